# Optimizing a Trainium2 kernel written in Bass

```python
import jax
import jax.numpy as jnp
from jax import lax
import numpy as np

D_MODEL = 1024
BATCH = 4
SEQ = 4096
DEPTH = 2

GRID_W = 64
CTX_LEN = 256
HEAD_DIM = 64
D_MIX = 1024
RWKV_HEADS = 4
RWKV_W = RWKV_HEADS * HEAD_DIM
DECAY_LORA = 64
ICLR_LORA = 64
GATE_LORA = 128
GQA_HEADS = 4
GQA_KV_HEADS = 2
GQA_W = GQA_HEADS * HEAD_DIM
GQA_KV_W = GQA_KV_HEADS * HEAD_DIM
MLA_HEADS = 4
MLA_NOPE = 64
MLA_ROPE = 32
MLA_V = 64
MLA_Q_LORA = 256
MLA_KV_LORA = 128
NAT_HEADS = 4
NAT_W = NAT_HEADS * HEAD_DIM
NAT_WIN_R = 8
NAT_WIN_C = 16
D_FF = 2816
N_EXPERTS = 8
TOP_K = 2
Q_BLOCK = 128
ROPE_THETA = 10000.0
RMS_EPS = 1e-6
GN_EPS = 64e-5
RWKV_COLS = 3 * RWKV_W + DECAY_LORA + ICLR_LORA + GATE_LORA
GQA_COLS = GQA_W + 2 * GQA_KV_W
MLA_COLS = MLA_Q_LORA + MLA_KV_LORA + MLA_ROPE
NAT_COLS = 3 * NAT_W
IN_COLS = RWKV_COLS + GQA_COLS + MLA_COLS + NAT_COLS
GROUP_SPLITS = [RWKV_COLS, RWKV_COLS + GQA_COLS, RWKV_COLS + GQA_COLS + MLA_COLS]
RWKV_SPLITS = [RWKV_W, 2 * RWKV_W, 3 * RWKV_W, 3 * RWKV_W + DECAY_LORA, 3 * RWKV_W + DECAY_LORA + ICLR_LORA]
N_DENSE = (DEPTH + 1) // 2
N_MOE = DEPTH // 2

kernel_name = 'hybrid_diffusion_parallel_heads'

f32 = jnp.float32


def rms_norm(x, g, eps=RMS_EPS):
    xf = x.astype(f32)
    y = xf * lax.rsqrt(jnp.mean(jnp.square(xf), axis=-1, keepdims=True) + eps)
    return (y * g.astype(f32)).astype(x.dtype)


def heads(t, n):
    return t.reshape(t.shape[:-1] + (n, t.shape[-1] // n))


def to_heads_first(t):
    return t.transpose(0, 2, 1, 3)


def q_groups(q, n_groups):
    B, T, H, d = q.shape
    return q.reshape(B, T, n_groups, H // n_groups, d).transpose(0, 2, 3, 1, 4)


def merge_groups(o):
    B, G, R, T, d = o.shape
    return o.transpose(0, 3, 1, 2, 4).reshape(B, T, G * R * d)


def axial_rope_tables(n, rot_dim):
    t = jnp.arange(n)
    row = (t // GRID_W).astype(f32)
    col = (t % GRID_W).astype(f32)
    quarter = rot_dim // 4
    inv_freq = ROPE_THETA ** (-jnp.arange(quarter, dtype=f32) / quarter)
    ang = jnp.concatenate([row[:, None] * inv_freq, col[:, None] * inv_freq], axis=-1)
    return jnp.cos(ang)[:, None, :], jnp.sin(ang)[:, None, :]


def apply_rope(x, cos, sin):
    half = x.shape[-1] // 2
    x1 = x[..., :half].astype(f32)
    x2 = x[..., half:].astype(f32)
    return jnp.concatenate([x1 * cos - x2 * sin, x1 * sin + x2 * cos], axis=-1).astype(x.dtype)


def attend(q, k, v, scale):
    s = jnp.einsum('bgrqd,bgkd->bgrqk', q, k).astype(f32) * scale
    p = jax.nn.softmax(s, axis=-1).astype(v.dtype)
    return jnp.einsum('bgrqk,bgkd->bgrqd', p, v)


def blocked_attend(q, k, v, scale):
    B, G, R, N, dq = q.shape
    nb = N // Q_BLOCK
    qb = jnp.moveaxis(q.reshape(B, G, R, nb, Q_BLOCK, dq), 3, 0)
    ob = lax.map(lambda qi: attend(qi, k, v, scale), qb)
    return jnp.moveaxis(ob, 0, 3).reshape(B, G, R, N, v.shape[-1])


def centred_shift(p):
    zero = jnp.zeros_like(p[:, :1])
    prev = jnp.concatenate([zero, p[:, :-1]], axis=1)
    nxt = jnp.concatenate([p[:, 1:], zero], axis=1)
    return 0.5 * (prev + nxt)


def rwkv_step(S, inp):
    r, w, k, v, kk, a = inp
    sa = jnp.einsum('bhvk,bhk->bhv', S, -kk)
    S = S * w[:, :, None, :] + sa[..., None] * (kk * a)[:, :, None, :] + v[..., None] * k[:, :, None, :]
    return S, jnp.einsum('bhvk,bhk->bhv', S, r)


def rwkv_scan(seqs):
    B, T, H, dh = seqs[0].shape
    tm = tuple(jnp.moveaxis(s.astype(f32), 1, 0) for s in seqs)
    _, y = lax.scan(rwkv_step, jnp.zeros((B, H, dh, dh), f32), tm)
    return jnp.moveaxis(y, 0, 1)


def rwkv_mixer(p_lat, p_ctx, mu, w0, w_up, a0, a_up, g_up, k_k, k_a, r_k, ln_g, ln_b, need_ctx):
    n_ctx = p_ctx.shape[1]

    def shared(p):
        p = p + mu * (centred_shift(p) - p)
        r, k, v, xw, xa, xg = jnp.split(p, RWKV_SPLITS, axis=-1)
        kk = heads(k * k_k, RWKV_HEADS).astype(f32)
        kk = kk * lax.rsqrt(jnp.sum(kk * kk, axis=-1, keepdims=True) + 1e-12)
        return r, k, v, xw, xa, xg, kk

    def directional(f, d):
        r, k, v, xw, xa, _, kk = f
        w_log = -jax.nn.softplus(-(w0[d] + jnp.tanh(xw) @ w_up[d])) - 0.5
        decay = jnp.exp(-jnp.exp(w_log.astype(f32)))
        a = jax.nn.sigmoid(a0[d] + xa @ a_up[d])
        kd = k * (1.0 + (a - 1.0) * k_a)
        return tuple(heads(t, RWKV_HEADS) for t in (r, decay, kd, v)) + (kk, heads(a, RWKV_HEADS))

    def read_out(y, dirf):
        r, _, kd, v = dirf[:4]
        mean = jnp.mean(y, axis=-1, keepdims=True)
        var = jnp.mean(jnp.square(y - mean), axis=-1, keepdims=True)
        yn = ((y - mean) * lax.rsqrt(var + GN_EPS)).reshape(y.shape[:2] + (RWKV_W,))
        yn = yn * ln_g.astype(f32) + ln_b.astype(f32)
        bonus = jnp.sum((r * kd * r_k).astype(f32), axis=-1, keepdims=True) * v.astype(f32)
        return yn + bonus.reshape(yn.shape)

    f_lat, f_ctx = shared(p_lat), shared(p_ctx)
    outs_lat, outs_ctx = [], []
    for d in range(2):
        d_lat, d_ctx = directional(f_lat, d), directional(f_ctx, d)
        if d == 0:
            seqs = tuple(jnp.concatenate([sc, sl], axis=1) for sc, sl in zip(d_ctx, d_lat))
        else:
            seqs = tuple(jnp.concatenate([sc[:, ::-1], sl[:, ::-1]], axis=1) for sc, sl in zip(d_ctx, d_lat))
        y = rwkv_scan(seqs)
        y_ctx, y_lat = y[:, :n_ctx], y[:, n_ctx:]
        if d == 1:
            y_ctx, y_lat = y_ctx[:, ::-1], y_lat[:, ::-1]
        outs_lat.append(read_out(y_lat, d_lat))
        if need_ctx:
            outs_ctx.append(read_out(y_ctx, d_ctx))
    y_lat = ((outs_lat[0] + outs_lat[1]) * (jax.nn.sigmoid(f_lat[5]) @ g_up)).astype(p_lat.dtype)
    y_ctx = None
    if need_ctx:
        y_ctx = ((outs_ctx[0] + outs_ctx[1]) * (jax.nn.sigmoid(f_ctx[5]) @ g_up)).astype(p_ctx.dtype)
    return y_lat, y_ctx


def gqa_mixer(p_lat, p_ctx, q_g, k_g, cos, sin, need_ctx):
    scale = HEAD_DIM ** -0.5

    def split(p):
        return jnp.split(p, [GQA_W, GQA_W + GQA_KV_W], axis=-1)

    q_l, k_l, v_l = split(p_lat)
    q_c, k_c, v_c = split(p_ctx)
    q_l = apply_rope(rms_norm(heads(q_l, GQA_HEADS), q_g), cos, sin)
    k_l = apply_rope(rms_norm(heads(k_l, GQA_KV_HEADS), k_g), cos, sin)
    k_c = rms_norm(heads(k_c, GQA_KV_HEADS), k_g)
    v_l, v_c = heads(v_l, GQA_KV_HEADS), heads(v_c, GQA_KV_HEADS)
    keys = to_heads_first(jnp.concatenate([k_l, k_c], axis=1))
    vals = to_heads_first(jnp.concatenate([v_l, v_c], axis=1))
    y_lat = merge_groups(blocked_attend(q_groups(q_l, GQA_KV_HEADS), keys, vals, scale))
    y_ctx = None
    if need_ctx:
        q_c = rms_norm(heads(q_c, GQA_HEADS), q_g)
        y_ctx = merge_groups(attend(q_groups(q_c, GQA_KV_HEADS), to_heads_first(k_c), to_heads_first(v_c), scale))
    return y_lat, y_ctx


def mla_mixer(p_lat, p_ctx, q_norm_g, w_uq, kv_norm_g, w_ukv, cos, sin, need_ctx):
    scale = (MLA_NOPE + MLA_ROPE) ** -0.5

    def split(p):
        return jnp.split(p, [MLA_Q_LORA, MLA_Q_LORA + MLA_KV_LORA], axis=-1)

    def queries(cq, rope):
        q = heads(rms_norm(cq, q_norm_g) @ w_uq, MLA_HEADS)
        q_nope, q_rope = q[..., :MLA_NOPE], q[..., MLA_NOPE:]
        if rope:
            q_rope = apply_rope(q_rope, cos, sin)
        return jnp.concatenate([q_nope, q_rope], axis=-1)

    def keys_values(ckv, kr, rope):
        kv = heads(rms_norm(ckv, kv_norm_g) @ w_ukv, MLA_HEADS)
        k_nope, v = kv[..., :MLA_NOPE], kv[..., MLA_NOPE:]
        kr = kr[:, :, None, :]
        if rope:
            kr = apply_rope(kr, cos, sin)
        k = jnp.concatenate([k_nope, jnp.broadcast_to(kr, k_nope.shape[:-1] + (MLA_ROPE,))], axis=-1)
        return k, v

    cq_l, ckv_l, kr_l = split(p_lat)
    cq_c, ckv_c, kr_c = split(p_ctx)
    k_l, v_l = keys_values(ckv_l, kr_l, True)
    k_c, v_c = keys_values(ckv_c, kr_c, False)
    keys = to_heads_first(jnp.concatenate([k_l, k_c], axis=1))
    vals = to_heads_first(jnp.concatenate([v_l, v_c], axis=1))
    y_lat = merge_groups(blocked_attend(q_groups(queries(cq_l, True), MLA_HEADS), keys, vals, scale))
    y_ctx = None
    if need_ctx:
        y_ctx = merge_groups(attend(q_groups(queries(cq_c, False), MLA_HEADS), to_heads_first(k_c), to_heads_first(v_c), scale))
    return y_lat, y_ctx


def nat_mixer(p_lat, p_ctx, bias_tab, need_ctx):
    scale = HEAD_DIM ** -0.5
    q_l, k_l, v_l = (heads(t, NAT_HEADS) for t in jnp.split(p_lat, [NAT_W, 2 * NAT_W], axis=-1))
    q_c, k_c, v_c = (heads(t, NAT_HEADS) for t in jnp.split(p_ctx, [NAT_W, 2 * NAT_W], axis=-1))
    k_c, v_c = to_heads_first(k_c), to_heads_first(v_c)
    B, N, H, d = q_l.shape
    rows = N // GRID_W
    win_r = min(NAT_WIN_R, rows)

    def grid(t):
        return to_heads_first(t).reshape(B, H, rows, GRID_W, d)

    q_g, k_g, v_g = grid(q_l), grid(k_l), grid(v_l)
    row_start = jnp.clip(jnp.arange(rows) - win_r // 2, 0, rows - win_r)
    col = jnp.arange(GRID_W)
    col_idx = jnp.clip(col - NAT_WIN_C // 2, 0, GRID_W - NAT_WIN_C)[:, None] + jnp.arange(NAT_WIN_C)
    col_off = col_idx - col[:, None] + (NAT_WIN_C - 1)
    bias_col = bias_tab[:, :, col_off]
    n_win = win_r * NAT_WIN_C

    def row_block(inp):
        q_row, rs, i = inp
        k_win = lax.dynamic_slice_in_dim(k_g, rs, win_r, axis=2)[:, :, :, col_idx]
        v_win = lax.dynamic_slice_in_dim(v_g, rs, win_r, axis=2)[:, :, :, col_idx]
        bias = bias_col[:, rs + jnp.arange(win_r) - i + (NAT_WIN_R - 1)].transpose(0, 2, 1, 3)
        s_win = jnp.einsum('bhqd,bhrqcd->bhqrc', q_row, k_win).astype(f32) * scale + bias.astype(f32)
        s_ctx = jnp.einsum('bhqd,bhkd->bhqk', q_row, k_c).astype(f32) * scale
        s = jnp.concatenate([s_win.reshape(B, H, GRID_W, n_win), s_ctx], axis=-1)
        p = jax.nn.softmax(s, axis=-1).astype(v_g.dtype)
        p_win = p[..., :n_win].reshape(B, H, GRID_W, win_r, NAT_WIN_C)
        return jnp.einsum('bhqrc,bhrqcd->bhqd', p_win, v_win) + jnp.einsum('bhqk,bhkd->bhqd', p[..., n_win:], v_c)

    o = lax.map(row_block, (jnp.moveaxis(q_g, 2, 0), row_start, jnp.arange(rows)))
    y_lat = o.transpose(1, 0, 3, 2, 4).reshape(B, N, H * d)
    y_ctx = None
    if need_ctx:
        y_ctx = merge_groups(attend(q_groups(q_c, NAT_HEADS), k_c, v_c, scale))
    return y_lat, y_ctx


def swiglu(h, wg, wu, wd):
    return (jax.nn.silu(h @ wg) * (h @ wu)) @ wd


def moe_swiglu(h, router, wg, wu, wd):
    logits = (h @ router).astype(f32)
    top_vals, top_idx = lax.top_k(logits, TOP_K)
    top_w = jax.nn.softmax(top_vals, axis=-1)
    gates = jnp.sum(jax.nn.one_hot(top_idx, N_EXPERTS, dtype=f32) * top_w[..., None], axis=-2).astype(h.dtype)
    return sum(gates[..., e:e + 1] * swiglu(h, wg[e], wu[e], wd[e]) for e in range(N_EXPERTS))


def modulation(cond, w, b):
    return jnp.split(jax.nn.silu(cond) @ w + b, 6, axis=-1)


def setup_inputs(seed: int = 0) -> dict:
    key = jax.random.key(seed)
    ks = iter(jax.random.split(key, 48))

    def nrm(shape, scale):
        return scale * jax.random.normal(next(ks), shape, f32)

    return {
        'x': nrm((BATCH, SEQ, D_MODEL), 1.0),
        'c': nrm((BATCH, D_MODEL), 1.0),
        'ctx': nrm((BATCH, CTX_LEN, D_MODEL), 1.0),
        'c_ctx': nrm((D_MODEL,), 1.0),
        'mod_w': nrm((DEPTH, D_MODEL, 6 * D_MODEL), 0.5 * D_MODEL ** -0.5),
        'mod_b': nrm((DEPTH, 6 * D_MODEL), 0.02),
        'norm_g': 1.0 + nrm((DEPTH, 4, D_MODEL), 0.05),
        'w_in': nrm((DEPTH, D_MODEL, IN_COLS), D_MODEL ** -0.5),
        'w_out': nrm((DEPTH, D_MIX, D_MODEL), D_MIX ** -0.5),
        'rwkv_mu': jax.random.uniform(next(ks), (DEPTH, RWKV_COLS), f32),
        'rwkv_w0': jax.random.uniform(next(ks), (DEPTH, 2, RWKV_W), f32, -6.0, 1.0),
        'rwkv_w_up': nrm((DEPTH, 2, DECAY_LORA, RWKV_W), DECAY_LORA ** -0.5),
        'rwkv_a0': nrm((DEPTH, 2, RWKV_W), 0.1),
        'rwkv_a_up': nrm((DEPTH, 2, ICLR_LORA, RWKV_W), ICLR_LORA ** -0.5),
        'rwkv_g_up': nrm((DEPTH, GATE_LORA, RWKV_W), GATE_LORA ** -0.5),
        'rwkv_k_k': 0.85 + nrm((DEPTH, RWKV_W), 0.05),
        'rwkv_k_a': 1.0 + nrm((DEPTH, RWKV_W), 0.05),
        'rwkv_r_k': nrm((DEPTH, RWKV_HEADS, HEAD_DIM), 0.1),
        'rwkv_ln_g': 1.0 + nrm((DEPTH, RWKV_W), 0.05),
        'rwkv_ln_b': nrm((DEPTH, RWKV_W), 0.02),
        'gqa_q_g': 1.0 + nrm((DEPTH, HEAD_DIM), 0.05),
        'gqa_k_g': 1.0 + nrm((DEPTH, HEAD_DIM), 0.05),
        'mla_q_norm_g': 1.0 + nrm((DEPTH, MLA_Q_LORA), 0.05),
        'mla_w_uq': nrm((DEPTH, MLA_Q_LORA, MLA_HEADS * (MLA_NOPE + MLA_ROPE)), MLA_Q_LORA ** -0.5),
        'mla_kv_norm_g': 1.0 + nrm((DEPTH, MLA_KV_LORA), 0.05),
        'mla_w_ukv': nrm((DEPTH, MLA_KV_LORA, MLA_HEADS * (MLA_NOPE + MLA_V)), MLA_KV_LORA ** -0.5),
        'nat_bias': nrm((DEPTH, NAT_HEADS, 2 * NAT_WIN_R - 1, 2 * NAT_WIN_C - 1), 0.1),
        'ffn_w_gate': nrm((N_DENSE, D_MODEL, D_FF), D_MODEL ** -0.5),
        'ffn_w_up': nrm((N_DENSE, D_MODEL, D_FF), D_MODEL ** -0.5),
        'ffn_w_down': nrm((N_DENSE, D_FF, D_MODEL), D_FF ** -0.5),
        'moe_router': nrm((N_MOE, D_MODEL, N_EXPERTS), D_MODEL ** -0.5),
        'moe_w_gate': nrm((N_MOE, N_EXPERTS, D_MODEL, D_FF), D_MODEL ** -0.5),
        'moe_w_up': nrm((N_MOE, N_EXPERTS, D_MODEL, D_FF), D_MODEL ** -0.5),
        'moe_w_down': nrm((N_MOE, N_EXPERTS, D_FF, D_MODEL), D_FF ** -0.5),
    }


def reference(x, c, ctx, c_ctx, mod_w, mod_b, norm_g, w_in, w_out, rwkv_mu, rwkv_w0, rwkv_w_up, rwkv_a0,
              rwkv_a_up, rwkv_g_up, rwkv_k_k, rwkv_k_a, rwkv_r_k, rwkv_ln_g, rwkv_ln_b, gqa_q_g, gqa_k_g,
              mla_q_norm_g, mla_w_uq, mla_kv_norm_g, mla_w_ukv, nat_bias, ffn_w_gate, ffn_w_up, ffn_w_down,
              moe_router, moe_w_gate, moe_w_up, moe_w_down):
    n_lat, n_ctx = x.shape[1], ctx.shape[1]
    cos_g, sin_g = axial_rope_tables(n_lat, HEAD_DIM)
    cos_m, sin_m = axial_rope_tables(n_lat, MLA_ROPE)
    xc = ctx
    for l in range(DEPTH):
        need_ctx = l < DEPTH - 1
        m_lat = [m[:, None, :] for m in modulation(c, mod_w[l], mod_b[l])]
        m_ctx = modulation(c_ctx, mod_w[l], mod_b[l])
        gains = norm_g[l]

        h_lat = rms_norm(x, gains[0]) * (1.0 + m_lat[1]) + m_lat[0]
        h_ctx = rms_norm(xc, gains[0]) * (1.0 + m_ctx[1]) + m_ctx[0]
        rw_l, gq_l, ml_l, na_l = jnp.split(h_lat @ w_in[l], GROUP_SPLITS, axis=-1)
        rw_c, gq_c, ml_c, na_c = jnp.split(h_ctx @ w_in[l], GROUP_SPLITS, axis=-1)
        o_rw = rwkv_mixer(rw_l, rw_c, rwkv_mu[l], rwkv_w0[l], rwkv_w_up[l], rwkv_a0[l], rwkv_a_up[l], rwkv_g_up[l],
                          rwkv_k_k[l], rwkv_k_a[l], rwkv_r_k[l], rwkv_ln_g[l], rwkv_ln_b[l], need_ctx)
        o_gq = gqa_mixer(gq_l, gq_c, gqa_q_g[l], gqa_k_g[l], cos_g, sin_g, need_ctx)
        o_ml = mla_mixer(ml_l, ml_c, mla_q_norm_g[l], mla_w_uq[l], mla_kv_norm_g[l], mla_w_ukv[l], cos_m, sin_m, need_ctx)
        o_na = nat_mixer(na_l, na_c, nat_bias[l], need_ctx)
        y_lat = jnp.concatenate([o_rw[0], o_gq[0], o_ml[0], o_na[0]], axis=-1) @ w_out[l]
        x = x + m_lat[2] * rms_norm(y_lat, gains[1])
        if need_ctx:
            y_ctx = jnp.concatenate([o_rw[1], o_gq[1], o_ml[1], o_na[1]], axis=-1) @ w_out[l]
            xc = xc + m_ctx[2] * rms_norm(y_ctx, gains[1])

        h = rms_norm(x, gains[2]) * (1.0 + m_lat[4]) + m_lat[3]
        if need_ctx:
            h = jnp.concatenate([rms_norm(xc, gains[2]) * (1.0 + m_ctx[4]) + m_ctx[3], h], axis=1)
        if l % 2 == 0:
            f = swiglu(h, ffn_w_gate[l // 2], ffn_w_up[l // 2], ffn_w_down[l // 2])
        else:
            f = moe_swiglu(h, moe_router[l // 2], moe_w_gate[l // 2], moe_w_up[l // 2], moe_w_down[l // 2])
        if need_ctx:
            xc = xc + m_ctx[5] * rms_norm(f[:, :n_ctx], gains[3])
            f = f[:, n_ctx:]
        x = x + m_lat[5] * rms_norm(f, gains[3])
    return x
```

```python
import numpy as np
from contextlib import ExitStack
import concourse.bass as bass
import concourse.mybir as mybir
from concourse.bass_utils import run_bass_kernel_spmd

F32 = mybir.dt.float32
BF16 = mybir.dt.bfloat16
AF = mybir.ActivationFunctionType
ALU = mybir.AluOpType
AX = mybir.AxisListType

COMPUTE = ('pe', 'act', 'dve', 'pool')
QUEUES = ('pe', 'act', 'dve', 'pool', 'sp')
NRING = 8

D = 1024
NL = 4096
NC_ = 256
NT = NL + NC_
NTILE = NT // 128
INC = 2720
RW0, GQ0, ML0, NA0 = 0, 1024, 1536, 1952
DFF = 2816
NFF = DFF // 128
NE = 8
RMS_EPS = 1e-6


class Dep:
    __slots__ = ('w', 'r', 'name')

    def __init__(self, name=''):
        self.w = None
        self.r = {}
        self.name = name


class Prog:
    def __init__(self, nc):
        self.nc = nc
        self.q = {e: [] for e in QUEUES}
        self.cnt = {e: 0 for e in COMPUTE}
        self.seen = {e: {} for e in QUEUES}
        self.sems = {}
        self.dma_cnt = {}
        self.dma_next = {e: 0 for e in QUEUES}
        self.ninstr = 0

    def alloc_sems(self, stack):
        for e in COMPUTE:
            self.sems[e] = stack.enter_context(self.nc.semaphore('s_' + e))
        for qn in ('sp', 'pool', 'act'):
            for j in range(NRING):
                key = ('dma', qn, j)
                self.sems[key] = stack.enter_context(self.nc.semaphore('d_%s_%d' % (qn, j)))
                self.dma_cnt[key] = 0

    def _need(self, queue, key, count, waits):
        if self.seen[queue].get(key, 0) >= count:
            return
        if waits.get(key, 0) < count:
            waits[key] = count

    def _emit_waits(self, queue, waits):
        for key, count in waits.items():
            sem = self.sems[key]
            val = count * (16 if isinstance(key, tuple) else 1)
            self.q[queue].append(lambda e, sem=sem, val=val: e.wait_ge(sem, val))
            self.seen[queue][key] = count
            self.ninstr += 1

    def _collect(self, queue, reads, writes):
        waits = {}
        for t in reads:
            if t.w is not None:
                self._need(queue, t.w[0], t.w[1], waits)
        for t in writes:
            if t.w is not None:
                if not (queue == 'pe' and t.w[0] == 'pe'):
                    self._need(queue, t.w[0], t.w[1], waits)
            for k, c in t.r.items():
                if k == queue:
                    continue
                self._need(queue, k, c, waits)
        return waits

    def op(self, queue, fn, reads=(), writes=()):
        waits = self._collect(queue, reads, writes)
        self._emit_waits(queue, waits)
        self.cnt[queue] += 1
        c = self.cnt[queue]
        sem = self.sems[queue]
        self.q[queue].append(lambda e, fn=fn, sem=sem: fn(e).then_inc(sem, 1))
        self.ninstr += 1
        for t in reads:
            if t.r.get(queue, 0) < c:
                t.r[queue] = c
        for t in writes:
            t.w = (queue, c)
            t.r = {}

    def dma(self, queue, fn, reads=(), writes=()):
        j = self.dma_next[queue]
        self.dma_next[queue] = (j + 1) % NRING
        key = ('dma', queue, j)
        waits = self._collect(queue, reads, writes)
        n = self.dma_cnt[key]
        if n > 0:
            self._need(queue, key, n, waits)
        self._emit_waits(queue, waits)
        self.dma_cnt[key] = n + 1
        sem = self.sems[key]
        self.q[queue].append(lambda e, fn=fn, sem=sem: fn(e).then_inc(sem, 16))
        self.ninstr += 1
        for t in reads:
            t.r[key] = n + 1
        for t in writes:
            t.w = (key, n + 1)
            t.r = {}

    def barrier(self, queues=QUEUES):
        for qn in queues:
            waits = {}
            for e in COMPUTE:
                if self.cnt[e] > 0 and e != qn:
                    self._need(qn, e, self.cnt[e], waits)
            for key, n in self.dma_cnt.items():
                if n > 0:
                    self._need(qn, key, n, waits)
            self._emit_waits(qn, waits)

    def emit(self):
        nc = self.nc
        with nc.Block() as block:
            @block.tensor
            def _(e):
                for f in self.q['pe']:
                    f(e)

            @block.scalar
            def _(e):
                for f in self.q['act']:
                    f(e)

            @block.vector
            def _(e):
                for f in self.q['dve']:
                    f(e)

            @block.gpsimd
            def _(e):
                for f in self.q['pool']:
                    f(e)

            @block.sync
            def _(e):
                for f in self.q['sp']:
                    f(e)


class Ring:
    def __init__(self, K, st, name, shape, dtype, n, psum=False):
        self.bufs = []
        for i in range(n):
            if psum:
                full = [128, 512] if dtype == F32 else [128, 1024]
                n = 1
                for d_ in shape[1:]:
                    n *= d_
                assert n <= full[1]
                t = st.enter_context(K.nc.psum_tensor(uname('%s%d' % (name, i)), full, dtype))
                t = t[0:shape[0], 0:n]
                if len(shape) == 3:
                    t = t.rearrange("p (a b) -> p a b", b=shape[2])
            else:
                t = st.enter_context(K.nc.sbuf_tensor(uname('%s%d' % (name, i)), shape, dtype))
            self.bufs.append((t, Dep('%s%d' % (name, i))))
        self.i = 0

    def next(self):
        b = self.bufs[self.i]
        self.i = (self.i + 1) % len(self.bufs)
        return b


class K:
    LVL = 99
    UID = 0


def uname(name):
    K.UID += 1
    return '%s_u%d' % (name, K.UID)


def sb(st, name, shape, dtype):
    return st.enter_context(K.nc.sbuf_tensor(uname(name), shape, dtype)), Dep(name)


def ps(st, name, shape, dtype):
    return st.enter_context(K.nc.psum_tensor(uname(name), shape, dtype)), Dep(name)


def dram(name, shape, dtype):
    return K.nc.dram_tensor(name, shape, dtype, kind="Internal").ap()


def rms_rstd(x_ap, xdep, n, sq, sqd, ss, ssd, col):
    P = K.P
    P.op('dve', lambda e: e.tensor_tensor(out=sq[:, 0:n], in0=x_ap, in1=x_ap, op=ALU.mult), [xdep], [sqd])
    P.op('dve', lambda e: e.tensor_reduce(out=ss[:, col:col + 1], in_=sq[:, 0:n], axis=AX.X, op=ALU.add), [sqd], [ssd])
    P.op('act', lambda e: e.activation(out=ss[:, col:col + 1], in_=ss[:, col:col + 1], func=AF.Sqrt, bias=RMS_EPS, scale=1.0 / n), [ssd], [ssd])
    P.op('dve', lambda e: e.reciprocal(out=ss[:, col:col + 1], in_=ss[:, col:col + 1]), [ssd], [ssd])


def phase_mod(l, I, modL, modLd, modC, modCd):
    nc, P = K.nc, K.P
    with ExitStack() as st:
        cv, cvd = sb(st, 'cv', [128, 2, 8], F32)
        cs, csd = sb(st, 'cs', [128, 2, 8], F32)
        crep, crepd = sb(st, 'crep', [128, 2, 8, 128], BF16)
        gb, gbd = sb(st, 'gb', [128, 4, D], F32)
        wst = Ring(K, st, 'mw_st', [128, 8, 512], F32, 2)
        wbf = Ring(K, st, 'mw_bf', [128, 8, 512], BF16, 2)
        bb = Ring(K, st, 'mbb', [128, 512], F32, 2)
        pm = Ring(K, st, 'pmod', [128, 512], F32, 4, psum=True)
        P.dma('sp', lambda e: e.dma_start(out=cv[:, 0, :], in_=I['c'].rearrange("(k p) -> p k", p=128), allow_slow_non_contiguous=True), [], [cvd])
        P.dma('sp', lambda e: e.dma_start(out=cv[:, 1, :], in_=I['c_ctx'].rearrange("(k p) -> p k", p=128), allow_slow_non_contiguous=True), [], [cvd])
        P.dma('sp', lambda e: e.dma_start(out=gb[:], in_=I['norm_g'][l].partition_broadcast(128)), [], [gbd])
        P.op('act', lambda e: e.activation(out=cs[:], in_=cv[:], func=AF.Silu), [cvd], [csd])
        P.op('dve', lambda e: e.tensor_copy(out=crep[:], in_=cs[:].unsqueeze(3).to_broadcast([128, 2, 8, 128])), [csd], [crepd])
        mw = I['mod_w'][l].rearrange("(k p) n -> p k n", p=128)
        for nb in range(12):
            (ws, wsd) = wst.next()
            (wb, wbd) = wbf.next()
            (bt, btd) = bb.next()
            P.dma('sp', lambda e, ws=ws, nb=nb: e.dma_start(out=ws[:], in_=mw[:, :, nb * 512:(nb + 1) * 512]), [], [wsd])
            P.dma('sp', lambda e, bt=bt, nb=nb: e.dma_start(out=bt[:], in_=I['mod_b'][l, nb * 512:(nb + 1) * 512].partition_broadcast(128)), [], [btd])
            P.op('pool', lambda e, ws=ws, wb=wb: e.tensor_copy(out=wb[:], in_=ws[:]), [wsd], [wbd])
            j, off = nb // 2, (nb % 2) * 512
            for s, (mt, mtd) in enumerate(((modL, modLd), (modC, modCd))):
                (pt, ptd) = pm.next()
                for kc in range(8):
                    P.op('pe', lambda e, pt=pt, wb=wb, kc=kc, s=s: e.matmul(pt[:], lhsT=crep[:, s, kc, :], rhs=wb[:, kc, :], start=(kc == 0), stop=(kc == 7)),
                         [crepd, wbd], [ptd])
                P.op('dve', lambda e, pt=pt, bt=bt, mt=mt, j=j, off=off: e.tensor_tensor(out=mt[:, j, off:off + 512], in0=pt[:], in1=bt[:], op=ALU.add),
                     [ptd, btd], [mtd])
        for (mt, mtd) in ((modL, modLd), (modC, modCd)):
            for j, gi, plus1 in ((1, 0, True), (2, 1, False), (4, 2, True), (5, 3, False)):
                if plus1:
                    P.op('dve', lambda e, mt=mt, j=j, gi=gi: e.scalar_tensor_tensor(out=mt[:, j, :], in0=mt[:, j, :], scalar=1.0, in1=gb[:, gi, :], op0=ALU.add, op1=ALU.mult),
                         [mtd, gbd], [mtd])
                else:
                    P.op('dve', lambda e, mt=mt, j=j, gi=gi: e.tensor_tensor(out=mt[:, j, :], in0=mt[:, j, :], in1=gb[:, gi, :], op=ALU.mult),
                         [mtd, gbd], [mtd])
        P.barrier()


def phase_in(l, I, S, modL, modLd, modC, modCd, ident, identd):
    nc, P = K.nc, K.P
    with ExitStack() as st:
        wbf, wbfd = sb(st, 'win_bf', [128, 8, INC], BF16)
        wst = Ring(K, st, 'win_st', [128, INC], F32, 2)
        xr = Ring(K, st, 'in_x', [128, D], F32, 3)
        hr = Ring(K, st, 'in_h', [128, D], F32, 2)
        hbr = Ring(K, st, 'in_hb', [128, D], BF16, 2)
        hTr = Ring(K, st, 'in_hT', [128, 8, 128], BF16, 2)
        sq, sqd = sb(st, 'in_sq', [128, D], F32)
        ssr = Ring(K, st, 'in_ss', [128, 1], F32, 4)
        pr = Ring(K, st, 'in_p', [128, INC], F32, 2)
        ptr = Ring(K, st, 'in_pT', [128, 8, 128], BF16, 2, psum=True)
        pmr = Ring(K, st, 'in_pm', [128, 512], F32, 4, psum=True)
        win = I['w_in'][l].rearrange("(k p) n -> p k n", p=128)
        for kc in range(8):
            (ws, wsd) = wst.next()
            P.dma('sp', lambda e, ws=ws, kc=kc: e.dma_start(out=ws[:], in_=win[:, kc, :]), [], [wsd])
            P.op('pool', lambda e, ws=ws, kc=kc: e.tensor_copy(out=wbf[:, kc, :], in_=ws[:]), [wsd], [wbfd])
        for t in range(NTILE):
            mt, mtd = (modC, modCd) if t < 2 else (modL, modLd)
            (x, xd) = xr.next()
            (h, hd) = hr.next()
            (hb, hbd) = hbr.next()
            (hT, hTd) = hTr.next()
            (ss, ssd) = ssr.next()
            (pt, ptd) = pr.next()
            (pT, pTd) = ptr.next()
            P.dma('sp', lambda e, x=x, t=t: e.dma_start(out=x[:], in_=S['xs'][t * 128:(t + 1) * 128, :]), [S['xs_d'][t]], [xd])
            rms_rstd(x[:], xd, D, sq, sqd, ss, ssd, 0)
            P.op('dve', lambda e, h=h, x=x, ss=ss, mt=mt: e.scalar_tensor_tensor(out=h[:], in0=x[:], scalar=ss[:, 0:1], in1=mt[:, 1, :], op0=ALU.mult, op1=ALU.mult),
                 [xd, ssd, mtd], [hd])
            P.op('pool', lambda e, h=h, hb=hb, mt=mt: e.tensor_tensor(out=hb[:], in0=h[:], in1=mt[:, 0, :], op=ALU.add), [hd, mtd], [hbd])
            for kc in range(8):
                P.op('pe', lambda e, pT=pT, hb=hb, kc=kc: e.transpose(out=pT[:, kc, :], in_=hb[:, kc * 128:(kc + 1) * 128], identity=ident[:]),
                     [hbd, identd], [pTd])
            P.op('act', lambda e, hT=hT, pT=pT: e.copy(out=hT[:], in_=pT[:]), [pTd], [hTd])
            for nb in range(6):
                c0 = nb * 512
                cw = min(512, INC - c0)
                (pm, pmd) = pmr.next()
                for kc in range(8):
                    P.op('pe', lambda e, pm=pm, hT=hT, kc=kc, c0=c0, cw=cw: e.matmul(pm[:, 0:cw], lhsT=hT[:, kc, :], rhs=wbf[:, kc, c0:c0 + cw], start=(kc == 0), stop=(kc == 7)),
                         [hTd, wbfd], [pmd])
                if nb % 2 == 0:
                    P.op('act', lambda e, pm=pm, pt=pt, c0=c0, cw=cw: e.copy(out=pt[:, c0:c0 + cw], in_=pm[:, 0:cw]), [pmd], [ptd])
                else:
                    P.op('dve', lambda e, pm=pm, pt=pt, c0=c0, cw=cw: e.tensor_copy(out=pt[:, c0:c0 + cw], in_=pm[:, 0:cw]), [pmd], [ptd])
            P.dma('sp', lambda e, pt=pt, t=t: e.dma_start(out=S['p'][t * 128:(t + 1) * 128, :], in_=pt[:]), [ptd], [S['p_d'][t]])
        P.barrier()


def attn_finalize(st_rings, O, Od, h, ot, otd, nq, ident32, ident32d):
    P = K.P
    osbr, ptr, rvr = st_rings
    (osb, osbd) = osbr.next()
    P.op('dve', lambda e: e.tensor_copy(out=osb[:, 0:nq], in_=O[:, 0:nq]), [Od], [osbd])
    for j in range(nq // 128):
        (pt, ptd) = ptr.next()
        (rv, rvd) = rvr.next()
        P.op('pe', lambda e, pt=pt, osb=osb, j=j: e.transpose(out=pt[:], in_=osb[:, j * 128:(j + 1) * 128], identity=ident32[0:65, 0:65]),
             [osbd, ident32d], [ptd])
        P.op('dve', lambda e, pt=pt, rv=rv: e.reciprocal(out=rv[:], in_=pt[:, 64:65]), [ptd], [rvd])
        P.op('dve', lambda e, pt=pt, rv=rv, j=j: e.tensor_scalar(out=ot[:, j, h * 64:(h + 1) * 64], in0=pt[:, 0:64], scalar1=rv[:, 0:1], scalar2=None, op0=ALU.mult),
             [ptd, rvd], [otd])


def attn_core(st, S, QT, QTd, KT, KTd, kvmap, Vaug, Vaugd, scale, col0, need_ctx, ident32, ident32d, tag):
    P = K.P
    sr = Ring(K, st, tag + '_S', [128, 512], F32, 3, psum=True)
    orr = Ring(K, st, tag + '_O', [65, 512], F32, 2, psum=True)
    ptr = Ring(K, st, tag + '_fT', [128, 65], F32, 2, psum=True)
    pr = Ring(K, st, tag + '_P', [128, 512], BF16, 3)
    osbr = Ring(K, st, tag + '_osb', [65, 512], F32, 2)
    rvr = Ring(K, st, tag + '_rv', [128, 1], F32, 4)
    otr = Ring(K, st, tag + '_ot', [128, 4, 256], F32, 2)
    blocks = []
    if need_ctx:
        blocks.append((0, 256, [0, 1]))
    for qb in range(8):
        blocks.append((256 + qb * 512, 512, list(range(NTILE))))
    for (q0, nq, kts) in blocks:
        (ot, otd) = otr.next()
        for h in range(4):
            g = kvmap[h]
            (O, Od) = orr.next()
            for i, kt in enumerate(kts):
                (Sp, Spd) = sr.next()
                (Pt, Ptd) = pr.next()
                P.op('pe', lambda e, Sp=Sp, g=g, h=h, kt=kt, q0=q0, nq=nq: e.matmul(Sp[:, 0:nq], lhsT=KT(g)[:, kt * 128:(kt + 1) * 128], rhs=QT(h)[:, q0:q0 + nq], start=True, stop=True),
                     [QTd, KTd], [Spd])
                P.op('act', lambda e, Sp=Sp, Pt=Pt, nq=nq: e.activation(out=Pt[:, 0:nq], in_=Sp[:, 0:nq], func=AF.Exp, scale=scale), [Spd], [Ptd])
                P.op('pe', lambda e, O=O, g=g, kt=kt, Pt=Pt, nq=nq, i=i, n=len(kts): e.matmul(O[:, 0:nq], lhsT=Vaug[:, kt, g, :], rhs=Pt[:, 0:nq], start=(i == 0), stop=(i == n - 1)),
                     [Vaugd, Ptd], [Od])
            attn_finalize((osbr, ptr, rvr), O, Od, h, ot, otd, nq, ident32, ident32d)
        nj = nq // 128
        P.dma('sp', lambda e, ot=ot, q0=q0, nj=nj: e.dma_start(out=S['ocat'][q0:q0 + nj * 128, col0:col0 + 256].rearrange("(j p) c -> p j c", p=128), in_=ot[:, 0:nj, :]),
              [otd], [S['ocat_d'][t] for t in range(q0 // 128, q0 // 128 + nj)])


def phase_gqa(l, I, S, need_ctx, ident, identd, ident32, ident32d):
    nc, P = K.nc, K.P
    with ExitStack() as st:
        QKT, QKTd = sb(st, 'gq_QKT', [64, 6, NT], BF16)
        Vaug, Vaugd = sb(st, 'gq_V', [128, NTILE, 2, 65], BF16)
        with ExitStack() as st2:
            gain, gaind = sb(st2, 'gq_gain', [128, 6, 64], F32)
            xr = Ring(K, st2, 'gq_x', [128, 512], F32, 3)
            sq, sqd = sb(st2, 'gq_sq', [128, 384], F32)
            ssr = Ring(K, st2, 'gq_ss', [128, 6], F32, 3)
            qnr = Ring(K, st2, 'gq_qn', [128, 6, 64], F32, 2)
            qbr = Ring(K, st2, 'gq_qb', [128, 6, 64], BF16, 2)
            csr = Ring(K, st2, 'gq_cs', [128, 2, 32], F32, 3)
            t1r = Ring(K, st2, 'gq_t1', [128, 6, 32], F32, 2)
            t2r = Ring(K, st2, 'gq_t2', [128, 6, 32], F32, 2)
            pTr = Ring(K, st2, 'gq_pT', [64, 6, 128], BF16, 2, psum=True)
            P.dma('sp', lambda e: e.dma_start(out=gain[:, 0:4, :], in_=I['gqa_q_g'][l:l + 1, :].partition_broadcast(128).to_broadcast([128, 4, 64])), [], [gaind])
            P.dma('sp', lambda e: e.dma_start(out=gain[:, 4:6, :], in_=I['gqa_k_g'][l:l + 1, :].partition_broadcast(128).to_broadcast([128, 2, 64])), [], [gaind])
            P.op('pool', lambda e: e.memset(Vaug[:, :, :, 64:65], 1.0), [], [Vaugd])
            for t in range(NTILE):
                (x, xd) = xr.next()
                (ss, ssd) = ssr.next()
                (qn, qnd) = qnr.next()
                (qb, qbd) = qbr.next()
                (pT, pTd) = pTr.next()
                P.dma('sp', lambda e, x=x, t=t: e.dma_start(out=x[:], in_=S['p'][t * 128:(t + 1) * 128, GQ0:GQ0 + 512]), [S['p_d'][t]], [xd])
                P.op('dve', lambda e, x=x: e.tensor_tensor(out=sq[:], in0=x[:, 0:384], in1=x[:, 0:384], op=ALU.mult), [xd], [sqd])
                P.op('dve', lambda e, ss=ss: e.tensor_reduce(out=ss[:], in_=sq[:].rearrange("p (g d) -> p g d", d=64), axis=AX.X, op=ALU.add), [sqd], [ssd])
                P.op('act', lambda e, ss=ss: e.activation(out=ss[:], in_=ss[:], func=AF.Sqrt, bias=RMS_EPS, scale=1.0 / 64), [ssd], [ssd])
                P.op('dve', lambda e, ss=ss: e.reciprocal(out=ss[:], in_=ss[:]), [ssd], [ssd])
                P.op('dve', lambda e, x=x, qn=qn, ss=ss: e.tensor_tensor(out=qn[:], in0=x[:, 0:384].rearrange("p (g d) -> p g d", d=64), in1=ss[:].unsqueeze(2).to_broadcast([128, 6, 64]), op=ALU.mult),
                     [xd, ssd], [qnd])
                P.op('pool', lambda e, x=x, t=t: e.tensor_copy(out=Vaug[:, t, :, 0:64], in_=x[:, 384:512].rearrange("p (g d) -> p g d", d=64)), [xd], [Vaugd])
                if t < 2:
                    P.op('dve', lambda e, qn=qn, qb=qb: e.tensor_tensor(out=qb[:], in0=qn[:], in1=gain[:], op=ALU.mult), [qnd, gaind], [qbd])
                else:
                    (cs, csd) = csr.next()
                    (t1, t1d) = t1r.next()
                    (t2, t2d) = t2r.next()
                    r0 = (t - 2) * 128
                    P.dma('sp', lambda e, cs=cs, r0=r0: e.dma_start(out=cs[:], in_=I['rope_g'][r0:r0 + 128, :, :]), [], [csd])
                    P.op('dve', lambda e, qn=qn: e.tensor_tensor(out=qn[:], in0=qn[:], in1=gain[:], op=ALU.mult), [qnd, gaind], [qnd])
                    cosb = lambda cs=cs: cs[:, 0, :].unsqueeze(1).to_broadcast([128, 6, 32])
                    sinb = lambda cs=cs: cs[:, 1, :].unsqueeze(1).to_broadcast([128, 6, 32])
                    P.op('dve', lambda e, t1=t1, qn=qn, cosb=cosb: e.tensor_tensor(out=t1[:], in0=qn[:, :, 0:32], in1=cosb(), op=ALU.mult), [qnd, csd], [t1d])
                    P.op('pool', lambda e, t2=t2, qn=qn, sinb=sinb: e.tensor_tensor(out=t2[:], in0=qn[:, :, 32:64], in1=sinb(), op=ALU.mult), [qnd, csd], [t2d])
                    P.op('dve', lambda e, t1=t1, t2=t2, qb=qb: e.tensor_tensor(out=qb[:, :, 0:32], in0=t1[:], in1=t2[:], op=ALU.subtract), [t1d, t2d], [qbd])
                    P.op('dve', lambda e, t1=t1, qn=qn, sinb=sinb: e.tensor_tensor(out=t1[:], in0=qn[:, :, 0:32], in1=sinb(), op=ALU.mult), [qnd, csd], [t1d])
                    P.op('pool', lambda e, t2=t2, qn=qn, cosb=cosb: e.tensor_tensor(out=t2[:], in0=qn[:, :, 32:64], in1=cosb(), op=ALU.mult), [qnd, csd], [t2d])
                    P.op('dve', lambda e, t1=t1, t2=t2, qb=qb: e.tensor_tensor(out=qb[:, :, 32:64], in0=t1[:], in1=t2[:], op=ALU.add), [t1d, t2d], [qbd])
                for g in range(6):
                    P.op('pe', lambda e, pT=pT, qb=qb, g=g: e.transpose(out=pT[:, g, :], in_=qb[:, g, :], identity=ident[:]), [qbd, identd], [pTd])
                P.op('act', lambda e, pT=pT, t=t: e.copy(out=QKT[:, :, t * 128:(t + 1) * 128], in_=pT[:]), [pTd], [QKTd])
            P.barrier()
        attn_core(st, S, lambda h: QKT[:, h, :], QKTd, lambda g: QKT[:, 4 + g, :], QKTd, [0, 0, 1, 1], Vaug, Vaugd, 0.125, 256,
                  need_ctx, ident32, ident32d, 'gq')
        P.barrier()


def phase_mla(l, I, S, need_ctx, ident, identd, ident32, ident32d):
    nc, P = K.nc, K.P
    with ExitStack() as st:
        QKT, QKTd = sb(st, 'ml_QKT', [128, 8, NT], BF16)
        Vaug, Vaugd = sb(st, 'ml_V', [128, NTILE, 4, 65], BF16)
        with ExitStack() as st2:
            gain, gaind = sb(st2, 'ml_gain', [128, 384], F32)
            wst, wstd = sb(st2, 'ml_wst', [128, 2, 512], F32)
            wuq, wuqd = sb(st2, 'ml_wuq', [128, 2, 384], BF16)
            wukv, wukvd = sb(st2, 'ml_wukv', [128, 512], BF16)
            xr = Ring(K, st2, 'ml_x', [128, 416], F32, 3)
            sq, sqd = sb(st2, 'ml_sq', [128, 384], F32)
            ssr = Ring(K, st2, 'ml_ss', [128, 2], F32, 3)
            cnr = Ring(K, st2, 'ml_cn', [128, 384], F32, 2)
            cbr = Ring(K, st2, 'ml_cb', [128, 384], BF16, 2)
            cTr = Ring(K, st2, 'ml_cT', [128, 3, 128], BF16, 2)
            qsr = Ring(K, st2, 'ml_qs', [128, 4, 96], F32, 2)
            csr = Ring(K, st2, 'ml_cs', [128, 2, 16], F32, 3)
            t1r = Ring(K, st2, 'ml_t1', [128, 5, 16], F32, 2)
            t2r = Ring(K, st2, 'ml_t2', [128, 5, 16], F32, 2)
            rr = Ring(K, st2, 'ml_r', [128, 5, 32], F32, 2)
            qkr = Ring(K, st2, 'ml_qk', [128, 8, 128], BF16, 2)
            for (qk_, qkd_) in qkr.bufs:
                P.op('pool', lambda e, qk_=qk_: e.memset(qk_[:], 0.0), [], [qkd_])
            pcT = Ring(K, st2, 'ml_pcT', [128, 3, 128], BF16, 2, psum=True)
            pq = Ring(K, st2, 'ml_pq', [128, 384], F32, 1, psum=True)
            pkv = Ring(K, st2, 'ml_pkv', [128, 512], F32, 2, psum=True)
            pT2 = Ring(K, st2, 'ml_pT2', [128, 8, 128], BF16, 2, psum=True)
            P.dma('sp', lambda e: e.dma_start(out=gain[:, 0:256], in_=I['mla_q_norm_g'][l:l + 1, :].partition_broadcast(128)), [], [gaind])
            P.dma('sp', lambda e: e.dma_start(out=gain[:, 256:384], in_=I['mla_kv_norm_g'][l:l + 1, :].partition_broadcast(128)), [], [gaind])
            P.dma('sp', lambda e: e.dma_start(out=wst[:, :, 0:384], in_=I['mla_w_uq'][l].rearrange("(k p) n -> p k n", p=128)), [], [wstd])
            P.op('pool', lambda e: e.tensor_copy(out=wuq[:], in_=wst[:, :, 0:384]), [wstd], [wuqd])
            P.dma('sp', lambda e: e.dma_start(out=wst[:, 0, :], in_=I['mla_w_ukv'][l]), [wuqd], [wstd])
            P.op('pool', lambda e: e.tensor_copy(out=wukv[:], in_=wst[:, 0, :]), [wstd], [wukvd])
            P.op('pool', lambda e: e.memset(Vaug[:, :, :, 64:65], 1.0), [], [Vaugd])
            for t in range(NTILE):
                (x, xd) = xr.next()
                (ss, ssd) = ssr.next()
                (cn, cnd) = cnr.next()
                (cb, cbd) = cbr.next()
                (cT, cTd) = cTr.next()
                (qs, qsd) = qsr.next()
                (qk, qkd) = qkr.next()
                (r, rd) = rr.next()
                (pc, pcd) = pcT.next()
                (pqt, pqd) = pq.next()
                (pk, pkd) = pkv.next()
                (pT, pTd) = pT2.next()
                P.dma('sp', lambda e, x=x, t=t: e.dma_start(out=x[:], in_=S['p'][t * 128:(t + 1) * 128, ML0:ML0 + 416]), [S['p_d'][t]], [xd])
                if K.LVL < 2:
                    continue
                P.op('dve', lambda e, x=x: e.tensor_tensor(out=sq[:], in0=x[:, 0:384], in1=x[:, 0:384], op=ALU.mult), [xd], [sqd])
                P.op('dve', lambda e, ss=ss: e.tensor_reduce(out=ss[:, 0:1], in_=sq[:, 0:256], axis=AX.X, op=ALU.add), [sqd], [ssd])
                P.op('dve', lambda e, ss=ss: e.tensor_reduce(out=ss[:, 1:2], in_=sq[:, 256:384], axis=AX.X, op=ALU.add), [sqd], [ssd])
                P.op('act', lambda e, ss=ss: e.activation(out=ss[:, 0:1], in_=ss[:, 0:1], func=AF.Sqrt, bias=RMS_EPS, scale=1.0 / 256), [ssd], [ssd])
                P.op('act', lambda e, ss=ss: e.activation(out=ss[:, 1:2], in_=ss[:, 1:2], func=AF.Sqrt, bias=RMS_EPS, scale=1.0 / 128), [ssd], [ssd])
                P.op('dve', lambda e, ss=ss: e.reciprocal(out=ss[:], in_=ss[:]), [ssd], [ssd])
                P.op('dve', lambda e, x=x, cn=cn, ss=ss: e.scalar_tensor_tensor(out=cn[:, 0:256], in0=x[:, 0:256], scalar=ss[:, 0:1], in1=gain[:, 0:256], op0=ALU.mult, op1=ALU.mult),
                     [xd, ssd, gaind], [cnd])
                P.op('dve', lambda e, x=x, cn=cn, ss=ss: e.scalar_tensor_tensor(out=cn[:, 256:384], in0=x[:, 256:384], scalar=ss[:, 1:2], in1=gain[:, 256:384], op0=ALU.mult, op1=ALU.mult),
                     [xd, ssd, gaind], [cnd])
                P.op('pool', lambda e, cn=cn, cb=cb: e.tensor_copy(out=cb[:], in_=cn[:]), [cnd], [cbd])
                if K.LVL < 3:
                    continue
                for j in range(3):
                    P.op('pe', lambda e, pc=pc, cb=cb, j=j: e.transpose(out=pc[:, j, :], in_=cb[:, j * 128:(j + 1) * 128], identity=ident[:]), [cbd, identd], [pcd])
                if K.LVL < 2.3:
                    continue
                P.op('act', lambda e, cT=cT, pc=pc: e.copy(out=cT[:], in_=pc[:]), [pcd], [cTd])
                if K.LVL < 2.6:
                    continue
                for j in range(2):
                    P.op('pe', lambda e, pqt=pqt, cT=cT, j=j: e.matmul(pqt[:], lhsT=cT[:, j, :], rhs=wuq[:, j, :], start=(j == 0), stop=(j == 1)), [cTd, wuqd], [pqd])
                if K.LVL < 2.8:
                    continue
                for hf in range(2):
                    P.op('pe', lambda e, pk=pk, cT=cT, hf=hf: e.matmul(pk[:, hf * 256:(hf + 1) * 256], lhsT=cT[:, 2, :], rhs=wukv[:, hf * 256:(hf + 1) * 256], start=True, stop=True), [cTd, wukvd], [pkd])
                if K.LVL < 4:
                    continue
                P.op('act', lambda e, qs=qs, pqt=pqt: e.copy(out=qs[:], in_=pqt[:].rearrange("p (h d) -> p h d", d=96)), [pqd], [qsd])
                P.op('dve', lambda e, pk=pk, t=t: e.tensor_copy(out=Vaug[:, t, :, 0:64], in_=pk[:].rearrange("p (h d) -> p h d", d=128)[:, :, 64:128]), [pkd], [Vaugd])
                P.op('dve', lambda e, pk=pk, qk=qk: e.tensor_copy(out=qk[:, 4:8, 0:64], in_=pk[:].rearrange("p (h d) -> p h d", d=128)[:, :, 0:64]), [pkd], [qkd])
                P.op('pool', lambda e, qs=qs, qk=qk: e.tensor_copy(out=qk[:, 0:4, 0:64], in_=qs[:, :, 0:64]), [qsd], [qkd])
                P.op('pool', lambda e, r=r, qs=qs: e.tensor_copy(out=r[:, 0:4, :], in_=qs[:, :, 64:96]), [qsd], [rd])
                P.op('pool', lambda e, r=r, x=x: e.tensor_copy(out=r[:, 4, :], in_=x[:, 384:416]), [xd], [rd])
                if K.LVL < 5:
                    continue
                if t < 2:
                    P.op('dve', lambda e, r=r, qk=qk: e.tensor_copy(out=qk[:, 0:4, 64:96], in_=r[:, 0:4, :]), [rd], [qkd])
                    P.op('dve', lambda e, r=r, qk=qk: e.tensor_copy(out=qk[:, 4:8, 64:96], in_=r[:, 4, :].unsqueeze(1).to_broadcast([128, 4, 32])), [rd], [qkd])
                else:
                    (cs, csd) = csr.next()
                    (t1, t1d) = t1r.next()
                    (t2, t2d) = t2r.next()
                    r0 = (t - 2) * 128
                    P.dma('sp', lambda e, cs=cs, r0=r0: e.dma_start(out=cs[:], in_=I['rope_m'][r0:r0 + 128, :, :]), [], [csd])
                    cosb = lambda cs=cs: cs[:, 0, :].unsqueeze(1).to_broadcast([128, 5, 16])
                    sinb = lambda cs=cs: cs[:, 1, :].unsqueeze(1).to_broadcast([128, 5, 16])
                    P.op('dve', lambda e, t1=t1, r=r, cosb=cosb: e.tensor_tensor(out=t1[:], in0=r[:, :, 0:16], in1=cosb(), op=ALU.mult), [rd, csd], [t1d])
                    P.op('pool', lambda e, t2=t2, r=r, sinb=sinb: e.tensor_tensor(out=t2[:], in0=r[:, :, 16:32], in1=sinb(), op=ALU.mult), [rd, csd], [t2d])
                    P.op('dve', lambda e, t1=t1, t2=t2: e.tensor_tensor(out=t1[:], in0=t1[:], in1=t2[:], op=ALU.subtract), [t1d, t2d], [t1d])
                    P.op('dve', lambda e, t1=t1, qk=qk: e.tensor_copy(out=qk[:, 0:4, 64:80], in_=t1[:, 0:4, :]), [t1d], [qkd])
                    P.op('dve', lambda e, t1=t1, qk=qk: e.tensor_copy(out=qk[:, 4:8, 64:80], in_=t1[:, 4, :].unsqueeze(1).to_broadcast([128, 4, 16])), [t1d], [qkd])
                    (t1, t1d) = t1r.next()
                    (t2, t2d) = t2r.next()
                    P.op('dve', lambda e, t1=t1, r=r, sinb=sinb: e.tensor_tensor(out=t1[:], in0=r[:, :, 0:16], in1=sinb(), op=ALU.mult), [rd, csd], [t1d])
                    P.op('pool', lambda e, t2=t2, r=r, cosb=cosb: e.tensor_tensor(out=t2[:], in0=r[:, :, 16:32], in1=cosb(), op=ALU.mult), [rd, csd], [t2d])
                    P.op('dve', lambda e, t1=t1, t2=t2: e.tensor_tensor(out=t1[:], in0=t1[:], in1=t2[:], op=ALU.add), [t1d, t2d], [t1d])
                    P.op('dve', lambda e, t1=t1, qk=qk: e.tensor_copy(out=qk[:, 0:4, 80:96], in_=t1[:, 0:4, :]), [t1d], [qkd])
                    P.op('dve', lambda e, t1=t1, qk=qk: e.tensor_copy(out=qk[:, 4:8, 80:96], in_=t1[:, 4, :].unsqueeze(1).to_broadcast([128, 4, 16])), [t1d], [qkd])
                if K.LVL < 6:
                    continue
                for g in range(8):
                    P.op('pe', lambda e, pT=pT, qk=qk, g=g: e.transpose(out=pT[:, g, :], in_=qk[:, g, :], identity=ident[:]), [qkd, identd], [pTd])
                P.op('act', lambda e, pT=pT, t=t: e.copy(out=QKT[:, :, t * 128:(t + 1) * 128], in_=pT[:]), [pTd], [QKTd])
            P.barrier()
        if K.LVL < 7:
            return
        attn_core(st, S, lambda h: QKT[:, h, :], QKTd, lambda g: QKT[:, 4 + g, :], QKTd, [0, 1, 2, 3], Vaug, Vaugd, 96.0 ** -0.5, 512,
                  need_ctx, ident32, ident32d, 'ml')
        P.barrier()


BIG = 30000.0


def nat_plan():
    variants = {}
    plan = []
    for i in range(64):
        rs = min(max(i - 4, 0), 56)
        tiles = []
        for m in range(rs // 2, (rs + 7) // 2 + 1):
            dd = []
            for r in (2 * m, 2 * m + 1):
                dd.append(r - i + 7 if rs <= r < rs + 8 else -1)
            key = tuple(dd)
            if key not in variants:
                variants[key] = len(variants)
            tiles.append((2 + m, variants[key]))
        plan.append(tiles)
    vlist = [None] * len(variants)
    for k, v in variants.items():
        vlist[v] = k
    return plan, vlist


def nat_bias_table(nat_bias_l):
    plan, vlist = nat_plan()
    c = np.arange(64)
    cs = np.clip(c - 8, 0, 48)
    cp = np.arange(64)
    inwin = (cp[:, None] >= cs[None, :]) & (cp[:, None] < cs[None, :] + 16)
    off = np.clip(cp[:, None] - c[None, :] + 15, 0, 30)
    tab = np.full((128, 4, len(vlist), 64), -BIG, np.float32)
    for v, (d0, d1) in enumerate(vlist):
        for half, d in enumerate((d0, d1)):
            if d < 0:
                continue
            for h in range(4):
                vals = nat_bias_l[h, d][off]
                tab[half * 64:(half + 1) * 64, h, v, :] = np.where(inwin, vals, np.float32(-BIG))
    return tab


def phase_nat(l, I, S, need_ctx, ident, identd, ident32, ident32d):
    nc, P = K.nc, K.P
    plan, vlist = nat_plan()
    NV = len(vlist)
    with ExitStack() as st:
        QKT, QKTd = sb(st, 'na_QKT', [64, 8, NT], BF16)
        Vaug, Vaugd = sb(st, 'na_V', [128, NTILE, 4, 65], BF16)
        tb, tbd = sb(st, 'na_tb', [128, 4, NV, 64], BF16)
        with ExitStack() as st2:
            tbs, tbsd = sb(st2, 'na_tbs', [128, 4, NV, 64], F32)
            xr = Ring(K, st2, 'na_x', [128, 768], F32, 3)
            xbr = Ring(K, st2, 'na_xb', [128, 512], BF16, 2)
            pTr = Ring(K, st2, 'na_pT', [64, 8, 128], BF16, 2, psum=True)
            P.dma('sp', lambda e: e.dma_start(out=tbs[:], in_=I['nat_tab'][l]), [], [tbsd])
            P.op('dve', lambda e: e.tensor_scalar(out=tb[:], in0=tbs[:], scalar1=8.0, scalar2=None, op0=ALU.mult), [tbsd], [tbd])
            P.op('pool', lambda e: e.memset(Vaug[:, :, :, 64:65], 1.0), [], [Vaugd])
            for t in range(NTILE):
                (x, xd) = xr.next()
                (xb, xbd) = xbr.next()
                (pT, pTd) = pTr.next()
                P.dma('sp', lambda e, x=x, t=t: e.dma_start(out=x[:], in_=S['p'][t * 128:(t + 1) * 128, NA0:NA0 + 768]), [S['p_d'][t]], [xd])
                P.op('dve', lambda e, x=x, xb=xb: e.tensor_copy(out=xb[:], in_=x[:, 0:512]), [xd], [xbd])
                P.op('pool', lambda e, x=x, t=t: e.tensor_copy(out=Vaug[:, t, :, 0:64], in_=x[:, 512:768].rearrange("p (h d) -> p h d", d=64)), [xd], [Vaugd])
                for g in range(8):
                    P.op('pe', lambda e, pT=pT, xb=xb, g=g: e.transpose(out=pT[:, g, :], in_=xb[:, g * 64:(g + 1) * 64], identity=ident[:]), [xbd, identd], [pTd])
                P.op('act', lambda e, pT=pT, t=t: e.copy(out=QKT[:, :, t * 128:(t + 1) * 128], in_=pT[:]), [pTd], [QKTd])
            P.barrier()
        sr = Ring(K, st, 'na_S', [128, 512], F32, 3, psum=True)
        orr = Ring(K, st, 'na_O', [65, 512], F32, 2, psum=True)
        ptr = Ring(K, st, 'na_fT', [128, 65], F32, 2, psum=True)
        pr = Ring(K, st, 'na_P', [128, 512], BF16, 3)
        osbr = Ring(K, st, 'na_osb', [65, 512], F32, 2)
        rvr = Ring(K, st, 'na_rv', [128, 1], F32, 4)
        otr = Ring(K, st, 'na_ot', [128, 4, 256], F32, 2)
        blocks = []
        if need_ctx:
            blocks.append(None)
        for qb in range(8):
            blocks.append(qb)
        for qb in blocks:
            (ot, otd) = otr.next()
            if qb is None:
                q0, nq = 0, 256
            else:
                q0, nq = 256 + qb * 512, 512
            for h in range(4):
                (O, Od) = orr.next()
                if qb is None:
                    for i, kt in enumerate((0, 1)):
                        (Sp, Spd) = sr.next()
                        (Pt, Ptd) = pr.next()
                        P.op('pe', lambda e, Sp=Sp, h=h, kt=kt: e.matmul(Sp[:, 0:256], lhsT=QKT[:, 4 + h, kt * 128:(kt + 1) * 128], rhs=QKT[:, h, 0:256], start=True, stop=True),
                             [QKTd], [Spd])
                        P.op('act', lambda e, Sp=Sp, Pt=Pt: e.activation(out=Pt[:, 0:256], in_=Sp[:, 0:256], func=AF.Exp, scale=0.125), [Spd], [Ptd])
                        P.op('pe', lambda e, O=O, h=h, kt=kt, Pt=Pt, i=i: e.matmul(O[:, 0:256], lhsT=Vaug[:, kt, h, :], rhs=Pt[:, 0:256], start=(i == 0), stop=(i == 1)),
                             [Vaugd, Ptd], [Od])
                else:
                    for ri in range(8):
                        i = qb * 8 + ri
                        qt0 = 256 + i * 64
                        tiles = [(kt, None) for kt in (0, 1)] + plan[i]
                        (Sp, Spd) = sr.next()
                        (Pt, Ptd) = pr.next()
                        for j, (kt, v) in enumerate(tiles):
                            P.op('pe', lambda e, Sp=Sp, h=h, kt=kt, qt0=qt0, j=j, v=v: e.matmul(Sp[:, j * 64:(j + 1) * 64], lhsT=QKT[:, 4 + h, kt * 128:(kt + 1) * 128], rhs=QKT[:, h, qt0:qt0 + 64], start=True, stop=(v is None)),
                                 [QKTd], [Spd])
                            if v is not None:
                                P.op('pe', lambda e, Sp=Sp, h=h, j=j, v=v: e.matmul(Sp[:, j * 64:(j + 1) * 64], lhsT=ident[:], rhs=tb[:, h, v, :], start=False, stop=True),
                                     [identd, tbd], [Spd])
                        nk = len(tiles)
                        P.op('act', lambda e, Sp=Sp, Pt=Pt, nk=nk: e.activation(out=Pt[:, 0:nk * 64], in_=Sp[:, 0:nk * 64], func=AF.Exp, scale=0.125), [Spd], [Ptd])
                        for j, (kt, v) in enumerate(tiles):
                            P.op('pe', lambda e, O=O, h=h, kt=kt, Pt=Pt, j=j, ri=ri, nk=nk: e.matmul(O[:, ri * 64:(ri + 1) * 64], lhsT=Vaug[:, kt, h, :], rhs=Pt[:, j * 64:(j + 1) * 64], start=(j == 0), stop=(j == nk - 1)),
                                 [Vaugd, Ptd], [Od])
                attn_finalize((osbr, ptr, rvr), O, Od, h, ot, otd, nq, ident32, ident32d)
            nj = nq // 128
            P.dma('sp', lambda e, ot=ot, q0=q0, nj=nj: e.dma_start(out=S['ocat'][q0:q0 + nj * 128, 768:1024].rearrange("(j p) c -> p j c", p=128), in_=ot[:, 0:nj, :]),
                  [otd], [S['ocat_d'][t] for t in range(q0 // 128, q0 // 128 + nj)])
        P.barrier()


def o_tt(q, out, in0, in1, op, rd, wr):
    K.P.op(q, lambda e: e.tensor_tensor(out=out, in0=in0, in1=in1, op=op), rd, wr)


def o_stt(q, out, in0, scalar, in1, op0, op1, rd, wr):
    K.P.op(q, lambda e: e.scalar_tensor_tensor(out=out, in0=in0, scalar=scalar, in1=in1, op0=op0, op1=op1), rd, wr)


def o_ts(q, out, in0, s1, op0, rd, wr):
    K.P.op(q, lambda e: e.tensor_scalar(out=out, in0=in0, scalar1=s1, scalar2=None, op0=op0), rd, wr)


def o_act(out, in_, func, rd, wr, **kw):
    K.P.op('act', lambda e: e.activation(out=out, in_=in_, func=func, **kw), rd, wr)


def o_red(q, out, in_, op, rd, wr):
    K.P.op(q, lambda e: e.tensor_reduce(out=out, in_=in_, axis=AX.X, op=op), rd, wr)


def o_mm(out, lhsT, rhs, rd, wr, start=True, stop=True):
    K.P.op('pe', lambda e: e.matmul(out, lhsT=lhsT, rhs=rhs, start=start, stop=stop), rd, wr)


def o_tr(out, in_, ident, rd, wr):
    K.P.op('pe', lambda e: e.transpose(out=out, in_=in_, identity=ident), rd, wr)


def o_cp(q, out, in_, rd, wr):
    if q == 'act':
        K.P.op('act', lambda e: e.copy(out=out, in_=in_), rd, wr)
    else:
        K.P.op(q, lambda e: e.tensor_copy(out=out, in_=in_), rd, wr)


def o_rcp(out, in_, rd, wr):
    K.P.op('dve', lambda e: e.reciprocal(out=out, in_=in_), rd, wr)


def o_ms(q, out, val, wr):
    K.P.op(q, lambda e: e.memset(out, val), [], wr)


def o_dma(out, in_, rd, wr, q='sp', **kw):
    K.P.dma(q, lambda e: e.dma_start(out=out, in_=in_, **kw), rd, wr)


def bc_load(st, name, src_row, n):
    t, d = sb(st, name, [128, n], F32)
    o_dma(t[:], src_row.partition_broadcast(128), [], [d])
    return t, d


RCH = 8


def rev_tile(c):
    return 1 - c if c < 2 else 35 - c


def phase_rwkv(l, I, S, need_ctx, ident32, ident32d, J32, J32d):
    rwkv_prep(l, I, S, ident32, ident32d, J32, J32d)
    rwkv_scan(S)
    rwkv_readout(l, I, S, need_ctx, J32, J32d)


def rwkv_prep(l, I, S, ident32, ident32d, J32, J32d):
    P = K.P
    MUL, ADD, SUB = ALU.mult, ALU.add, ALU.subtract
    with ExitStack() as st:
        xr = Ring(K, st, 'rv_x', [128, 1024], F32, 2)
        xo = Ring(K, st, 'rv_o', [128, 1024], F32, 2)
        pr = Ring(K, st, 'rv_ps', [128, 512], F32, 2, psum=True)
        for c in range(NTILE):
            tt_ = rev_tile(c)
            (x, xd) = xr.next()
            (o, od) = xo.next()
            o_dma(x[:], S['p'][tt_ * 128:(tt_ + 1) * 128, 0:1024], [S['p_d'][tt_]], [xd])
            for hf in range(2):
                (ps_, psd) = pr.next()
                o_mm(ps_[:], J32[:], x[:, hf * 512:(hf + 1) * 512], [J32d, xd], [psd])
                o_cp('act' if hf else 'dve', o[:, hf * 512:(hf + 1) * 512], ps_[:], [psd], [od])
            o_dma(S['prw1'][c * 128:(c + 1) * 128, :], o[:], [od], [S['prw1_d'][c]])
        P.barrier()
    with ExitStack() as st:
        mub, mubd = bc_load(st, 'rp_mu', I['rwkv_mu'][l, :], 1024)
        kkb, kkbd = bc_load(st, 'rp_kk', I['rwkv_k_k'][l, :], 256)
        kab, kabd = bc_load(st, 'rp_ka', I['rwkv_k_a'][l, :], 256)
        rkb, rkbd = bc_load(st, 'rp_rk', I['rwkv_r_k'][l].rearrange("h d -> (h d)"), 256)
        omka, omkad = sb(st, 'rp_omka', [128, 256], F32)
        K.P.op('dve', lambda e: e.tensor_scalar(out=omka[:], in0=kab[:], scalar1=-1.0, scalar2=1.0, op0=MUL, op1=ADD), [kabd], [omkad])
        w0b, a0b, wup, aup = [], [], [], []
        for d in range(2):
            w0b.append(bc_load(st, 'rp_w0%d' % d, I['rwkv_w0'][l, d, :], 256))
            a0b.append(bc_load(st, 'rp_a0%d' % d, I['rwkv_a0'][l, d, :], 256))
            t, td = sb(st, 'rp_wup%d' % d, [64, 256], F32)
            o_dma(t[:], I['rwkv_w_up'][l, d], [], [td])
            wup.append((t, td))
            t, td = sb(st, 'rp_aup%d' % d, [64, 256], F32)
            o_dma(t[:], I['rwkv_a_up'][l, d], [], [td])
            aup.append((t, td))
        gup, gupd = sb(st, 'rp_gup', [128, 256], F32)
        o_dma(gup[:], I['rwkv_g_up'][l], [], [gupd])

        xr = Ring(K, st, 'rp_x', [128, 1024], F32, 2)
        pvr = Ring(K, st, 'rp_pv', [128, 1024], F32, 2)
        nxr = Ring(K, st, 'rp_nx', [128, 1024], F32, 2)
        xsr = Ring(K, st, 'rp_xs', [128, 1024], F32, 2)
        kkr = Ring(K, st, 'rp_kkn', [128, 256], F32, 2)
        sqr = Ring(K, st, 'rp_sq', [128, 256], F32, 2)
        ssr = Ring(K, st, 'rp_ss', [128, 4], F32, 4)
        smr = Ring(K, st, 'rp_sm', [128, 128], F32, 4)
        sTr = Ring(K, st, 'rp_sT', [128, 128], F32, 4)
        ur = Ring(K, st, 'rp_u', [128, 256], F32, 2)
        ar_ = Ring(K, st, 'rp_a', [128, 256], F32, 2)
        mr = Ring(K, st, 'rp_m', [128, 256], F32, 2)
        kdr = Ring(K, st, 'rp_kd', [128, 256], F32, 3)
        kkar = Ring(K, st, 'rp_kka', [128, 256], F32, 3)
        bor = Ring(K, st, 'rp_bo', [128, 256], F32, 3)
        gtr = Ring(K, st, 'rp_gt', [128, 256], F32, 2)
        nkkr = Ring(K, st, 'rp_nkk', [128, 4, 2, 64], F32, 2)
        rrr = Ring(K, st, 'rp_rr', [128, 4, 2, 64], F32, 2)
        decr = Ring(K, st, 'rp_dec', [128, 4, 2, 64], F32, 2)
        fakr = Ring(K, st, 'rp_fak', [128, 128, 4, 2], F32, 2)
        farr = Ring(K, st, 'rp_far', [128, 128, 4, 2], F32, 2)
        fwr = Ring(K, st, 'rp_fw', [128, 128, 4], F32, 2)
        for ring in (fakr, farr):
            for (b_, bd_) in ring.bufs:
                o_ms('pool', b_[:], 0.0, [bd_])
        pbig = Ring(K, st, 'rp_pb', [128, 256], F32, 3, psum=True)
        ptr_ = Ring(K, st, 'rp_pt', [128, 128], F32, 3, psum=True)

        def v3(ap):
            return ap.rearrange("p (h k) -> p h k", k=64)

        for c in range(NTILE):
            first = c in (0, 2)
            last = c in (1, NTILE - 1)
            r0 = c * 128
            (nkk, nkkd) = nkkr.next()
            (rr, rrd) = rrr.next()
            (dec, decd) = decr.next()
            for d in range(2):
                if d == 0:
                    src = lambda a, b: S['p'][a:b, 0:1024]
                    sdeps = S['p_d']
                else:
                    src = lambda a, b: S['prw1'][a:b, :]
                    sdeps = S['prw1_d']
                nb = [sdeps[c]] + ([sdeps[c - 1]] if c > 0 else []) + ([sdeps[c + 1]] if c < NTILE - 1 else [])
                (x, xd) = xr.next()
                (pv, pvd) = pvr.next()
                (nx, nxd) = nxr.next()
                (xs, xsd) = xsr.next()
                o_dma(x[:], src(r0, r0 + 128), nb, [xd])
                if first:
                    o_ms('pool', pv[:], 0.0, [pvd])
                    o_dma(pv[1:128, :], src(r0, r0 + 127), nb, [pvd])
                else:
                    o_dma(pv[:], src(r0 - 1, r0 + 127), nb, [pvd])
                if last:
                    o_ms('pool', nx[:], 0.0, [nxd])
                    o_dma(nx[0:127, :], src(r0 + 1, r0 + 128), nb, [nxd])
                else:
                    o_dma(nx[:], src(r0 + 1, r0 + 129), nb, [nxd])
                o_tt('pool', pv[:], pv[:], nx[:], ADD, [pvd, nxd], [pvd])
                o_stt('dve', pv[:], pv[:], 0.5, x[:], MUL, SUB, [pvd, xd], [pvd])
                o_tt('pool', pv[:], pv[:], mub[:], MUL, [pvd, mubd], [pvd])
                o_tt('dve', xs[:], x[:], pv[:], ADD, [xd, pvd], [xsd])
                r_ = xs[:, 0:256]
                k_ = xs[:, 256:512]
                v_ = xs[:, 512:768]
                (kkn, kknd) = kkr.next()
                (sq, sqd) = sqr.next()
                (ss, ssd) = ssr.next()
                o_tt('dve', kkn[:], k_, kkb[:], MUL, [xsd, kkbd], [kknd])
                o_tt('pool', sq[:], kkn[:], kkn[:], MUL, [kknd], [sqd])
                o_red('dve', ss[:], v3(sq[:]), ADD, [sqd], [ssd])
                o_act(ss[:], ss[:], AF.Sqrt, [ssd], [ssd], bias=1e-12, scale=1.0)
                o_rcp(ss[:], ss[:], [ssd], [ssd])
                o_tt('dve', v3(kkn[:]), v3(kkn[:]), ss[:].unsqueeze(2).to_broadcast([128, 4, 64]), MUL, [kknd, ssd], [kknd])
                o_ts('dve', nkk[:, :, d, :], v3(kkn[:]), -1.0, MUL, [kknd], [nkkd])
                o_cp('pool', rr[:, :, d, :], v3(r_), [xsd], [rrd])
                (tw, twd) = smr.next()
                (twT, twTd) = sTr.next()
                (pt, ptd) = ptr_.next()
                (pb, pbd) = pbig.next()
                (u, ud) = ur.next()
                o_act(tw[:, 0:64], xs[:, 768:832], AF.Tanh, [xsd], [twd])
                o_tr(pt[0:64, :], tw[:, 0:64], ident32[:], [twd, ident32d], [ptd])
                o_cp('act', twT[0:64, :], pt[0:64, :], [ptd], [twTd])
                o_mm(pb[:], twT[0:64, :], wup[d][0][:], [twTd, wup[d][1]], [pbd])
                o_tt('dve', u[:], pb[:], w0b[d][0][:], ADD, [pbd, w0b[d][1]], [ud])
                o_act(u[:], u[:], AF.Sigmoid, [ud], [ud])
                o_act(dec[:, :, d, :], v3(u[:]), AF.Exp, [ud], [decd], scale=-0.6065306597126334)
                (xa, xad) = smr.next()
                (xaT, xaTd) = sTr.next()
                (pt, ptd) = ptr_.next()
                (pb, pbd) = pbig.next()
                (a, ad) = ar_.next()
                o_cp('pool', xa[:, 0:64], xs[:, 832:896], [xsd], [xad])
                o_tr(pt[0:64, :], xa[:, 0:64], ident32[:], [xad, ident32d], [ptd])
                o_cp('act', xaT[0:64, :], pt[0:64, :], [ptd], [xaTd])
                o_mm(pb[:], xaT[0:64, :], aup[d][0][:], [xaTd, aup[d][1]], [pbd])
                o_tt('dve', a[:], pb[:], a0b[d][0][:], ADD, [pbd, a0b[d][1]], [ad])
                o_act(a[:], a[:], AF.Sigmoid, [ad], [ad])
                (m, md) = mr.next()
                (kd, kdd) = kdr.next()
                (kka, kkad) = kkar.next()
                (bo, bod) = bor.next()
                o_tt('pool', m[:], a[:], kab[:], MUL, [ad, kabd], [md])
                o_tt('pool', m[:], m[:], omka[:], ADD, [md, omkad], [md])
                o_tt('dve', kd[:], k_, m[:], MUL, [xsd, md], [kdd])
                o_tt('pool', kka[:], kkn[:], a[:], MUL, [kknd, ad], [kkad])
                (ss2, ss2d) = ssr.next()
                o_tt('dve', m[:], r_, kd[:], MUL, [xsd, kdd], [md])
                o_tt('pool', m[:], m[:], rkb[:], MUL, [md, rkbd], [md])
                o_red('dve', ss2[:], v3(m[:]), ADD, [md], [ss2d])
                o_tt('dve', v3(bo[:]), v3(v_), ss2[:].unsqueeze(2).to_broadcast([128, 4, 64]), MUL, [xsd, ss2d], [bod])
                o_dma(S['V2'][d, r0:r0 + 128, :], v_, [xsd], [S['V2_d'][c]])
                o_dma(S['KKA'][d, r0:r0 + 128, :], kka[:], [kkad], [S['KKA_d'][c]])
                o_dma(S['KD'][d, r0:r0 + 128, :], kd[:], [kdd], [S['KD_d'][c]])
                o_dma(S['BON'][d, r0:r0 + 128, :], bo[:], [bod], [S['BON_d'][c]])
                if d == 0:
                    (sg, sgd) = smr.next()
                    (sgT, sgTd) = sTr.next()
                    (pt, ptd) = ptr_.next()
                    (pb, pbd) = pbig.next()
                    (gt, gtd) = gtr.next()
                    o_act(sg[:], xs[:, 896:1024], AF.Sigmoid, [xsd], [sgd])
                    o_tr(pt[:], sg[:], ident32[:], [sgd, ident32d], [ptd])
                    o_cp('act', sgT[:], pt[:], [ptd], [sgTd])
                    o_mm(pb[:], sgT[:], gup[:], [sgTd, gupd], [pbd])
                    o_cp('dve', gt[:], pb[:], [pbd], [gtd])
                    o_dma(S['GATE'][r0:r0 + 128, :], gt[:], [gtd], [S['GATE_d'][c]])
            (fak, fakd) = fakr.next()
            (far, fard) = farr.next()
            (fw, fwd) = fwr.next()
            for h in range(4):
                for (srcT, srcd, dstF, dstd) in ((nkk, nkkd, fak, fakd), (rr, rrd, far, fard)):
                    (pt, ptd) = ptr_.next()
                    o_tr(pt[:], srcT[:, h, :, :].rearrange("p d k -> p (d k)"), ident32[:], [srcd, ident32d], [ptd])
                    eng_ = 'act' if h % 2 == 0 else 'dve'
                    o_cp(eng_, dstF[0:64, :, h, 0], pt[0:64, :], [ptd], [dstd])
                    o_cp(eng_, dstF[64:128, :, h, 1], pt[64:128, :], [ptd], [dstd])
                (pt, ptd) = ptr_.next()
                o_tr(pt[:], dec[:, h, :, :].rearrange("p d k -> p (d k)"), ident32[:], [decd, ident32d], [ptd])
                o_cp('act', fw[:, :, h], pt[:], [ptd], [fwd])
            o_dma(S['AKK'][:, r0:r0 + 128, :], fak[:].rearrange("p s h d -> p s (h d)"), [fakd], [S['AKK_d'][c]])
            o_dma(S['AR'][:, r0:r0 + 128, :], far[:].rearrange("p s h d -> p s (h d)"), [fard], [S['AR_d'][c]])
            o_dma(S['WD'][:, r0:r0 + 128, :], fw[:], [fwd], [S['WD_d'][c]])
        P.barrier()


def rwkv_scan(S, T=None):
    P = K.P
    T = NT if T is None else T
    CH = RCH
    nch = T // CH
    with ExitStack() as st:
        ST, STd = sb(st, 'sc_ST', [128, 256], F32)
        o_ms('pool', ST[:], 0.0, [STd])
        ST3 = ST[:].rearrange("p (h v) -> p h v", v=64)
        NB = 2
        akk = [sb(st, 'sc_akk%d' % i, [128, CH, 8], F32) for i in range(NB)]
        ar = [sb(st, 'sc_ar%d' % i, [128, CH, 8], F32) for i in range(NB)]
        wt = [sb(st, 'sc_wt%d' % i, [128, CH, 4], F32) for i in range(NB)]
        bt = [sb(st, 'sc_bt%d' % i, [4, CH, 4, 128], F32) for i in range(NB)]
        rt = [sb(st, 'sc_rt%d' % i, [4, CH, 256], F32) for i in range(NB)]
        rtv = [Dep('sc_rtv%d' % i) for i in range(NB)]
        yst = [sb(st, 'sc_y%d' % i, [2, CH, 256], F32) for i in range(NB)]
        for (b_, bd_) in bt:
            o_ms('pool', b_[:], 0.0, [bd_])
        sar = Ring(K, st, 'sc_sa', [128, 256], F32, 2, psum=True)
        yr = Ring(K, st, 'sc_yp', [128, 256], F32, 2, psum=True)
        ur = Ring(K, st, 'sc_u', [128, 256], F32, 2, psum=True)

        def load_chunk(c):
            i = c % NB
            s0 = c * CH
            tl = [s0 // 128]
            o_dma(akk[i][0][:], S['AKK'][:, s0:s0 + CH, :], [S['AKK_d'][t] for t in tl], [akk[i][1]])
            o_dma(ar[i][0][:], S['AR'][:, s0:s0 + CH, :], [S['AR_d'][t] for t in tl], [ar[i][1]])
            o_dma(wt[i][0][:], S['WD'][:, s0:s0 + CH, :], [S['WD_d'][t] for t in tl], [wt[i][1]])
            for which, nm in ((0, 'KKA'), (1, 'KD')):
                for d in range(2):
                    row = which * 2 + d
                    o_dma(bt[i][0][row:row + 1, :, :, d * 64:(d + 1) * 64],
                          S[nm][d:d + 1, s0:s0 + CH, :].rearrange("o s (h k) -> o s h k", k=64),
                          [S[nm + '_d'][t] for t in tl], [bt[i][1]])
            o_dma(rt[i][0][2:4, :, :], S['V2'][:, s0:s0 + CH, :], [S['V2_d'][t] for t in tl], [rtv[i]])

        load_chunk(0)
        pending = None
        for s in range(T):
            c, j = divmod(s, CH)
            i = c % NB
            if j == 0 and c + 1 < nch:
                load_chunk(c + 1)
            (SA, SAd) = sar.next()
            for h in range(4):
                o_mm(SA[0:2, h * 64:(h + 1) * 64], akk[i][0][:, j, 2 * h:2 * h + 2], ST[:, h * 64:(h + 1) * 64], [akk[i][1], STd], [SAd])
            if pending is not None:
                pending()
                pending = None
            o_cp('act', rt[i][0][0:2, j, :], SA[0:2, :], [SAd], [rt[i][1]])
            (U, Ud) = ur.next()
            for h in range(4):
                o_mm(U[:, h * 64:(h + 1) * 64], bt[i][0][0:4, j, h, :], rt[i][0][0:4, j, h * 64:(h + 1) * 64], [bt[i][1], rt[i][1], rtv[i]], [Ud])
            o_tt('dve', ST3, ST3, wt[i][0][:, j, :].unsqueeze(2).to_broadcast([128, 4, 64]), ALU.mult, [STd, wt[i][1]], [STd])
            o_tt('dve', ST[:], ST[:], U[:], ALU.add, [STd, Ud], [STd])

            def pend(i=i, j=j):
                (Yp, Ypd) = yr.next()
                for h in range(4):
                    o_mm(Yp[0:2, h * 64:(h + 1) * 64], ar[i][0][:, j, 2 * h:2 * h + 2], ST[:, h * 64:(h + 1) * 64], [ar[i][1], STd], [Ypd])
                o_cp('act', yst[i][0][0:2, j, :], Yp[0:2, :], [Ypd], [yst[i][1]])
            pending = pend
            if j == CH - 1:
                pending()
                pending = None
                s0 = c * CH
                o_dma(S['Y2'][:, s0:s0 + CH, :], yst[i][0][0:2, :, :], [yst[i][1]], [S['Y2_d'][s0 // 128]])
        P.barrier()


def rwkv_readout(l, I, S, need_ctx, J32, J32d):
    P = K.P
    MUL, ADD = ALU.mult, ALU.add
    with ExitStack() as st:
        lng, lngd = bc_load(st, 'ro_lng', I['rwkv_ln_g'][l, :], 256)
        lnb, lnbd = bc_load(st, 'ro_lnb', I['rwkv_ln_b'][l, :], 256)
        yr_ = Ring(K, st, 'ro_y', [128, 256], F32, 3)
        br_ = Ring(K, st, 'ro_b', [128, 256], F32, 3)
        gr_ = Ring(K, st, 'ro_g', [128, 256], F32, 2)
        ycr = Ring(K, st, 'ro_yc', [128, 256], F32, 3)
        sqr = Ring(K, st, 'ro_sq', [128, 256], F32, 2)
        smr = Ring(K, st, 'ro_sm', [128, 4], F32, 6)
        otr = Ring(K, st, 'ro_o', [128, 256], F32, 2)
        psr = Ring(K, st, 'ro_ps', [128, 256], F32, 2, psum=True)

        def v3(ap):
            return ap.rearrange("p (h k) -> p h k", k=64)

        def bc4(ap):
            return ap.unsqueeze(2).to_broadcast([128, 4, 64])

        for t in range(0 if need_ctx else 2, NTILE):
            outs = []
            for d in range(2):
                c = t if d == 0 else rev_tile(t)
                r0 = c * 128
                (y, yd) = yr_.next()
                (b, bd) = br_.next()
                (yc, ycd) = ycr.next()
                (sq, sqd) = sqr.next()
                (sm, smd) = smr.next()
                (vr, vrd) = smr.next()
                o_dma(y[:], S['Y2'][d, r0:r0 + 128, :], [S['Y2_d'][c]], [yd])
                o_dma(b[:], S['BON'][d, r0:r0 + 128, :], [S['BON_d'][c]], [bd])
                o_red('dve', sm[:], v3(y[:]), ADD, [yd], [smd])
                o_ts('dve', sm[:], sm[:], -1.0 / 64, MUL, [smd], [smd])
                o_tt('dve', v3(yc[:]), v3(y[:]), bc4(sm[:]), ADD, [yd, smd], [ycd])
                o_tt('pool', sq[:], yc[:], yc[:], MUL, [ycd], [sqd])
                o_red('dve', vr[:], v3(sq[:]), ADD, [sqd], [vrd])
                o_act(vr[:], vr[:], AF.Sqrt, [vrd], [vrd], bias=64e-5, scale=1.0 / 64)
                o_rcp(vr[:], vr[:], [vrd], [vrd])
                o_tt('dve', v3(yc[:]), v3(yc[:]), bc4(vr[:]), MUL, [ycd, vrd], [ycd])
                o_tt('pool', yc[:], yc[:], lng[:], MUL, [ycd, lngd], [ycd])
                o_tt('pool', yc[:], yc[:], lnb[:], ADD, [ycd, lnbd], [ycd])
                o_tt('dve', yc[:], yc[:], b[:], ADD, [ycd, bd], [ycd])
                outs.append((yc, ycd))
            (ps_, psd) = psr.next()
            (g, gd) = gr_.next()
            (ot, otd) = otr.next()
            o_dma(g[:], S['GATE'][t * 128:(t + 1) * 128, :], [S['GATE_d'][t]], [gd])
            o_mm(ps_[:], J32[:], outs[1][0][:], [J32d, outs[1][1]], [psd])
            o_tt('dve', ot[:], outs[0][0][:], ps_[:], ADD, [outs[0][1], psd], [otd])
            o_tt('dve', ot[:], ot[:], g[:], MUL, [otd, gd], [otd])
            o_dma(S['ocat'][t * 128:(t + 1) * 128, 0:256], ot[:], [otd], [S['ocat_d'][t]])
        P.barrier()


def phase_out(l, I, S, need_ctx, modL, modLd, modC, modCd, ident, identd):
    P = K.P
    MUL, ADD = ALU.mult, ALU.add
    with ExitStack() as st:
        wbf, wbfd = sb(st, 'wo_bf', [128, 8, D], BF16)
        wst = Ring(K, st, 'wo_st', [128, D], F32, 2)
        ocr = Ring(K, st, 'wo_oc', [128, D], F32, 2)
        ocbr = Ring(K, st, 'wo_ocb', [128, D], BF16, 2)
        oTr = Ring(K, st, 'wo_oT', [128, 8, 128], BF16, 2)
        xr = Ring(K, st, 'wo_x', [128, D], F32, 2)
        yr = Ring(K, st, 'wo_y', [128, D], F32, 2)
        sq, sqd = sb(st, 'wo_sq', [128, D], F32)
        ssr = Ring(K, st, 'wo_ss', [128, 1], F32, 4)
        pTr = Ring(K, st, 'wo_pT', [128, 8, 128], BF16, 2, psum=True)
        pyr = Ring(K, st, 'wo_py', [128, 512], F32, 4, psum=True)
        wo = I['w_out'][l].rearrange("(k p) n -> p k n", p=128)
        for kc in range(8):
            (ws, wsd) = wst.next()
            o_dma(ws[:], wo[:, kc, :], [], [wsd])
            o_cp('pool', wbf[:, kc, :], ws[:], [wsd], [wbfd])
        for t in range(0 if need_ctx else 2, NTILE):
            mt, mtd = (modC, modCd) if t < 2 else (modL, modLd)
            (oc, ocd) = ocr.next()
            (ocb, ocbd) = ocbr.next()
            (oT, oTd) = oTr.next()
            (x, xd) = xr.next()
            (y, yd) = yr.next()
            (ss, ssd) = ssr.next()
            (pT, pTd) = pTr.next()
            o_dma(oc[:], S['ocat'][t * 128:(t + 1) * 128, :], [S['ocat_d'][t]], [ocd])
            o_dma(x[:], S['xs'][t * 128:(t + 1) * 128, :], [S['xs_d'][t]], [xd])
            o_cp('pool', ocb[:], oc[:], [ocd], [ocbd])
            for kc in range(8):
                o_tr(pT[:, kc, :], ocb[:, kc * 128:(kc + 1) * 128], ident[:], [ocbd, identd], [pTd])
            o_cp('act', oT[:], pT[:], [pTd], [oTd])
            for hf in range(2):
                (py, pyd) = pyr.next()
                for kc in range(8):
                    o_mm(py[:], oT[:, kc, :], wbf[:, kc, hf * 512:(hf + 1) * 512], [oTd, wbfd], [pyd], start=(kc == 0), stop=(kc == 7))
                o_cp('act' if hf else 'dve', y[:, hf * 512:(hf + 1) * 512], py[:], [pyd], [yd])
            rms_rstd(y[:], yd, D, sq, sqd, ss, ssd, 0)
            o_stt('dve', y[:], y[:], ss[:, 0:1], mt[:, 2, :], MUL, MUL, [yd, ssd, mtd], [yd])
            o_tt('pool', x[:], x[:], y[:], ADD, [xd, yd], [xd])
            o_dma(S['xs'][t * 128:(t + 1) * 128, :], x[:], [xd], [S['xs_d'][t]])
        P.barrier()


def ffn_convert(I, S, moe, li):
    P = K.P
    NEx = NE if moe else 1
    tag = 'm' if moe else 'f'
    with ExitStack() as st:
        ldr = Ring(K, st, 'cv_ld', [128, DFF], F32, 3)
        cvr = Ring(K, st, 'cv_bf', [128, DFF], BF16, 3)
        n = 0
        engs = ('dve', 'pool', 'act')
        for e in range(NEx):
            for (nm, dst) in (('gate', 'wgb' + tag), ('up', 'wub' + tag)):
                src = (I['moe_w_' + nm][li, e] if moe else I['ffn_w_' + nm][li]).rearrange("(k p) n -> p k n", p=128)
                for kc in range(8):
                    (ld, ldd) = ldr.next()
                    (cv, cvd) = cvr.next()
                    o_dma(ld[:], src[:, kc, :], [], [ldd])
                    o_cp(engs[n % 3], cv[:], ld[:], [ldd], [cvd])
                    n += 1
                    for q4 in range(2):
                        f0, f1 = q4 * 11, (q4 + 1) * 11
                        o_dma(S[dst][e, f0:f1, :, kc, :].rearrange("f p n -> p f n"),
                              cv[:, f0 * 128:f1 * 128].rearrange("p (f n) -> p f n", n=128), [cvd], [Dep()])
            srcd = (I['moe_w_down'][li, e] if moe else I['ffn_w_down'][li]).rearrange("(f p) n -> p f n", p=128)
            for f0 in range(0, NFF, 2):
                (ld, ldd) = ldr.next()
                (cv, cvd) = cvr.next()
                o_dma(ld[:, 0:2048].rearrange("p (f n) -> p f n", n=1024), srcd[:, f0:f0 + 2, :], [], [ldd])
                o_cp(engs[n % 3], cv[:, 0:2048], ld[:, 0:2048], [ldd], [cvd])
                n += 1
                o_dma(S['wdb' + tag][e, f0:f0 + 2, :, :].rearrange("f p n -> p f n"),
                      cv[:, 0:2048].rearrange("p (f n) -> p f n", n=1024), [cvd], [Dep()])
        P.barrier()


def phase_ffn(l, I, S, need_ctx, modL, modLd, modC, modCd, ident32, ident32d, out_ap, out_d):
    P = K.P
    MUL, ADD, SUB = ALU.mult, ALU.add, ALU.subtract
    moe = (l % 2 == 1)
    li = l // 2
    NEx = NE if moe else 1
    tag = 'm' if moe else 'f'
    last_layer = not need_ctx
    ffn_convert(I, S, moe, li)
    tiles = list(range(0 if need_ctx else 2, NTILE))
    with ExitStack() as st:
        xrr = Ring(K, st, 'ff_x', [128, 2, D], F32, 2)
        hfr = Ring(K, st, 'ff_hf', [128, D], F32, 2)
        hTr = Ring(K, st, 'ff_hT', [128, 8, 256], BF16, 2)
        h32r = Ring(K, st, 'ff_h32', [128, 8, 128], F32, 2)
        sq, sqd = sb(st, 'ff_sq', [128, D], F32)
        ssr = Ring(K, st, 'ff_ss', [128, 1], F32, 4)
        accr = Ring(K, st, 'ff_acc', [128, 2, D], F32, 2)
        gtr = Ring(K, st, 'ff_gate', [128, 2, 8], F32, 2)
        smr = Ring(K, st, 'ff_sm', [128, 8], F32, 8)
        s1r = Ring(K, st, 'ff_s1', [128, 1], F32, 12)
        wgr = Ring(K, st, 'ff_wg', [128, 8, 128], BF16, 3)
        wur = Ring(K, st, 'ff_wu', [128, 8, 128], BF16, 3)
        wdr = Ring(K, st, 'ff_wd', [128, D], BF16, 3)
        sgr = Ring(K, st, 'ff_sg', [128, 256], F32, 2)
        GTr = Ring(K, st, 'ff_GT', [128, 256], BF16, 3)
        pTr = Ring(K, st, 'ff_pT', [128, 4, 128], F32, 1, psum=True)
        gur = Ring(K, st, 'ff_gu', [128, 2, 256], F32, 2, psum=True)
        ops_ = [[ps(st, 'ff_o%d%d' % (a, b), [128, 512], F32) for b in range(2)] for a in range(2)]
        plr = Ring(K, st, 'ff_pl', [128, 8], F32, 1, psum=True)
        if moe:
            rt_, rtd = sb(st, 'ff_router', [128, 8, 8], F32)
            o_dma(rt_[:], I['moe_router'][li].rearrange("(k p) e -> p k e", p=128), [], [rtd])
        for blk in range(len(tiles) // 2):
            tl = tiles[2 * blk:2 * blk + 2]
            (xr, xrd) = xrr.next()
            (hT, hTd) = hTr.next()
            (acc, accd) = accr.next()
            (gate, gated) = gtr.next()
            for jj, t in enumerate(tl):
                mt, mtd = (modC, modCd) if t < 2 else (modL, modLd)
                (hf, hfd) = hfr.next()
                (h32, h32d) = h32r.next()
                (ss, ssd) = ssr.next()
                o_dma(xr[:, jj, :], S['xs'][t * 128:(t + 1) * 128, :], [S['xs_d'][t]], [xrd])
                rms_rstd(xr[:, jj, :], xrd, D, sq, sqd, ss, ssd, 0)
                o_stt('dve', hf[:], xr[:, jj, :], ss[:, 0:1], mt[:, 4, :], MUL, MUL, [xrd, ssd, mtd], [hfd])
                o_tt('pool', hf[:], hf[:], mt[:, 3, :], ADD, [hfd, mtd], [hfd])
                for q4 in range(2):
                    (pT, pTd) = pTr.next()
                    for kk_ in range(4):
                        kc = q4 * 4 + kk_
                        o_tr(pT[:, kk_, :], hf[:, kc * 128:(kc + 1) * 128], ident32[:], [hfd, ident32d], [pTd])
                    o_cp('act', h32[:, q4 * 4:(q4 + 1) * 4, :], pT[:], [pTd], [h32d])
                    o_cp('pool', hT[:, q4 * 4:(q4 + 1) * 4, jj * 128:(jj + 1) * 128], h32[:, q4 * 4:(q4 + 1) * 4, :], [h32d], [hTd])
                if moe:
                    (pl, pld) = plr.next()
                    for kc in range(8):
                        o_mm(pl[:], h32[:, kc, :], rt_[:, kc, :], [h32d, rtd], [pld], start=(kc == 0), stop=(kc == 7))
                    (lg, lgd) = smr.next()
                    (lg2, lg2d) = smr.next()
                    (mk1, mk1d) = smr.next()
                    (mk2, mk2d) = smr.next()
                    (m1, m1d) = s1r.next()
                    (m2, m2d) = s1r.next()
                    (ex, exd) = s1r.next()
                    (w1, w1d) = s1r.next()
                    (w2, w2d) = s1r.next()
                    o_cp('dve', lg[:], pl[:], [pld], [lgd])
                    o_red('dve', m1[:], lg[:], ALU.max, [lgd], [m1d])
                    o_ts('dve', mk1[:], lg[:], m1[:, 0:1], ALU.is_equal, [lgd, m1d], [mk1d])
                    o_stt('dve', lg2[:], mk1[:], -1.0e30, lg[:], MUL, ADD, [mk1d, lgd], [lg2d])
                    o_red('dve', m2[:], lg2[:], ALU.max, [lg2d], [m2d])
                    o_ts('dve', mk2[:], lg2[:], m2[:, 0:1], ALU.is_equal, [lg2d, m2d], [mk2d])
                    o_tt('dve', ex[:], m2[:], m1[:], SUB, [m2d, m1d], [exd])
                    o_act(ex[:], ex[:], AF.Exp, [exd], [exd])
                    o_ts('dve', w1[:], ex[:], 1.0, ADD, [exd], [w1d])
                    o_rcp(w1[:], w1[:], [w1d], [w1d])
                    o_tt('dve', w2[:], ex[:], w1[:], MUL, [exd, w1d], [w2d])
                    o_ts('dve', gate[:, jj, :], mk1[:], w1[:, 0:1], MUL, [mk1d, w1d], [gated])
                    o_stt('dve', gate[:, jj, :], mk2[:], w2[:, 0:1], gate[:, jj, :], MUL, ADD, [mk2d, w2d, gated], [gated])
            for e in range(NEx):
                for f in range(NFF):
                    (wg, wgd) = wgr.next()
                    (wu, wud) = wur.next()
                    (wd, wdd) = wdr.next()
                    (gu, gud) = gur.next()
                    (sg, sgd) = sgr.next()
                    (GT, GTd) = GTr.next()
                    o_dma(wg[:], S['wgb' + tag][e, f], [], [wgd])
                    o_dma(wu[:], S['wub' + tag][e, f], [], [wud], q='pool')
                    o_dma(wd[:], S['wdb' + tag][e, f], [], [wdd])
                    for kc in range(8):
                        o_mm(gu[:, 0, :], wg[:, kc, :], hT[:, kc, :], [wgd, hTd], [gud], start=(kc == 0), stop=(kc == 7))
                    for kc in range(8):
                        o_mm(gu[:, 1, :], wu[:, kc, :], hT[:, kc, :], [wud, hTd], [gud], start=(kc == 0), stop=(kc == 7))
                    o_act(sg[:], gu[:, 0, :], AF.Silu, [gud], [sgd])
                    o_tt('dve', GT[:], sg[:], gu[:, 1, :], MUL, [sgd, gud], [GTd])
                    for jj in range(2):
                        for hf_ in range(2):
                            (op_, opd) = ops_[jj][hf_]
                            o_mm(op_[:], GT[:, jj * 128:(jj + 1) * 128], wd[:, hf_ * 512:(hf_ + 1) * 512], [GTd, wdd], [opd],
                                 start=(f == 0), stop=(f == NFF - 1))
                for jj in range(2):
                    for hf_ in range(2):
                        (op_, opd) = ops_[jj][hf_]
                        dst = acc[:, jj, hf_ * 512:(hf_ + 1) * 512]
                        if not moe:
                            o_cp('act', dst, op_[:], [opd], [accd])
                        elif e == 0:
                            o_ts('dve', dst, op_[:], gate[:, jj, e:e + 1], MUL, [opd, gated], [accd])
                        else:
                            o_stt('dve', dst, op_[:], gate[:, jj, e:e + 1], dst, MUL, ADD, [opd, gated, accd], [accd])
            for jj, t in enumerate(tl):
                mt, mtd = (modC, modCd) if t < 2 else (modL, modLd)
                (ss, ssd) = ssr.next()
                rms_rstd(acc[:, jj, :], accd, D, sq, sqd, ss, ssd, 0)
                o_stt('dve', acc[:, jj, :], acc[:, jj, :], ss[:, 0:1], mt[:, 5, :], MUL, MUL, [accd, ssd, mtd], [accd])
                o_tt('pool', acc[:, jj, :], acc[:, jj, :], xr[:, jj, :], ADD, [accd, xrd], [accd])
                if last_layer:
                    o_dma(out_ap[(t - 2) * 128:(t - 1) * 128, :], acc[:, jj, :], [accd], [out_d])
                else:
                    o_dma(S['xs'][t * 128:(t + 1) * 128, :], acc[:, jj, :], [accd], [S['xs_d'][t]])
        P.barrier()


IN_NAMES = ['x', 'c', 'ctx', 'c_ctx', 'mod_w', 'mod_b', 'norm_g', 'w_in', 'w_out', 'rwkv_mu', 'rwkv_w0', 'rwkv_w_up',
            'rwkv_a0', 'rwkv_a_up', 'rwkv_g_up', 'rwkv_k_k', 'rwkv_k_a', 'rwkv_r_k', 'rwkv_ln_g', 'rwkv_ln_b',
            'gqa_q_g', 'gqa_k_g', 'mla_q_norm_g', 'mla_w_uq', 'mla_kv_norm_g', 'mla_w_ukv', 'nat_bias',
            'ffn_w_gate', 'ffn_w_up', 'ffn_w_down', 'moe_router', 'moe_w_gate', 'moe_w_up', 'moe_w_down']


def build(shapes, stop_after=None, debug=(), only=None, layers=(0, 1), scan_T=None):
    nc = bass.Bass("TRN2", target_bir_lowering=False)
    K.nc = nc
    P = Prog(nc)
    K.P = P
    I = {}
    for name in IN_NAMES:
        I[name] = nc.dram_tensor(name, list(shapes[name]), F32, kind="ExternalInput").ap()
    out = nc.dram_tensor("out", [NL, D], F32, kind="ExternalOutput").ap()
    out_d = Dep('out')
    S = {}

    def scratch(name, shape, dtype=F32, tiled=True):
        S[name] = dram(name, shape, dtype)
        S[name + '_d'] = [Dep('%s%d' % (name, t)) for t in range(NTILE)] if tiled else Dep(name)

    scratch('xs', [NT, D])
    scratch('p', [NT, INC])
    scratch('ocat', [NT, D])
    scratch('prw1', [NT, 1024])
    for nm in ('V2', 'KKA', 'KD', 'BON', 'Y2'):
        scratch(nm, [2, NT, 256])
    scratch('GATE', [NT, 256])
    scratch('AKK', [128, NT, 8])
    scratch('AR', [128, NT, 8])
    scratch('WD', [128, NT, 4])
    for tag, ne in (('f', 1), ('m', NE)):
        scratch('wgb' + tag, [ne, NFF, 128, 8, 128], BF16, tiled=False)
        scratch('wub' + tag, [ne, NFF, 128, 8, 128], BF16, tiled=False)
        scratch('wdb' + tag, [ne, NFF, 128, D], BF16, tiled=False)
    I['rope_g'] = nc.dram_tensor('rope_g', [NL, 2, 32], F32, kind='ExternalInput').ap()
    I['rope_m'] = nc.dram_tensor('rope_m', [NL, 2, 16], F32, kind='ExternalInput').ap()
    I['nat_tab'] = nc.dram_tensor('nat_tab', [2, 128, 4, len(nat_plan()[1]), 64], F32, kind='ExternalInput').ap()
    with ExitStack() as st:
        P.alloc_sems(st)
        ident, identd = sb(st, 'ident', [128, 128], BF16)
        modL, modLd = sb(st, 'modL', [128, 6, D], F32)
        modC, modCd = sb(st, 'modC', [128, 6, D], F32)
        ident32, ident32d = sb(st, 'ident32', [128, 128], F32)
        J32, J32d = sb(st, 'J32', [128, 128], F32)
        P.op('pool', lambda e: e.memset(ident[:], 1.0), [], [identd])
        P.op('pool', lambda e: e.memset(ident32[:], 1.0), [], [ident32d])
        P.op('pool', lambda e: e.memset(J32[:], 1.0), [], [J32d])
        P.op('pool', lambda e: e.affine_select(out=ident32[:], in_=ident32[:], pattern=[[-1, 128]], compare_op=ALU.is_equal, fill=0.0, base=0, channel_multiplier=1),
             [ident32d], [ident32d])
        P.op('pool', lambda e: e.affine_select(out=ident[:], in_=ident[:], pattern=[[-1, 128]], compare_op=ALU.is_equal, fill=0.0, base=0, channel_multiplier=1),
             [identd], [identd])
        P.op('pool', lambda e: e.affine_select(out=J32[:], in_=J32[:], pattern=[[1, 128]], compare_op=ALU.is_equal, fill=0.0, base=-127, channel_multiplier=1),
             [J32d], [J32d])
        P.dma('sp', lambda e: e.dma_start(out=S['xs'][0:NC_, :], in_=I['ctx'][:, :]), [], S['xs_d'][0:2])
        for j in range(4):
            P.dma('sp', lambda e, j=j: e.dma_start(out=S['xs'][NC_ + j * 1024:NC_ + (j + 1) * 1024, :], in_=I['x'][j * 1024:(j + 1) * 1024, :]),
                  [], S['xs_d'][2 + 8 * j:2 + 8 * (j + 1)])

        def want(name):
            return only is None or name in only

        for l in layers:
            need_ctx = (l == 0)
            if want('mod'):
                phase_mod(l, I, modL, modLd, modC, modCd)
            if want('in'):
                phase_in(l, I, S, modL, modLd, modC, modCd, ident, identd)
            if want('rwkv'):
                if scan_T is None:
                    phase_rwkv(l, I, S, need_ctx, ident32, ident32d, J32, J32d)
                else:
                    rwkv_prep(l, I, S, ident32, ident32d, J32, J32d)
                    rwkv_scan(S, scan_T)
            if want('gqa'):
                phase_gqa(l, I, S, need_ctx, ident, identd, ident32, ident32d)
            if want('mla'):
                phase_mla(l, I, S, need_ctx, ident, identd, ident32, ident32d)
            if want('nat'):
                phase_nat(l, I, S, need_ctx, ident, identd, ident32, ident32d)
            if stop_after == ('attn', l):
                break
            if want('out'):
                phase_out(l, I, S, need_ctx, modL, modLd, modC, modCd, ident, identd)
            if stop_after == ('out', l):
                break
            if want('ffn'):
                phase_ffn(l, I, S, need_ctx, modL, modLd, modC, modCd, ident32, ident32d, out, out_d)
        for name in debug:
            src = S[name]
            d = nc.dram_tensor('dbg_' + name, list(src.shape), src.dtype, kind="ExternalOutput").ap()
            dd = Dep('dbg_' + name)
            deps = S[name + '_d'] if isinstance(S[name + '_d'], list) else [S[name + '_d']]
            P.dma('sp', lambda e, d=d, src=src: e.dma_start(out=d, in_=src), deps, [dd])
        P.barrier()
        P.emit()
    return nc


def core_shapes(inputs):
    sh = {k: tuple(np.asarray(v).shape) for k, v in inputs.items()}
    sh['x'] = (NL, D)
    sh['c'] = (D,)
    sh['ctx'] = (NC_, D)
    return sh


def rope_tables(rot_dim):
    t = np.arange(NL)
    row = (t // 64).astype(np.float32)
    col = (t % 64).astype(np.float32)
    quarter = rot_dim // 4
    inv_freq = (np.float32(10000.0) ** (-np.arange(quarter, dtype=np.float32) / np.float32(quarter))).astype(np.float32)
    ang = np.concatenate([row[:, None] * inv_freq, col[:, None] * inv_freq], axis=-1).astype(np.float32)
    return np.ascontiguousarray(np.stack([np.cos(ang), np.sin(ang)], axis=1).astype(np.float32))


def core_inputs(inputs, b, shared=None):
    if shared is None:
        shared = {k: np.ascontiguousarray(np.asarray(v, dtype=np.float32)) for k, v in inputs.items() if k not in ('x', 'c', 'ctx')}
        shared['rope_g'] = rope_tables(64)
        shared['rope_m'] = rope_tables(32)
        nb = np.asarray(inputs['nat_bias'], np.float32)
        shared['nat_tab'] = np.ascontiguousarray(np.stack([nat_bias_table(nb[0]), nat_bias_table(nb[1])], 0))
    m = dict(shared)
    m['x'] = np.ascontiguousarray(np.asarray(inputs['x'][b], dtype=np.float32))
    m['c'] = np.ascontiguousarray(np.asarray(inputs['c'][b], dtype=np.float32))
    m['ctx'] = np.ascontiguousarray(np.asarray(inputs['ctx'][b], dtype=np.float32))
    return m


def kernel(**inputs):
    nb = 4
    shapes = core_shapes(inputs)
    nc = build(shapes)
    first = core_inputs(inputs, 0)
    shared = {k: v for k, v in first.items() if k not in ('x', 'c', 'ctx')}
    maps = [first] + [core_inputs(inputs, b, shared) for b in range(1, nb)]
    res = run_bass_kernel_spmd(nc, maps, core_ids=list(range(nb)))
    return np.stack([np.asarray(res.results[b]['out'], dtype=np.float32) for b in range(nb)], axis=0)
```

```python
import numpy as np
from contextlib import ExitStack
import concourse.bass as bass
import concourse.mybir as mybir
from concourse.bass_utils import run_bass_kernel_spmd

F32 = mybir.dt.float32
BF16 = mybir.dt.bfloat16
AF = mybir.ActivationFunctionType
ALU = mybir.AluOpType
AX = mybir.AxisListType

COMPUTE = ('pe', 'act', 'dve', 'pool')
QUEUES = ('pe', 'act', 'dve', 'pool', 'sp')
NRING = 8

D = 1024
NL = 4096
NC_ = 256
NT = NL + NC_
NTILE = NT // 128
INC = 2720
RW0, GQ0, ML0, NA0 = 0, 1024, 1536, 1952
DFF = 2816
NFF = DFF // 128
NE = 8
RMS_EPS = 1e-6


class Dep:
    __slots__ = ('w', 'r', 'name')

    def __init__(self, name=''):
        self.w = None
        self.r = {}
        self.name = name


class Prog:
    def __init__(self, nc):
        self.nc = nc
        self.q = {e: [] for e in QUEUES}
        self.cnt = {e: 0 for e in COMPUTE}
        self.seen = {e: {} for e in QUEUES}
        self.sems = {}
        self.dma_cnt = {}
        self.dma_next = {e: 0 for e in QUEUES}
        self.ninstr = 0

    def alloc_sems(self, stack):
        for e in COMPUTE:
            self.sems[e] = stack.enter_context(self.nc.semaphore('s_' + e))
        for qn in ('sp', 'pool', 'act'):
            for j in range(NRING):
                key = ('dma', qn, j)
                self.sems[key] = stack.enter_context(self.nc.semaphore('d_%s_%d' % (qn, j)))
                self.dma_cnt[key] = 0

    def _need(self, queue, key, count, waits):
        if self.seen[queue].get(key, 0) >= count:
            return
        if waits.get(key, 0) < count:
            waits[key] = count

    def _emit_waits(self, queue, waits):
        for key, count in waits.items():
            sem = self.sems[key]
            val = count * (16 if isinstance(key, tuple) else 1)
            self.q[queue].append(lambda e, sem=sem, val=val: e.wait_ge(sem, val))
            self.seen[queue][key] = count
            self.ninstr += 1

    def _collect(self, queue, reads, writes):
        waits = {}
        for t in reads:
            if t.w is not None:
                self._need(queue, t.w[0], t.w[1], waits)
        for t in writes:
            if t.w is not None:
                if not (queue == 'pe' and t.w[0] == 'pe'):
                    self._need(queue, t.w[0], t.w[1], waits)
            for k, c in t.r.items():
                if k == queue:
                    continue
                self._need(queue, k, c, waits)
        return waits

    def op(self, queue, fn, reads=(), writes=()):
        waits = self._collect(queue, reads, writes)
        self._emit_waits(queue, waits)
        self.cnt[queue] += 1
        c = self.cnt[queue]
        sem = self.sems[queue]
        self.q[queue].append(lambda e, fn=fn, sem=sem: fn(e).then_inc(sem, 1))
        self.ninstr += 1
        for t in reads:
            if t.r.get(queue, 0) < c:
                t.r[queue] = c
        for t in writes:
            t.w = (queue, c)
            t.r = {}

    def dma(self, queue, fn, reads=(), writes=()):
        j = self.dma_next[queue]
        self.dma_next[queue] = (j + 1) % NRING
        key = ('dma', queue, j)
        waits = self._collect(queue, reads, writes)
        n = self.dma_cnt[key]
        if n > 0:
            self._need(queue, key, n, waits)
        self._emit_waits(queue, waits)
        self.dma_cnt[key] = n + 1
        sem = self.sems[key]
        self.q[queue].append(lambda e, fn=fn, sem=sem: fn(e).then_inc(sem, 16))
        self.ninstr += 1
        for t in reads:
            t.r[key] = n + 1
        for t in writes:
            t.w = (key, n + 1)
            t.r = {}

    def barrier(self, queues=QUEUES):
        for qn in queues:
            waits = {}
            for e in COMPUTE:
                if self.cnt[e] > 0 and e != qn:
                    self._need(qn, e, self.cnt[e], waits)
            for key, n in self.dma_cnt.items():
                if n > 0:
                    self._need(qn, key, n, waits)
            self._emit_waits(qn, waits)

    def emit(self):
        nc = self.nc
        with nc.Block() as block:
            @block.tensor
            def _(e):
                for f in self.q['pe']:
                    f(e)

            @block.scalar
            def _(e):
                for f in self.q['act']:
                    f(e)

            @block.vector
            def _(e):
                for f in self.q['dve']:
                    f(e)

            @block.gpsimd
            def _(e):
                for f in self.q['pool']:
                    f(e)

            @block.sync
            def _(e):
                for f in self.q['sp']:
                    f(e)


class Ring:
    def __init__(self, K, st, name, shape, dtype, n, psum=False):
        self.bufs = []
        for i in range(n):
            if psum:
                full = [128, 512] if dtype == F32 else [128, 1024]
                n = 1
                for d_ in shape[1:]:
                    n *= d_
                assert n <= full[1]
                t = st.enter_context(K.nc.psum_tensor(uname('%s%d' % (name, i)), full, dtype))
                t = t[0:shape[0], 0:n]
                if len(shape) == 3:
                    t = t.rearrange("p (a b) -> p a b", b=shape[2])
            else:
                t = st.enter_context(K.nc.sbuf_tensor(uname('%s%d' % (name, i)), shape, dtype))
            self.bufs.append((t, Dep('%s%d' % (name, i))))
        self.i = 0

    def next(self):
        b = self.bufs[self.i]
        self.i = (self.i + 1) % len(self.bufs)
        return b


class K:
    LVL = 99
    UID = 0


def uname(name):
    K.UID += 1
    return '%s_u%d' % (name, K.UID)


def sb(st, name, shape, dtype):
    return st.enter_context(K.nc.sbuf_tensor(uname(name), shape, dtype)), Dep(name)


def ps(st, name, shape, dtype):
    return st.enter_context(K.nc.psum_tensor(uname(name), shape, dtype)), Dep(name)


def dram(name, shape, dtype):
    return K.nc.dram_tensor(name, shape, dtype, kind="Internal").ap()


def rms_rstd(x_ap, xdep, n, sq, sqd, ss, ssd, col):
    P = K.P
    P.op('dve', lambda e: e.tensor_tensor(out=sq[:, 0:n], in0=x_ap, in1=x_ap, op=ALU.mult), [xdep], [sqd])
    P.op('dve', lambda e: e.tensor_reduce(out=ss[:, col:col + 1], in_=sq[:, 0:n], axis=AX.X, op=ALU.add), [sqd], [ssd])
    P.op('act', lambda e: e.activation(out=ss[:, col:col + 1], in_=ss[:, col:col + 1], func=AF.Sqrt, bias=RMS_EPS, scale=1.0 / n), [ssd], [ssd])
    P.op('dve', lambda e: e.reciprocal(out=ss[:, col:col + 1], in_=ss[:, col:col + 1]), [ssd], [ssd])


def phase_mod(l, I, modL, modLd, modC, modCd):
    nc, P = K.nc, K.P
    with ExitStack() as st:
        cv, cvd = sb(st, 'cv', [128, 2, 8], F32)
        cs, csd = sb(st, 'cs', [128, 2, 8], F32)
        crep, crepd = sb(st, 'crep', [128, 2, 8, 128], BF16)
        gb, gbd = sb(st, 'gb', [128, 4, D], F32)
        wst = Ring(K, st, 'mw_st', [128, 8, 512], F32, 2)
        wbf = Ring(K, st, 'mw_bf', [128, 8, 512], BF16, 2)
        bb = Ring(K, st, 'mbb', [128, 512], F32, 2)
        pm = Ring(K, st, 'pmod', [128, 512], F32, 4, psum=True)
        P.dma('sp', lambda e: e.dma_start(out=cv[:, 0, :], in_=I['c'].rearrange("(k p) -> p k", p=128), allow_slow_non_contiguous=True), [], [cvd])
        P.dma('sp', lambda e: e.dma_start(out=cv[:, 1, :], in_=I['c_ctx'].rearrange("(k p) -> p k", p=128), allow_slow_non_contiguous=True), [], [cvd])
        P.dma('sp', lambda e: e.dma_start(out=gb[:], in_=I['norm_g'][l].partition_broadcast(128)), [], [gbd])
        P.op('act', lambda e: e.activation(out=cs[:], in_=cv[:], func=AF.Silu), [cvd], [csd])
        P.op('dve', lambda e: e.tensor_copy(out=crep[:], in_=cs[:].unsqueeze(3).to_broadcast([128, 2, 8, 128])), [csd], [crepd])
        mw = I['mod_w'][l].rearrange("(k p) n -> p k n", p=128)
        for nb in range(12):
            (ws, wsd) = wst.next()
            (wb, wbd) = wbf.next()
            (bt, btd) = bb.next()
            P.dma('sp', lambda e, ws=ws, nb=nb: e.dma_start(out=ws[:], in_=mw[:, :, nb * 512:(nb + 1) * 512]), [], [wsd])
            P.dma('sp', lambda e, bt=bt, nb=nb: e.dma_start(out=bt[:], in_=I['mod_b'][l, nb * 512:(nb + 1) * 512].partition_broadcast(128)), [], [btd])
            P.op('pool', lambda e, ws=ws, wb=wb: e.tensor_copy(out=wb[:], in_=ws[:]), [wsd], [wbd])
            j, off = nb // 2, (nb % 2) * 512
            for s, (mt, mtd) in enumerate(((modL, modLd), (modC, modCd))):
                (pt, ptd) = pm.next()
                for kc in range(8):
                    P.op('pe', lambda e, pt=pt, wb=wb, kc=kc, s=s: e.matmul(pt[:], lhsT=crep[:, s, kc, :], rhs=wb[:, kc, :], start=(kc == 0), stop=(kc == 7)),
                         [crepd, wbd], [ptd])
                P.op('dve', lambda e, pt=pt, bt=bt, mt=mt, j=j, off=off: e.tensor_tensor(out=mt[:, j, off:off + 512], in0=pt[:], in1=bt[:], op=ALU.add),
                     [ptd, btd], [mtd])
        for (mt, mtd) in ((modL, modLd), (modC, modCd)):
            for j, gi, plus1 in ((1, 0, True), (2, 1, False), (4, 2, True), (5, 3, False)):
                if plus1:
                    P.op('dve', lambda e, mt=mt, j=j, gi=gi: e.scalar_tensor_tensor(out=mt[:, j, :], in0=mt[:, j, :], scalar=1.0, in1=gb[:, gi, :], op0=ALU.add, op1=ALU.mult),
                         [mtd, gbd], [mtd])
                else:
                    P.op('dve', lambda e, mt=mt, j=j, gi=gi: e.tensor_tensor(out=mt[:, j, :], in0=mt[:, j, :], in1=gb[:, gi, :], op=ALU.mult),
                         [mtd, gbd], [mtd])
        P.barrier()


def phase_in(l, I, S, modL, modLd, modC, modCd, ident, identd):
    nc, P = K.nc, K.P
    with ExitStack() as st:
        wbf, wbfd = sb(st, 'win_bf', [128, 8, INC], BF16)
        wst = Ring(K, st, 'win_st', [128, INC], F32, 2)
        xr = Ring(K, st, 'in_x', [128, D], F32, 3)
        hr = Ring(K, st, 'in_h', [128, D], F32, 2)
        hbr = Ring(K, st, 'in_hb', [128, D], BF16, 2)
        hTr = Ring(K, st, 'in_hT', [128, 8, 128], BF16, 2)
        sq, sqd = sb(st, 'in_sq', [128, D], F32)
        ssr = Ring(K, st, 'in_ss', [128, 1], F32, 4)
        pr = Ring(K, st, 'in_p', [128, INC], F32, 2)
        ptr = Ring(K, st, 'in_pT', [128, 8, 128], BF16, 2, psum=True)
        pmr = Ring(K, st, 'in_pm', [128, 512], F32, 4, psum=True)
        win = I['w_in'][l].rearrange("(k p) n -> p k n", p=128)
        for kc in range(8):
            (ws, wsd) = wst.next()
            P.dma('sp', lambda e, ws=ws, kc=kc: e.dma_start(out=ws[:], in_=win[:, kc, :]), [], [wsd])
            P.op('pool', lambda e, ws=ws, kc=kc: e.tensor_copy(out=wbf[:, kc, :], in_=ws[:]), [wsd], [wbfd])
        for t in range(NTILE):
            mt, mtd = (modC, modCd) if t < 2 else (modL, modLd)
            (x, xd) = xr.next()
            (h, hd) = hr.next()
            (hb, hbd) = hbr.next()
            (hT, hTd) = hTr.next()
            (ss, ssd) = ssr.next()
            (pt, ptd) = pr.next()
            (pT, pTd) = ptr.next()
            P.dma('sp', lambda e, x=x, t=t: e.dma_start(out=x[:], in_=S['xs'][t * 128:(t + 1) * 128, :]), [S['xs_d'][t]], [xd])
            rms_rstd(x[:], xd, D, sq, sqd, ss, ssd, 0)
            P.op('dve', lambda e, h=h, x=x, ss=ss, mt=mt: e.scalar_tensor_tensor(out=h[:], in0=x[:], scalar=ss[:, 0:1], in1=mt[:, 1, :], op0=ALU.mult, op1=ALU.mult),
                 [xd, ssd, mtd], [hd])
            P.op('pool', lambda e, h=h, hb=hb, mt=mt: e.tensor_tensor(out=hb[:], in0=h[:], in1=mt[:, 0, :], op=ALU.add), [hd, mtd], [hbd])
            for kc in range(8):
                P.op('pe', lambda e, pT=pT, hb=hb, kc=kc: e.transpose(out=pT[:, kc, :], in_=hb[:, kc * 128:(kc + 1) * 128], identity=ident[:]),
                     [hbd, identd], [pTd])
            P.op('act', lambda e, hT=hT, pT=pT: e.copy(out=hT[:], in_=pT[:]), [pTd], [hTd])
            for nb in range(6):
                c0 = nb * 512
                cw = min(512, INC - c0)
                (pm, pmd) = pmr.next()
                for kc in range(8):
                    P.op('pe', lambda e, pm=pm, hT=hT, kc=kc, c0=c0, cw=cw: e.matmul(pm[:, 0:cw], lhsT=hT[:, kc, :], rhs=wbf[:, kc, c0:c0 + cw], start=(kc == 0), stop=(kc == 7)),
                         [hTd, wbfd], [pmd])
                if nb % 2 == 0:
                    P.op('act', lambda e, pm=pm, pt=pt, c0=c0, cw=cw: e.copy(out=pt[:, c0:c0 + cw], in_=pm[:, 0:cw]), [pmd], [ptd])
                else:
                    P.op('dve', lambda e, pm=pm, pt=pt, c0=c0, cw=cw: e.tensor_copy(out=pt[:, c0:c0 + cw], in_=pm[:, 0:cw]), [pmd], [ptd])
            P.dma('sp', lambda e, pt=pt, t=t: e.dma_start(out=S['p'][t * 128:(t + 1) * 128, :], in_=pt[:]), [ptd], [S['p_d'][t]])
        P.barrier()


def attn_finalize(st_rings, O, Od, h, ot, otd, nq, ident32, ident32d):
    P = K.P
    osbr, ptr, rvr = st_rings
    (osb, osbd) = osbr.next()
    P.op('dve', lambda e: e.tensor_copy(out=osb[:, 0:nq], in_=O[:, 0:nq]), [Od], [osbd])
    for j in range(nq // 128):
        (pt, ptd) = ptr.next()
        (rv, rvd) = rvr.next()
        P.op('pe', lambda e, pt=pt, osb=osb, j=j: e.transpose(out=pt[:], in_=osb[:, j * 128:(j + 1) * 128], identity=ident32[0:65, 0:65]),
             [osbd, ident32d], [ptd])
        P.op('dve', lambda e, pt=pt, rv=rv: e.reciprocal(out=rv[:], in_=pt[:, 64:65]), [ptd], [rvd])
        P.op('dve', lambda e, pt=pt, rv=rv, j=j: e.tensor_scalar(out=ot[:, j, h * 64:(h + 1) * 64], in0=pt[:, 0:64], scalar1=rv[:, 0:1], scalar2=None, op0=ALU.mult),
             [ptd, rvd], [otd])


def attn_core(st, S, QT, QTd, KT, KTd, kvmap, Vaug, Vaugd, scale, col0, need_ctx, ident32, ident32d, tag):
    P = K.P
    sr = Ring(K, st, tag + '_S', [128, 512], F32, 3, psum=True)
    orr = Ring(K, st, tag + '_O', [65, 512], F32, 2, psum=True)
    ptr = Ring(K, st, tag + '_fT', [128, 65], F32, 2, psum=True)
    pr = Ring(K, st, tag + '_P', [128, 512], BF16, 3)
    osbr = Ring(K, st, tag + '_osb', [65, 512], F32, 2)
    rvr = Ring(K, st, tag + '_rv', [128, 1], F32, 4)
    otr = Ring(K, st, tag + '_ot', [128, 4, 256], F32, 2)
    blocks = []
    if need_ctx:
        blocks.append((0, 256, [0, 1]))
    for qb in range(8):
        blocks.append((256 + qb * 512, 512, list(range(NTILE))))
    for (q0, nq, kts) in blocks:
        (ot, otd) = otr.next()
        for h in range(4):
            g = kvmap[h]
            (O, Od) = orr.next()
            pend = None
            for i, kt in enumerate(kts):
                (Sp, Spd) = sr.next()
                (Pt, Ptd) = pr.next()
                P.op('pe', lambda e, Sp=Sp, g=g, h=h, kt=kt, q0=q0, nq=nq: e.matmul(Sp[:, 0:nq], lhsT=KT(g)[:, kt * 128:(kt + 1) * 128], rhs=QT(h)[:, q0:q0 + nq], start=True, stop=True),
                     [QTd, KTd], [Spd])
                P.op('act', lambda e, Sp=Sp, Pt=Pt, nq=nq: e.activation(out=Pt[:, 0:nq], in_=Sp[:, 0:nq], func=AF.Exp, scale=scale), [Spd], [Ptd])
                if pend is not None:
                    pend()

                def pend(O=O, Od=Od, g=g, kt=kt, Pt=Pt, Ptd=Ptd, nq=nq, i=i, n=len(kts)):
                    P.op('pe', lambda e: e.matmul(O[:, 0:nq], lhsT=Vaug[:, kt, g, :], rhs=Pt[:, 0:nq], start=(i == 0), stop=(i == n - 1)),
                         [Vaugd, Ptd], [Od])
            pend()
            attn_finalize((osbr, ptr, rvr), O, Od, h, ot, otd, nq, ident32, ident32d)
        nj = nq // 128
        P.dma('sp', lambda e, ot=ot, q0=q0, nj=nj: e.dma_start(out=S['ocat'][q0:q0 + nj * 128, col0:col0 + 256].rearrange("(j p) c -> p j c", p=128), in_=ot[:, 0:nj, :]),
              [otd], [S['ocat_d'][t] for t in range(q0 // 128, q0 // 128 + nj)])


def phase_gqa(l, I, S, need_ctx, ident, identd, ident32, ident32d):
    nc, P = K.nc, K.P
    with ExitStack() as st:
        QKT, QKTd = sb(st, 'gq_QKT', [64, 6, NT], BF16)
        Vaug, Vaugd = sb(st, 'gq_V', [128, NTILE, 2, 65], BF16)
        with ExitStack() as st2:
            gain, gaind = sb(st2, 'gq_gain', [128, 6, 64], F32)
            xr = Ring(K, st2, 'gq_x', [128, 512], F32, 3)
            sq, sqd = sb(st2, 'gq_sq', [128, 384], F32)
            ssr = Ring(K, st2, 'gq_ss', [128, 6], F32, 3)
            qnr = Ring(K, st2, 'gq_qn', [128, 6, 64], F32, 2)
            qbr = Ring(K, st2, 'gq_qb', [128, 6, 64], BF16, 2)
            csr = Ring(K, st2, 'gq_cs', [128, 2, 32], F32, 3)
            t1r = Ring(K, st2, 'gq_t1', [128, 6, 32], F32, 2)
            t2r = Ring(K, st2, 'gq_t2', [128, 6, 32], F32, 2)
            pTr = Ring(K, st2, 'gq_pT', [64, 6, 128], BF16, 2, psum=True)
            P.dma('sp', lambda e: e.dma_start(out=gain[:, 0:4, :], in_=I['gqa_q_g'][l:l + 1, :].partition_broadcast(128).to_broadcast([128, 4, 64])), [], [gaind])
            P.dma('sp', lambda e: e.dma_start(out=gain[:, 4:6, :], in_=I['gqa_k_g'][l:l + 1, :].partition_broadcast(128).to_broadcast([128, 2, 64])), [], [gaind])
            P.op('pool', lambda e: e.memset(Vaug[:, :, :, 64:65], 1.0), [], [Vaugd])
            for t in range(NTILE):
                (x, xd) = xr.next()
                (ss, ssd) = ssr.next()
                (qn, qnd) = qnr.next()
                (qb, qbd) = qbr.next()
                (pT, pTd) = pTr.next()
                P.dma('sp', lambda e, x=x, t=t: e.dma_start(out=x[:], in_=S['p'][t * 128:(t + 1) * 128, GQ0:GQ0 + 512]), [S['p_d'][t]], [xd])
                P.op('dve', lambda e, x=x: e.tensor_tensor(out=sq[:], in0=x[:, 0:384], in1=x[:, 0:384], op=ALU.mult), [xd], [sqd])
                P.op('dve', lambda e, ss=ss: e.tensor_reduce(out=ss[:], in_=sq[:].rearrange("p (g d) -> p g d", d=64), axis=AX.X, op=ALU.add), [sqd], [ssd])
                P.op('act', lambda e, ss=ss: e.activation(out=ss[:], in_=ss[:], func=AF.Sqrt, bias=RMS_EPS, scale=1.0 / 64), [ssd], [ssd])
                P.op('dve', lambda e, ss=ss: e.reciprocal(out=ss[:], in_=ss[:]), [ssd], [ssd])
                P.op('dve', lambda e, x=x, qn=qn, ss=ss: e.tensor_tensor(out=qn[:], in0=x[:, 0:384].rearrange("p (g d) -> p g d", d=64), in1=ss[:].unsqueeze(2).to_broadcast([128, 6, 64]), op=ALU.mult),
                     [xd, ssd], [qnd])
                P.op('pool', lambda e, x=x, t=t: e.tensor_copy(out=Vaug[:, t, :, 0:64], in_=x[:, 384:512].rearrange("p (g d) -> p g d", d=64)), [xd], [Vaugd])
                if t < 2:
                    P.op('dve', lambda e, qn=qn, qb=qb: e.tensor_tensor(out=qb[:], in0=qn[:], in1=gain[:], op=ALU.mult), [qnd, gaind], [qbd])
                else:
                    (cs, csd) = csr.next()
                    (t1, t1d) = t1r.next()
                    (t2, t2d) = t2r.next()
                    r0 = (t - 2) * 128
                    P.dma('sp', lambda e, cs=cs, r0=r0: e.dma_start(out=cs[:], in_=I['rope_g'][r0:r0 + 128, :, :]), [], [csd])
                    P.op('dve', lambda e, qn=qn: e.tensor_tensor(out=qn[:], in0=qn[:], in1=gain[:], op=ALU.mult), [qnd, gaind], [qnd])
                    cosb = lambda cs=cs: cs[:, 0, :].unsqueeze(1).to_broadcast([128, 6, 32])
                    sinb = lambda cs=cs: cs[:, 1, :].unsqueeze(1).to_broadcast([128, 6, 32])
                    P.op('dve', lambda e, t1=t1, qn=qn, cosb=cosb: e.tensor_tensor(out=t1[:], in0=qn[:, :, 0:32], in1=cosb(), op=ALU.mult), [qnd, csd], [t1d])
                    P.op('pool', lambda e, t2=t2, qn=qn, sinb=sinb: e.tensor_tensor(out=t2[:], in0=qn[:, :, 32:64], in1=sinb(), op=ALU.mult), [qnd, csd], [t2d])
                    P.op('dve', lambda e, t1=t1, t2=t2, qb=qb: e.tensor_tensor(out=qb[:, :, 0:32], in0=t1[:], in1=t2[:], op=ALU.subtract), [t1d, t2d], [qbd])
                    P.op('dve', lambda e, t1=t1, qn=qn, sinb=sinb: e.tensor_tensor(out=t1[:], in0=qn[:, :, 0:32], in1=sinb(), op=ALU.mult), [qnd, csd], [t1d])
                    P.op('pool', lambda e, t2=t2, qn=qn, cosb=cosb: e.tensor_tensor(out=t2[:], in0=qn[:, :, 32:64], in1=cosb(), op=ALU.mult), [qnd, csd], [t2d])
                    P.op('dve', lambda e, t1=t1, t2=t2, qb=qb: e.tensor_tensor(out=qb[:, :, 32:64], in0=t1[:], in1=t2[:], op=ALU.add), [t1d, t2d], [qbd])
                for g in range(6):
                    P.op('pe', lambda e, pT=pT, qb=qb, g=g: e.transpose(out=pT[:, g, :], in_=qb[:, g, :], identity=ident[:]), [qbd, identd], [pTd])
                P.op('act', lambda e, pT=pT, t=t: e.copy(out=QKT[:, :, t * 128:(t + 1) * 128], in_=pT[:]), [pTd], [QKTd])
            P.barrier()
        attn_core(st, S, lambda h: QKT[:, h, :], QKTd, lambda g: QKT[:, 4 + g, :], QKTd, [0, 0, 1, 1], Vaug, Vaugd, 0.125, 256,
                  need_ctx, ident32, ident32d, 'gq')
        P.barrier()


def phase_mla(l, I, S, need_ctx, ident, identd, ident32, ident32d):
    nc, P = K.nc, K.P
    with ExitStack() as st:
        QKT, QKTd = sb(st, 'ml_QKT', [128, 8, NT], BF16)
        Vaug, Vaugd = sb(st, 'ml_V', [128, NTILE, 4, 65], BF16)
        with ExitStack() as st2:
            gain, gaind = sb(st2, 'ml_gain', [128, 384], F32)
            wst, wstd = sb(st2, 'ml_wst', [128, 2, 512], F32)
            wuq, wuqd = sb(st2, 'ml_wuq', [128, 2, 384], BF16)
            wukv, wukvd = sb(st2, 'ml_wukv', [128, 512], BF16)
            xr = Ring(K, st2, 'ml_x', [128, 416], F32, 3)
            sq, sqd = sb(st2, 'ml_sq', [128, 384], F32)
            ssr = Ring(K, st2, 'ml_ss', [128, 2], F32, 3)
            cnr = Ring(K, st2, 'ml_cn', [128, 384], F32, 2)
            cbr = Ring(K, st2, 'ml_cb', [128, 384], BF16, 2)
            cTr = Ring(K, st2, 'ml_cT', [128, 3, 128], BF16, 2)
            qsr = Ring(K, st2, 'ml_qs', [128, 4, 96], F32, 2)
            csr = Ring(K, st2, 'ml_cs', [128, 2, 16], F32, 3)
            t1r = Ring(K, st2, 'ml_t1', [128, 5, 16], F32, 2)
            t2r = Ring(K, st2, 'ml_t2', [128, 5, 16], F32, 2)
            rr = Ring(K, st2, 'ml_r', [128, 5, 32], F32, 2)
            qkr = Ring(K, st2, 'ml_qk', [128, 8, 128], BF16, 2)
            for (qk_, qkd_) in qkr.bufs:
                P.op('pool', lambda e, qk_=qk_: e.memset(qk_[:], 0.0), [], [qkd_])
            pcT = Ring(K, st2, 'ml_pcT', [128, 3, 128], BF16, 2, psum=True)
            pq = Ring(K, st2, 'ml_pq', [128, 384], F32, 1, psum=True)
            pkv = Ring(K, st2, 'ml_pkv', [128, 512], F32, 2, psum=True)
            pT2 = Ring(K, st2, 'ml_pT2', [128, 8, 128], BF16, 2, psum=True)
            P.dma('sp', lambda e: e.dma_start(out=gain[:, 0:256], in_=I['mla_q_norm_g'][l:l + 1, :].partition_broadcast(128)), [], [gaind])
            P.dma('sp', lambda e: e.dma_start(out=gain[:, 256:384], in_=I['mla_kv_norm_g'][l:l + 1, :].partition_broadcast(128)), [], [gaind])
            P.dma('sp', lambda e: e.dma_start(out=wst[:, :, 0:384], in_=I['mla_w_uq'][l].rearrange("(k p) n -> p k n", p=128)), [], [wstd])
            P.op('pool', lambda e: e.tensor_copy(out=wuq[:], in_=wst[:, :, 0:384]), [wstd], [wuqd])
            P.dma('sp', lambda e: e.dma_start(out=wst[:, 0, :], in_=I['mla_w_ukv'][l]), [wuqd], [wstd])
            P.op('pool', lambda e: e.tensor_copy(out=wukv[:], in_=wst[:, 0, :]), [wstd], [wukvd])
            P.op('pool', lambda e: e.memset(Vaug[:, :, :, 64:65], 1.0), [], [Vaugd])
            for t in range(NTILE):
                (x, xd) = xr.next()
                (ss, ssd) = ssr.next()
                (cn, cnd) = cnr.next()
                (cb, cbd) = cbr.next()
                (cT, cTd) = cTr.next()
                (qs, qsd) = qsr.next()
                (qk, qkd) = qkr.next()
                (r, rd) = rr.next()
                (pc, pcd) = pcT.next()
                (pqt, pqd) = pq.next()
                (pk, pkd) = pkv.next()
                (pT, pTd) = pT2.next()
                P.dma('sp', lambda e, x=x, t=t: e.dma_start(out=x[:], in_=S['p'][t * 128:(t + 1) * 128, ML0:ML0 + 416]), [S['p_d'][t]], [xd])
                if K.LVL < 2:
                    continue
                P.op('dve', lambda e, x=x: e.tensor_tensor(out=sq[:], in0=x[:, 0:384], in1=x[:, 0:384], op=ALU.mult), [xd], [sqd])
                P.op('dve', lambda e, ss=ss: e.tensor_reduce(out=ss[:, 0:1], in_=sq[:, 0:256], axis=AX.X, op=ALU.add), [sqd], [ssd])
                P.op('dve', lambda e, ss=ss: e.tensor_reduce(out=ss[:, 1:2], in_=sq[:, 256:384], axis=AX.X, op=ALU.add), [sqd], [ssd])
                P.op('act', lambda e, ss=ss: e.activation(out=ss[:, 0:1], in_=ss[:, 0:1], func=AF.Sqrt, bias=RMS_EPS, scale=1.0 / 256), [ssd], [ssd])
                P.op('act', lambda e, ss=ss: e.activation(out=ss[:, 1:2], in_=ss[:, 1:2], func=AF.Sqrt, bias=RMS_EPS, scale=1.0 / 128), [ssd], [ssd])
                P.op('dve', lambda e, ss=ss: e.reciprocal(out=ss[:], in_=ss[:]), [ssd], [ssd])
                P.op('dve', lambda e, x=x, cn=cn, ss=ss: e.scalar_tensor_tensor(out=cn[:, 0:256], in0=x[:, 0:256], scalar=ss[:, 0:1], in1=gain[:, 0:256], op0=ALU.mult, op1=ALU.mult),
                     [xd, ssd, gaind], [cnd])
                P.op('dve', lambda e, x=x, cn=cn, ss=ss: e.scalar_tensor_tensor(out=cn[:, 256:384], in0=x[:, 256:384], scalar=ss[:, 1:2], in1=gain[:, 256:384], op0=ALU.mult, op1=ALU.mult),
                     [xd, ssd, gaind], [cnd])
                P.op('pool', lambda e, cn=cn, cb=cb: e.tensor_copy(out=cb[:], in_=cn[:]), [cnd], [cbd])
                if K.LVL < 3:
                    continue
                for j in range(3):
                    P.op('pe', lambda e, pc=pc, cb=cb, j=j: e.transpose(out=pc[:, j, :], in_=cb[:, j * 128:(j + 1) * 128], identity=ident[:]), [cbd, identd], [pcd])
                if K.LVL < 2.3:
                    continue
                P.op('act', lambda e, cT=cT, pc=pc: e.copy(out=cT[:], in_=pc[:]), [pcd], [cTd])
                if K.LVL < 2.6:
                    continue
                for j in range(2):
                    P.op('pe', lambda e, pqt=pqt, cT=cT, j=j: e.matmul(pqt[:], lhsT=cT[:, j, :], rhs=wuq[:, j, :], start=(j == 0), stop=(j == 1)), [cTd, wuqd], [pqd])
                if K.LVL < 2.8:
                    continue
                for hf in range(2):
                    P.op('pe', lambda e, pk=pk, cT=cT, hf=hf: e.matmul(pk[:, hf * 256:(hf + 1) * 256], lhsT=cT[:, 2, :], rhs=wukv[:, hf * 256:(hf + 1) * 256], start=True, stop=True), [cTd, wukvd], [pkd])
                if K.LVL < 4:
                    continue
                P.op('act', lambda e, qs=qs, pqt=pqt: e.copy(out=qs[:], in_=pqt[:].rearrange("p (h d) -> p h d", d=96)), [pqd], [qsd])
                P.op('dve', lambda e, pk=pk, t=t: e.tensor_copy(out=Vaug[:, t, :, 0:64], in_=pk[:].rearrange("p (h d) -> p h d", d=128)[:, :, 64:128]), [pkd], [Vaugd])
                P.op('dve', lambda e, pk=pk, qk=qk: e.tensor_copy(out=qk[:, 4:8, 0:64], in_=pk[:].rearrange("p (h d) -> p h d", d=128)[:, :, 0:64]), [pkd], [qkd])
                P.op('pool', lambda e, qs=qs, qk=qk: e.tensor_copy(out=qk[:, 0:4, 0:64], in_=qs[:, :, 0:64]), [qsd], [qkd])
                P.op('pool', lambda e, r=r, qs=qs: e.tensor_copy(out=r[:, 0:4, :], in_=qs[:, :, 64:96]), [qsd], [rd])
                P.op('pool', lambda e, r=r, x=x: e.tensor_copy(out=r[:, 4, :], in_=x[:, 384:416]), [xd], [rd])
                if K.LVL < 5:
                    continue
                if t < 2:
                    P.op('dve', lambda e, r=r, qk=qk: e.tensor_copy(out=qk[:, 0:4, 64:96], in_=r[:, 0:4, :]), [rd], [qkd])
                    P.op('dve', lambda e, r=r, qk=qk: e.tensor_copy(out=qk[:, 4:8, 64:96], in_=r[:, 4, :].unsqueeze(1).to_broadcast([128, 4, 32])), [rd], [qkd])
                else:
                    (cs, csd) = csr.next()
                    (t1, t1d) = t1r.next()
                    (t2, t2d) = t2r.next()
                    r0 = (t - 2) * 128
                    P.dma('sp', lambda e, cs=cs, r0=r0: e.dma_start(out=cs[:], in_=I['rope_m'][r0:r0 + 128, :, :]), [], [csd])
                    cosb = lambda cs=cs: cs[:, 0, :].unsqueeze(1).to_broadcast([128, 5, 16])
                    sinb = lambda cs=cs: cs[:, 1, :].unsqueeze(1).to_broadcast([128, 5, 16])
                    P.op('dve', lambda e, t1=t1, r=r, cosb=cosb: e.tensor_tensor(out=t1[:], in0=r[:, :, 0:16], in1=cosb(), op=ALU.mult), [rd, csd], [t1d])
                    P.op('pool', lambda e, t2=t2, r=r, sinb=sinb: e.tensor_tensor(out=t2[:], in0=r[:, :, 16:32], in1=sinb(), op=ALU.mult), [rd, csd], [t2d])
                    P.op('dve', lambda e, t1=t1, t2=t2: e.tensor_tensor(out=t1[:], in0=t1[:], in1=t2[:], op=ALU.subtract), [t1d, t2d], [t1d])
                    P.op('dve', lambda e, t1=t1, qk=qk: e.tensor_copy(out=qk[:, 0:4, 64:80], in_=t1[:, 0:4, :]), [t1d], [qkd])
                    P.op('dve', lambda e, t1=t1, qk=qk: e.tensor_copy(out=qk[:, 4:8, 64:80], in_=t1[:, 4, :].unsqueeze(1).to_broadcast([128, 4, 16])), [t1d], [qkd])
                    (t1, t1d) = t1r.next()
                    (t2, t2d) = t2r.next()
                    P.op('dve', lambda e, t1=t1, r=r, sinb=sinb: e.tensor_tensor(out=t1[:], in0=r[:, :, 0:16], in1=sinb(), op=ALU.mult), [rd, csd], [t1d])
                    P.op('pool', lambda e, t2=t2, r=r, cosb=cosb: e.tensor_tensor(out=t2[:], in0=r[:, :, 16:32], in1=cosb(), op=ALU.mult), [rd, csd], [t2d])
                    P.op('dve', lambda e, t1=t1, t2=t2: e.tensor_tensor(out=t1[:], in0=t1[:], in1=t2[:], op=ALU.add), [t1d, t2d], [t1d])
                    P.op('dve', lambda e, t1=t1, qk=qk: e.tensor_copy(out=qk[:, 0:4, 80:96], in_=t1[:, 0:4, :]), [t1d], [qkd])
                    P.op('dve', lambda e, t1=t1, qk=qk: e.tensor_copy(out=qk[:, 4:8, 80:96], in_=t1[:, 4, :].unsqueeze(1).to_broadcast([128, 4, 16])), [t1d], [qkd])
                if K.LVL < 6:
                    continue
                for g in range(8):
                    P.op('pe', lambda e, pT=pT, qk=qk, g=g: e.transpose(out=pT[:, g, :], in_=qk[:, g, :], identity=ident[:]), [qkd, identd], [pTd])
                P.op('act', lambda e, pT=pT, t=t: e.copy(out=QKT[:, :, t * 128:(t + 1) * 128], in_=pT[:]), [pTd], [QKTd])
            P.barrier()
        if K.LVL < 7:
            return
        attn_core(st, S, lambda h: QKT[:, h, :], QKTd, lambda g: QKT[:, 4 + g, :], QKTd, [0, 1, 2, 3], Vaug, Vaugd, 96.0 ** -0.5, 512,
                  need_ctx, ident32, ident32d, 'ml')
        P.barrier()


BIG = 30000.0


def nat_plan():
    variants = {}
    plan = []
    for i in range(64):
        rs = min(max(i - 4, 0), 56)
        tiles = []
        for m in range(rs // 2, (rs + 7) // 2 + 1):
            dd = []
            for r in (2 * m, 2 * m + 1):
                dd.append(r - i + 7 if rs <= r < rs + 8 else -1)
            key = tuple(dd)
            if key not in variants:
                variants[key] = len(variants)
            tiles.append((2 + m, variants[key]))
        plan.append(tiles)
    vlist = [None] * len(variants)
    for k, v in variants.items():
        vlist[v] = k
    return plan, vlist


def nat_bias_table(nat_bias_l):
    plan, vlist = nat_plan()
    c = np.arange(64)
    cs = np.clip(c - 8, 0, 48)
    cp = np.arange(64)
    inwin = (cp[:, None] >= cs[None, :]) & (cp[:, None] < cs[None, :] + 16)
    off = np.clip(cp[:, None] - c[None, :] + 15, 0, 30)
    tab = np.full((128, 4, len(vlist), 64), -BIG, np.float32)
    for v, (d0, d1) in enumerate(vlist):
        for half, d in enumerate((d0, d1)):
            if d < 0:
                continue
            for h in range(4):
                vals = nat_bias_l[h, d][off]
                tab[half * 64:(half + 1) * 64, h, v, :] = np.where(inwin, vals, np.float32(-BIG))
    return tab


def phase_nat(l, I, S, need_ctx, ident, identd, ident32, ident32d):
    nc, P = K.nc, K.P
    plan, vlist = nat_plan()
    NV = len(vlist)
    with ExitStack() as st:
        QKT, QKTd = sb(st, 'na_QKT', [64, 8, NT], BF16)
        Vaug, Vaugd = sb(st, 'na_V', [128, NTILE, 4, 65], BF16)
        tb, tbd = sb(st, 'na_tb', [128, 4, NV, 64], BF16)
        with ExitStack() as st2:
            tbs, tbsd = sb(st2, 'na_tbs', [128, 4, NV, 64], F32)
            xr = Ring(K, st2, 'na_x', [128, 768], F32, 3)
            xbr = Ring(K, st2, 'na_xb', [128, 512], BF16, 2)
            pTr = Ring(K, st2, 'na_pT', [64, 8, 128], BF16, 2, psum=True)
            P.dma('sp', lambda e: e.dma_start(out=tbs[:], in_=I['nat_tab'][l]), [], [tbsd])
            P.op('dve', lambda e: e.tensor_scalar(out=tb[:], in0=tbs[:], scalar1=8.0, scalar2=None, op0=ALU.mult), [tbsd], [tbd])
            P.op('pool', lambda e: e.memset(Vaug[:, :, :, 64:65], 1.0), [], [Vaugd])
            for t in range(NTILE):
                (x, xd) = xr.next()
                (xb, xbd) = xbr.next()
                (pT, pTd) = pTr.next()
                P.dma('sp', lambda e, x=x, t=t: e.dma_start(out=x[:], in_=S['p'][t * 128:(t + 1) * 128, NA0:NA0 + 768]), [S['p_d'][t]], [xd])
                P.op('dve', lambda e, x=x, xb=xb: e.tensor_copy(out=xb[:], in_=x[:, 0:512]), [xd], [xbd])
                P.op('pool', lambda e, x=x, t=t: e.tensor_copy(out=Vaug[:, t, :, 0:64], in_=x[:, 512:768].rearrange("p (h d) -> p h d", d=64)), [xd], [Vaugd])
                for g in range(8):
                    P.op('pe', lambda e, pT=pT, xb=xb, g=g: e.transpose(out=pT[:, g, :], in_=xb[:, g * 64:(g + 1) * 64], identity=ident[:]), [xbd, identd], [pTd])
                P.op('act', lambda e, pT=pT, t=t: e.copy(out=QKT[:, :, t * 128:(t + 1) * 128], in_=pT[:]), [pTd], [QKTd])
            P.barrier()
        sr = Ring(K, st, 'na_S', [128, 512], F32, 3, psum=True)
        orr = Ring(K, st, 'na_O', [65, 512], F32, 2, psum=True)
        ptr = Ring(K, st, 'na_fT', [128, 65], F32, 2, psum=True)
        pr = Ring(K, st, 'na_P', [128, 512], BF16, 3)
        osbr = Ring(K, st, 'na_osb', [65, 512], F32, 2)
        rvr = Ring(K, st, 'na_rv', [128, 1], F32, 4)
        otr = Ring(K, st, 'na_ot', [128, 4, 256], F32, 2)
        blocks = []
        if need_ctx:
            blocks.append(None)
        for qb in range(8):
            blocks.append(qb)
        for qb in blocks:
            (ot, otd) = otr.next()
            if qb is None:
                q0, nq = 0, 256
            else:
                q0, nq = 256 + qb * 512, 512
            for h in range(4):
                (O, Od) = orr.next()
                if qb is None:
                    for i, kt in enumerate((0, 1)):
                        (Sp, Spd) = sr.next()
                        (Pt, Ptd) = pr.next()
                        P.op('pe', lambda e, Sp=Sp, h=h, kt=kt: e.matmul(Sp[:, 0:256], lhsT=QKT[:, 4 + h, kt * 128:(kt + 1) * 128], rhs=QKT[:, h, 0:256], start=True, stop=True),
                             [QKTd], [Spd])
                        P.op('act', lambda e, Sp=Sp, Pt=Pt: e.activation(out=Pt[:, 0:256], in_=Sp[:, 0:256], func=AF.Exp, scale=0.125), [Spd], [Ptd])
                        P.op('pe', lambda e, O=O, h=h, kt=kt, Pt=Pt, i=i: e.matmul(O[:, 0:256], lhsT=Vaug[:, kt, h, :], rhs=Pt[:, 0:256], start=(i == 0), stop=(i == 1)),
                             [Vaugd, Ptd], [Od])
                else:
                    for ri in range(8):
                        i = qb * 8 + ri
                        qt0 = 256 + i * 64
                        tiles = [(kt, None) for kt in (0, 1)] + plan[i]
                        (Sp, Spd) = sr.next()
                        (Pt, Ptd) = pr.next()
                        for j, (kt, v) in enumerate(tiles):
                            P.op('pe', lambda e, Sp=Sp, h=h, kt=kt, qt0=qt0, j=j, v=v: e.matmul(Sp[:, j * 64:(j + 1) * 64], lhsT=QKT[:, 4 + h, kt * 128:(kt + 1) * 128], rhs=QKT[:, h, qt0:qt0 + 64], start=True, stop=(v is None)),
                                 [QKTd], [Spd])
                            if v is not None:
                                P.op('pe', lambda e, Sp=Sp, h=h, j=j, v=v: e.matmul(Sp[:, j * 64:(j + 1) * 64], lhsT=ident[:], rhs=tb[:, h, v, :], start=False, stop=True),
                                     [identd, tbd], [Spd])
                        nk = len(tiles)
                        P.op('act', lambda e, Sp=Sp, Pt=Pt, nk=nk: e.activation(out=Pt[:, 0:nk * 64], in_=Sp[:, 0:nk * 64], func=AF.Exp, scale=0.125), [Spd], [Ptd])
                        for j, (kt, v) in enumerate(tiles):
                            P.op('pe', lambda e, O=O, h=h, kt=kt, Pt=Pt, j=j, ri=ri, nk=nk: e.matmul(O[:, ri * 64:(ri + 1) * 64], lhsT=Vaug[:, kt, h, :], rhs=Pt[:, j * 64:(j + 1) * 64], start=(j == 0), stop=(j == nk - 1)),
                                 [Vaugd, Ptd], [Od])
                attn_finalize((osbr, ptr, rvr), O, Od, h, ot, otd, nq, ident32, ident32d)
            nj = nq // 128
            P.dma('sp', lambda e, ot=ot, q0=q0, nj=nj: e.dma_start(out=S['ocat'][q0:q0 + nj * 128, 768:1024].rearrange("(j p) c -> p j c", p=128), in_=ot[:, 0:nj, :]),
                  [otd], [S['ocat_d'][t] for t in range(q0 // 128, q0 // 128 + nj)])
        P.barrier()


def o_tt(q, out, in0, in1, op, rd, wr):
    K.P.op(q, lambda e: e.tensor_tensor(out=out, in0=in0, in1=in1, op=op), rd, wr)


def o_stt(q, out, in0, scalar, in1, op0, op1, rd, wr):
    K.P.op(q, lambda e: e.scalar_tensor_tensor(out=out, in0=in0, scalar=scalar, in1=in1, op0=op0, op1=op1), rd, wr)


def o_ts(q, out, in0, s1, op0, rd, wr):
    K.P.op(q, lambda e: e.tensor_scalar(out=out, in0=in0, scalar1=s1, scalar2=None, op0=op0), rd, wr)


def o_act(out, in_, func, rd, wr, **kw):
    K.P.op('act', lambda e: e.activation(out=out, in_=in_, func=func, **kw), rd, wr)


def o_red(q, out, in_, op, rd, wr):
    K.P.op(q, lambda e: e.tensor_reduce(out=out, in_=in_, axis=AX.X, op=op), rd, wr)


def o_mm(out, lhsT, rhs, rd, wr, start=True, stop=True):
    K.P.op('pe', lambda e: e.matmul(out, lhsT=lhsT, rhs=rhs, start=start, stop=stop), rd, wr)


def o_tr(out, in_, ident, rd, wr):
    K.P.op('pe', lambda e: e.transpose(out=out, in_=in_, identity=ident), rd, wr)


def o_cp(q, out, in_, rd, wr):
    if q == 'act':
        K.P.op('act', lambda e: e.copy(out=out, in_=in_), rd, wr)
    else:
        K.P.op(q, lambda e: e.tensor_copy(out=out, in_=in_), rd, wr)


def o_rcp(out, in_, rd, wr):
    K.P.op('dve', lambda e: e.reciprocal(out=out, in_=in_), rd, wr)


def o_ms(q, out, val, wr):
    K.P.op(q, lambda e: e.memset(out, val), [], wr)


def o_dma(out, in_, rd, wr, q='sp', **kw):
    K.P.dma(q, lambda e: e.dma_start(out=out, in_=in_, **kw), rd, wr)


def bc_load(st, name, src_row, n):
    t, d = sb(st, name, [128, n], F32)
    o_dma(t[:], src_row.partition_broadcast(128), [], [d])
    return t, d


RCH = 8


def rev_tile(c):
    return 1 - c if c < 2 else 35 - c


def phase_rwkv(l, I, S, need_ctx, ident32, ident32d, J32, J32d):
    rwkv_prep(l, I, S, ident32, ident32d, J32, J32d)
    rwkv_scan(S)
    rwkv_readout(l, I, S, need_ctx, J32, J32d)


def rwkv_prep(l, I, S, ident32, ident32d, J32, J32d):
    P = K.P
    MUL, ADD, SUB = ALU.mult, ALU.add, ALU.subtract
    with ExitStack() as st:
        xr = Ring(K, st, 'rv_x', [128, 1024], F32, 2)
        xo = Ring(K, st, 'rv_o', [128, 1024], F32, 2)
        pr = Ring(K, st, 'rv_ps', [128, 512], F32, 2, psum=True)
        for c in range(NTILE):
            tt_ = rev_tile(c)
            (x, xd) = xr.next()
            (o, od) = xo.next()
            o_dma(x[:], S['p'][tt_ * 128:(tt_ + 1) * 128, 0:1024], [S['p_d'][tt_]], [xd])
            for hf in range(2):
                (ps_, psd) = pr.next()
                o_mm(ps_[:], J32[:], x[:, hf * 512:(hf + 1) * 512], [J32d, xd], [psd])
                o_cp('act' if hf else 'dve', o[:, hf * 512:(hf + 1) * 512], ps_[:], [psd], [od])
            o_dma(S['prw1'][c * 128:(c + 1) * 128, :], o[:], [od], [S['prw1_d'][c]])
        P.barrier()
    with ExitStack() as st:
        mub, mubd = bc_load(st, 'rp_mu', I['rwkv_mu'][l, :], 1024)
        kkb, kkbd = bc_load(st, 'rp_kk', I['rwkv_k_k'][l, :], 256)
        kab, kabd = bc_load(st, 'rp_ka', I['rwkv_k_a'][l, :], 256)
        rkb, rkbd = bc_load(st, 'rp_rk', I['rwkv_r_k'][l].rearrange("h d -> (h d)"), 256)
        omka, omkad = sb(st, 'rp_omka', [128, 256], F32)
        K.P.op('dve', lambda e: e.tensor_scalar(out=omka[:], in0=kab[:], scalar1=-1.0, scalar2=1.0, op0=MUL, op1=ADD), [kabd], [omkad])
        w0b, a0b, wup, aup = [], [], [], []
        for d in range(2):
            w0b.append(bc_load(st, 'rp_w0%d' % d, I['rwkv_w0'][l, d, :], 256))
            a0b.append(bc_load(st, 'rp_a0%d' % d, I['rwkv_a0'][l, d, :], 256))
            t, td = sb(st, 'rp_wup%d' % d, [64, 256], F32)
            o_dma(t[:], I['rwkv_w_up'][l, d], [], [td])
            wup.append((t, td))
            t, td = sb(st, 'rp_aup%d' % d, [64, 256], F32)
            o_dma(t[:], I['rwkv_a_up'][l, d], [], [td])
            aup.append((t, td))
        gup, gupd = sb(st, 'rp_gup', [128, 256], F32)
        o_dma(gup[:], I['rwkv_g_up'][l], [], [gupd])

        xr = Ring(K, st, 'rp_x', [128, 1024], F32, 2)
        pvr = Ring(K, st, 'rp_pv', [128, 1024], F32, 2)
        nxr = Ring(K, st, 'rp_nx', [128, 1024], F32, 2)
        xsr = Ring(K, st, 'rp_xs', [128, 1024], F32, 2)
        kkr = Ring(K, st, 'rp_kkn', [128, 256], F32, 2)
        sqr = Ring(K, st, 'rp_sq', [128, 256], F32, 2)
        ssr = Ring(K, st, 'rp_ss', [128, 4], F32, 4)
        smr = Ring(K, st, 'rp_sm', [128, 128], F32, 4)
        sTr = Ring(K, st, 'rp_sT', [128, 128], F32, 4)
        ur = Ring(K, st, 'rp_u', [128, 256], F32, 2)
        ar_ = Ring(K, st, 'rp_a', [128, 256], F32, 2)
        mr = Ring(K, st, 'rp_m', [128, 256], F32, 2)
        kdr = Ring(K, st, 'rp_kd', [128, 256], F32, 3)
        kkar = Ring(K, st, 'rp_kka', [128, 256], F32, 3)
        bor = Ring(K, st, 'rp_bo', [128, 256], F32, 3)
        gtr = Ring(K, st, 'rp_gt', [128, 256], F32, 2)
        nkkr = Ring(K, st, 'rp_nkk', [128, 4, 2, 64], F32, 2)
        rrr = Ring(K, st, 'rp_rr', [128, 4, 2, 64], F32, 2)
        decr = Ring(K, st, 'rp_dec', [128, 4, 2, 64], F32, 2)
        fakr = Ring(K, st, 'rp_fak', [128, 128, 4, 2], F32, 2)
        farr = Ring(K, st, 'rp_far', [128, 128, 4, 2], F32, 2)
        fwr = Ring(K, st, 'rp_fw', [128, 128, 4], F32, 2)
        for ring in (fakr, farr):
            for (b_, bd_) in ring.bufs:
                o_ms('pool', b_[:], 0.0, [bd_])
        pbig = Ring(K, st, 'rp_pb', [128, 256], F32, 3, psum=True)
        ptr_ = Ring(K, st, 'rp_pt', [128, 128], F32, 3, psum=True)

        def v3(ap):
            return ap.rearrange("p (h k) -> p h k", k=64)

        for c in range(NTILE):
            first = c in (0, 2)
            last = c in (1, NTILE - 1)
            r0 = c * 128
            (nkk, nkkd) = nkkr.next()
            (rr, rrd) = rrr.next()
            (dec, decd) = decr.next()
            for d in range(2):
                if d == 0:
                    src = lambda a, b: S['p'][a:b, 0:1024]
                    sdeps = S['p_d']
                else:
                    src = lambda a, b: S['prw1'][a:b, :]
                    sdeps = S['prw1_d']
                nb = [sdeps[c]] + ([sdeps[c - 1]] if c > 0 else []) + ([sdeps[c + 1]] if c < NTILE - 1 else [])
                (x, xd) = xr.next()
                (pv, pvd) = pvr.next()
                (nx, nxd) = nxr.next()
                (xs, xsd) = xsr.next()
                o_dma(x[:], src(r0, r0 + 128), nb, [xd])
                if first:
                    o_ms('pool', pv[:], 0.0, [pvd])
                    o_dma(pv[1:128, :], src(r0, r0 + 127), nb, [pvd])
                else:
                    o_dma(pv[:], src(r0 - 1, r0 + 127), nb, [pvd])
                if last:
                    o_ms('pool', nx[:], 0.0, [nxd])
                    o_dma(nx[0:127, :], src(r0 + 1, r0 + 128), nb, [nxd])
                else:
                    o_dma(nx[:], src(r0 + 1, r0 + 129), nb, [nxd])
                o_tt('pool', pv[:], pv[:], nx[:], ADD, [pvd, nxd], [pvd])
                o_stt('dve', pv[:], pv[:], 0.5, x[:], MUL, SUB, [pvd, xd], [pvd])
                o_tt('pool', pv[:], pv[:], mub[:], MUL, [pvd, mubd], [pvd])
                o_tt('dve', xs[:], x[:], pv[:], ADD, [xd, pvd], [xsd])
                r_ = xs[:, 0:256]
                k_ = xs[:, 256:512]
                v_ = xs[:, 512:768]
                (kkn, kknd) = kkr.next()
                (sq, sqd) = sqr.next()
                (ss, ssd) = ssr.next()
                o_tt('dve', kkn[:], k_, kkb[:], MUL, [xsd, kkbd], [kknd])
                o_tt('pool', sq[:], kkn[:], kkn[:], MUL, [kknd], [sqd])
                o_red('dve', ss[:], v3(sq[:]), ADD, [sqd], [ssd])
                o_act(ss[:], ss[:], AF.Sqrt, [ssd], [ssd], bias=1e-12, scale=1.0)
                o_rcp(ss[:], ss[:], [ssd], [ssd])
                o_tt('dve', v3(kkn[:]), v3(kkn[:]), ss[:].unsqueeze(2).to_broadcast([128, 4, 64]), MUL, [kknd, ssd], [kknd])
                o_ts('dve', nkk[:, :, d, :], v3(kkn[:]), -1.0, MUL, [kknd], [nkkd])
                o_cp('pool', rr[:, :, d, :], v3(r_), [xsd], [rrd])
                (tw, twd) = smr.next()
                (twT, twTd) = sTr.next()
                (pt, ptd) = ptr_.next()
                (pb, pbd) = pbig.next()
                (u, ud) = ur.next()
                o_act(tw[:, 0:64], xs[:, 768:832], AF.Tanh, [xsd], [twd])
                o_tr(pt[0:64, :], tw[:, 0:64], ident32[:], [twd, ident32d], [ptd])
                o_cp('act', twT[0:64, :], pt[0:64, :], [ptd], [twTd])
                o_mm(pb[:], twT[0:64, :], wup[d][0][:], [twTd, wup[d][1]], [pbd])
                o_tt('dve', u[:], pb[:], w0b[d][0][:], ADD, [pbd, w0b[d][1]], [ud])
                o_act(u[:], u[:], AF.Sigmoid, [ud], [ud])
                o_act(dec[:, :, d, :], v3(u[:]), AF.Exp, [ud], [decd], scale=-0.6065306597126334)
                (xa, xad) = smr.next()
                (xaT, xaTd) = sTr.next()
                (pt, ptd) = ptr_.next()
                (pb, pbd) = pbig.next()
                (a, ad) = ar_.next()
                o_cp('pool', xa[:, 0:64], xs[:, 832:896], [xsd], [xad])
                o_tr(pt[0:64, :], xa[:, 0:64], ident32[:], [xad, ident32d], [ptd])
                o_cp('act', xaT[0:64, :], pt[0:64, :], [ptd], [xaTd])
                o_mm(pb[:], xaT[0:64, :], aup[d][0][:], [xaTd, aup[d][1]], [pbd])
                o_tt('dve', a[:], pb[:], a0b[d][0][:], ADD, [pbd, a0b[d][1]], [ad])
                o_act(a[:], a[:], AF.Sigmoid, [ad], [ad])
                (m, md) = mr.next()
                (kd, kdd) = kdr.next()
                (kka, kkad) = kkar.next()
                (bo, bod) = bor.next()
                o_tt('pool', m[:], a[:], kab[:], MUL, [ad, kabd], [md])
                o_tt('pool', m[:], m[:], omka[:], ADD, [md, omkad], [md])
                o_tt('dve', kd[:], k_, m[:], MUL, [xsd, md], [kdd])
                o_tt('pool', kka[:], kkn[:], a[:], MUL, [kknd, ad], [kkad])
                (ss2, ss2d) = ssr.next()
                o_tt('dve', m[:], r_, kd[:], MUL, [xsd, kdd], [md])
                o_tt('pool', m[:], m[:], rkb[:], MUL, [md, rkbd], [md])
                o_red('dve', ss2[:], v3(m[:]), ADD, [md], [ss2d])
                o_tt('dve', v3(bo[:]), v3(v_), ss2[:].unsqueeze(2).to_broadcast([128, 4, 64]), MUL, [xsd, ss2d], [bod])
                o_dma(S['V2'][d, r0:r0 + 128, :], v_, [xsd], [S['V2_d'][c]])
                o_dma(S['KKA'][d, r0:r0 + 128, :], kka[:], [kkad], [S['KKA_d'][c]])
                o_dma(S['KD'][d, r0:r0 + 128, :], kd[:], [kdd], [S['KD_d'][c]])
                o_dma(S['BON'][d, r0:r0 + 128, :], bo[:], [bod], [S['BON_d'][c]])
                if d == 0:
                    (sg, sgd) = smr.next()
                    (sgT, sgTd) = sTr.next()
                    (pt, ptd) = ptr_.next()
                    (pb, pbd) = pbig.next()
                    (gt, gtd) = gtr.next()
                    o_act(sg[:], xs[:, 896:1024], AF.Sigmoid, [xsd], [sgd])
                    o_tr(pt[:], sg[:], ident32[:], [sgd, ident32d], [ptd])
                    o_cp('act', sgT[:], pt[:], [ptd], [sgTd])
                    o_mm(pb[:], sgT[:], gup[:], [sgTd, gupd], [pbd])
                    o_cp('dve', gt[:], pb[:], [pbd], [gtd])
                    o_dma(S['GATE'][r0:r0 + 128, :], gt[:], [gtd], [S['GATE_d'][c]])
            (fak, fakd) = fakr.next()
            (far, fard) = farr.next()
            (fw, fwd) = fwr.next()
            for h in range(4):
                for (srcT, srcd, dstF, dstd) in ((nkk, nkkd, fak, fakd), (rr, rrd, far, fard)):
                    (pt, ptd) = ptr_.next()
                    o_tr(pt[:], srcT[:, h, :, :].rearrange("p d k -> p (d k)"), ident32[:], [srcd, ident32d], [ptd])
                    eng_ = 'act' if h % 2 == 0 else 'dve'
                    o_cp(eng_, dstF[0:64, :, h, 0], pt[0:64, :], [ptd], [dstd])
                    o_cp(eng_, dstF[64:128, :, h, 1], pt[64:128, :], [ptd], [dstd])
                (pt, ptd) = ptr_.next()
                o_tr(pt[:], dec[:, h, :, :].rearrange("p d k -> p (d k)"), ident32[:], [decd, ident32d], [ptd])
                o_cp('act', fw[:, :, h], pt[:], [ptd], [fwd])
            o_dma(S['AKK'][:, r0:r0 + 128, :], fak[:].rearrange("p s h d -> p s (h d)"), [fakd], [S['AKK_d'][c]])
            o_dma(S['AR'][:, r0:r0 + 128, :], far[:].rearrange("p s h d -> p s (h d)"), [fard], [S['AR_d'][c]])
            o_dma(S['WD'][:, r0:r0 + 128, :], fw[:], [fwd], [S['WD_d'][c]])
        P.barrier()


def rwkv_scan(S, T=None):
    P = K.P
    T = NT if T is None else T
    CH = RCH
    nch = T // CH
    with ExitStack() as st:
        ST, _ = sb(st, 'sc_ST', [128, 256], F32)
        STd = [Dep('sc_ST0'), Dep('sc_ST1')]
        for ch in range(2):
            o_ms('pool', ST[:, ch * 128:(ch + 1) * 128], 0.0, [STd[ch]])
        NB = 3
        akk = [sb(st, 'sc_akk%d' % i, [128, CH, 8], F32) for i in range(NB)]
        ar = [sb(st, 'sc_ar%d' % i, [128, CH, 8], F32) for i in range(NB)]
        am = [sb(st, 'sc_am%d' % i, [128, CH, 4, 4], F32) for i in range(NB)]
        wt = [sb(st, 'sc_wt%d' % i, [128, CH, 4], F32) for i in range(NB)]
        bt = [sb(st, 'sc_bt%d' % i, [6, CH, 4, 128], F32) for i in range(NB)]
        rt = [sb(st, 'sc_rt%d' % i, [6, CH, 256], F32) for i in range(NB)]
        rtv = [Dep('sc_rtv%d' % i) for i in range(NB)]
        rts = [[Dep('sc_rts%d_%d' % (i, ch)) for ch in range(2)] for i in range(NB)]
        amf, amfd = sb(st, 'sc_amf', [128, 4, 4], F32)
        yfin, yfind = sb(st, 'sc_yfin', [4, 256], F32)
        for (b_, bd_) in bt:
            o_ms('pool', b_[:], 0.0, [bd_])
        sar = [Ring(K, st, 'sc_sa%d' % ch, [128, 128], F32, 2, psum=True) for ch in range(2)]
        ur = [Ring(K, st, 'sc_u%d' % ch, [128, 128], F32, 2, psum=True) for ch in range(2)]

        def load_chunk(c):
            i = c % NB
            s0 = c * CH
            tl = [s0 // 128]
            o_dma(akk[i][0][:], S['AKK'][:, s0:s0 + CH, :], [S['AKK_d'][t] for t in tl], [akk[i][1]])
            o_dma(ar[i][0][:], S['AR'][:, s0:s0 + CH, :], [S['AR_d'][t] for t in tl], [ar[i][1]])
            o_dma(wt[i][0][:], S['WD'][:, s0:s0 + CH, :], [S['WD_d'][t] for t in tl], [wt[i][1]])
            for which, nm in ((0, 'KKA'), (1, 'KD')):
                for d in range(2):
                    row = d if which == 0 else 4 + d
                    o_dma(bt[i][0][row:row + 1, :, :, d * 64:(d + 1) * 64],
                          S[nm][d:d + 1, s0:s0 + CH, :].rearrange("o s (h k) -> o s h k", k=64),
                          [S[nm + '_d'][t] for t in tl], [bt[i][1]], q='pool')
            o_dma(rt[i][0][4:6, :, :], S['V2'][:, s0:s0 + CH, :], [S['V2_d'][t] for t in tl], [rtv[i]])
            a4 = akk[i][0][:].rearrange("p s (h d) -> p s h d", d=2)
            r4 = ar[i][0][:].rearrange("p s (h d) -> p s h d", d=2)
            o_cp('pool', am[i][0][:, :, :, 0:2], a4, [akk[i][1]], [am[i][1]])
            o_cp('pool', am[i][0][:, 1:CH, :, 2:4], r4[:, 0:CH - 1, :, :], [ar[i][1]], [am[i][1]])
            if c == 0:
                o_ms('pool', am[i][0][:, 0, :, 2:4], 0.0, [am[i][1]])
            else:
                ip = (c - 1) % NB
                rp = ar[ip][0][:].rearrange("p s (h d) -> p s h d", d=2)
                o_cp('pool', am[i][0][:, 0, :, 2:4], rp[:, CH - 1, :, :], [ar[ip][1]], [am[i][1]])

        load_chunk(0)
        if nch > 1:
            load_chunk(1)
        for s in range(T):
            c, j = divmod(s, CH)
            i = c % NB
            if j == 0 and c + 2 < nch:
                load_chunk(c + 2)
            SAs = [sar[ch].next() for ch in range(2)]
            Us = [ur[ch].next() for ch in range(2)]
            for ch in range(2):
                (SA, SAd) = SAs[ch]
                for hh in range(2):
                    h = ch * 2 + hh
                    o_mm(SA[0:4, hh * 64:(hh + 1) * 64], am[i][0][:, j, h, :], ST[:, h * 64:(h + 1) * 64], [am[i][1], STd[ch]], [SAd])
            for ch in range(2):
                (SA, SAd) = SAs[ch]
                (U, Ud) = Us[ch]
                o_cp('act', rt[i][0][0:4, j, ch * 128:(ch + 1) * 128], SA[0:4, :], [SAd], [rts[i][ch]])
                for hh in range(2):
                    h = ch * 2 + hh
                    o_mm(U[:, hh * 64:(hh + 1) * 64], bt[i][0][0:6, j, h, :], rt[i][0][0:6, j, h * 64:(h + 1) * 64],
                         [bt[i][1], rts[i][ch], rtv[i]], [Ud])
            for ch in range(2):
                (U, Ud) = Us[ch]
                STc = ST[:, ch * 128:(ch + 1) * 128]
                o_tt('dve', STc.rearrange("p (h v) -> p h v", v=64), STc.rearrange("p (h v) -> p h v", v=64),
                     wt[i][0][:, j, ch * 2:ch * 2 + 2].unsqueeze(2).to_broadcast([128, 2, 64]), ALU.mult, [STd[ch], wt[i][1]], [STd[ch]])
                o_tt('dve', STc, STc, U[:], ALU.add, [STd[ch], Ud], [STd[ch]])
            if j == CH - 1:
                s0 = c * CH
                if c == 0:
                    o_dma(S['Y2'][:, 0:CH - 1, :], rt[i][0][2:4, 1:CH, :], rts[i], [S['Y2_d'][0]])
                else:
                    o_dma(S['Y2'][:, s0 - 1:s0 + CH - 1, :], rt[i][0][2:4, :, :], rts[i], [S['Y2_d'][(s0 - 1) // 128]])
        il = (nch - 1) % NB
        rl = ar[il][0][:].rearrange("p s (h d) -> p s h d", d=2)
        o_ms('pool', amf[:], 0.0, [amfd])
        o_cp('pool', amf[:, :, 2:4], rl[:, CH - 1, :, :], [ar[il][1], amfd], [amfd])
        for ch in range(2):
            (SA, SAd) = sar[ch].next()
            for hh in range(2):
                h = ch * 2 + hh
                o_mm(SA[0:4, hh * 64:(hh + 1) * 64], amf[:, h, :], ST[:, h * 64:(h + 1) * 64], [amfd, STd[ch]], [SAd])
            o_cp('act', yfin[0:4, ch * 128:(ch + 1) * 128], SA[0:4, :], [SAd], [yfind])
        o_dma(S['Y2'][:, T - 1, :], yfin[2:4, :], [yfind], [S['Y2_d'][(T - 1) // 128]])
        P.barrier()


def rwkv_readout(l, I, S, need_ctx, J32, J32d):
    P = K.P
    MUL, ADD = ALU.mult, ALU.add
    with ExitStack() as st:
        lng, lngd = bc_load(st, 'ro_lng', I['rwkv_ln_g'][l, :], 256)
        lnb, lnbd = bc_load(st, 'ro_lnb', I['rwkv_ln_b'][l, :], 256)
        yr_ = Ring(K, st, 'ro_y', [128, 256], F32, 3)
        br_ = Ring(K, st, 'ro_b', [128, 256], F32, 3)
        gr_ = Ring(K, st, 'ro_g', [128, 256], F32, 2)
        ycr = Ring(K, st, 'ro_yc', [128, 256], F32, 3)
        sqr = Ring(K, st, 'ro_sq', [128, 256], F32, 2)
        smr = Ring(K, st, 'ro_sm', [128, 4], F32, 6)
        otr = Ring(K, st, 'ro_o', [128, 256], F32, 2)
        psr = Ring(K, st, 'ro_ps', [128, 256], F32, 2, psum=True)

        def v3(ap):
            return ap.rearrange("p (h k) -> p h k", k=64)

        def bc4(ap):
            return ap.unsqueeze(2).to_broadcast([128, 4, 64])

        for t in range(0 if need_ctx else 2, NTILE):
            outs = []
            for d in range(2):
                c = t if d == 0 else rev_tile(t)
                r0 = c * 128
                (y, yd) = yr_.next()
                (b, bd) = br_.next()
                (yc, ycd) = ycr.next()
                (sq, sqd) = sqr.next()
                (sm, smd) = smr.next()
                (vr, vrd) = smr.next()
                o_dma(y[:], S['Y2'][d, r0:r0 + 128, :], [S['Y2_d'][c]], [yd])
                o_dma(b[:], S['BON'][d, r0:r0 + 128, :], [S['BON_d'][c]], [bd])
                o_red('dve', sm[:], v3(y[:]), ADD, [yd], [smd])
                o_ts('dve', sm[:], sm[:], -1.0 / 64, MUL, [smd], [smd])
                o_tt('dve', v3(yc[:]), v3(y[:]), bc4(sm[:]), ADD, [yd, smd], [ycd])
                o_tt('pool', sq[:], yc[:], yc[:], MUL, [ycd], [sqd])
                o_red('dve', vr[:], v3(sq[:]), ADD, [sqd], [vrd])
                o_act(vr[:], vr[:], AF.Sqrt, [vrd], [vrd], bias=64e-5, scale=1.0 / 64)
                o_rcp(vr[:], vr[:], [vrd], [vrd])
                o_tt('dve', v3(yc[:]), v3(yc[:]), bc4(vr[:]), MUL, [ycd, vrd], [ycd])
                o_tt('pool', yc[:], yc[:], lng[:], MUL, [ycd, lngd], [ycd])
                o_tt('pool', yc[:], yc[:], lnb[:], ADD, [ycd, lnbd], [ycd])
                o_tt('dve', yc[:], yc[:], b[:], ADD, [ycd, bd], [ycd])
                outs.append((yc, ycd))
            (ps_, psd) = psr.next()
            (g, gd) = gr_.next()
            (ot, otd) = otr.next()
            o_dma(g[:], S['GATE'][t * 128:(t + 1) * 128, :], [S['GATE_d'][t]], [gd])
            o_mm(ps_[:], J32[:], outs[1][0][:], [J32d, outs[1][1]], [psd])
            o_tt('dve', ot[:], outs[0][0][:], ps_[:], ADD, [outs[0][1], psd], [otd])
            o_tt('dve', ot[:], ot[:], g[:], MUL, [otd, gd], [otd])
            o_dma(S['ocat'][t * 128:(t + 1) * 128, 0:256], ot[:], [otd], [S['ocat_d'][t]])
        P.barrier()


def phase_out(l, I, S, need_ctx, modL, modLd, modC, modCd, ident, identd):
    P = K.P
    MUL, ADD = ALU.mult, ALU.add
    with ExitStack() as st:
        wbf, wbfd = sb(st, 'wo_bf', [128, 8, D], BF16)
        wst = Ring(K, st, 'wo_st', [128, D], F32, 2)
        ocr = Ring(K, st, 'wo_oc', [128, D], F32, 2)
        ocbr = Ring(K, st, 'wo_ocb', [128, D], BF16, 2)
        oTr = Ring(K, st, 'wo_oT', [128, 8, 128], BF16, 2)
        xr = Ring(K, st, 'wo_x', [128, D], F32, 2)
        yr = Ring(K, st, 'wo_y', [128, D], F32, 2)
        sq, sqd = sb(st, 'wo_sq', [128, D], F32)
        ssr = Ring(K, st, 'wo_ss', [128, 1], F32, 4)
        pTr = Ring(K, st, 'wo_pT', [128, 8, 128], BF16, 2, psum=True)
        pyr = Ring(K, st, 'wo_py', [128, 512], F32, 4, psum=True)
        wo = I['w_out'][l].rearrange("(k p) n -> p k n", p=128)
        for kc in range(8):
            (ws, wsd) = wst.next()
            o_dma(ws[:], wo[:, kc, :], [], [wsd])
            o_cp('pool', wbf[:, kc, :], ws[:], [wsd], [wbfd])
        for t in range(0 if need_ctx else 2, NTILE):
            mt, mtd = (modC, modCd) if t < 2 else (modL, modLd)
            (oc, ocd) = ocr.next()
            (ocb, ocbd) = ocbr.next()
            (oT, oTd) = oTr.next()
            (x, xd) = xr.next()
            (y, yd) = yr.next()
            (ss, ssd) = ssr.next()
            (pT, pTd) = pTr.next()
            o_dma(oc[:], S['ocat'][t * 128:(t + 1) * 128, :], [S['ocat_d'][t]], [ocd])
            o_dma(x[:], S['xs'][t * 128:(t + 1) * 128, :], [S['xs_d'][t]], [xd])
            o_cp('pool', ocb[:], oc[:], [ocd], [ocbd])
            for kc in range(8):
                o_tr(pT[:, kc, :], ocb[:, kc * 128:(kc + 1) * 128], ident[:], [ocbd, identd], [pTd])
            o_cp('act', oT[:], pT[:], [pTd], [oTd])
            for hf in range(2):
                (py, pyd) = pyr.next()
                for kc in range(8):
                    o_mm(py[:], oT[:, kc, :], wbf[:, kc, hf * 512:(hf + 1) * 512], [oTd, wbfd], [pyd], start=(kc == 0), stop=(kc == 7))
                o_cp('act' if hf else 'dve', y[:, hf * 512:(hf + 1) * 512], py[:], [pyd], [yd])
            rms_rstd(y[:], yd, D, sq, sqd, ss, ssd, 0)
            o_stt('dve', y[:], y[:], ss[:, 0:1], mt[:, 2, :], MUL, MUL, [yd, ssd, mtd], [yd])
            o_tt('pool', x[:], x[:], y[:], ADD, [xd, yd], [xd])
            o_dma(S['xs'][t * 128:(t + 1) * 128, :], x[:], [xd], [S['xs_d'][t]])
        P.barrier()


def ffn_convert(I, S, moe, li):
    P = K.P
    NEx = NE if moe else 1
    tag = 'm' if moe else 'f'
    with ExitStack() as st:
        ldr = Ring(K, st, 'cv_ld', [128, DFF], F32, 3)
        cvr = Ring(K, st, 'cv_bf', [128, DFF], BF16, 3)
        n = 0
        engs = ('dve', 'pool', 'act')
        for e in range(NEx):
            for (nm, dst) in (('gate', 'wgb' + tag), ('up', 'wub' + tag)):
                src = (I['moe_w_' + nm][li, e] if moe else I['ffn_w_' + nm][li]).rearrange("(k p) n -> p k n", p=128)
                for kc in range(8):
                    (ld, ldd) = ldr.next()
                    (cv, cvd) = cvr.next()
                    o_dma(ld[:], src[:, kc, :], [], [ldd])
                    o_cp(engs[n % 3], cv[:], ld[:], [ldd], [cvd])
                    n += 1
                    for q4 in range(2):
                        f0, f1 = q4 * 11, (q4 + 1) * 11
                        o_dma(S[dst][e, f0:f1, :, kc, :].rearrange("f p n -> p f n"),
                              cv[:, f0 * 128:f1 * 128].rearrange("p (f n) -> p f n", n=128), [cvd], [Dep()])
            srcd = (I['moe_w_down'][li, e] if moe else I['ffn_w_down'][li]).rearrange("(f p) n -> p f n", p=128)
            for f0 in range(0, NFF, 2):
                (ld, ldd) = ldr.next()
                (cv, cvd) = cvr.next()
                o_dma(ld[:, 0:2048].rearrange("p (f n) -> p f n", n=1024), srcd[:, f0:f0 + 2, :], [], [ldd])
                o_cp(engs[n % 3], cv[:, 0:2048], ld[:, 0:2048], [ldd], [cvd])
                n += 1
                o_dma(S['wdb' + tag][e, f0:f0 + 2, :, :].rearrange("f p n -> p f n"),
                      cv[:, 0:2048].rearrange("p (f n) -> p f n", n=1024), [cvd], [Dep()])
        P.barrier()


def phase_ffn(l, I, S, need_ctx, modL, modLd, modC, modCd, ident32, ident32d, out_ap, out_d):
    P = K.P
    MUL, ADD, SUB = ALU.mult, ALU.add, ALU.subtract
    moe = (l % 2 == 1)
    li = l // 2
    NEx = NE if moe else 1
    tag = 'm' if moe else 'f'
    last_layer = not need_ctx
    ffn_convert(I, S, moe, li)
    tiles = list(range(0 if need_ctx else 2, NTILE))
    with ExitStack() as st:
        xrr = Ring(K, st, 'ff_x', [128, 2, D], F32, 2)
        hfr = Ring(K, st, 'ff_hf', [128, D], F32, 2)
        hTr = Ring(K, st, 'ff_hT', [128, 8, 256], BF16, 2)
        h32r = Ring(K, st, 'ff_h32', [128, 8, 128], F32, 2)
        sq, sqd = sb(st, 'ff_sq', [128, D], F32)
        ssr = Ring(K, st, 'ff_ss', [128, 1], F32, 4)
        accr = Ring(K, st, 'ff_acc', [128, 2, D], F32, 2)
        gtr = Ring(K, st, 'ff_gate', [128, 2, 8], F32, 2)
        smr = Ring(K, st, 'ff_sm', [128, 8], F32, 8)
        s1r = Ring(K, st, 'ff_s1', [128, 1], F32, 12)
        wgr = Ring(K, st, 'ff_wg', [128, 8, 128], BF16, 3)
        wur = Ring(K, st, 'ff_wu', [128, 8, 128], BF16, 3)
        wdr = Ring(K, st, 'ff_wd', [128, D], BF16, 3)
        sgr = Ring(K, st, 'ff_sg', [128, 256], F32, 2)
        GTr = Ring(K, st, 'ff_GT', [128, 256], BF16, 3)
        pTr = Ring(K, st, 'ff_pT', [128, 4, 128], F32, 1, psum=True)
        gur = Ring(K, st, 'ff_gu', [128, 2, 256], F32, 2, psum=True)
        ops_ = [[ps(st, 'ff_o%d%d' % (a, b), [128, 512], F32) for b in range(2)] for a in range(2)]
        plr = Ring(K, st, 'ff_pl', [128, 8], F32, 1, psum=True)
        if moe:
            rt_, rtd = sb(st, 'ff_router', [128, 8, 8], F32)
            o_dma(rt_[:], I['moe_router'][li].rearrange("(k p) e -> p k e", p=128), [], [rtd])
        for blk in range(len(tiles) // 2):
            tl = tiles[2 * blk:2 * blk + 2]
            (xr, xrd) = xrr.next()
            (hT, hTd) = hTr.next()
            (acc, accd) = accr.next()
            (gate, gated) = gtr.next()
            for jj, t in enumerate(tl):
                mt, mtd = (modC, modCd) if t < 2 else (modL, modLd)
                (hf, hfd) = hfr.next()
                (h32, h32d) = h32r.next()
                (ss, ssd) = ssr.next()
                o_dma(xr[:, jj, :], S['xs'][t * 128:(t + 1) * 128, :], [S['xs_d'][t]], [xrd])
                rms_rstd(xr[:, jj, :], xrd, D, sq, sqd, ss, ssd, 0)
                o_stt('dve', hf[:], xr[:, jj, :], ss[:, 0:1], mt[:, 4, :], MUL, MUL, [xrd, ssd, mtd], [hfd])
                o_tt('pool', hf[:], hf[:], mt[:, 3, :], ADD, [hfd, mtd], [hfd])
                for q4 in range(2):
                    (pT, pTd) = pTr.next()
                    for kk_ in range(4):
                        kc = q4 * 4 + kk_
                        o_tr(pT[:, kk_, :], hf[:, kc * 128:(kc + 1) * 128], ident32[:], [hfd, ident32d], [pTd])
                    o_cp('act', h32[:, q4 * 4:(q4 + 1) * 4, :], pT[:], [pTd], [h32d])
                    o_cp('pool', hT[:, q4 * 4:(q4 + 1) * 4, jj * 128:(jj + 1) * 128], h32[:, q4 * 4:(q4 + 1) * 4, :], [h32d], [hTd])
                if moe:
                    (pl, pld) = plr.next()
                    for kc in range(8):
                        o_mm(pl[:], h32[:, kc, :], rt_[:, kc, :], [h32d, rtd], [pld], start=(kc == 0), stop=(kc == 7))
                    (lg, lgd) = smr.next()
                    (lg2, lg2d) = smr.next()
                    (mk1, mk1d) = smr.next()
                    (mk2, mk2d) = smr.next()
                    (m1, m1d) = s1r.next()
                    (m2, m2d) = s1r.next()
                    (ex, exd) = s1r.next()
                    (w1, w1d) = s1r.next()
                    (w2, w2d) = s1r.next()
                    o_cp('dve', lg[:], pl[:], [pld], [lgd])
                    o_red('dve', m1[:], lg[:], ALU.max, [lgd], [m1d])
                    o_ts('dve', mk1[:], lg[:], m1[:, 0:1], ALU.is_equal, [lgd, m1d], [mk1d])
                    o_stt('dve', lg2[:], mk1[:], -1.0e30, lg[:], MUL, ADD, [mk1d, lgd], [lg2d])
                    o_red('dve', m2[:], lg2[:], ALU.max, [lg2d], [m2d])
                    o_ts('dve', mk2[:], lg2[:], m2[:, 0:1], ALU.is_equal, [lg2d, m2d], [mk2d])
                    o_tt('dve', ex[:], m2[:], m1[:], SUB, [m2d, m1d], [exd])
                    o_act(ex[:], ex[:], AF.Exp, [exd], [exd])
                    o_ts('dve', w1[:], ex[:], 1.0, ADD, [exd], [w1d])
                    o_rcp(w1[:], w1[:], [w1d], [w1d])
                    o_tt('dve', w2[:], ex[:], w1[:], MUL, [exd, w1d], [w2d])
                    o_ts('dve', gate[:, jj, :], mk1[:], w1[:, 0:1], MUL, [mk1d, w1d], [gated])
                    o_stt('dve', gate[:, jj, :], mk2[:], w2[:, 0:1], gate[:, jj, :], MUL, ADD, [mk2d, w2d, gated], [gated])
            for e in range(NEx):
                for f in range(NFF):
                    (wg, wgd) = wgr.next()
                    (wu, wud) = wur.next()
                    (wd, wdd) = wdr.next()
                    (gu, gud) = gur.next()
                    (sg, sgd) = sgr.next()
                    (GT, GTd) = GTr.next()
                    o_dma(wg[:], S['wgb' + tag][e, f], [], [wgd])
                    o_dma(wu[:], S['wub' + tag][e, f], [], [wud], q='pool')
                    o_dma(wd[:], S['wdb' + tag][e, f], [], [wdd])
                    for kc in range(8):
                        o_mm(gu[:, 0, :], wg[:, kc, :], hT[:, kc, :], [wgd, hTd], [gud], start=(kc == 0), stop=(kc == 7))
                    for kc in range(8):
                        o_mm(gu[:, 1, :], wu[:, kc, :], hT[:, kc, :], [wud, hTd], [gud], start=(kc == 0), stop=(kc == 7))
                    o_act(sg[:], gu[:, 0, :], AF.Silu, [gud], [sgd])
                    o_tt('dve', GT[:], sg[:], gu[:, 1, :], MUL, [sgd, gud], [GTd])
                    for jj in range(2):
                        for hf_ in range(2):
                            (op_, opd) = ops_[jj][hf_]
                            o_mm(op_[:], GT[:, jj * 128:(jj + 1) * 128], wd[:, hf_ * 512:(hf_ + 1) * 512], [GTd, wdd], [opd],
                                 start=(f == 0), stop=(f == NFF - 1))
                for jj in range(2):
                    for hf_ in range(2):
                        (op_, opd) = ops_[jj][hf_]
                        dst = acc[:, jj, hf_ * 512:(hf_ + 1) * 512]
                        if not moe:
                            o_cp('act', dst, op_[:], [opd], [accd])
                        elif e == 0:
                            o_ts('dve', dst, op_[:], gate[:, jj, e:e + 1], MUL, [opd, gated], [accd])
                        else:
                            o_stt('dve', dst, op_[:], gate[:, jj, e:e + 1], dst, MUL, ADD, [opd, gated, accd], [accd])
            for jj, t in enumerate(tl):
                mt, mtd = (modC, modCd) if t < 2 else (modL, modLd)
                (ss, ssd) = ssr.next()
                rms_rstd(acc[:, jj, :], accd, D, sq, sqd, ss, ssd, 0)
                o_stt('dve', acc[:, jj, :], acc[:, jj, :], ss[:, 0:1], mt[:, 5, :], MUL, MUL, [accd, ssd, mtd], [accd])
                o_tt('pool', acc[:, jj, :], acc[:, jj, :], xr[:, jj, :], ADD, [accd, xrd], [accd])
                if last_layer:
                    o_dma(out_ap[(t - 2) * 128:(t - 1) * 128, :], acc[:, jj, :], [accd], [out_d])
                else:
                    o_dma(S['xs'][t * 128:(t + 1) * 128, :], acc[:, jj, :], [accd], [S['xs_d'][t]])
        P.barrier()


IN_NAMES = ['x', 'c', 'ctx', 'c_ctx', 'mod_w', 'mod_b', 'norm_g', 'w_in', 'w_out', 'rwkv_mu', 'rwkv_w0', 'rwkv_w_up',
            'rwkv_a0', 'rwkv_a_up', 'rwkv_g_up', 'rwkv_k_k', 'rwkv_k_a', 'rwkv_r_k', 'rwkv_ln_g', 'rwkv_ln_b',
            'gqa_q_g', 'gqa_k_g', 'mla_q_norm_g', 'mla_w_uq', 'mla_kv_norm_g', 'mla_w_ukv', 'nat_bias',
            'ffn_w_gate', 'ffn_w_up', 'ffn_w_down', 'moe_router', 'moe_w_gate', 'moe_w_up', 'moe_w_down']


def build(shapes, stop_after=None, debug=(), only=None, layers=(0, 1), scan_T=None):
    nc = bass.Bass("TRN2", target_bir_lowering=False)
    K.nc = nc
    P = Prog(nc)
    K.P = P
    I = {}
    for name in IN_NAMES:
        I[name] = nc.dram_tensor(name, list(shapes[name]), F32, kind="ExternalInput").ap()
    out = nc.dram_tensor("out", [NL, D], F32, kind="ExternalOutput").ap()
    out_d = Dep('out')
    S = {}

    def scratch(name, shape, dtype=F32, tiled=True):
        S[name] = dram(name, shape, dtype)
        S[name + '_d'] = [Dep('%s%d' % (name, t)) for t in range(NTILE)] if tiled else Dep(name)

    scratch('xs', [NT, D])
    scratch('p', [NT, INC])
    scratch('ocat', [NT, D])
    scratch('prw1', [NT, 1024])
    for nm in ('V2', 'KKA', 'KD', 'BON', 'Y2'):
        scratch(nm, [2, NT, 256])
    scratch('GATE', [NT, 256])
    scratch('AKK', [128, NT, 8])
    scratch('AR', [128, NT, 8])
    scratch('WD', [128, NT, 4])
    for tag, ne in (('f', 1), ('m', NE)):
        scratch('wgb' + tag, [ne, NFF, 128, 8, 128], BF16, tiled=False)
        scratch('wub' + tag, [ne, NFF, 128, 8, 128], BF16, tiled=False)
        scratch('wdb' + tag, [ne, NFF, 128, D], BF16, tiled=False)
    I['rope_g'] = nc.dram_tensor('rope_g', [NL, 2, 32], F32, kind='ExternalInput').ap()
    I['rope_m'] = nc.dram_tensor('rope_m', [NL, 2, 16], F32, kind='ExternalInput').ap()
    I['nat_tab'] = nc.dram_tensor('nat_tab', [2, 128, 4, len(nat_plan()[1]), 64], F32, kind='ExternalInput').ap()
    with ExitStack() as st:
        P.alloc_sems(st)
        ident, identd = sb(st, 'ident', [128, 128], BF16)
        modL, modLd = sb(st, 'modL', [128, 6, D], F32)
        modC, modCd = sb(st, 'modC', [128, 6, D], F32)
        ident32, ident32d = sb(st, 'ident32', [128, 128], F32)
        J32, J32d = sb(st, 'J32', [128, 128], F32)
        P.op('pool', lambda e: e.memset(ident[:], 1.0), [], [identd])
        P.op('pool', lambda e: e.memset(ident32[:], 1.0), [], [ident32d])
        P.op('pool', lambda e: e.memset(J32[:], 1.0), [], [J32d])
        P.op('pool', lambda e: e.affine_select(out=ident32[:], in_=ident32[:], pattern=[[-1, 128]], compare_op=ALU.is_equal, fill=0.0, base=0, channel_multiplier=1),
             [ident32d], [ident32d])
        P.op('pool', lambda e: e.affine_select(out=ident[:], in_=ident[:], pattern=[[-1, 128]], compare_op=ALU.is_equal, fill=0.0, base=0, channel_multiplier=1),
             [identd], [identd])
        P.op('pool', lambda e: e.affine_select(out=J32[:], in_=J32[:], pattern=[[1, 128]], compare_op=ALU.is_equal, fill=0.0, base=-127, channel_multiplier=1),
             [J32d], [J32d])
        P.dma('sp', lambda e: e.dma_start(out=S['xs'][0:NC_, :], in_=I['ctx'][:, :]), [], S['xs_d'][0:2])
        for j in range(4):
            P.dma('sp', lambda e, j=j: e.dma_start(out=S['xs'][NC_ + j * 1024:NC_ + (j + 1) * 1024, :], in_=I['x'][j * 1024:(j + 1) * 1024, :]),
                  [], S['xs_d'][2 + 8 * j:2 + 8 * (j + 1)])

        def want(name):
            return only is None or name in only

        for l in layers:
            need_ctx = (l == 0)
            if want('mod'):
                phase_mod(l, I, modL, modLd, modC, modCd)
            if want('in'):
                phase_in(l, I, S, modL, modLd, modC, modCd, ident, identd)
            if want('rwkv'):
                if scan_T is None:
                    phase_rwkv(l, I, S, need_ctx, ident32, ident32d, J32, J32d)
                else:
                    rwkv_prep(l, I, S, ident32, ident32d, J32, J32d)
                    rwkv_scan(S, scan_T)
            if want('gqa'):
                phase_gqa(l, I, S, need_ctx, ident, identd, ident32, ident32d)
            if want('mla'):
                phase_mla(l, I, S, need_ctx, ident, identd, ident32, ident32d)
            if want('nat'):
                phase_nat(l, I, S, need_ctx, ident, identd, ident32, ident32d)
            if stop_after == ('attn', l):
                break
            if want('out'):
                phase_out(l, I, S, need_ctx, modL, modLd, modC, modCd, ident, identd)
            if stop_after == ('out', l):
                break
            if want('ffn'):
                phase_ffn(l, I, S, need_ctx, modL, modLd, modC, modCd, ident32, ident32d, out, out_d)
        for name in debug:
            src = S[name]
            d = nc.dram_tensor('dbg_' + name, list(src.shape), src.dtype, kind="ExternalOutput").ap()
            dd = Dep('dbg_' + name)
            deps = S[name + '_d'] if isinstance(S[name + '_d'], list) else [S[name + '_d']]
            P.dma('sp', lambda e, d=d, src=src: e.dma_start(out=d, in_=src), deps, [dd])
        P.barrier()
        P.emit()
    return nc


def core_shapes(inputs):
    sh = {k: tuple(np.asarray(v).shape) for k, v in inputs.items()}
    sh['x'] = (NL, D)
    sh['c'] = (D,)
    sh['ctx'] = (NC_, D)
    return sh


def rope_tables(rot_dim):
    t = np.arange(NL)
    row = (t // 64).astype(np.float32)
    col = (t % 64).astype(np.float32)
    quarter = rot_dim // 4
    inv_freq = (np.float32(10000.0) ** (-np.arange(quarter, dtype=np.float32) / np.float32(quarter))).astype(np.float32)
    ang = np.concatenate([row[:, None] * inv_freq, col[:, None] * inv_freq], axis=-1).astype(np.float32)
    return np.ascontiguousarray(np.stack([np.cos(ang), np.sin(ang)], axis=1).astype(np.float32))


def core_inputs(inputs, b, shared=None):
    if shared is None:
        shared = {k: np.ascontiguousarray(np.asarray(v, dtype=np.float32)) for k, v in inputs.items() if k not in ('x', 'c', 'ctx')}
        shared['rope_g'] = rope_tables(64)
        shared['rope_m'] = rope_tables(32)
        nb = np.asarray(inputs['nat_bias'], np.float32)
        shared['nat_tab'] = np.ascontiguousarray(np.stack([nat_bias_table(nb[0]), nat_bias_table(nb[1])], 0))
    m = dict(shared)
    m['x'] = np.ascontiguousarray(np.asarray(inputs['x'][b], dtype=np.float32))
    m['c'] = np.ascontiguousarray(np.asarray(inputs['c'][b], dtype=np.float32))
    m['ctx'] = np.ascontiguousarray(np.asarray(inputs['ctx'][b], dtype=np.float32))
    return m


def kernel(**inputs):
    nb = 4
    shapes = core_shapes(inputs)
    nc = build(shapes)
    first = core_inputs(inputs, 0)
    shared = {k: v for k, v in first.items() if k not in ('x', 'c', 'ctx')}
    maps = [first] + [core_inputs(inputs, b, shared) for b in range(1, nb)]
    res = run_bass_kernel_spmd(nc, maps, core_ids=list(range(nb)))
    return np.stack([np.asarray(res.results[b]['out'], dtype=np.float32) for b in range(nb)], axis=0)
```

```python
import numpy as np
from contextlib import ExitStack
import concourse.bass as bass
import concourse.mybir as mybir
from concourse.bass_utils import run_bass_kernel_spmd

F32 = mybir.dt.float32
BF16 = mybir.dt.bfloat16
AF = mybir.ActivationFunctionType
ALU = mybir.AluOpType
AX = mybir.AxisListType

COMPUTE = ('pe', 'act', 'dve', 'pool')
QUEUES = ('pe', 'act', 'dve', 'pool', 'sp')
NRING = 8

D = 1024
NL = 4096
NC_ = 256
NT = NL + NC_
NTILE = NT // 128
INC = 2720
RW0, GQ0, ML0, NA0 = 0, 1024, 1536, 1952
DFF = 2816
NFF = DFF // 128
NE = 8
RMS_EPS = 1e-6


class Dep:
    __slots__ = ('w', 'r', 'name')

    def __init__(self, name=''):
        self.w = None
        self.r = {}
        self.name = name


class Prog:
    def __init__(self, nc):
        self.nc = nc
        self.q = {e: [] for e in QUEUES}
        self.cnt = {e: 0 for e in COMPUTE}
        self.seen = {e: {} for e in QUEUES}
        self.sems = {}
        self.dma_cnt = {}
        self.dma_next = {e: 0 for e in QUEUES}
        self.ninstr = 0

    def alloc_sems(self, stack):
        for e in COMPUTE:
            self.sems[e] = stack.enter_context(self.nc.semaphore('s_' + e))
        for qn in ('sp', 'pool', 'act'):
            for j in range(NRING):
                key = ('dma', qn, j)
                self.sems[key] = stack.enter_context(self.nc.semaphore('d_%s_%d' % (qn, j)))
                self.dma_cnt[key] = 0

    def _need(self, queue, key, count, waits):
        if self.seen[queue].get(key, 0) >= count:
            return
        if waits.get(key, 0) < count:
            waits[key] = count

    def _emit_waits(self, queue, waits):
        for key, count in waits.items():
            sem = self.sems[key]
            val = count * (16 if isinstance(key, tuple) else 1)
            self.q[queue].append(lambda e, sem=sem, val=val: e.wait_ge(sem, val))
            self.seen[queue][key] = count
            self.ninstr += 1

    def _collect(self, queue, reads, writes):
        waits = {}
        for t in reads:
            if t.w is not None:
                self._need(queue, t.w[0], t.w[1], waits)
        for t in writes:
            if t.w is not None:
                if not (queue == 'pe' and t.w[0] == 'pe'):
                    self._need(queue, t.w[0], t.w[1], waits)
            for k, c in t.r.items():
                if k == queue:
                    continue
                self._need(queue, k, c, waits)
        return waits

    def op(self, queue, fn, reads=(), writes=()):
        waits = self._collect(queue, reads, writes)
        self._emit_waits(queue, waits)
        self.cnt[queue] += 1
        c = self.cnt[queue]
        sem = self.sems[queue]
        self.q[queue].append(lambda e, fn=fn, sem=sem: fn(e).then_inc(sem, 1))
        self.ninstr += 1
        for t in reads:
            if t.r.get(queue, 0) < c:
                t.r[queue] = c
        for t in writes:
            t.w = (queue, c)
            t.r = {}

    def dma(self, queue, fn, reads=(), writes=()):
        j = self.dma_next[queue]
        self.dma_next[queue] = (j + 1) % NRING
        key = ('dma', queue, j)
        waits = self._collect(queue, reads, writes)
        n = self.dma_cnt[key]
        if n > 0:
            self._need(queue, key, n, waits)
        self._emit_waits(queue, waits)
        self.dma_cnt[key] = n + 1
        sem = self.sems[key]
        self.q[queue].append(lambda e, fn=fn, sem=sem: fn(e).then_inc(sem, 16))
        self.ninstr += 1
        for t in reads:
            t.r[key] = n + 1
        for t in writes:
            t.w = (key, n + 1)
            t.r = {}

    def barrier(self, queues=QUEUES):
        for qn in queues:
            waits = {}
            for e in COMPUTE:
                if self.cnt[e] > 0 and e != qn:
                    self._need(qn, e, self.cnt[e], waits)
            for key, n in self.dma_cnt.items():
                if n > 0:
                    self._need(qn, key, n, waits)
            self._emit_waits(qn, waits)

    def emit(self):
        nc = self.nc
        with nc.Block() as block:
            @block.tensor
            def _(e):
                for f in self.q['pe']:
                    f(e)

            @block.scalar
            def _(e):
                for f in self.q['act']:
                    f(e)

            @block.vector
            def _(e):
                for f in self.q['dve']:
                    f(e)

            @block.gpsimd
            def _(e):
                for f in self.q['pool']:
                    f(e)

            @block.sync
            def _(e):
                for f in self.q['sp']:
                    f(e)


class Ring:
    def __init__(self, K, st, name, shape, dtype, n, psum=False):
        self.bufs = []
        for i in range(n):
            if psum:
                full = [128, 512] if dtype == F32 else [128, 1024]
                n = 1
                for d_ in shape[1:]:
                    n *= d_
                assert n <= full[1]
                t = st.enter_context(K.nc.psum_tensor(uname('%s%d' % (name, i)), full, dtype))
                t = t[0:shape[0], 0:n]
                if len(shape) == 3:
                    t = t.rearrange("p (a b) -> p a b", b=shape[2])
            else:
                t = st.enter_context(K.nc.sbuf_tensor(uname('%s%d' % (name, i)), shape, dtype))
            self.bufs.append((t, Dep('%s%d' % (name, i))))
        self.i = 0

    def next(self):
        b = self.bufs[self.i]
        self.i = (self.i + 1) % len(self.bufs)
        return b


class K:
    LVL = 99
    UID = 0


def uname(name):
    K.UID += 1
    return '%s_u%d' % (name, K.UID)


def sb(st, name, shape, dtype):
    return st.enter_context(K.nc.sbuf_tensor(uname(name), shape, dtype)), Dep(name)


def ps(st, name, shape, dtype):
    return st.enter_context(K.nc.psum_tensor(uname(name), shape, dtype)), Dep(name)


def dram(name, shape, dtype):
    return K.nc.dram_tensor(name, shape, dtype, kind="Internal").ap()


def rms_rstd(x_ap, xdep, n, sq, sqd, ss, ssd, col):
    P = K.P
    P.op('dve', lambda e: e.tensor_tensor(out=sq[:, 0:n], in0=x_ap, in1=x_ap, op=ALU.mult), [xdep], [sqd])
    P.op('dve', lambda e: e.tensor_reduce(out=ss[:, col:col + 1], in_=sq[:, 0:n], axis=AX.X, op=ALU.add), [sqd], [ssd])
    P.op('act', lambda e: e.activation(out=ss[:, col:col + 1], in_=ss[:, col:col + 1], func=AF.Sqrt, bias=RMS_EPS, scale=1.0 / n), [ssd], [ssd])
    P.op('dve', lambda e: e.reciprocal(out=ss[:, col:col + 1], in_=ss[:, col:col + 1]), [ssd], [ssd])


def phase_mod(l, I, modL, modLd, modC, modCd):
    nc, P = K.nc, K.P
    with ExitStack() as st:
        cv, cvd = sb(st, 'cv', [128, 2, 8], F32)
        cs, csd = sb(st, 'cs', [128, 2, 8], F32)
        crep, crepd = sb(st, 'crep', [128, 2, 8, 128], BF16)
        gb, gbd = sb(st, 'gb', [128, 4, D], F32)
        wst = Ring(K, st, 'mw_st', [128, 8, 512], F32, 2)
        wbf = Ring(K, st, 'mw_bf', [128, 8, 512], BF16, 2)
        bb = Ring(K, st, 'mbb', [128, 512], F32, 2)
        pm = Ring(K, st, 'pmod', [128, 512], F32, 4, psum=True)
        P.dma('sp', lambda e: e.dma_start(out=cv[:, 0, :], in_=I['c'].rearrange("(k p) -> p k", p=128), allow_slow_non_contiguous=True), [], [cvd])
        P.dma('sp', lambda e: e.dma_start(out=cv[:, 1, :], in_=I['c_ctx'].rearrange("(k p) -> p k", p=128), allow_slow_non_contiguous=True), [], [cvd])
        P.dma('sp', lambda e: e.dma_start(out=gb[:], in_=I['norm_g'][l].partition_broadcast(128)), [], [gbd])
        P.op('act', lambda e: e.activation(out=cs[:], in_=cv[:], func=AF.Silu), [cvd], [csd])
        P.op('dve', lambda e: e.tensor_copy(out=crep[:], in_=cs[:].unsqueeze(3).to_broadcast([128, 2, 8, 128])), [csd], [crepd])
        mw = I['mod_w'][l].rearrange("(k p) n -> p k n", p=128)
        for nb in range(12):
            (ws, wsd) = wst.next()
            (wb, wbd) = wbf.next()
            (bt, btd) = bb.next()
            P.dma('sp', lambda e, ws=ws, nb=nb: e.dma_start(out=ws[:], in_=mw[:, :, nb * 512:(nb + 1) * 512]), [], [wsd])
            P.dma('sp', lambda e, bt=bt, nb=nb: e.dma_start(out=bt[:], in_=I['mod_b'][l, nb * 512:(nb + 1) * 512].partition_broadcast(128)), [], [btd])
            P.op('pool', lambda e, ws=ws, wb=wb: e.tensor_copy(out=wb[:], in_=ws[:]), [wsd], [wbd])
            j, off = nb // 2, (nb % 2) * 512
            for s, (mt, mtd) in enumerate(((modL, modLd), (modC, modCd))):
                (pt, ptd) = pm.next()
                for kc in range(8):
                    P.op('pe', lambda e, pt=pt, wb=wb, kc=kc, s=s: e.matmul(pt[:], lhsT=crep[:, s, kc, :], rhs=wb[:, kc, :], start=(kc == 0), stop=(kc == 7)),
                         [crepd, wbd], [ptd])
                P.op('dve', lambda e, pt=pt, bt=bt, mt=mt, j=j, off=off: e.tensor_tensor(out=mt[:, j, off:off + 512], in0=pt[:], in1=bt[:], op=ALU.add),
                     [ptd, btd], [mtd])
        for (mt, mtd) in ((modL, modLd), (modC, modCd)):
            for j, gi, plus1 in ((1, 0, True), (2, 1, False), (4, 2, True), (5, 3, False)):
                if plus1:
                    P.op('dve', lambda e, mt=mt, j=j, gi=gi: e.scalar_tensor_tensor(out=mt[:, j, :], in0=mt[:, j, :], scalar=1.0, in1=gb[:, gi, :], op0=ALU.add, op1=ALU.mult),
                         [mtd, gbd], [mtd])
                else:
                    P.op('dve', lambda e, mt=mt, j=j, gi=gi: e.tensor_tensor(out=mt[:, j, :], in0=mt[:, j, :], in1=gb[:, gi, :], op=ALU.mult),
                         [mtd, gbd], [mtd])
        P.barrier()


def phase_in(l, I, S, modL, modLd, modC, modCd, ident, identd):
    nc, P = K.nc, K.P
    with ExitStack() as st:
        wbf, wbfd = sb(st, 'win_bf', [128, 8, INC], BF16)
        wst = Ring(K, st, 'win_st', [128, INC], F32, 2)
        xr = Ring(K, st, 'in_x', [128, D], F32, 3)
        hr = Ring(K, st, 'in_h', [128, D], F32, 2)
        hbr = Ring(K, st, 'in_hb', [128, D], BF16, 2)
        hTr = Ring(K, st, 'in_hT', [128, 8, 128], BF16, 2)
        sq, sqd = sb(st, 'in_sq', [128, D], F32)
        ssr = Ring(K, st, 'in_ss', [128, 1], F32, 4)
        pr = Ring(K, st, 'in_p', [128, INC], F32, 2)
        ptr = Ring(K, st, 'in_pT', [128, 8, 128], BF16, 2, psum=True)
        pmr = Ring(K, st, 'in_pm', [128, 512], F32, 4, psum=True)
        win = I['w_in'][l].rearrange("(k p) n -> p k n", p=128)
        for kc in range(8):
            (ws, wsd) = wst.next()
            P.dma('sp', lambda e, ws=ws, kc=kc: e.dma_start(out=ws[:], in_=win[:, kc, :]), [], [wsd])
            P.op('pool', lambda e, ws=ws, kc=kc: e.tensor_copy(out=wbf[:, kc, :], in_=ws[:]), [wsd], [wbfd])
        for t in range(NTILE):
            mt, mtd = (modC, modCd) if t < 2 else (modL, modLd)
            (x, xd) = xr.next()
            (h, hd) = hr.next()
            (hb, hbd) = hbr.next()
            (hT, hTd) = hTr.next()
            (ss, ssd) = ssr.next()
            (pt, ptd) = pr.next()
            (pT, pTd) = ptr.next()
            P.dma('sp', lambda e, x=x, t=t: e.dma_start(out=x[:], in_=S['xs'][t * 128:(t + 1) * 128, :]), [S['xs_d'][t]], [xd])
            rms_rstd(x[:], xd, D, sq, sqd, ss, ssd, 0)
            P.op('dve', lambda e, h=h, x=x, ss=ss, mt=mt: e.scalar_tensor_tensor(out=h[:], in0=x[:], scalar=ss[:, 0:1], in1=mt[:, 1, :], op0=ALU.mult, op1=ALU.mult),
                 [xd, ssd, mtd], [hd])
            P.op('pool', lambda e, h=h, hb=hb, mt=mt: e.tensor_tensor(out=hb[:], in0=h[:], in1=mt[:, 0, :], op=ALU.add), [hd, mtd], [hbd])
            for kc in range(8):
                P.op('pe', lambda e, pT=pT, hb=hb, kc=kc: e.transpose(out=pT[:, kc, :], in_=hb[:, kc * 128:(kc + 1) * 128], identity=ident[:]),
                     [hbd, identd], [pTd])
            P.op('act', lambda e, hT=hT, pT=pT: e.copy(out=hT[:], in_=pT[:]), [pTd], [hTd])
            for nb in range(6):
                c0 = nb * 512
                cw = min(512, INC - c0)
                (pm, pmd) = pmr.next()
                for kc in range(8):
                    P.op('pe', lambda e, pm=pm, hT=hT, kc=kc, c0=c0, cw=cw: e.matmul(pm[:, 0:cw], lhsT=hT[:, kc, :], rhs=wbf[:, kc, c0:c0 + cw], start=(kc == 0), stop=(kc == 7)),
                         [hTd, wbfd], [pmd])
                if nb % 2 == 0:
                    P.op('act', lambda e, pm=pm, pt=pt, c0=c0, cw=cw: e.copy(out=pt[:, c0:c0 + cw], in_=pm[:, 0:cw]), [pmd], [ptd])
                else:
                    P.op('dve', lambda e, pm=pm, pt=pt, c0=c0, cw=cw: e.tensor_copy(out=pt[:, c0:c0 + cw], in_=pm[:, 0:cw]), [pmd], [ptd])
            P.dma('sp', lambda e, pt=pt, t=t: e.dma_start(out=S['p'][t * 128:(t + 1) * 128, :], in_=pt[:]), [ptd], [S['p_d'][t]])
        P.barrier()


def attn_finalize(st_rings, O, Od, h, ot, otd, nq, ident32, ident32d):
    P = K.P
    osbr, ptr, rvr = st_rings
    (osb, osbd) = osbr.next()
    P.op('dve', lambda e: e.tensor_copy(out=osb[:, 0:nq], in_=O[:, 0:nq]), [Od], [osbd])
    for j in range(nq // 128):
        (pt, ptd) = ptr.next()
        (rv, rvd) = rvr.next()
        P.op('pe', lambda e, pt=pt, osb=osb, j=j: e.transpose(out=pt[:], in_=osb[:, j * 128:(j + 1) * 128], identity=ident32[0:65, 0:65]),
             [osbd, ident32d], [ptd])
        P.op('dve', lambda e, pt=pt, rv=rv: e.reciprocal(out=rv[:], in_=pt[:, 64:65]), [ptd], [rvd])
        P.op('dve', lambda e, pt=pt, rv=rv, j=j: e.tensor_scalar(out=ot[:, j, h * 64:(h + 1) * 64], in0=pt[:, 0:64], scalar1=rv[:, 0:1], scalar2=None, op0=ALU.mult),
             [ptd, rvd], [otd])


def attn_core(st, S, QT, QTd, KT, KTd, kvmap, Vaug, Vaugd, scale, col0, need_ctx, ident32, ident32d, tag):
    P = K.P
    sr = Ring(K, st, tag + '_S', [128, 512], F32, 3, psum=True)
    orr = Ring(K, st, tag + '_O', [65, 512], F32, 2, psum=True)
    ptr = Ring(K, st, tag + '_fT', [128, 65], F32, 2, psum=True)
    pr = Ring(K, st, tag + '_P', [128, 512], BF16, 3)
    osbr = Ring(K, st, tag + '_osb', [65, 512], F32, 2)
    rvr = Ring(K, st, tag + '_rv', [128, 1], F32, 4)
    otr = Ring(K, st, tag + '_ot', [128, 4, 256], F32, 2)
    blocks = []
    if need_ctx:
        blocks.append((0, 256, [0, 1]))
    for qb in range(8):
        blocks.append((256 + qb * 512, 512, list(range(NTILE))))
    for (q0, nq, kts) in blocks:
        (ot, otd) = otr.next()
        for h in range(4):
            g = kvmap[h]
            (O, Od) = orr.next()
            pend = None
            for i, kt in enumerate(kts):
                (Sp, Spd) = sr.next()
                (Pt, Ptd) = pr.next()
                P.op('pe', lambda e, Sp=Sp, g=g, h=h, kt=kt, q0=q0, nq=nq: e.matmul(Sp[:, 0:nq], lhsT=KT(g)[:, kt * 128:(kt + 1) * 128], rhs=QT(h)[:, q0:q0 + nq], start=True, stop=True),
                     [QTd, KTd], [Spd])
                P.op('act', lambda e, Sp=Sp, Pt=Pt, nq=nq: e.activation(out=Pt[:, 0:nq], in_=Sp[:, 0:nq], func=AF.Exp, scale=scale), [Spd], [Ptd])
                if pend is not None:
                    pend()

                def pend(O=O, Od=Od, g=g, kt=kt, Pt=Pt, Ptd=Ptd, nq=nq, i=i, n=len(kts)):
                    P.op('pe', lambda e: e.matmul(O[:, 0:nq], lhsT=Vaug[:, kt, g, :], rhs=Pt[:, 0:nq], start=(i == 0), stop=(i == n - 1)),
                         [Vaugd, Ptd], [Od])
            pend()
            attn_finalize((osbr, ptr, rvr), O, Od, h, ot, otd, nq, ident32, ident32d)
        nj = nq // 128
        P.dma('sp', lambda e, ot=ot, q0=q0, nj=nj: e.dma_start(out=S['ocat'][q0:q0 + nj * 128, col0:col0 + 256].rearrange("(j p) c -> p j c", p=128), in_=ot[:, 0:nj, :]),
              [otd], [S['ocat_d'][t] for t in range(q0 // 128, q0 // 128 + nj)])


def phase_gqa(l, I, S, need_ctx, ident, identd, ident32, ident32d):
    nc, P = K.nc, K.P
    with ExitStack() as st:
        QKT, QKTd = sb(st, 'gq_QKT', [64, 6, NT], BF16)
        Vaug, Vaugd = sb(st, 'gq_V', [128, NTILE, 2, 65], BF16)
        with ExitStack() as st2:
            gain, gaind = sb(st2, 'gq_gain', [128, 6, 64], F32)
            xr = Ring(K, st2, 'gq_x', [128, 512], F32, 3)
            sq, sqd = sb(st2, 'gq_sq', [128, 384], F32)
            ssr = Ring(K, st2, 'gq_ss', [128, 6], F32, 3)
            qnr = Ring(K, st2, 'gq_qn', [128, 6, 64], F32, 2)
            qbr = Ring(K, st2, 'gq_qb', [128, 6, 64], BF16, 2)
            csr = Ring(K, st2, 'gq_cs', [128, 2, 32], F32, 3)
            t1r = Ring(K, st2, 'gq_t1', [128, 6, 32], F32, 2)
            t2r = Ring(K, st2, 'gq_t2', [128, 6, 32], F32, 2)
            pTr = Ring(K, st2, 'gq_pT', [64, 6, 128], BF16, 2, psum=True)
            P.dma('sp', lambda e: e.dma_start(out=gain[:, 0:4, :], in_=I['gqa_q_g'][l:l + 1, :].partition_broadcast(128).to_broadcast([128, 4, 64])), [], [gaind])
            P.dma('sp', lambda e: e.dma_start(out=gain[:, 4:6, :], in_=I['gqa_k_g'][l:l + 1, :].partition_broadcast(128).to_broadcast([128, 2, 64])), [], [gaind])
            P.op('pool', lambda e: e.memset(Vaug[:, :, :, 64:65], 1.0), [], [Vaugd])
            for t in range(NTILE):
                (x, xd) = xr.next()
                (ss, ssd) = ssr.next()
                (qn, qnd) = qnr.next()
                (qb, qbd) = qbr.next()
                (pT, pTd) = pTr.next()
                P.dma('sp', lambda e, x=x, t=t: e.dma_start(out=x[:], in_=S['p'][t * 128:(t + 1) * 128, GQ0:GQ0 + 512]), [S['p_d'][t]], [xd])
                P.op('dve', lambda e, x=x: e.tensor_tensor(out=sq[:], in0=x[:, 0:384], in1=x[:, 0:384], op=ALU.mult), [xd], [sqd])
                P.op('dve', lambda e, ss=ss: e.tensor_reduce(out=ss[:], in_=sq[:].rearrange("p (g d) -> p g d", d=64), axis=AX.X, op=ALU.add), [sqd], [ssd])
                P.op('act', lambda e, ss=ss: e.activation(out=ss[:], in_=ss[:], func=AF.Sqrt, bias=RMS_EPS, scale=1.0 / 64), [ssd], [ssd])
                P.op('dve', lambda e, ss=ss: e.reciprocal(out=ss[:], in_=ss[:]), [ssd], [ssd])
                P.op('dve', lambda e, x=x, qn=qn, ss=ss: e.tensor_tensor(out=qn[:], in0=x[:, 0:384].rearrange("p (g d) -> p g d", d=64), in1=ss[:].unsqueeze(2).to_broadcast([128, 6, 64]), op=ALU.mult),
                     [xd, ssd], [qnd])
                P.op('pool', lambda e, x=x, t=t: e.tensor_copy(out=Vaug[:, t, :, 0:64], in_=x[:, 384:512].rearrange("p (g d) -> p g d", d=64)), [xd], [Vaugd])
                if t < 2:
                    P.op('dve', lambda e, qn=qn, qb=qb: e.tensor_tensor(out=qb[:], in0=qn[:], in1=gain[:], op=ALU.mult), [qnd, gaind], [qbd])
                else:
                    (cs, csd) = csr.next()
                    (t1, t1d) = t1r.next()
                    (t2, t2d) = t2r.next()
                    r0 = (t - 2) * 128
                    P.dma('sp', lambda e, cs=cs, r0=r0: e.dma_start(out=cs[:], in_=I['rope_g'][r0:r0 + 128, :, :]), [], [csd])
                    P.op('dve', lambda e, qn=qn: e.tensor_tensor(out=qn[:], in0=qn[:], in1=gain[:], op=ALU.mult), [qnd, gaind], [qnd])
                    cosb = lambda cs=cs: cs[:, 0, :].unsqueeze(1).to_broadcast([128, 6, 32])
                    sinb = lambda cs=cs: cs[:, 1, :].unsqueeze(1).to_broadcast([128, 6, 32])
                    P.op('dve', lambda e, t1=t1, qn=qn, cosb=cosb: e.tensor_tensor(out=t1[:], in0=qn[:, :, 0:32], in1=cosb(), op=ALU.mult), [qnd, csd], [t1d])
                    P.op('pool', lambda e, t2=t2, qn=qn, sinb=sinb: e.tensor_tensor(out=t2[:], in0=qn[:, :, 32:64], in1=sinb(), op=ALU.mult), [qnd, csd], [t2d])
                    P.op('dve', lambda e, t1=t1, t2=t2, qb=qb: e.tensor_tensor(out=qb[:, :, 0:32], in0=t1[:], in1=t2[:], op=ALU.subtract), [t1d, t2d], [qbd])
                    P.op('dve', lambda e, t1=t1, qn=qn, sinb=sinb: e.tensor_tensor(out=t1[:], in0=qn[:, :, 0:32], in1=sinb(), op=ALU.mult), [qnd, csd], [t1d])
                    P.op('pool', lambda e, t2=t2, qn=qn, cosb=cosb: e.tensor_tensor(out=t2[:], in0=qn[:, :, 32:64], in1=cosb(), op=ALU.mult), [qnd, csd], [t2d])
                    P.op('dve', lambda e, t1=t1, t2=t2, qb=qb: e.tensor_tensor(out=qb[:, :, 32:64], in0=t1[:], in1=t2[:], op=ALU.add), [t1d, t2d], [qbd])
                for g in range(6):
                    P.op('pe', lambda e, pT=pT, qb=qb, g=g: e.transpose(out=pT[:, g, :], in_=qb[:, g, :], identity=ident[:]), [qbd, identd], [pTd])
                P.op('act', lambda e, pT=pT, t=t: e.copy(out=QKT[:, :, t * 128:(t + 1) * 128], in_=pT[:]), [pTd], [QKTd])
            P.barrier()
        attn_core(st, S, lambda h: QKT[:, h, :], QKTd, lambda g: QKT[:, 4 + g, :], QKTd, [0, 0, 1, 1], Vaug, Vaugd, 0.125, 256,
                  need_ctx, ident32, ident32d, 'gq')
        P.barrier()


def phase_mla(l, I, S, need_ctx, ident, identd, ident32, ident32d):
    nc, P = K.nc, K.P
    with ExitStack() as st:
        QKT, QKTd = sb(st, 'ml_QKT', [128, 8, NT], BF16)
        Vaug, Vaugd = sb(st, 'ml_V', [128, NTILE, 4, 65], BF16)
        with ExitStack() as st2:
            gain, gaind = sb(st2, 'ml_gain', [128, 384], F32)
            wst, wstd = sb(st2, 'ml_wst', [128, 2, 512], F32)
            wuq, wuqd = sb(st2, 'ml_wuq', [128, 2, 384], BF16)
            wukv, wukvd = sb(st2, 'ml_wukv', [128, 512], BF16)
            xr = Ring(K, st2, 'ml_x', [128, 416], F32, 3)
            sq, sqd = sb(st2, 'ml_sq', [128, 384], F32)
            ssr = Ring(K, st2, 'ml_ss', [128, 2], F32, 3)
            cnr = Ring(K, st2, 'ml_cn', [128, 384], F32, 2)
            cbr = Ring(K, st2, 'ml_cb', [128, 384], BF16, 2)
            cTr = Ring(K, st2, 'ml_cT', [128, 3, 128], BF16, 2)
            qsr = Ring(K, st2, 'ml_qs', [128, 4, 96], F32, 2)
            csr = Ring(K, st2, 'ml_cs', [128, 2, 16], F32, 3)
            t1r = Ring(K, st2, 'ml_t1', [128, 5, 16], F32, 2)
            t2r = Ring(K, st2, 'ml_t2', [128, 5, 16], F32, 2)
            rr = Ring(K, st2, 'ml_r', [128, 5, 32], F32, 2)
            qkr = Ring(K, st2, 'ml_qk', [128, 8, 128], BF16, 2)
            for (qk_, qkd_) in qkr.bufs:
                P.op('pool', lambda e, qk_=qk_: e.memset(qk_[:], 0.0), [], [qkd_])
            pcT = Ring(K, st2, 'ml_pcT', [128, 3, 128], BF16, 2, psum=True)
            pq = Ring(K, st2, 'ml_pq', [128, 384], F32, 1, psum=True)
            pkv = Ring(K, st2, 'ml_pkv', [128, 512], F32, 2, psum=True)
            pT2 = Ring(K, st2, 'ml_pT2', [128, 8, 128], BF16, 2, psum=True)
            P.dma('sp', lambda e: e.dma_start(out=gain[:, 0:256], in_=I['mla_q_norm_g'][l:l + 1, :].partition_broadcast(128)), [], [gaind])
            P.dma('sp', lambda e: e.dma_start(out=gain[:, 256:384], in_=I['mla_kv_norm_g'][l:l + 1, :].partition_broadcast(128)), [], [gaind])
            P.dma('sp', lambda e: e.dma_start(out=wst[:, :, 0:384], in_=I['mla_w_uq'][l].rearrange("(k p) n -> p k n", p=128)), [], [wstd])
            P.op('pool', lambda e: e.tensor_copy(out=wuq[:], in_=wst[:, :, 0:384]), [wstd], [wuqd])
            P.dma('sp', lambda e: e.dma_start(out=wst[:, 0, :], in_=I['mla_w_ukv'][l]), [wuqd], [wstd])
            P.op('pool', lambda e: e.tensor_copy(out=wukv[:], in_=wst[:, 0, :]), [wstd], [wukvd])
            P.op('pool', lambda e: e.memset(Vaug[:, :, :, 64:65], 1.0), [], [Vaugd])
            for t in range(NTILE):
                (x, xd) = xr.next()
                (ss, ssd) = ssr.next()
                (cn, cnd) = cnr.next()
                (cb, cbd) = cbr.next()
                (cT, cTd) = cTr.next()
                (qs, qsd) = qsr.next()
                (qk, qkd) = qkr.next()
                (r, rd) = rr.next()
                (pc, pcd) = pcT.next()
                (pqt, pqd) = pq.next()
                (pk, pkd) = pkv.next()
                (pT, pTd) = pT2.next()
                P.dma('sp', lambda e, x=x, t=t: e.dma_start(out=x[:], in_=S['p'][t * 128:(t + 1) * 128, ML0:ML0 + 416]), [S['p_d'][t]], [xd])
                if K.LVL < 2:
                    continue
                P.op('dve', lambda e, x=x: e.tensor_tensor(out=sq[:], in0=x[:, 0:384], in1=x[:, 0:384], op=ALU.mult), [xd], [sqd])
                P.op('dve', lambda e, ss=ss: e.tensor_reduce(out=ss[:, 0:1], in_=sq[:, 0:256], axis=AX.X, op=ALU.add), [sqd], [ssd])
                P.op('dve', lambda e, ss=ss: e.tensor_reduce(out=ss[:, 1:2], in_=sq[:, 256:384], axis=AX.X, op=ALU.add), [sqd], [ssd])
                P.op('act', lambda e, ss=ss: e.activation(out=ss[:, 0:1], in_=ss[:, 0:1], func=AF.Sqrt, bias=RMS_EPS, scale=1.0 / 256), [ssd], [ssd])
                P.op('act', lambda e, ss=ss: e.activation(out=ss[:, 1:2], in_=ss[:, 1:2], func=AF.Sqrt, bias=RMS_EPS, scale=1.0 / 128), [ssd], [ssd])
                P.op('dve', lambda e, ss=ss: e.reciprocal(out=ss[:], in_=ss[:]), [ssd], [ssd])
                P.op('dve', lambda e, x=x, cn=cn, ss=ss: e.scalar_tensor_tensor(out=cn[:, 0:256], in0=x[:, 0:256], scalar=ss[:, 0:1], in1=gain[:, 0:256], op0=ALU.mult, op1=ALU.mult),
                     [xd, ssd, gaind], [cnd])
                P.op('dve', lambda e, x=x, cn=cn, ss=ss: e.scalar_tensor_tensor(out=cn[:, 256:384], in0=x[:, 256:384], scalar=ss[:, 1:2], in1=gain[:, 256:384], op0=ALU.mult, op1=ALU.mult),
                     [xd, ssd, gaind], [cnd])
                P.op('pool', lambda e, cn=cn, cb=cb: e.tensor_copy(out=cb[:], in_=cn[:]), [cnd], [cbd])
                if K.LVL < 3:
                    continue
                for j in range(3):
                    P.op('pe', lambda e, pc=pc, cb=cb, j=j: e.transpose(out=pc[:, j, :], in_=cb[:, j * 128:(j + 1) * 128], identity=ident[:]), [cbd, identd], [pcd])
                if K.LVL < 2.3:
                    continue
                P.op('act', lambda e, cT=cT, pc=pc: e.copy(out=cT[:], in_=pc[:]), [pcd], [cTd])
                if K.LVL < 2.6:
                    continue
                for j in range(2):
                    P.op('pe', lambda e, pqt=pqt, cT=cT, j=j: e.matmul(pqt[:], lhsT=cT[:, j, :], rhs=wuq[:, j, :], start=(j == 0), stop=(j == 1)), [cTd, wuqd], [pqd])
                if K.LVL < 2.8:
                    continue
                for hf in range(2):
                    P.op('pe', lambda e, pk=pk, cT=cT, hf=hf: e.matmul(pk[:, hf * 256:(hf + 1) * 256], lhsT=cT[:, 2, :], rhs=wukv[:, hf * 256:(hf + 1) * 256], start=True, stop=True), [cTd, wukvd], [pkd])
                if K.LVL < 4:
                    continue
                P.op('act', lambda e, qs=qs, pqt=pqt: e.copy(out=qs[:], in_=pqt[:].rearrange("p (h d) -> p h d", d=96)), [pqd], [qsd])
                P.op('dve', lambda e, pk=pk, t=t: e.tensor_copy(out=Vaug[:, t, :, 0:64], in_=pk[:].rearrange("p (h d) -> p h d", d=128)[:, :, 64:128]), [pkd], [Vaugd])
                P.op('dve', lambda e, pk=pk, qk=qk: e.tensor_copy(out=qk[:, 4:8, 0:64], in_=pk[:].rearrange("p (h d) -> p h d", d=128)[:, :, 0:64]), [pkd], [qkd])
                P.op('pool', lambda e, qs=qs, qk=qk: e.tensor_copy(out=qk[:, 0:4, 0:64], in_=qs[:, :, 0:64]), [qsd], [qkd])
                P.op('pool', lambda e, r=r, qs=qs: e.tensor_copy(out=r[:, 0:4, :], in_=qs[:, :, 64:96]), [qsd], [rd])
                P.op('pool', lambda e, r=r, x=x: e.tensor_copy(out=r[:, 4, :], in_=x[:, 384:416]), [xd], [rd])
                if K.LVL < 5:
                    continue
                if t < 2:
                    P.op('dve', lambda e, r=r, qk=qk: e.tensor_copy(out=qk[:, 0:4, 64:96], in_=r[:, 0:4, :]), [rd], [qkd])
                    P.op('dve', lambda e, r=r, qk=qk: e.tensor_copy(out=qk[:, 4:8, 64:96], in_=r[:, 4, :].unsqueeze(1).to_broadcast([128, 4, 32])), [rd], [qkd])
                else:
                    (cs, csd) = csr.next()
                    (t1, t1d) = t1r.next()
                    (t2, t2d) = t2r.next()
                    r0 = (t - 2) * 128
                    P.dma('sp', lambda e, cs=cs, r0=r0: e.dma_start(out=cs[:], in_=I['rope_m'][r0:r0 + 128, :, :]), [], [csd])
                    cosb = lambda cs=cs: cs[:, 0, :].unsqueeze(1).to_broadcast([128, 5, 16])
                    sinb = lambda cs=cs: cs[:, 1, :].unsqueeze(1).to_broadcast([128, 5, 16])
                    P.op('dve', lambda e, t1=t1, r=r, cosb=cosb: e.tensor_tensor(out=t1[:], in0=r[:, :, 0:16], in1=cosb(), op=ALU.mult), [rd, csd], [t1d])
                    P.op('pool', lambda e, t2=t2, r=r, sinb=sinb: e.tensor_tensor(out=t2[:], in0=r[:, :, 16:32], in1=sinb(), op=ALU.mult), [rd, csd], [t2d])
                    P.op('dve', lambda e, t1=t1, t2=t2: e.tensor_tensor(out=t1[:], in0=t1[:], in1=t2[:], op=ALU.subtract), [t1d, t2d], [t1d])
                    P.op('dve', lambda e, t1=t1, qk=qk: e.tensor_copy(out=qk[:, 0:4, 64:80], in_=t1[:, 0:4, :]), [t1d], [qkd])
                    P.op('dve', lambda e, t1=t1, qk=qk: e.tensor_copy(out=qk[:, 4:8, 64:80], in_=t1[:, 4, :].unsqueeze(1).to_broadcast([128, 4, 16])), [t1d], [qkd])
                    (t1, t1d) = t1r.next()
                    (t2, t2d) = t2r.next()
                    P.op('dve', lambda e, t1=t1, r=r, sinb=sinb: e.tensor_tensor(out=t1[:], in0=r[:, :, 0:16], in1=sinb(), op=ALU.mult), [rd, csd], [t1d])
                    P.op('pool', lambda e, t2=t2, r=r, cosb=cosb: e.tensor_tensor(out=t2[:], in0=r[:, :, 16:32], in1=cosb(), op=ALU.mult), [rd, csd], [t2d])
                    P.op('dve', lambda e, t1=t1, t2=t2: e.tensor_tensor(out=t1[:], in0=t1[:], in1=t2[:], op=ALU.add), [t1d, t2d], [t1d])
                    P.op('dve', lambda e, t1=t1, qk=qk: e.tensor_copy(out=qk[:, 0:4, 80:96], in_=t1[:, 0:4, :]), [t1d], [qkd])
                    P.op('dve', lambda e, t1=t1, qk=qk: e.tensor_copy(out=qk[:, 4:8, 80:96], in_=t1[:, 4, :].unsqueeze(1).to_broadcast([128, 4, 16])), [t1d], [qkd])
                if K.LVL < 6:
                    continue
                for g in range(8):
                    P.op('pe', lambda e, pT=pT, qk=qk, g=g: e.transpose(out=pT[:, g, :], in_=qk[:, g, :], identity=ident[:]), [qkd, identd], [pTd])
                P.op('act', lambda e, pT=pT, t=t: e.copy(out=QKT[:, :, t * 128:(t + 1) * 128], in_=pT[:]), [pTd], [QKTd])
            P.barrier()
        if K.LVL < 7:
            return
        attn_core(st, S, lambda h: QKT[:, h, :], QKTd, lambda g: QKT[:, 4 + g, :], QKTd, [0, 1, 2, 3], Vaug, Vaugd, 96.0 ** -0.5, 512,
                  need_ctx, ident32, ident32d, 'ml')
        P.barrier()


BIG = 30000.0


def nat_plan():
    variants = {}
    plan = []
    for i in range(64):
        rs = min(max(i - 4, 0), 56)
        tiles = []
        for m in range(rs // 2, (rs + 7) // 2 + 1):
            dd = []
            for r in (2 * m, 2 * m + 1):
                dd.append(r - i + 7 if rs <= r < rs + 8 else -1)
            key = tuple(dd)
            if key not in variants:
                variants[key] = len(variants)
            tiles.append((2 + m, variants[key]))
        plan.append(tiles)
    vlist = [None] * len(variants)
    for k, v in variants.items():
        vlist[v] = k
    return plan, vlist


def nat_bias_table(nat_bias_l):
    plan, vlist = nat_plan()
    c = np.arange(64)
    cs = np.clip(c - 8, 0, 48)
    cp = np.arange(64)
    inwin = (cp[:, None] >= cs[None, :]) & (cp[:, None] < cs[None, :] + 16)
    off = np.clip(cp[:, None] - c[None, :] + 15, 0, 30)
    tab = np.full((128, 4, len(vlist), 64), -BIG, np.float32)
    for v, (d0, d1) in enumerate(vlist):
        for half, d in enumerate((d0, d1)):
            if d < 0:
                continue
            for h in range(4):
                vals = nat_bias_l[h, d][off]
                tab[half * 64:(half + 1) * 64, h, v, :] = np.where(inwin, vals, np.float32(-BIG))
    return tab


def phase_nat(l, I, S, need_ctx, ident, identd, ident32, ident32d):
    nc, P = K.nc, K.P
    plan, vlist = nat_plan()
    NV = len(vlist)
    with ExitStack() as st:
        QKT, QKTd = sb(st, 'na_QKT', [64, 8, NT], BF16)
        Vaug, Vaugd = sb(st, 'na_V', [128, NTILE, 4, 65], BF16)
        tb, tbd = sb(st, 'na_tb', [128, 4, NV, 64], BF16)
        with ExitStack() as st2:
            tbs, tbsd = sb(st2, 'na_tbs', [128, 4, NV, 64], F32)
            xr = Ring(K, st2, 'na_x', [128, 768], F32, 3)
            xbr = Ring(K, st2, 'na_xb', [128, 512], BF16, 2)
            pTr = Ring(K, st2, 'na_pT', [64, 8, 128], BF16, 2, psum=True)
            P.dma('sp', lambda e: e.dma_start(out=tbs[:], in_=I['nat_tab'][l]), [], [tbsd])
            P.op('dve', lambda e: e.tensor_scalar(out=tb[:], in0=tbs[:], scalar1=8.0, scalar2=None, op0=ALU.mult), [tbsd], [tbd])
            P.op('pool', lambda e: e.memset(Vaug[:, :, :, 64:65], 1.0), [], [Vaugd])
            for t in range(NTILE):
                (x, xd) = xr.next()
                (xb, xbd) = xbr.next()
                (pT, pTd) = pTr.next()
                P.dma('sp', lambda e, x=x, t=t: e.dma_start(out=x[:], in_=S['p'][t * 128:(t + 1) * 128, NA0:NA0 + 768]), [S['p_d'][t]], [xd])
                P.op('dve', lambda e, x=x, xb=xb: e.tensor_copy(out=xb[:], in_=x[:, 0:512]), [xd], [xbd])
                P.op('pool', lambda e, x=x, t=t: e.tensor_copy(out=Vaug[:, t, :, 0:64], in_=x[:, 512:768].rearrange("p (h d) -> p h d", d=64)), [xd], [Vaugd])
                for g in range(8):
                    P.op('pe', lambda e, pT=pT, xb=xb, g=g: e.transpose(out=pT[:, g, :], in_=xb[:, g * 64:(g + 1) * 64], identity=ident[:]), [xbd, identd], [pTd])
                P.op('act', lambda e, pT=pT, t=t: e.copy(out=QKT[:, :, t * 128:(t + 1) * 128], in_=pT[:]), [pTd], [QKTd])
            P.barrier()
        sr = Ring(K, st, 'na_S', [128, 512], F32, 3, psum=True)
        orr = Ring(K, st, 'na_O', [65, 512], F32, 2, psum=True)
        ptr = Ring(K, st, 'na_fT', [128, 65], F32, 2, psum=True)
        pr = Ring(K, st, 'na_P', [128, 512], BF16, 3)
        osbr = Ring(K, st, 'na_osb', [65, 512], F32, 2)
        rvr = Ring(K, st, 'na_rv', [128, 1], F32, 4)
        otr = Ring(K, st, 'na_ot', [128, 4, 256], F32, 2)
        blocks = []
        if need_ctx:
            blocks.append(None)
        for qb in range(8):
            blocks.append(qb)
        for qb in blocks:
            (ot, otd) = otr.next()
            if qb is None:
                q0, nq = 0, 256
            else:
                q0, nq = 256 + qb * 512, 512
            for h in range(4):
                (O, Od) = orr.next()
                if qb is None:
                    for i, kt in enumerate((0, 1)):
                        (Sp, Spd) = sr.next()
                        (Pt, Ptd) = pr.next()
                        P.op('pe', lambda e, Sp=Sp, h=h, kt=kt: e.matmul(Sp[:, 0:256], lhsT=QKT[:, 4 + h, kt * 128:(kt + 1) * 128], rhs=QKT[:, h, 0:256], start=True, stop=True),
                             [QKTd], [Spd])
                        P.op('act', lambda e, Sp=Sp, Pt=Pt: e.activation(out=Pt[:, 0:256], in_=Sp[:, 0:256], func=AF.Exp, scale=0.125), [Spd], [Ptd])
                        P.op('pe', lambda e, O=O, h=h, kt=kt, Pt=Pt, i=i: e.matmul(O[:, 0:256], lhsT=Vaug[:, kt, h, :], rhs=Pt[:, 0:256], start=(i == 0), stop=(i == 1)),
                             [Vaugd, Ptd], [Od])
                else:
                    for ri in range(8):
                        i = qb * 8 + ri
                        qt0 = 256 + i * 64
                        tiles = [(kt, None) for kt in (0, 1)] + plan[i]
                        (Sp, Spd) = sr.next()
                        (Pt, Ptd) = pr.next()
                        for j, (kt, v) in enumerate(tiles):
                            P.op('pe', lambda e, Sp=Sp, h=h, kt=kt, qt0=qt0, j=j, v=v: e.matmul(Sp[:, j * 64:(j + 1) * 64], lhsT=QKT[:, 4 + h, kt * 128:(kt + 1) * 128], rhs=QKT[:, h, qt0:qt0 + 64], start=True, stop=(v is None)),
                                 [QKTd], [Spd])
                            if v is not None:
                                P.op('pe', lambda e, Sp=Sp, h=h, j=j, v=v: e.matmul(Sp[:, j * 64:(j + 1) * 64], lhsT=ident[:], rhs=tb[:, h, v, :], start=False, stop=True),
                                     [identd, tbd], [Spd])
                        nk = len(tiles)
                        P.op('act', lambda e, Sp=Sp, Pt=Pt, nk=nk: e.activation(out=Pt[:, 0:nk * 64], in_=Sp[:, 0:nk * 64], func=AF.Exp, scale=0.125), [Spd], [Ptd])
                        for j, (kt, v) in enumerate(tiles):
                            P.op('pe', lambda e, O=O, h=h, kt=kt, Pt=Pt, j=j, ri=ri, nk=nk: e.matmul(O[:, ri * 64:(ri + 1) * 64], lhsT=Vaug[:, kt, h, :], rhs=Pt[:, j * 64:(j + 1) * 64], start=(j == 0), stop=(j == nk - 1)),
                                 [Vaugd, Ptd], [Od])
                attn_finalize((osbr, ptr, rvr), O, Od, h, ot, otd, nq, ident32, ident32d)
            nj = nq // 128
            P.dma('sp', lambda e, ot=ot, q0=q0, nj=nj: e.dma_start(out=S['ocat'][q0:q0 + nj * 128, 768:1024].rearrange("(j p) c -> p j c", p=128), in_=ot[:, 0:nj, :]),
                  [otd], [S['ocat_d'][t] for t in range(q0 // 128, q0 // 128 + nj)])
        P.barrier()


def o_tt(q, out, in0, in1, op, rd, wr):
    K.P.op(q, lambda e: e.tensor_tensor(out=out, in0=in0, in1=in1, op=op), rd, wr)


def o_stt(q, out, in0, scalar, in1, op0, op1, rd, wr):
    K.P.op(q, lambda e: e.scalar_tensor_tensor(out=out, in0=in0, scalar=scalar, in1=in1, op0=op0, op1=op1), rd, wr)


def o_ts(q, out, in0, s1, op0, rd, wr):
    K.P.op(q, lambda e: e.tensor_scalar(out=out, in0=in0, scalar1=s1, scalar2=None, op0=op0), rd, wr)


def o_act(out, in_, func, rd, wr, **kw):
    K.P.op('act', lambda e: e.activation(out=out, in_=in_, func=func, **kw), rd, wr)


def o_red(q, out, in_, op, rd, wr):
    K.P.op(q, lambda e: e.tensor_reduce(out=out, in_=in_, axis=AX.X, op=op), rd, wr)


def o_mm(out, lhsT, rhs, rd, wr, start=True, stop=True):
    K.P.op('pe', lambda e: e.matmul(out, lhsT=lhsT, rhs=rhs, start=start, stop=stop), rd, wr)


def o_tr(out, in_, ident, rd, wr):
    K.P.op('pe', lambda e: e.transpose(out=out, in_=in_, identity=ident), rd, wr)


def o_cp(q, out, in_, rd, wr):
    if q == 'act':
        K.P.op('act', lambda e: e.copy(out=out, in_=in_), rd, wr)
    else:
        K.P.op(q, lambda e: e.tensor_copy(out=out, in_=in_), rd, wr)


def o_rcp(out, in_, rd, wr):
    K.P.op('dve', lambda e: e.reciprocal(out=out, in_=in_), rd, wr)


def o_ms(q, out, val, wr):
    K.P.op(q, lambda e: e.memset(out, val), [], wr)


def o_dma(out, in_, rd, wr, q='sp', **kw):
    K.P.dma(q, lambda e: e.dma_start(out=out, in_=in_, **kw), rd, wr)


def bc_load(st, name, src_row, n):
    t, d = sb(st, name, [128, n], F32)
    o_dma(t[:], src_row.partition_broadcast(128), [], [d])
    return t, d


RCH = 8


def rev_tile(c):
    return 1 - c if c < 2 else 35 - c


def phase_rwkv(l, I, S, need_ctx, ident32, ident32d, J32, J32d):
    rwkv_prep(l, I, S, ident32, ident32d, J32, J32d)
    rwkv_scan(S)
    rwkv_readout(l, I, S, need_ctx, J32, J32d)


def rwkv_prep(l, I, S, ident32, ident32d, J32, J32d):
    P = K.P
    MUL, ADD, SUB = ALU.mult, ALU.add, ALU.subtract
    with ExitStack() as st:
        xr = Ring(K, st, 'rv_x', [128, 1024], F32, 2)
        xo = Ring(K, st, 'rv_o', [128, 1024], F32, 2)
        pr = Ring(K, st, 'rv_ps', [128, 512], F32, 2, psum=True)
        for c in range(NTILE):
            tt_ = rev_tile(c)
            (x, xd) = xr.next()
            (o, od) = xo.next()
            o_dma(x[:], S['p'][tt_ * 128:(tt_ + 1) * 128, 0:1024], [S['p_d'][tt_]], [xd])
            for hf in range(2):
                (ps_, psd) = pr.next()
                o_mm(ps_[:], J32[:], x[:, hf * 512:(hf + 1) * 512], [J32d, xd], [psd])
                o_cp('act' if hf else 'dve', o[:, hf * 512:(hf + 1) * 512], ps_[:], [psd], [od])
            o_dma(S['prw1'][c * 128:(c + 1) * 128, :], o[:], [od], [S['prw1_d'][c]])
        P.barrier()
    with ExitStack() as st:
        mub, mubd = bc_load(st, 'rp_mu', I['rwkv_mu'][l, :], 1024)
        kkb, kkbd = bc_load(st, 'rp_kk', I['rwkv_k_k'][l, :], 256)
        kab, kabd = bc_load(st, 'rp_ka', I['rwkv_k_a'][l, :], 256)
        rkb, rkbd = bc_load(st, 'rp_rk', I['rwkv_r_k'][l].rearrange("h d -> (h d)"), 256)
        omka, omkad = sb(st, 'rp_omka', [128, 256], F32)
        K.P.op('dve', lambda e: e.tensor_scalar(out=omka[:], in0=kab[:], scalar1=-1.0, scalar2=1.0, op0=MUL, op1=ADD), [kabd], [omkad])
        w0b, a0b, wup, aup = [], [], [], []
        for d in range(2):
            w0b.append(bc_load(st, 'rp_w0%d' % d, I['rwkv_w0'][l, d, :], 256))
            a0b.append(bc_load(st, 'rp_a0%d' % d, I['rwkv_a0'][l, d, :], 256))
            t, td = sb(st, 'rp_wup%d' % d, [64, 256], F32)
            o_dma(t[:], I['rwkv_w_up'][l, d], [], [td])
            wup.append((t, td))
            t, td = sb(st, 'rp_aup%d' % d, [64, 256], F32)
            o_dma(t[:], I['rwkv_a_up'][l, d], [], [td])
            aup.append((t, td))
        gup, gupd = sb(st, 'rp_gup', [128, 256], F32)
        o_dma(gup[:], I['rwkv_g_up'][l], [], [gupd])

        xr = Ring(K, st, 'rp_x', [128, 1024], F32, 2)
        pvr = Ring(K, st, 'rp_pv', [128, 1024], F32, 2)
        nxr = Ring(K, st, 'rp_nx', [128, 1024], F32, 2)
        xsr = Ring(K, st, 'rp_xs', [128, 1024], F32, 2)
        kkr = Ring(K, st, 'rp_kkn', [128, 256], F32, 2)
        sqr = Ring(K, st, 'rp_sq', [128, 256], F32, 2)
        ssr = Ring(K, st, 'rp_ss', [128, 4], F32, 4)
        smr = Ring(K, st, 'rp_sm', [128, 128], F32, 4)
        sTr = Ring(K, st, 'rp_sT', [128, 128], F32, 4)
        ur = Ring(K, st, 'rp_u', [128, 256], F32, 2)
        ar_ = Ring(K, st, 'rp_a', [128, 256], F32, 2)
        mr = Ring(K, st, 'rp_m', [128, 256], F32, 2)
        kdr = Ring(K, st, 'rp_kd', [128, 256], F32, 3)
        kkar = Ring(K, st, 'rp_kka', [128, 256], F32, 3)
        bor = Ring(K, st, 'rp_bo', [128, 256], F32, 3)
        gtr = Ring(K, st, 'rp_gt', [128, 256], F32, 2)
        nkkr = Ring(K, st, 'rp_nkk', [128, 4, 2, 64], F32, 2)
        rrr = Ring(K, st, 'rp_rr', [128, 4, 2, 64], F32, 2)
        decr = Ring(K, st, 'rp_dec', [128, 4, 2, 64], F32, 2)
        fakr = Ring(K, st, 'rp_fak', [128, 128, 4, 2], F32, 2)
        farr = Ring(K, st, 'rp_far', [128, 128, 4, 2], F32, 2)
        fwr = Ring(K, st, 'rp_fw', [128, 128, 4], F32, 2)
        for ring in (fakr, farr):
            for (b_, bd_) in ring.bufs:
                o_ms('pool', b_[:], 0.0, [bd_])
        pbig = Ring(K, st, 'rp_pb', [128, 256], F32, 3, psum=True)
        ptr_ = Ring(K, st, 'rp_pt', [128, 128], F32, 3, psum=True)

        def v3(ap):
            return ap.rearrange("p (h k) -> p h k", k=64)

        for c in range(NTILE):
            first = c in (0, 2)
            last = c in (1, NTILE - 1)
            r0 = c * 128
            (nkk, nkkd) = nkkr.next()
            (rr, rrd) = rrr.next()
            (dec, decd) = decr.next()
            for d in range(2):
                if d == 0:
                    src = lambda a, b: S['p'][a:b, 0:1024]
                    sdeps = S['p_d']
                else:
                    src = lambda a, b: S['prw1'][a:b, :]
                    sdeps = S['prw1_d']
                nb = [sdeps[c]] + ([sdeps[c - 1]] if c > 0 else []) + ([sdeps[c + 1]] if c < NTILE - 1 else [])
                (x, xd) = xr.next()
                (pv, pvd) = pvr.next()
                (nx, nxd) = nxr.next()
                (xs, xsd) = xsr.next()
                o_dma(x[:], src(r0, r0 + 128), nb, [xd])
                if first:
                    o_ms('pool', pv[:], 0.0, [pvd])
                    o_dma(pv[1:128, :], src(r0, r0 + 127), nb, [pvd])
                else:
                    o_dma(pv[:], src(r0 - 1, r0 + 127), nb, [pvd])
                if last:
                    o_ms('pool', nx[:], 0.0, [nxd])
                    o_dma(nx[0:127, :], src(r0 + 1, r0 + 128), nb, [nxd])
                else:
                    o_dma(nx[:], src(r0 + 1, r0 + 129), nb, [nxd])
                o_tt('pool', pv[:], pv[:], nx[:], ADD, [pvd, nxd], [pvd])
                o_stt('dve', pv[:], pv[:], 0.5, x[:], MUL, SUB, [pvd, xd], [pvd])
                o_tt('pool', pv[:], pv[:], mub[:], MUL, [pvd, mubd], [pvd])
                o_tt('dve', xs[:], x[:], pv[:], ADD, [xd, pvd], [xsd])
                r_ = xs[:, 0:256]
                k_ = xs[:, 256:512]
                v_ = xs[:, 512:768]
                (kkn, kknd) = kkr.next()
                (sq, sqd) = sqr.next()
                (ss, ssd) = ssr.next()
                o_tt('dve', kkn[:], k_, kkb[:], MUL, [xsd, kkbd], [kknd])
                o_tt('pool', sq[:], kkn[:], kkn[:], MUL, [kknd], [sqd])
                o_red('dve', ss[:], v3(sq[:]), ADD, [sqd], [ssd])
                o_act(ss[:], ss[:], AF.Sqrt, [ssd], [ssd], bias=1e-12, scale=1.0)
                o_rcp(ss[:], ss[:], [ssd], [ssd])
                o_tt('dve', v3(kkn[:]), v3(kkn[:]), ss[:].unsqueeze(2).to_broadcast([128, 4, 64]), MUL, [kknd, ssd], [kknd])
                o_ts('dve', nkk[:, :, d, :], v3(kkn[:]), -1.0, MUL, [kknd], [nkkd])
                o_cp('pool', rr[:, :, d, :], v3(r_), [xsd], [rrd])
                (tw, twd) = smr.next()
                (twT, twTd) = sTr.next()
                (pt, ptd) = ptr_.next()
                (pb, pbd) = pbig.next()
                (u, ud) = ur.next()
                o_act(tw[:, 0:64], xs[:, 768:832], AF.Tanh, [xsd], [twd])
                o_tr(pt[0:64, :], tw[:, 0:64], ident32[:], [twd, ident32d], [ptd])
                o_cp('act', twT[0:64, :], pt[0:64, :], [ptd], [twTd])
                o_mm(pb[:], twT[0:64, :], wup[d][0][:], [twTd, wup[d][1]], [pbd])
                o_tt('dve', u[:], pb[:], w0b[d][0][:], ADD, [pbd, w0b[d][1]], [ud])
                o_act(u[:], u[:], AF.Sigmoid, [ud], [ud])
                o_act(dec[:, :, d, :], v3(u[:]), AF.Exp, [ud], [decd], scale=-0.6065306597126334)
                (xa, xad) = smr.next()
                (xaT, xaTd) = sTr.next()
                (pt, ptd) = ptr_.next()
                (pb, pbd) = pbig.next()
                (a, ad) = ar_.next()
                o_cp('pool', xa[:, 0:64], xs[:, 832:896], [xsd], [xad])
                o_tr(pt[0:64, :], xa[:, 0:64], ident32[:], [xad, ident32d], [ptd])
                o_cp('act', xaT[0:64, :], pt[0:64, :], [ptd], [xaTd])
                o_mm(pb[:], xaT[0:64, :], aup[d][0][:], [xaTd, aup[d][1]], [pbd])
                o_tt('dve', a[:], pb[:], a0b[d][0][:], ADD, [pbd, a0b[d][1]], [ad])
                o_act(a[:], a[:], AF.Sigmoid, [ad], [ad])
                (m, md) = mr.next()
                (kd, kdd) = kdr.next()
                (kka, kkad) = kkar.next()
                (bo, bod) = bor.next()
                o_tt('pool', m[:], a[:], kab[:], MUL, [ad, kabd], [md])
                o_tt('pool', m[:], m[:], omka[:], ADD, [md, omkad], [md])
                o_tt('dve', kd[:], k_, m[:], MUL, [xsd, md], [kdd])
                o_tt('pool', kka[:], kkn[:], a[:], MUL, [kknd, ad], [kkad])
                (ss2, ss2d) = ssr.next()
                o_tt('dve', m[:], r_, kd[:], MUL, [xsd, kdd], [md])
                o_tt('pool', m[:], m[:], rkb[:], MUL, [md, rkbd], [md])
                o_red('dve', ss2[:], v3(m[:]), ADD, [md], [ss2d])
                o_tt('dve', v3(bo[:]), v3(v_), ss2[:].unsqueeze(2).to_broadcast([128, 4, 64]), MUL, [xsd, ss2d], [bod])
                o_dma(S['V2'][d, r0:r0 + 128, :], v_, [xsd], [S['V2_d'][c]])
                o_dma(S['KKA'][d, r0:r0 + 128, :], kka[:], [kkad], [S['KKA_d'][c]])
                o_dma(S['KD'][d, r0:r0 + 128, :], kd[:], [kdd], [S['KD_d'][c]])
                o_dma(S['BON'][d, r0:r0 + 128, :], bo[:], [bod], [S['BON_d'][c]])
                if d == 0:
                    (sg, sgd) = smr.next()
                    (sgT, sgTd) = sTr.next()
                    (pt, ptd) = ptr_.next()
                    (pb, pbd) = pbig.next()
                    (gt, gtd) = gtr.next()
                    o_act(sg[:], xs[:, 896:1024], AF.Sigmoid, [xsd], [sgd])
                    o_tr(pt[:], sg[:], ident32[:], [sgd, ident32d], [ptd])
                    o_cp('act', sgT[:], pt[:], [ptd], [sgTd])
                    o_mm(pb[:], sgT[:], gup[:], [sgTd, gupd], [pbd])
                    o_cp('dve', gt[:], pb[:], [pbd], [gtd])
                    o_dma(S['GATE'][r0:r0 + 128, :], gt[:], [gtd], [S['GATE_d'][c]])
            (fak, fakd) = fakr.next()
            (far, fard) = farr.next()
            (fw, fwd) = fwr.next()
            for h in range(4):
                for (srcT, srcd, dstF, dstd) in ((nkk, nkkd, fak, fakd), (rr, rrd, far, fard)):
                    (pt, ptd) = ptr_.next()
                    o_tr(pt[:], srcT[:, h, :, :].rearrange("p d k -> p (d k)"), ident32[:], [srcd, ident32d], [ptd])
                    eng_ = 'act' if h % 2 == 0 else 'dve'
                    o_cp(eng_, dstF[0:64, :, h, 0], pt[0:64, :], [ptd], [dstd])
                    o_cp(eng_, dstF[64:128, :, h, 1], pt[64:128, :], [ptd], [dstd])
                (pt, ptd) = ptr_.next()
                o_tr(pt[:], dec[:, h, :, :].rearrange("p d k -> p (d k)"), ident32[:], [decd, ident32d], [ptd])
                o_cp('act', fw[:, :, h], pt[:], [ptd], [fwd])
            o_dma(S['AKK'][:, r0:r0 + 128, :], fak[:].rearrange("p s h d -> p s (h d)"), [fakd], [S['AKK_d'][c]])
            o_dma(S['AR'][:, r0:r0 + 128, :], far[:].rearrange("p s h d -> p s (h d)"), [fard], [S['AR_d'][c]])
            o_dma(S['WD'][:, r0:r0 + 128, :], fw[:], [fwd], [S['WD_d'][c]])
        P.barrier()


def rwkv_scan(S, T=None):
    P = K.P
    T = NT if T is None else T
    CH = RCH
    nch = T // CH
    with ExitStack() as st:
        ST, _ = sb(st, 'sc_ST', [128, 256], F32)
        STd = [Dep('sc_ST%d' % h) for h in range(4)]
        for ch in range(4):
            o_ms('pool', ST[:, ch * 64:(ch + 1) * 64], 0.0, [STd[ch]])
        NB = 3
        akk = [sb(st, 'sc_akk%d' % i, [128, CH, 8], F32) for i in range(NB)]
        ar = [sb(st, 'sc_ar%d' % i, [128, CH, 8], F32) for i in range(NB)]
        am = [sb(st, 'sc_am%d' % i, [128, CH, 4, 4], F32) for i in range(NB)]
        wt = [sb(st, 'sc_wt%d' % i, [128, CH, 4], F32) for i in range(NB)]
        bt = [sb(st, 'sc_bt%d' % i, [6, CH, 4, 128], F32) for i in range(NB)]
        rt = [sb(st, 'sc_rt%d' % i, [6, CH, 256], F32) for i in range(NB)]
        rtv = [Dep('sc_rtv%d' % i) for i in range(NB)]
        rts = [[Dep('sc_rts%d_%d' % (i, ch)) for ch in range(4)] for i in range(NB)]
        amf, amfd = sb(st, 'sc_amf', [128, 4, 4], F32)
        yfin, yfind = sb(st, 'sc_yfin', [4, 256], F32)
        for (b_, bd_) in bt:
            o_ms('pool', b_[:], 0.0, [bd_])
        sar = [Ring(K, st, 'sc_sa%d' % ch, [128, 64], F32, 1, psum=True) for ch in range(4)]
        ur = [Ring(K, st, 'sc_u%d' % ch, [128, 64], F32, 1, psum=True) for ch in range(4)]

        def load_chunk(c):
            i = c % NB
            s0 = c * CH
            tl = [s0 // 128]
            o_dma(akk[i][0][:], S['AKK'][:, s0:s0 + CH, :], [S['AKK_d'][t] for t in tl], [akk[i][1]])
            o_dma(ar[i][0][:], S['AR'][:, s0:s0 + CH, :], [S['AR_d'][t] for t in tl], [ar[i][1]])
            o_dma(wt[i][0][:], S['WD'][:, s0:s0 + CH, :], [S['WD_d'][t] for t in tl], [wt[i][1]])
            for which, nm in ((0, 'KKA'), (1, 'KD')):
                for d in range(2):
                    row = d if which == 0 else 4 + d
                    o_dma(bt[i][0][row:row + 1, :, :, d * 64:(d + 1) * 64],
                          S[nm][d:d + 1, s0:s0 + CH, :].rearrange("o s (h k) -> o s h k", k=64),
                          [S[nm + '_d'][t] for t in tl], [bt[i][1]], q='pool')
            o_dma(rt[i][0][4:6, :, :], S['V2'][:, s0:s0 + CH, :], [S['V2_d'][t] for t in tl], [rtv[i]])
            a4 = akk[i][0][:].rearrange("p s (h d) -> p s h d", d=2)
            r4 = ar[i][0][:].rearrange("p s (h d) -> p s h d", d=2)
            o_cp('pool', am[i][0][:, :, :, 0:2], a4, [akk[i][1]], [am[i][1]])
            o_cp('pool', am[i][0][:, 1:CH, :, 2:4], r4[:, 0:CH - 1, :, :], [ar[i][1]], [am[i][1]])
            if c == 0:
                o_ms('pool', am[i][0][:, 0, :, 2:4], 0.0, [am[i][1]])
            else:
                ip = (c - 1) % NB
                rp = ar[ip][0][:].rearrange("p s (h d) -> p s h d", d=2)
                o_cp('pool', am[i][0][:, 0, :, 2:4], rp[:, CH - 1, :, :], [ar[ip][1]], [am[i][1]])

        load_chunk(0)
        if nch > 1:
            load_chunk(1)
        for s in range(T):
            c, j = divmod(s, CH)
            i = c % NB
            if j == 0 and c + 2 < nch:
                load_chunk(c + 2)
            SAs = [sar[ch].next() for ch in range(4)]
            Us = [ur[ch].next() for ch in range(4)]
            for h in range(4):
                (SA, SAd) = SAs[h]
                o_mm(SA[0:4, :], am[i][0][:, j, h, :], ST[:, h * 64:(h + 1) * 64], [am[i][1], STd[h]], [SAd])
            for h in range(4):
                (SA, SAd) = SAs[h]
                (U, Ud) = Us[h]
                o_cp('act', rt[i][0][0:4, j, h * 64:(h + 1) * 64], SA[0:4, :], [SAd], [rts[i][h]])
                o_mm(U[:], bt[i][0][0:6, j, h, :], rt[i][0][0:6, j, h * 64:(h + 1) * 64], [bt[i][1], rts[i][h], rtv[i]], [Ud])
            for h in range(4):
                (U, Ud) = Us[h]
                STh = ST[:, h * 64:(h + 1) * 64]
                o_stt('dve', STh, STh, wt[i][0][:, j, h:h + 1], U[:], ALU.mult, ALU.add, [STd[h], wt[i][1], Ud], [STd[h]])
            if j == CH - 1:
                s0 = c * CH
                if c == 0:
                    o_dma(S['Y2'][:, 0:CH - 1, :], rt[i][0][2:4, 1:CH, :], rts[i], [S['Y2_d'][0]])
                else:
                    o_dma(S['Y2'][:, s0 - 1:s0 + CH - 1, :], rt[i][0][2:4, :, :], rts[i], [S['Y2_d'][(s0 - 1) // 128]])
        il = (nch - 1) % NB
        rl = ar[il][0][:].rearrange("p s (h d) -> p s h d", d=2)
        o_ms('pool', amf[:], 0.0, [amfd])
        o_cp('pool', amf[:, :, 2:4], rl[:, CH - 1, :, :], [ar[il][1], amfd], [amfd])
        for h in range(4):
            (SA, SAd) = sar[h].next()
            o_mm(SA[0:4, :], amf[:, h, :], ST[:, h * 64:(h + 1) * 64], [amfd, STd[h]], [SAd])
            o_cp('act', yfin[0:4, h * 64:(h + 1) * 64], SA[0:4, :], [SAd], [yfind])
        o_dma(S['Y2'][:, T - 1, :], yfin[2:4, :], [yfind], [S['Y2_d'][(T - 1) // 128]])
        P.barrier()


def rwkv_readout(l, I, S, need_ctx, J32, J32d):
    P = K.P
    MUL, ADD = ALU.mult, ALU.add
    with ExitStack() as st:
        lng, lngd = bc_load(st, 'ro_lng', I['rwkv_ln_g'][l, :], 256)
        lnb, lnbd = bc_load(st, 'ro_lnb', I['rwkv_ln_b'][l, :], 256)
        yr_ = Ring(K, st, 'ro_y', [128, 256], F32, 3)
        br_ = Ring(K, st, 'ro_b', [128, 256], F32, 3)
        gr_ = Ring(K, st, 'ro_g', [128, 256], F32, 2)
        ycr = Ring(K, st, 'ro_yc', [128, 256], F32, 3)
        sqr = Ring(K, st, 'ro_sq', [128, 256], F32, 2)
        smr = Ring(K, st, 'ro_sm', [128, 4], F32, 6)
        otr = Ring(K, st, 'ro_o', [128, 256], F32, 2)
        psr = Ring(K, st, 'ro_ps', [128, 256], F32, 2, psum=True)

        def v3(ap):
            return ap.rearrange("p (h k) -> p h k", k=64)

        def bc4(ap):
            return ap.unsqueeze(2).to_broadcast([128, 4, 64])

        for t in range(0 if need_ctx else 2, NTILE):
            outs = []
            for d in range(2):
                c = t if d == 0 else rev_tile(t)
                r0 = c * 128
                (y, yd) = yr_.next()
                (b, bd) = br_.next()
                (yc, ycd) = ycr.next()
                (sq, sqd) = sqr.next()
                (sm, smd) = smr.next()
                (vr, vrd) = smr.next()
                o_dma(y[:], S['Y2'][d, r0:r0 + 128, :], [S['Y2_d'][c]], [yd])
                o_dma(b[:], S['BON'][d, r0:r0 + 128, :], [S['BON_d'][c]], [bd])
                o_red('dve', sm[:], v3(y[:]), ADD, [yd], [smd])
                o_ts('dve', sm[:], sm[:], -1.0 / 64, MUL, [smd], [smd])
                o_tt('dve', v3(yc[:]), v3(y[:]), bc4(sm[:]), ADD, [yd, smd], [ycd])
                o_tt('pool', sq[:], yc[:], yc[:], MUL, [ycd], [sqd])
                o_red('dve', vr[:], v3(sq[:]), ADD, [sqd], [vrd])
                o_act(vr[:], vr[:], AF.Sqrt, [vrd], [vrd], bias=64e-5, scale=1.0 / 64)
                o_rcp(vr[:], vr[:], [vrd], [vrd])
                o_tt('dve', v3(yc[:]), v3(yc[:]), bc4(vr[:]), MUL, [ycd, vrd], [ycd])
                o_tt('pool', yc[:], yc[:], lng[:], MUL, [ycd, lngd], [ycd])
                o_tt('pool', yc[:], yc[:], lnb[:], ADD, [ycd, lnbd], [ycd])
                o_tt('dve', yc[:], yc[:], b[:], ADD, [ycd, bd], [ycd])
                outs.append((yc, ycd))
            (ps_, psd) = psr.next()
            (g, gd) = gr_.next()
            (ot, otd) = otr.next()
            o_dma(g[:], S['GATE'][t * 128:(t + 1) * 128, :], [S['GATE_d'][t]], [gd])
            o_mm(ps_[:], J32[:], outs[1][0][:], [J32d, outs[1][1]], [psd])
            o_tt('dve', ot[:], outs[0][0][:], ps_[:], ADD, [outs[0][1], psd], [otd])
            o_tt('dve', ot[:], ot[:], g[:], MUL, [otd, gd], [otd])
            o_dma(S['ocat'][t * 128:(t + 1) * 128, 0:256], ot[:], [otd], [S['ocat_d'][t]])
        P.barrier()


def phase_out(l, I, S, need_ctx, modL, modLd, modC, modCd, ident, identd):
    P = K.P
    MUL, ADD = ALU.mult, ALU.add
    with ExitStack() as st:
        wbf, wbfd = sb(st, 'wo_bf', [128, 8, D], BF16)
        wst = Ring(K, st, 'wo_st', [128, D], F32, 2)
        ocr = Ring(K, st, 'wo_oc', [128, D], F32, 2)
        ocbr = Ring(K, st, 'wo_ocb', [128, D], BF16, 2)
        oTr = Ring(K, st, 'wo_oT', [128, 8, 128], BF16, 2)
        xr = Ring(K, st, 'wo_x', [128, D], F32, 2)
        yr = Ring(K, st, 'wo_y', [128, D], F32, 2)
        sq, sqd = sb(st, 'wo_sq', [128, D], F32)
        ssr = Ring(K, st, 'wo_ss', [128, 1], F32, 4)
        pTr = Ring(K, st, 'wo_pT', [128, 8, 128], BF16, 2, psum=True)
        pyr = Ring(K, st, 'wo_py', [128, 512], F32, 4, psum=True)
        wo = I['w_out'][l].rearrange("(k p) n -> p k n", p=128)
        for kc in range(8):
            (ws, wsd) = wst.next()
            o_dma(ws[:], wo[:, kc, :], [], [wsd])
            o_cp('pool', wbf[:, kc, :], ws[:], [wsd], [wbfd])
        for t in range(0 if need_ctx else 2, NTILE):
            mt, mtd = (modC, modCd) if t < 2 else (modL, modLd)
            (oc, ocd) = ocr.next()
            (ocb, ocbd) = ocbr.next()
            (oT, oTd) = oTr.next()
            (x, xd) = xr.next()
            (y, yd) = yr.next()
            (ss, ssd) = ssr.next()
            (pT, pTd) = pTr.next()
            o_dma(oc[:], S['ocat'][t * 128:(t + 1) * 128, :], [S['ocat_d'][t]], [ocd])
            o_dma(x[:], S['xs'][t * 128:(t + 1) * 128, :], [S['xs_d'][t]], [xd])
            o_cp('pool', ocb[:], oc[:], [ocd], [ocbd])
            for kc in range(8):
                o_tr(pT[:, kc, :], ocb[:, kc * 128:(kc + 1) * 128], ident[:], [ocbd, identd], [pTd])
            o_cp('act', oT[:], pT[:], [pTd], [oTd])
            for hf in range(2):
                (py, pyd) = pyr.next()
                for kc in range(8):
                    o_mm(py[:], oT[:, kc, :], wbf[:, kc, hf * 512:(hf + 1) * 512], [oTd, wbfd], [pyd], start=(kc == 0), stop=(kc == 7))
                o_cp('act' if hf else 'dve', y[:, hf * 512:(hf + 1) * 512], py[:], [pyd], [yd])
            rms_rstd(y[:], yd, D, sq, sqd, ss, ssd, 0)
            o_stt('dve', y[:], y[:], ss[:, 0:1], mt[:, 2, :], MUL, MUL, [yd, ssd, mtd], [yd])
            o_tt('pool', x[:], x[:], y[:], ADD, [xd, yd], [xd])
            o_dma(S['xs'][t * 128:(t + 1) * 128, :], x[:], [xd], [S['xs_d'][t]])
        P.barrier()


def ffn_convert(I, S, moe, li):
    P = K.P
    NEx = NE if moe else 1
    tag = 'm' if moe else 'f'
    with ExitStack() as st:
        ldr = Ring(K, st, 'cv_ld', [128, DFF], F32, 3)
        cvr = Ring(K, st, 'cv_bf', [128, DFF], BF16, 3)
        n = 0
        engs = ('dve', 'pool', 'act')
        for e in range(NEx):
            for (nm, dst) in (('gate', 'wgb' + tag), ('up', 'wub' + tag)):
                src = (I['moe_w_' + nm][li, e] if moe else I['ffn_w_' + nm][li]).rearrange("(k p) n -> p k n", p=128)
                for kc in range(8):
                    (ld, ldd) = ldr.next()
                    (cv, cvd) = cvr.next()
                    o_dma(ld[:], src[:, kc, :], [], [ldd])
                    o_cp(engs[n % 3], cv[:], ld[:], [ldd], [cvd])
                    n += 1
                    for q4 in range(2):
                        f0, f1 = q4 * 11, (q4 + 1) * 11
                        o_dma(S[dst][e, f0:f1, :, kc, :].rearrange("f p n -> p f n"),
                              cv[:, f0 * 128:f1 * 128].rearrange("p (f n) -> p f n", n=128), [cvd], [Dep()])
            srcd = (I['moe_w_down'][li, e] if moe else I['ffn_w_down'][li]).rearrange("(f p) n -> p f n", p=128)
            for f0 in range(0, NFF, 2):
                (ld, ldd) = ldr.next()
                (cv, cvd) = cvr.next()
                o_dma(ld[:, 0:2048].rearrange("p (f n) -> p f n", n=1024), srcd[:, f0:f0 + 2, :], [], [ldd])
                o_cp(engs[n % 3], cv[:, 0:2048], ld[:, 0:2048], [ldd], [cvd])
                n += 1
                o_dma(S['wdb' + tag][e, f0:f0 + 2, :, :].rearrange("f p n -> p f n"),
                      cv[:, 0:2048].rearrange("p (f n) -> p f n", n=1024), [cvd], [Dep()])
        P.barrier()


def phase_ffn(l, I, S, need_ctx, modL, modLd, modC, modCd, ident32, ident32d, out_ap, out_d):
    P = K.P
    MUL, ADD, SUB = ALU.mult, ALU.add, ALU.subtract
    moe = (l % 2 == 1)
    li = l // 2
    NEx = NE if moe else 1
    tag = 'm' if moe else 'f'
    last_layer = not need_ctx
    ffn_convert(I, S, moe, li)
    tiles = list(range(0 if need_ctx else 2, NTILE))
    with ExitStack() as st:
        xrr = Ring(K, st, 'ff_x', [128, 2, D], F32, 2)
        hfr = Ring(K, st, 'ff_hf', [128, D], F32, 2)
        hTr = Ring(K, st, 'ff_hT', [128, 8, 256], BF16, 2)
        h32r = Ring(K, st, 'ff_h32', [128, 8, 128], F32, 2)
        sq, sqd = sb(st, 'ff_sq', [128, D], F32)
        ssr = Ring(K, st, 'ff_ss', [128, 1], F32, 4)
        accr = Ring(K, st, 'ff_acc', [128, 2, D], F32, 2)
        gtr = Ring(K, st, 'ff_gate', [128, 2, 8], F32, 2)
        smr = Ring(K, st, 'ff_sm', [128, 8], F32, 8)
        s1r = Ring(K, st, 'ff_s1', [128, 1], F32, 12)
        wgr = Ring(K, st, 'ff_wg', [128, 8, 128], BF16, 3)
        wur = Ring(K, st, 'ff_wu', [128, 8, 128], BF16, 3)
        wdr = Ring(K, st, 'ff_wd', [128, D], BF16, 3)
        sgr = Ring(K, st, 'ff_sg', [128, 256], F32, 2)
        GTr = Ring(K, st, 'ff_GT', [128, 256], BF16, 3)
        pTr = Ring(K, st, 'ff_pT', [128, 4, 128], F32, 1, psum=True)
        gur = Ring(K, st, 'ff_gu', [128, 2, 256], F32, 2, psum=True)
        ops_ = [[ps(st, 'ff_o%d%d' % (a, b), [128, 512], F32) for b in range(2)] for a in range(2)]
        plr = Ring(K, st, 'ff_pl', [128, 8], F32, 1, psum=True)
        if moe:
            rt_, rtd = sb(st, 'ff_router', [128, 8, 8], F32)
            o_dma(rt_[:], I['moe_router'][li].rearrange("(k p) e -> p k e", p=128), [], [rtd])
        for blk in range(len(tiles) // 2):
            tl = tiles[2 * blk:2 * blk + 2]
            (xr, xrd) = xrr.next()
            (hT, hTd) = hTr.next()
            (acc, accd) = accr.next()
            (gate, gated) = gtr.next()
            for jj, t in enumerate(tl):
                mt, mtd = (modC, modCd) if t < 2 else (modL, modLd)
                (hf, hfd) = hfr.next()
                (h32, h32d) = h32r.next()
                (ss, ssd) = ssr.next()
                o_dma(xr[:, jj, :], S['xs'][t * 128:(t + 1) * 128, :], [S['xs_d'][t]], [xrd])
                rms_rstd(xr[:, jj, :], xrd, D, sq, sqd, ss, ssd, 0)
                o_stt('dve', hf[:], xr[:, jj, :], ss[:, 0:1], mt[:, 4, :], MUL, MUL, [xrd, ssd, mtd], [hfd])
                o_tt('pool', hf[:], hf[:], mt[:, 3, :], ADD, [hfd, mtd], [hfd])
                for q4 in range(2):
                    (pT, pTd) = pTr.next()
                    for kk_ in range(4):
                        kc = q4 * 4 + kk_
                        o_tr(pT[:, kk_, :], hf[:, kc * 128:(kc + 1) * 128], ident32[:], [hfd, ident32d], [pTd])
                    o_cp('act', h32[:, q4 * 4:(q4 + 1) * 4, :], pT[:], [pTd], [h32d])
                    o_cp('pool', hT[:, q4 * 4:(q4 + 1) * 4, jj * 128:(jj + 1) * 128], h32[:, q4 * 4:(q4 + 1) * 4, :], [h32d], [hTd])
                if moe:
                    (pl, pld) = plr.next()
                    for kc in range(8):
                        o_mm(pl[:], h32[:, kc, :], rt_[:, kc, :], [h32d, rtd], [pld], start=(kc == 0), stop=(kc == 7))
                    (lg, lgd) = smr.next()
                    (lg2, lg2d) = smr.next()
                    (mk1, mk1d) = smr.next()
                    (mk2, mk2d) = smr.next()
                    (m1, m1d) = s1r.next()
                    (m2, m2d) = s1r.next()
                    (ex, exd) = s1r.next()
                    (w1, w1d) = s1r.next()
                    (w2, w2d) = s1r.next()
                    o_cp('dve', lg[:], pl[:], [pld], [lgd])
                    o_red('dve', m1[:], lg[:], ALU.max, [lgd], [m1d])
                    o_ts('dve', mk1[:], lg[:], m1[:, 0:1], ALU.is_equal, [lgd, m1d], [mk1d])
                    o_stt('dve', lg2[:], mk1[:], -1.0e30, lg[:], MUL, ADD, [mk1d, lgd], [lg2d])
                    o_red('dve', m2[:], lg2[:], ALU.max, [lg2d], [m2d])
                    o_ts('dve', mk2[:], lg2[:], m2[:, 0:1], ALU.is_equal, [lg2d, m2d], [mk2d])
                    o_tt('dve', ex[:], m2[:], m1[:], SUB, [m2d, m1d], [exd])
                    o_act(ex[:], ex[:], AF.Exp, [exd], [exd])
                    o_ts('dve', w1[:], ex[:], 1.0, ADD, [exd], [w1d])
                    o_rcp(w1[:], w1[:], [w1d], [w1d])
                    o_tt('dve', w2[:], ex[:], w1[:], MUL, [exd, w1d], [w2d])
                    o_ts('dve', gate[:, jj, :], mk1[:], w1[:, 0:1], MUL, [mk1d, w1d], [gated])
                    o_stt('dve', gate[:, jj, :], mk2[:], w2[:, 0:1], gate[:, jj, :], MUL, ADD, [mk2d, w2d, gated], [gated])
            for e in range(NEx):
                for f in range(NFF):
                    (wg, wgd) = wgr.next()
                    (wu, wud) = wur.next()
                    (wd, wdd) = wdr.next()
                    (gu, gud) = gur.next()
                    (sg, sgd) = sgr.next()
                    (GT, GTd) = GTr.next()
                    o_dma(wg[:], S['wgb' + tag][e, f], [], [wgd])
                    o_dma(wu[:], S['wub' + tag][e, f], [], [wud], q='pool')
                    o_dma(wd[:], S['wdb' + tag][e, f], [], [wdd])
                    for kc in range(8):
                        o_mm(gu[:, 0, :], wg[:, kc, :], hT[:, kc, :], [wgd, hTd], [gud], start=(kc == 0), stop=(kc == 7))
                    for kc in range(8):
                        o_mm(gu[:, 1, :], wu[:, kc, :], hT[:, kc, :], [wud, hTd], [gud], start=(kc == 0), stop=(kc == 7))
                    o_act(sg[:], gu[:, 0, :], AF.Silu, [gud], [sgd])
                    o_tt('dve', GT[:], sg[:], gu[:, 1, :], MUL, [sgd, gud], [GTd])
                    for jj in range(2):
                        for hf_ in range(2):
                            (op_, opd) = ops_[jj][hf_]
                            o_mm(op_[:], GT[:, jj * 128:(jj + 1) * 128], wd[:, hf_ * 512:(hf_ + 1) * 512], [GTd, wdd], [opd],
                                 start=(f == 0), stop=(f == NFF - 1))
                for jj in range(2):
                    for hf_ in range(2):
                        (op_, opd) = ops_[jj][hf_]
                        dst = acc[:, jj, hf_ * 512:(hf_ + 1) * 512]
                        if not moe:
                            o_cp('act', dst, op_[:], [opd], [accd])
                        elif e == 0:
                            o_ts('dve', dst, op_[:], gate[:, jj, e:e + 1], MUL, [opd, gated], [accd])
                        else:
                            o_stt('dve', dst, op_[:], gate[:, jj, e:e + 1], dst, MUL, ADD, [opd, gated, accd], [accd])
            for jj, t in enumerate(tl):
                mt, mtd = (modC, modCd) if t < 2 else (modL, modLd)
                (ss, ssd) = ssr.next()
                rms_rstd(acc[:, jj, :], accd, D, sq, sqd, ss, ssd, 0)
                o_stt('dve', acc[:, jj, :], acc[:, jj, :], ss[:, 0:1], mt[:, 5, :], MUL, MUL, [accd, ssd, mtd], [accd])
                o_tt('pool', acc[:, jj, :], acc[:, jj, :], xr[:, jj, :], ADD, [accd, xrd], [accd])
                if last_layer:
                    o_dma(out_ap[(t - 2) * 128:(t - 1) * 128, :], acc[:, jj, :], [accd], [out_d])
                else:
                    o_dma(S['xs'][t * 128:(t + 1) * 128, :], acc[:, jj, :], [accd], [S['xs_d'][t]])
        P.barrier()


IN_NAMES = ['x', 'c', 'ctx', 'c_ctx', 'mod_w', 'mod_b', 'norm_g', 'w_in', 'w_out', 'rwkv_mu', 'rwkv_w0', 'rwkv_w_up',
            'rwkv_a0', 'rwkv_a_up', 'rwkv_g_up', 'rwkv_k_k', 'rwkv_k_a', 'rwkv_r_k', 'rwkv_ln_g', 'rwkv_ln_b',
            'gqa_q_g', 'gqa_k_g', 'mla_q_norm_g', 'mla_w_uq', 'mla_kv_norm_g', 'mla_w_ukv', 'nat_bias',
            'ffn_w_gate', 'ffn_w_up', 'ffn_w_down', 'moe_router', 'moe_w_gate', 'moe_w_up', 'moe_w_down']


def build(shapes, stop_after=None, debug=(), only=None, layers=(0, 1), scan_T=None):
    nc = bass.Bass("TRN2", target_bir_lowering=False)
    K.nc = nc
    P = Prog(nc)
    K.P = P
    I = {}
    for name in IN_NAMES:
        I[name] = nc.dram_tensor(name, list(shapes[name]), F32, kind="ExternalInput").ap()
    out = nc.dram_tensor("out", [NL, D], F32, kind="ExternalOutput").ap()
    out_d = Dep('out')
    S = {}

    def scratch(name, shape, dtype=F32, tiled=True):
        S[name] = dram(name, shape, dtype)
        S[name + '_d'] = [Dep('%s%d' % (name, t)) for t in range(NTILE)] if tiled else Dep(name)

    scratch('xs', [NT, D])
    scratch('p', [NT, INC])
    scratch('ocat', [NT, D])
    scratch('prw1', [NT, 1024])
    for nm in ('V2', 'KKA', 'KD', 'BON', 'Y2'):
        scratch(nm, [2, NT, 256])
    scratch('GATE', [NT, 256])
    scratch('AKK', [128, NT, 8])
    scratch('AR', [128, NT, 8])
    scratch('WD', [128, NT, 4])
    for tag, ne in (('f', 1), ('m', NE)):
        scratch('wgb' + tag, [ne, NFF, 128, 8, 128], BF16, tiled=False)
        scratch('wub' + tag, [ne, NFF, 128, 8, 128], BF16, tiled=False)
        scratch('wdb' + tag, [ne, NFF, 128, D], BF16, tiled=False)
    I['rope_g'] = nc.dram_tensor('rope_g', [NL, 2, 32], F32, kind='ExternalInput').ap()
    I['rope_m'] = nc.dram_tensor('rope_m', [NL, 2, 16], F32, kind='ExternalInput').ap()
    I['nat_tab'] = nc.dram_tensor('nat_tab', [2, 128, 4, len(nat_plan()[1]), 64], F32, kind='ExternalInput').ap()
    with ExitStack() as st:
        P.alloc_sems(st)
        ident, identd = sb(st, 'ident', [128, 128], BF16)
        modL, modLd = sb(st, 'modL', [128, 6, D], F32)
        modC, modCd = sb(st, 'modC', [128, 6, D], F32)
        ident32, ident32d = sb(st, 'ident32', [128, 128], F32)
        J32, J32d = sb(st, 'J32', [128, 128], F32)
        P.op('pool', lambda e: e.memset(ident[:], 1.0), [], [identd])
        P.op('pool', lambda e: e.memset(ident32[:], 1.0), [], [ident32d])
        P.op('pool', lambda e: e.memset(J32[:], 1.0), [], [J32d])
        P.op('pool', lambda e: e.affine_select(out=ident32[:], in_=ident32[:], pattern=[[-1, 128]], compare_op=ALU.is_equal, fill=0.0, base=0, channel_multiplier=1),
             [ident32d], [ident32d])
        P.op('pool', lambda e: e.affine_select(out=ident[:], in_=ident[:], pattern=[[-1, 128]], compare_op=ALU.is_equal, fill=0.0, base=0, channel_multiplier=1),
             [identd], [identd])
        P.op('pool', lambda e: e.affine_select(out=J32[:], in_=J32[:], pattern=[[1, 128]], compare_op=ALU.is_equal, fill=0.0, base=-127, channel_multiplier=1),
             [J32d], [J32d])
        P.dma('sp', lambda e: e.dma_start(out=S['xs'][0:NC_, :], in_=I['ctx'][:, :]), [], S['xs_d'][0:2])
        for j in range(4):
            P.dma('sp', lambda e, j=j: e.dma_start(out=S['xs'][NC_ + j * 1024:NC_ + (j + 1) * 1024, :], in_=I['x'][j * 1024:(j + 1) * 1024, :]),
                  [], S['xs_d'][2 + 8 * j:2 + 8 * (j + 1)])

        def want(name):
            return only is None or name in only

        for l in layers:
            need_ctx = (l == 0)
            if want('mod'):
                phase_mod(l, I, modL, modLd, modC, modCd)
            if want('in'):
                phase_in(l, I, S, modL, modLd, modC, modCd, ident, identd)
            if want('rwkv'):
                if scan_T is None:
                    phase_rwkv(l, I, S, need_ctx, ident32, ident32d, J32, J32d)
                else:
                    rwkv_prep(l, I, S, ident32, ident32d, J32, J32d)
                    rwkv_scan(S, scan_T)
            if want('gqa'):
                phase_gqa(l, I, S, need_ctx, ident, identd, ident32, ident32d)
            if want('mla'):
                phase_mla(l, I, S, need_ctx, ident, identd, ident32, ident32d)
            if want('nat'):
                phase_nat(l, I, S, need_ctx, ident, identd, ident32, ident32d)
            if stop_after == ('attn', l):
                break
            if want('out'):
                phase_out(l, I, S, need_ctx, modL, modLd, modC, modCd, ident, identd)
            if stop_after == ('out', l):
                break
            if want('ffn'):
                phase_ffn(l, I, S, need_ctx, modL, modLd, modC, modCd, ident32, ident32d, out, out_d)
        for name in debug:
            src = S[name]
            d = nc.dram_tensor('dbg_' + name, list(src.shape), src.dtype, kind="ExternalOutput").ap()
            dd = Dep('dbg_' + name)
            deps = S[name + '_d'] if isinstance(S[name + '_d'], list) else [S[name + '_d']]
            P.dma('sp', lambda e, d=d, src=src: e.dma_start(out=d, in_=src), deps, [dd])
        P.barrier()
        P.emit()
    return nc


def core_shapes(inputs):
    sh = {k: tuple(np.asarray(v).shape) for k, v in inputs.items()}
    sh['x'] = (NL, D)
    sh['c'] = (D,)
    sh['ctx'] = (NC_, D)
    return sh


def rope_tables(rot_dim):
    t = np.arange(NL)
    row = (t // 64).astype(np.float32)
    col = (t % 64).astype(np.float32)
    quarter = rot_dim // 4
    inv_freq = (np.float32(10000.0) ** (-np.arange(quarter, dtype=np.float32) / np.float32(quarter))).astype(np.float32)
    ang = np.concatenate([row[:, None] * inv_freq, col[:, None] * inv_freq], axis=-1).astype(np.float32)
    return np.ascontiguousarray(np.stack([np.cos(ang), np.sin(ang)], axis=1).astype(np.float32))


def core_inputs(inputs, b, shared=None):
    if shared is None:
        shared = {k: np.ascontiguousarray(np.asarray(v, dtype=np.float32)) for k, v in inputs.items() if k not in ('x', 'c', 'ctx')}
        shared['rope_g'] = rope_tables(64)
        shared['rope_m'] = rope_tables(32)
        nb = np.asarray(inputs['nat_bias'], np.float32)
        shared['nat_tab'] = np.ascontiguousarray(np.stack([nat_bias_table(nb[0]), nat_bias_table(nb[1])], 0))
    m = dict(shared)
    m['x'] = np.ascontiguousarray(np.asarray(inputs['x'][b], dtype=np.float32))
    m['c'] = np.ascontiguousarray(np.asarray(inputs['c'][b], dtype=np.float32))
    m['ctx'] = np.ascontiguousarray(np.asarray(inputs['ctx'][b], dtype=np.float32))
    return m


def kernel(**inputs):
    nb = 4
    shapes = core_shapes(inputs)
    nc = build(shapes)
    first = core_inputs(inputs, 0)
    shared = {k: v for k, v in first.items() if k not in ('x', 'c', 'ctx')}
    maps = [first] + [core_inputs(inputs, b, shared) for b in range(1, nb)]
    res = run_bass_kernel_spmd(nc, maps, core_ids=list(range(nb)))
    return np.stack([np.asarray(res.results[b]['out'], dtype=np.float32) for b in range(nb)], axis=0)
```

```python
import numpy as np
from contextlib import ExitStack
import concourse.bass as bass
import concourse.mybir as mybir
from concourse.bass_utils import run_bass_kernel_spmd

F32 = mybir.dt.float32
BF16 = mybir.dt.bfloat16
AF = mybir.ActivationFunctionType
ALU = mybir.AluOpType
AX = mybir.AxisListType

COMPUTE = ('pe', 'act', 'dve', 'pool')
QUEUES = ('pe', 'act', 'dve', 'pool', 'sp')
NRING = 8

D = 1024
NL = 4096
NC_ = 256
NT = NL + NC_
NTILE = NT // 128
INC = 2720
RW0, GQ0, ML0, NA0 = 0, 1024, 1536, 1952
DFF = 2816
NFF = DFF // 128
NE = 8
RMS_EPS = 1e-6


class Dep:
    __slots__ = ('w', 'r', 'name')

    def __init__(self, name=''):
        self.w = None
        self.r = {}
        self.name = name


class Prog:
    def __init__(self, nc):
        self.nc = nc
        self.q = {e: [] for e in QUEUES}
        self.cnt = {e: 0 for e in COMPUTE}
        self.seen = {e: {} for e in QUEUES}
        self.sems = {}
        self.dma_cnt = {}
        self.dma_next = {e: 0 for e in QUEUES}
        self.ninstr = 0

    def alloc_sems(self, stack):
        for e in COMPUTE:
            self.sems[e] = stack.enter_context(self.nc.semaphore('s_' + e))
        for qn in ('sp', 'pool', 'act'):
            for j in range(NRING):
                key = ('dma', qn, j)
                self.sems[key] = stack.enter_context(self.nc.semaphore('d_%s_%d' % (qn, j)))
                self.dma_cnt[key] = 0

    def _need(self, queue, key, count, waits):
        if self.seen[queue].get(key, 0) >= count:
            return
        if waits.get(key, 0) < count:
            waits[key] = count

    def _emit_waits(self, queue, waits):
        for key, count in waits.items():
            sem = self.sems[key]
            val = count * (16 if isinstance(key, tuple) else 1)
            self.q[queue].append(lambda e, sem=sem, val=val: e.wait_ge(sem, val))
            self.seen[queue][key] = count
            self.ninstr += 1

    def _collect(self, queue, reads, writes):
        waits = {}
        for t in reads:
            if t.w is not None:
                self._need(queue, t.w[0], t.w[1], waits)
        for t in writes:
            if t.w is not None:
                if not (queue == 'pe' and t.w[0] == 'pe'):
                    self._need(queue, t.w[0], t.w[1], waits)
            for k, c in t.r.items():
                if k == queue:
                    continue
                self._need(queue, k, c, waits)
        return waits

    def op(self, queue, fn, reads=(), writes=()):
        waits = self._collect(queue, reads, writes)
        self._emit_waits(queue, waits)
        self.cnt[queue] += 1
        c = self.cnt[queue]
        sem = self.sems[queue]
        self.q[queue].append(lambda e, fn=fn, sem=sem: fn(e).then_inc(sem, 1))
        self.ninstr += 1
        for t in reads:
            if t.r.get(queue, 0) < c:
                t.r[queue] = c
        for t in writes:
            t.w = (queue, c)
            t.r = {}

    def dma(self, queue, fn, reads=(), writes=()):
        j = self.dma_next[queue]
        self.dma_next[queue] = (j + 1) % NRING
        key = ('dma', queue, j)
        waits = self._collect(queue, reads, writes)
        n = self.dma_cnt[key]
        if n > 0:
            self._need(queue, key, n, waits)
        self._emit_waits(queue, waits)
        self.dma_cnt[key] = n + 1
        sem = self.sems[key]
        self.q[queue].append(lambda e, fn=fn, sem=sem: fn(e).then_inc(sem, 16))
        self.ninstr += 1
        for t in reads:
            t.r[key] = n + 1
        for t in writes:
            t.w = (key, n + 1)
            t.r = {}

    def barrier(self, queues=QUEUES):
        for qn in queues:
            waits = {}
            for e in COMPUTE:
                if self.cnt[e] > 0 and e != qn:
                    self._need(qn, e, self.cnt[e], waits)
            for key, n in self.dma_cnt.items():
                if n > 0:
                    self._need(qn, key, n, waits)
            self._emit_waits(qn, waits)

    def emit(self):
        nc = self.nc
        with nc.Block() as block:
            @block.tensor
            def _(e):
                for f in self.q['pe']:
                    f(e)

            @block.scalar
            def _(e):
                for f in self.q['act']:
                    f(e)

            @block.vector
            def _(e):
                for f in self.q['dve']:
                    f(e)

            @block.gpsimd
            def _(e):
                for f in self.q['pool']:
                    f(e)

            @block.sync
            def _(e):
                for f in self.q['sp']:
                    f(e)


class Ring:
    def __init__(self, K, st, name, shape, dtype, n, psum=False):
        self.bufs = []
        for i in range(n):
            if psum:
                full = [128, 512] if dtype == F32 else [128, 1024]
                n = 1
                for d_ in shape[1:]:
                    n *= d_
                assert n <= full[1]
                t = st.enter_context(K.nc.psum_tensor(uname('%s%d' % (name, i)), full, dtype))
                t = t[0:shape[0], 0:n]
                if len(shape) == 3:
                    t = t.rearrange("p (a b) -> p a b", b=shape[2])
            else:
                t = st.enter_context(K.nc.sbuf_tensor(uname('%s%d' % (name, i)), shape, dtype))
            self.bufs.append((t, Dep('%s%d' % (name, i))))
        self.i = 0

    def next(self):
        b = self.bufs[self.i]
        self.i = (self.i + 1) % len(self.bufs)
        return b


class K:
    LVL = 99
    UID = 0


def uname(name):
    K.UID += 1
    return '%s_u%d' % (name, K.UID)


def sb(st, name, shape, dtype):
    return st.enter_context(K.nc.sbuf_tensor(uname(name), shape, dtype)), Dep(name)


def ps(st, name, shape, dtype):
    return st.enter_context(K.nc.psum_tensor(uname(name), shape, dtype)), Dep(name)


def dram(name, shape, dtype):
    return K.nc.dram_tensor(name, shape, dtype, kind="Internal").ap()


def rms_rstd(x_ap, xdep, n, sq, sqd, ss, ssd, col):
    P = K.P
    P.op('dve', lambda e: e.tensor_tensor(out=sq[:, 0:n], in0=x_ap, in1=x_ap, op=ALU.mult), [xdep], [sqd])
    P.op('dve', lambda e: e.tensor_reduce(out=ss[:, col:col + 1], in_=sq[:, 0:n], axis=AX.X, op=ALU.add), [sqd], [ssd])
    P.op('act', lambda e: e.activation(out=ss[:, col:col + 1], in_=ss[:, col:col + 1], func=AF.Sqrt, bias=RMS_EPS, scale=1.0 / n), [ssd], [ssd])
    P.op('dve', lambda e: e.reciprocal(out=ss[:, col:col + 1], in_=ss[:, col:col + 1]), [ssd], [ssd])


def phase_mod(l, I, modL, modLd, modC, modCd):
    nc, P = K.nc, K.P
    with ExitStack() as st:
        cv, cvd = sb(st, 'cv', [128, 2, 8], F32)
        cs, csd = sb(st, 'cs', [128, 2, 8], F32)
        crep, crepd = sb(st, 'crep', [128, 2, 8, 128], BF16)
        gb, gbd = sb(st, 'gb', [128, 4, D], F32)
        wst = Ring(K, st, 'mw_st', [128, 8, 512], F32, 2)
        wbf = Ring(K, st, 'mw_bf', [128, 8, 512], BF16, 2)
        bb = Ring(K, st, 'mbb', [128, 512], F32, 2)
        pm = Ring(K, st, 'pmod', [128, 512], F32, 4, psum=True)
        P.dma('sp', lambda e: e.dma_start(out=cv[:, 0, :], in_=I['c'].rearrange("(k p) -> p k", p=128), allow_slow_non_contiguous=True), [], [cvd])
        P.dma('sp', lambda e: e.dma_start(out=cv[:, 1, :], in_=I['c_ctx'].rearrange("(k p) -> p k", p=128), allow_slow_non_contiguous=True), [], [cvd])
        P.dma('sp', lambda e: e.dma_start(out=gb[:], in_=I['norm_g'][l].partition_broadcast(128)), [], [gbd])
        P.op('act', lambda e: e.activation(out=cs[:], in_=cv[:], func=AF.Silu), [cvd], [csd])
        P.op('dve', lambda e: e.tensor_copy(out=crep[:], in_=cs[:].unsqueeze(3).to_broadcast([128, 2, 8, 128])), [csd], [crepd])
        mw = I['mod_w'][l].rearrange("(k p) n -> p k n", p=128)
        for nb in range(12):
            (ws, wsd) = wst.next()
            (wb, wbd) = wbf.next()
            (bt, btd) = bb.next()
            P.dma('sp', lambda e, ws=ws, nb=nb: e.dma_start(out=ws[:], in_=mw[:, :, nb * 512:(nb + 1) * 512]), [], [wsd])
            P.dma('sp', lambda e, bt=bt, nb=nb: e.dma_start(out=bt[:], in_=I['mod_b'][l, nb * 512:(nb + 1) * 512].partition_broadcast(128)), [], [btd])
            P.op('pool', lambda e, ws=ws, wb=wb: e.tensor_copy(out=wb[:], in_=ws[:]), [wsd], [wbd])
            j, off = nb // 2, (nb % 2) * 512
            for s, (mt, mtd) in enumerate(((modL, modLd), (modC, modCd))):
                (pt, ptd) = pm.next()
                for kc in range(8):
                    P.op('pe', lambda e, pt=pt, wb=wb, kc=kc, s=s: e.matmul(pt[:], lhsT=crep[:, s, kc, :], rhs=wb[:, kc, :], start=(kc == 0), stop=(kc == 7)),
                         [crepd, wbd], [ptd])
                P.op('dve', lambda e, pt=pt, bt=bt, mt=mt, j=j, off=off: e.tensor_tensor(out=mt[:, j, off:off + 512], in0=pt[:], in1=bt[:], op=ALU.add),
                     [ptd, btd], [mtd])
        for (mt, mtd) in ((modL, modLd), (modC, modCd)):
            for j, gi, plus1 in ((1, 0, True), (2, 1, False), (4, 2, True), (5, 3, False)):
                if plus1:
                    P.op('dve', lambda e, mt=mt, j=j, gi=gi: e.scalar_tensor_tensor(out=mt[:, j, :], in0=mt[:, j, :], scalar=1.0, in1=gb[:, gi, :], op0=ALU.add, op1=ALU.mult),
                         [mtd, gbd], [mtd])
                else:
                    P.op('dve', lambda e, mt=mt, j=j, gi=gi: e.tensor_tensor(out=mt[:, j, :], in0=mt[:, j, :], in1=gb[:, gi, :], op=ALU.mult),
                         [mtd, gbd], [mtd])
        P.barrier()


def phase_in(l, I, S, modL, modLd, modC, modCd, ident, identd):
    nc, P = K.nc, K.P
    with ExitStack() as st:
        wbf, wbfd = sb(st, 'win_bf', [128, 8, INC], BF16)
        wst = Ring(K, st, 'win_st', [128, INC], F32, 2)
        xr = Ring(K, st, 'in_x', [128, D], F32, 3)
        hr = Ring(K, st, 'in_h', [128, D], F32, 2)
        hbr = Ring(K, st, 'in_hb', [128, D], BF16, 2)
        hTr = Ring(K, st, 'in_hT', [128, 8, 128], BF16, 2)
        sq, sqd = sb(st, 'in_sq', [128, D], F32)
        ssr = Ring(K, st, 'in_ss', [128, 1], F32, 4)
        pr = Ring(K, st, 'in_p', [128, INC], F32, 2)
        ptr = Ring(K, st, 'in_pT', [128, 8, 128], BF16, 2, psum=True)
        pmr = Ring(K, st, 'in_pm', [128, 512], F32, 4, psum=True)
        win = I['w_in'][l].rearrange("(k p) n -> p k n", p=128)
        for kc in range(8):
            (ws, wsd) = wst.next()
            P.dma('sp', lambda e, ws=ws, kc=kc: e.dma_start(out=ws[:], in_=win[:, kc, :]), [], [wsd])
            P.op('pool', lambda e, ws=ws, kc=kc: e.tensor_copy(out=wbf[:, kc, :], in_=ws[:]), [wsd], [wbfd])
        for t in range(NTILE):
            mt, mtd = (modC, modCd) if t < 2 else (modL, modLd)
            (x, xd) = xr.next()
            (h, hd) = hr.next()
            (hb, hbd) = hbr.next()
            (hT, hTd) = hTr.next()
            (ss, ssd) = ssr.next()
            (pt, ptd) = pr.next()
            (pT, pTd) = ptr.next()
            P.dma('sp', lambda e, x=x, t=t: e.dma_start(out=x[:], in_=S['xs'][t * 128:(t + 1) * 128, :]), [S['xs_d'][t]], [xd])
            rms_rstd(x[:], xd, D, sq, sqd, ss, ssd, 0)
            P.op('dve', lambda e, h=h, x=x, ss=ss, mt=mt: e.scalar_tensor_tensor(out=h[:], in0=x[:], scalar=ss[:, 0:1], in1=mt[:, 1, :], op0=ALU.mult, op1=ALU.mult),
                 [xd, ssd, mtd], [hd])
            P.op('pool', lambda e, h=h, hb=hb, mt=mt: e.tensor_tensor(out=hb[:], in0=h[:], in1=mt[:, 0, :], op=ALU.add), [hd, mtd], [hbd])
            for kc in range(8):
                P.op('pe', lambda e, pT=pT, hb=hb, kc=kc: e.transpose(out=pT[:, kc, :], in_=hb[:, kc * 128:(kc + 1) * 128], identity=ident[:]),
                     [hbd, identd], [pTd])
            P.op('act', lambda e, hT=hT, pT=pT: e.copy(out=hT[:], in_=pT[:]), [pTd], [hTd])
            for nb in range(6):
                c0 = nb * 512
                cw = min(512, INC - c0)
                (pm, pmd) = pmr.next()
                for kc in range(8):
                    P.op('pe', lambda e, pm=pm, hT=hT, kc=kc, c0=c0, cw=cw: e.matmul(pm[:, 0:cw], lhsT=hT[:, kc, :], rhs=wbf[:, kc, c0:c0 + cw], start=(kc == 0), stop=(kc == 7)),
                         [hTd, wbfd], [pmd])
                if nb % 2 == 0:
                    P.op('act', lambda e, pm=pm, pt=pt, c0=c0, cw=cw: e.copy(out=pt[:, c0:c0 + cw], in_=pm[:, 0:cw]), [pmd], [ptd])
                else:
                    P.op('dve', lambda e, pm=pm, pt=pt, c0=c0, cw=cw: e.tensor_copy(out=pt[:, c0:c0 + cw], in_=pm[:, 0:cw]), [pmd], [ptd])
            P.dma('sp', lambda e, pt=pt, t=t: e.dma_start(out=S['p'][t * 128:(t + 1) * 128, :], in_=pt[:]), [ptd], [S['p_d'][t]])
        P.barrier()


def attn_finalize(st_rings, O, Od, h, ot, otd, nq, ident32, ident32d):
    P = K.P
    osbr, ptr, rvr = st_rings
    (osb, osbd) = osbr.next()
    P.op('dve', lambda e: e.tensor_copy(out=osb[:, 0:nq], in_=O[:, 0:nq]), [Od], [osbd])
    for j in range(nq // 128):
        (pt, ptd) = ptr.next()
        (rv, rvd) = rvr.next()
        P.op('pe', lambda e, pt=pt, osb=osb, j=j: e.transpose(out=pt[:], in_=osb[:, j * 128:(j + 1) * 128], identity=ident32[0:65, 0:65]),
             [osbd, ident32d], [ptd])
        P.op('dve', lambda e, pt=pt, rv=rv: e.reciprocal(out=rv[:], in_=pt[:, 64:65]), [ptd], [rvd])
        P.op('dve', lambda e, pt=pt, rv=rv, j=j: e.tensor_scalar(out=ot[:, j, h * 64:(h + 1) * 64], in0=pt[:, 0:64], scalar1=rv[:, 0:1], scalar2=None, op0=ALU.mult),
             [ptd, rvd], [otd])


def attn_core(st, S, QT, QTd, KT, KTd, kvmap, Vaug, Vaugd, scale, col0, need_ctx, ident32, ident32d, tag):
    P = K.P
    sr = Ring(K, st, tag + '_S', [128, 512], F32, 3, psum=True)
    orr = Ring(K, st, tag + '_O', [65, 512], F32, 2, psum=True)
    ptr = Ring(K, st, tag + '_fT', [128, 65], F32, 2, psum=True)
    pr = Ring(K, st, tag + '_P', [128, 512], BF16, 3)
    osbr = Ring(K, st, tag + '_osb', [65, 512], F32, 2)
    rvr = Ring(K, st, tag + '_rv', [128, 1], F32, 4)
    otr = Ring(K, st, tag + '_ot', [128, 4, 256], F32, 2)
    blocks = []
    if need_ctx:
        blocks.append((0, 256, [0, 1]))
    for qb in range(8):
        blocks.append((256 + qb * 512, 512, list(range(NTILE))))
    for (q0, nq, kts) in blocks:
        (ot, otd) = otr.next()
        for h in range(4):
            g = kvmap[h]
            (O, Od) = orr.next()
            pend = None
            for i, kt in enumerate(kts):
                (Sp, Spd) = sr.next()
                (Pt, Ptd) = pr.next()
                P.op('pe', lambda e, Sp=Sp, g=g, h=h, kt=kt, q0=q0, nq=nq: e.matmul(Sp[:, 0:nq], lhsT=KT(g)[:, kt * 128:(kt + 1) * 128], rhs=QT(h)[:, q0:q0 + nq], start=True, stop=True),
                     [QTd, KTd], [Spd])
                P.op('act', lambda e, Sp=Sp, Pt=Pt, nq=nq: e.activation(out=Pt[:, 0:nq], in_=Sp[:, 0:nq], func=AF.Exp, scale=scale), [Spd], [Ptd])
                if pend is not None:
                    pend()

                def pend(O=O, Od=Od, g=g, kt=kt, Pt=Pt, Ptd=Ptd, nq=nq, i=i, n=len(kts)):
                    P.op('pe', lambda e: e.matmul(O[:, 0:nq], lhsT=Vaug[:, kt, g, :], rhs=Pt[:, 0:nq], start=(i == 0), stop=(i == n - 1)),
                         [Vaugd, Ptd], [Od])
            pend()
            attn_finalize((osbr, ptr, rvr), O, Od, h, ot, otd, nq, ident32, ident32d)
        nj = nq // 128
        P.dma('sp', lambda e, ot=ot, q0=q0, nj=nj: e.dma_start(out=S['ocat'][q0:q0 + nj * 128, col0:col0 + 256].rearrange("(j p) c -> p j c", p=128), in_=ot[:, 0:nj, :]),
              [otd], [S['ocat_d'][t] for t in range(q0 // 128, q0 // 128 + nj)])


def phase_gqa(l, I, S, need_ctx, ident, identd, ident32, ident32d):
    nc, P = K.nc, K.P
    with ExitStack() as st:
        QKT, QKTd = sb(st, 'gq_QKT', [64, 6, NT], BF16)
        Vaug, Vaugd = sb(st, 'gq_V', [128, NTILE, 2, 65], BF16)
        with ExitStack() as st2:
            gain, gaind = sb(st2, 'gq_gain', [128, 6, 64], F32)
            xr = Ring(K, st2, 'gq_x', [128, 512], F32, 3)
            sq, sqd = sb(st2, 'gq_sq', [128, 384], F32)
            ssr = Ring(K, st2, 'gq_ss', [128, 6], F32, 3)
            qnr = Ring(K, st2, 'gq_qn', [128, 6, 64], F32, 2)
            qbr = Ring(K, st2, 'gq_qb', [128, 6, 64], BF16, 2)
            csr = Ring(K, st2, 'gq_cs', [128, 2, 32], F32, 3)
            t1r = Ring(K, st2, 'gq_t1', [128, 6, 32], F32, 2)
            t2r = Ring(K, st2, 'gq_t2', [128, 6, 32], F32, 2)
            pTr = Ring(K, st2, 'gq_pT', [64, 6, 128], BF16, 2, psum=True)
            P.dma('sp', lambda e: e.dma_start(out=gain[:, 0:4, :], in_=I['gqa_q_g'][l:l + 1, :].partition_broadcast(128).to_broadcast([128, 4, 64])), [], [gaind])
            P.dma('sp', lambda e: e.dma_start(out=gain[:, 4:6, :], in_=I['gqa_k_g'][l:l + 1, :].partition_broadcast(128).to_broadcast([128, 2, 64])), [], [gaind])
            P.op('pool', lambda e: e.memset(Vaug[:, :, :, 64:65], 1.0), [], [Vaugd])
            for t in range(NTILE):
                (x, xd) = xr.next()
                (ss, ssd) = ssr.next()
                (qn, qnd) = qnr.next()
                (qb, qbd) = qbr.next()
                (pT, pTd) = pTr.next()
                P.dma('sp', lambda e, x=x, t=t: e.dma_start(out=x[:], in_=S['p'][t * 128:(t + 1) * 128, GQ0:GQ0 + 512]), [S['p_d'][t]], [xd])
                P.op('dve', lambda e, x=x: e.tensor_tensor(out=sq[:], in0=x[:, 0:384], in1=x[:, 0:384], op=ALU.mult), [xd], [sqd])
                P.op('dve', lambda e, ss=ss: e.tensor_reduce(out=ss[:], in_=sq[:].rearrange("p (g d) -> p g d", d=64), axis=AX.X, op=ALU.add), [sqd], [ssd])
                P.op('act', lambda e, ss=ss: e.activation(out=ss[:], in_=ss[:], func=AF.Sqrt, bias=RMS_EPS, scale=1.0 / 64), [ssd], [ssd])
                P.op('dve', lambda e, ss=ss: e.reciprocal(out=ss[:], in_=ss[:]), [ssd], [ssd])
                P.op('dve', lambda e, x=x, qn=qn, ss=ss: e.tensor_tensor(out=qn[:], in0=x[:, 0:384].rearrange("p (g d) -> p g d", d=64), in1=ss[:].unsqueeze(2).to_broadcast([128, 6, 64]), op=ALU.mult),
                     [xd, ssd], [qnd])
                P.op('pool', lambda e, x=x, t=t: e.tensor_copy(out=Vaug[:, t, :, 0:64], in_=x[:, 384:512].rearrange("p (g d) -> p g d", d=64)), [xd], [Vaugd])
                if t < 2:
                    P.op('dve', lambda e, qn=qn, qb=qb: e.tensor_tensor(out=qb[:], in0=qn[:], in1=gain[:], op=ALU.mult), [qnd, gaind], [qbd])
                else:
                    (cs, csd) = csr.next()
                    (t1, t1d) = t1r.next()
                    (t2, t2d) = t2r.next()
                    r0 = (t - 2) * 128
                    P.dma('sp', lambda e, cs=cs, r0=r0: e.dma_start(out=cs[:], in_=I['rope_g'][r0:r0 + 128, :, :]), [], [csd])
                    P.op('dve', lambda e, qn=qn: e.tensor_tensor(out=qn[:], in0=qn[:], in1=gain[:], op=ALU.mult), [qnd, gaind], [qnd])
                    cosb = lambda cs=cs: cs[:, 0, :].unsqueeze(1).to_broadcast([128, 6, 32])
                    sinb = lambda cs=cs: cs[:, 1, :].unsqueeze(1).to_broadcast([128, 6, 32])
                    P.op('dve', lambda e, t1=t1, qn=qn, cosb=cosb: e.tensor_tensor(out=t1[:], in0=qn[:, :, 0:32], in1=cosb(), op=ALU.mult), [qnd, csd], [t1d])
                    P.op('pool', lambda e, t2=t2, qn=qn, sinb=sinb: e.tensor_tensor(out=t2[:], in0=qn[:, :, 32:64], in1=sinb(), op=ALU.mult), [qnd, csd], [t2d])
                    P.op('dve', lambda e, t1=t1, t2=t2, qb=qb: e.tensor_tensor(out=qb[:, :, 0:32], in0=t1[:], in1=t2[:], op=ALU.subtract), [t1d, t2d], [qbd])
                    P.op('dve', lambda e, t1=t1, qn=qn, sinb=sinb: e.tensor_tensor(out=t1[:], in0=qn[:, :, 0:32], in1=sinb(), op=ALU.mult), [qnd, csd], [t1d])
                    P.op('pool', lambda e, t2=t2, qn=qn, cosb=cosb: e.tensor_tensor(out=t2[:], in0=qn[:, :, 32:64], in1=cosb(), op=ALU.mult), [qnd, csd], [t2d])
                    P.op('dve', lambda e, t1=t1, t2=t2, qb=qb: e.tensor_tensor(out=qb[:, :, 32:64], in0=t1[:], in1=t2[:], op=ALU.add), [t1d, t2d], [qbd])
                for g in range(6):
                    P.op('pe', lambda e, pT=pT, qb=qb, g=g: e.transpose(out=pT[:, g, :], in_=qb[:, g, :], identity=ident[:]), [qbd, identd], [pTd])
                P.op('act', lambda e, pT=pT, t=t: e.copy(out=QKT[:, :, t * 128:(t + 1) * 128], in_=pT[:]), [pTd], [QKTd])
            P.barrier()
        attn_core(st, S, lambda h: QKT[:, h, :], QKTd, lambda g: QKT[:, 4 + g, :], QKTd, [0, 0, 1, 1], Vaug, Vaugd, 0.125, 256,
                  need_ctx, ident32, ident32d, 'gq')
        P.barrier()


def phase_mla(l, I, S, need_ctx, ident, identd, ident32, ident32d):
    nc, P = K.nc, K.P
    with ExitStack() as st:
        QKT, QKTd = sb(st, 'ml_QKT', [128, 8, NT], BF16)
        Vaug, Vaugd = sb(st, 'ml_V', [128, NTILE, 4, 65], BF16)
        with ExitStack() as st2:
            gain, gaind = sb(st2, 'ml_gain', [128, 384], F32)
            wst, wstd = sb(st2, 'ml_wst', [128, 2, 512], F32)
            wuq, wuqd = sb(st2, 'ml_wuq', [128, 2, 384], BF16)
            wukv, wukvd = sb(st2, 'ml_wukv', [128, 512], BF16)
            xr = Ring(K, st2, 'ml_x', [128, 416], F32, 3)
            sq, sqd = sb(st2, 'ml_sq', [128, 384], F32)
            ssr = Ring(K, st2, 'ml_ss', [128, 2], F32, 3)
            cnr = Ring(K, st2, 'ml_cn', [128, 384], F32, 2)
            cbr = Ring(K, st2, 'ml_cb', [128, 384], BF16, 2)
            cTr = Ring(K, st2, 'ml_cT', [128, 3, 128], BF16, 2)
            qsr = Ring(K, st2, 'ml_qs', [128, 4, 96], F32, 2)
            csr = Ring(K, st2, 'ml_cs', [128, 2, 16], F32, 3)
            t1r = Ring(K, st2, 'ml_t1', [128, 5, 16], F32, 2)
            t2r = Ring(K, st2, 'ml_t2', [128, 5, 16], F32, 2)
            rr = Ring(K, st2, 'ml_r', [128, 5, 32], F32, 2)
            qkr = Ring(K, st2, 'ml_qk', [128, 8, 128], BF16, 2)
            for (qk_, qkd_) in qkr.bufs:
                P.op('pool', lambda e, qk_=qk_: e.memset(qk_[:], 0.0), [], [qkd_])
            pcT = Ring(K, st2, 'ml_pcT', [128, 3, 128], BF16, 2, psum=True)
            pq = Ring(K, st2, 'ml_pq', [128, 384], F32, 1, psum=True)
            pkv = Ring(K, st2, 'ml_pkv', [128, 512], F32, 2, psum=True)
            pT2 = Ring(K, st2, 'ml_pT2', [128, 8, 128], BF16, 2, psum=True)
            P.dma('sp', lambda e: e.dma_start(out=gain[:, 0:256], in_=I['mla_q_norm_g'][l:l + 1, :].partition_broadcast(128)), [], [gaind])
            P.dma('sp', lambda e: e.dma_start(out=gain[:, 256:384], in_=I['mla_kv_norm_g'][l:l + 1, :].partition_broadcast(128)), [], [gaind])
            P.dma('sp', lambda e: e.dma_start(out=wst[:, :, 0:384], in_=I['mla_w_uq'][l].rearrange("(k p) n -> p k n", p=128)), [], [wstd])
            P.op('pool', lambda e: e.tensor_copy(out=wuq[:], in_=wst[:, :, 0:384]), [wstd], [wuqd])
            P.dma('sp', lambda e: e.dma_start(out=wst[:, 0, :], in_=I['mla_w_ukv'][l]), [wuqd], [wstd])
            P.op('pool', lambda e: e.tensor_copy(out=wukv[:], in_=wst[:, 0, :]), [wstd], [wukvd])
            P.op('pool', lambda e: e.memset(Vaug[:, :, :, 64:65], 1.0), [], [Vaugd])
            for t in range(NTILE):
                (x, xd) = xr.next()
                (ss, ssd) = ssr.next()
                (cn, cnd) = cnr.next()
                (cb, cbd) = cbr.next()
                (cT, cTd) = cTr.next()
                (qs, qsd) = qsr.next()
                (qk, qkd) = qkr.next()
                (r, rd) = rr.next()
                (pc, pcd) = pcT.next()
                (pqt, pqd) = pq.next()
                (pk, pkd) = pkv.next()
                (pT, pTd) = pT2.next()
                P.dma('sp', lambda e, x=x, t=t: e.dma_start(out=x[:], in_=S['p'][t * 128:(t + 1) * 128, ML0:ML0 + 416]), [S['p_d'][t]], [xd])
                if K.LVL < 2:
                    continue
                P.op('dve', lambda e, x=x: e.tensor_tensor(out=sq[:], in0=x[:, 0:384], in1=x[:, 0:384], op=ALU.mult), [xd], [sqd])
                P.op('dve', lambda e, ss=ss: e.tensor_reduce(out=ss[:, 0:1], in_=sq[:, 0:256], axis=AX.X, op=ALU.add), [sqd], [ssd])
                P.op('dve', lambda e, ss=ss: e.tensor_reduce(out=ss[:, 1:2], in_=sq[:, 256:384], axis=AX.X, op=ALU.add), [sqd], [ssd])
                P.op('act', lambda e, ss=ss: e.activation(out=ss[:, 0:1], in_=ss[:, 0:1], func=AF.Sqrt, bias=RMS_EPS, scale=1.0 / 256), [ssd], [ssd])
                P.op('act', lambda e, ss=ss: e.activation(out=ss[:, 1:2], in_=ss[:, 1:2], func=AF.Sqrt, bias=RMS_EPS, scale=1.0 / 128), [ssd], [ssd])
                P.op('dve', lambda e, ss=ss: e.reciprocal(out=ss[:], in_=ss[:]), [ssd], [ssd])
                P.op('dve', lambda e, x=x, cn=cn, ss=ss: e.scalar_tensor_tensor(out=cn[:, 0:256], in0=x[:, 0:256], scalar=ss[:, 0:1], in1=gain[:, 0:256], op0=ALU.mult, op1=ALU.mult),
                     [xd, ssd, gaind], [cnd])
                P.op('dve', lambda e, x=x, cn=cn, ss=ss: e.scalar_tensor_tensor(out=cn[:, 256:384], in0=x[:, 256:384], scalar=ss[:, 1:2], in1=gain[:, 256:384], op0=ALU.mult, op1=ALU.mult),
                     [xd, ssd, gaind], [cnd])
                P.op('pool', lambda e, cn=cn, cb=cb: e.tensor_copy(out=cb[:], in_=cn[:]), [cnd], [cbd])
                if K.LVL < 3:
                    continue
                for j in range(3):
                    P.op('pe', lambda e, pc=pc, cb=cb, j=j: e.transpose(out=pc[:, j, :], in_=cb[:, j * 128:(j + 1) * 128], identity=ident[:]), [cbd, identd], [pcd])
                if K.LVL < 2.3:
                    continue
                P.op('act', lambda e, cT=cT, pc=pc: e.copy(out=cT[:], in_=pc[:]), [pcd], [cTd])
                if K.LVL < 2.6:
                    continue
                for j in range(2):
                    P.op('pe', lambda e, pqt=pqt, cT=cT, j=j: e.matmul(pqt[:], lhsT=cT[:, j, :], rhs=wuq[:, j, :], start=(j == 0), stop=(j == 1)), [cTd, wuqd], [pqd])
                if K.LVL < 2.8:
                    continue
                for hf in range(2):
                    P.op('pe', lambda e, pk=pk, cT=cT, hf=hf: e.matmul(pk[:, hf * 256:(hf + 1) * 256], lhsT=cT[:, 2, :], rhs=wukv[:, hf * 256:(hf + 1) * 256], start=True, stop=True), [cTd, wukvd], [pkd])
                if K.LVL < 4:
                    continue
                P.op('act', lambda e, qs=qs, pqt=pqt: e.copy(out=qs[:], in_=pqt[:].rearrange("p (h d) -> p h d", d=96)), [pqd], [qsd])
                P.op('dve', lambda e, pk=pk, t=t: e.tensor_copy(out=Vaug[:, t, :, 0:64], in_=pk[:].rearrange("p (h d) -> p h d", d=128)[:, :, 64:128]), [pkd], [Vaugd])
                P.op('dve', lambda e, pk=pk, qk=qk: e.tensor_copy(out=qk[:, 4:8, 0:64], in_=pk[:].rearrange("p (h d) -> p h d", d=128)[:, :, 0:64]), [pkd], [qkd])
                P.op('pool', lambda e, qs=qs, qk=qk: e.tensor_copy(out=qk[:, 0:4, 0:64], in_=qs[:, :, 0:64]), [qsd], [qkd])
                P.op('pool', lambda e, r=r, qs=qs: e.tensor_copy(out=r[:, 0:4, :], in_=qs[:, :, 64:96]), [qsd], [rd])
                P.op('pool', lambda e, r=r, x=x: e.tensor_copy(out=r[:, 4, :], in_=x[:, 384:416]), [xd], [rd])
                if K.LVL < 5:
                    continue
                if t < 2:
                    P.op('dve', lambda e, r=r, qk=qk: e.tensor_copy(out=qk[:, 0:4, 64:96], in_=r[:, 0:4, :]), [rd], [qkd])
                    P.op('dve', lambda e, r=r, qk=qk: e.tensor_copy(out=qk[:, 4:8, 64:96], in_=r[:, 4, :].unsqueeze(1).to_broadcast([128, 4, 32])), [rd], [qkd])
                else:
                    (cs, csd) = csr.next()
                    (t1, t1d) = t1r.next()
                    (t2, t2d) = t2r.next()
                    r0 = (t - 2) * 128
                    P.dma('sp', lambda e, cs=cs, r0=r0: e.dma_start(out=cs[:], in_=I['rope_m'][r0:r0 + 128, :, :]), [], [csd])
                    cosb = lambda cs=cs: cs[:, 0, :].unsqueeze(1).to_broadcast([128, 5, 16])
                    sinb = lambda cs=cs: cs[:, 1, :].unsqueeze(1).to_broadcast([128, 5, 16])
                    P.op('dve', lambda e, t1=t1, r=r, cosb=cosb: e.tensor_tensor(out=t1[:], in0=r[:, :, 0:16], in1=cosb(), op=ALU.mult), [rd, csd], [t1d])
                    P.op('pool', lambda e, t2=t2, r=r, sinb=sinb: e.tensor_tensor(out=t2[:], in0=r[:, :, 16:32], in1=sinb(), op=ALU.mult), [rd, csd], [t2d])
                    P.op('dve', lambda e, t1=t1, t2=t2: e.tensor_tensor(out=t1[:], in0=t1[:], in1=t2[:], op=ALU.subtract), [t1d, t2d], [t1d])
                    P.op('dve', lambda e, t1=t1, qk=qk: e.tensor_copy(out=qk[:, 0:4, 64:80], in_=t1[:, 0:4, :]), [t1d], [qkd])
                    P.op('dve', lambda e, t1=t1, qk=qk: e.tensor_copy(out=qk[:, 4:8, 64:80], in_=t1[:, 4, :].unsqueeze(1).to_broadcast([128, 4, 16])), [t1d], [qkd])
                    (t1, t1d) = t1r.next()
                    (t2, t2d) = t2r.next()
                    P.op('dve', lambda e, t1=t1, r=r, sinb=sinb: e.tensor_tensor(out=t1[:], in0=r[:, :, 0:16], in1=sinb(), op=ALU.mult), [rd, csd], [t1d])
                    P.op('pool', lambda e, t2=t2, r=r, cosb=cosb: e.tensor_tensor(out=t2[:], in0=r[:, :, 16:32], in1=cosb(), op=ALU.mult), [rd, csd], [t2d])
                    P.op('dve', lambda e, t1=t1, t2=t2: e.tensor_tensor(out=t1[:], in0=t1[:], in1=t2[:], op=ALU.add), [t1d, t2d], [t1d])
                    P.op('dve', lambda e, t1=t1, qk=qk: e.tensor_copy(out=qk[:, 0:4, 80:96], in_=t1[:, 0:4, :]), [t1d], [qkd])
                    P.op('dve', lambda e, t1=t1, qk=qk: e.tensor_copy(out=qk[:, 4:8, 80:96], in_=t1[:, 4, :].unsqueeze(1).to_broadcast([128, 4, 16])), [t1d], [qkd])
                if K.LVL < 6:
                    continue
                for g in range(8):
                    P.op('pe', lambda e, pT=pT, qk=qk, g=g: e.transpose(out=pT[:, g, :], in_=qk[:, g, :], identity=ident[:]), [qkd, identd], [pTd])
                P.op('act', lambda e, pT=pT, t=t: e.copy(out=QKT[:, :, t * 128:(t + 1) * 128], in_=pT[:]), [pTd], [QKTd])
            P.barrier()
        if K.LVL < 7:
            return
        attn_core(st, S, lambda h: QKT[:, h, :], QKTd, lambda g: QKT[:, 4 + g, :], QKTd, [0, 1, 2, 3], Vaug, Vaugd, 96.0 ** -0.5, 512,
                  need_ctx, ident32, ident32d, 'ml')
        P.barrier()


BIG = 30000.0


def nat_plan():
    variants = {}
    plan = []
    for i in range(64):
        rs = min(max(i - 4, 0), 56)
        tiles = []
        for m in range(rs // 2, (rs + 7) // 2 + 1):
            dd = []
            for r in (2 * m, 2 * m + 1):
                dd.append(r - i + 7 if rs <= r < rs + 8 else -1)
            key = tuple(dd)
            if key not in variants:
                variants[key] = len(variants)
            tiles.append((2 + m, variants[key]))
        plan.append(tiles)
    vlist = [None] * len(variants)
    for k, v in variants.items():
        vlist[v] = k
    return plan, vlist


def nat_bias_table(nat_bias_l):
    plan, vlist = nat_plan()
    c = np.arange(64)
    cs = np.clip(c - 8, 0, 48)
    cp = np.arange(64)
    inwin = (cp[:, None] >= cs[None, :]) & (cp[:, None] < cs[None, :] + 16)
    off = np.clip(cp[:, None] - c[None, :] + 15, 0, 30)
    tab = np.full((128, 4, len(vlist), 64), -BIG, np.float32)
    for v, (d0, d1) in enumerate(vlist):
        for half, d in enumerate((d0, d1)):
            if d < 0:
                continue
            for h in range(4):
                vals = nat_bias_l[h, d][off]
                tab[half * 64:(half + 1) * 64, h, v, :] = np.where(inwin, vals, np.float32(-BIG))
    return tab


def phase_nat(l, I, S, need_ctx, ident, identd, ident32, ident32d):
    nc, P = K.nc, K.P
    plan, vlist = nat_plan()
    NV = len(vlist)
    with ExitStack() as st:
        QKT, QKTd = sb(st, 'na_QKT', [64, 8, NT], BF16)
        Vaug, Vaugd = sb(st, 'na_V', [128, NTILE, 4, 65], BF16)
        tb, tbd = sb(st, 'na_tb', [128, 4, NV, 64], BF16)
        with ExitStack() as st2:
            tbs, tbsd = sb(st2, 'na_tbs', [128, 4, NV, 64], F32)
            xr = Ring(K, st2, 'na_x', [128, 768], F32, 3)
            xbr = Ring(K, st2, 'na_xb', [128, 512], BF16, 2)
            pTr = Ring(K, st2, 'na_pT', [64, 8, 128], BF16, 2, psum=True)
            P.dma('sp', lambda e: e.dma_start(out=tbs[:], in_=I['nat_tab'][l]), [], [tbsd])
            P.op('dve', lambda e: e.tensor_scalar(out=tb[:], in0=tbs[:], scalar1=8.0, scalar2=None, op0=ALU.mult), [tbsd], [tbd])
            P.op('pool', lambda e: e.memset(Vaug[:, :, :, 64:65], 1.0), [], [Vaugd])
            for t in range(NTILE):
                (x, xd) = xr.next()
                (xb, xbd) = xbr.next()
                (pT, pTd) = pTr.next()
                P.dma('sp', lambda e, x=x, t=t: e.dma_start(out=x[:], in_=S['p'][t * 128:(t + 1) * 128, NA0:NA0 + 768]), [S['p_d'][t]], [xd])
                P.op('dve', lambda e, x=x, xb=xb: e.tensor_copy(out=xb[:], in_=x[:, 0:512]), [xd], [xbd])
                P.op('pool', lambda e, x=x, t=t: e.tensor_copy(out=Vaug[:, t, :, 0:64], in_=x[:, 512:768].rearrange("p (h d) -> p h d", d=64)), [xd], [Vaugd])
                for g in range(8):
                    P.op('pe', lambda e, pT=pT, xb=xb, g=g: e.transpose(out=pT[:, g, :], in_=xb[:, g * 64:(g + 1) * 64], identity=ident[:]), [xbd, identd], [pTd])
                P.op('act', lambda e, pT=pT, t=t: e.copy(out=QKT[:, :, t * 128:(t + 1) * 128], in_=pT[:]), [pTd], [QKTd])
            P.barrier()
        sr = Ring(K, st, 'na_S', [128, 512], F32, 3, psum=True)
        orr = Ring(K, st, 'na_O', [65, 512], F32, 2, psum=True)
        ptr = Ring(K, st, 'na_fT', [128, 65], F32, 2, psum=True)
        pr = Ring(K, st, 'na_P', [128, 512], BF16, 3)
        osbr = Ring(K, st, 'na_osb', [65, 512], F32, 2)
        rvr = Ring(K, st, 'na_rv', [128, 1], F32, 4)
        otr = Ring(K, st, 'na_ot', [128, 4, 256], F32, 2)
        blocks = []
        if need_ctx:
            blocks.append(None)
        for qb in range(8):
            blocks.append(qb)
        for qb in blocks:
            (ot, otd) = otr.next()
            if qb is None:
                q0, nq = 0, 256
            else:
                q0, nq = 256 + qb * 512, 512
            for h in range(4):
                (O, Od) = orr.next()
                if qb is None:
                    for i, kt in enumerate((0, 1)):
                        (Sp, Spd) = sr.next()
                        (Pt, Ptd) = pr.next()
                        P.op('pe', lambda e, Sp=Sp, h=h, kt=kt: e.matmul(Sp[:, 0:256], lhsT=QKT[:, 4 + h, kt * 128:(kt + 1) * 128], rhs=QKT[:, h, 0:256], start=True, stop=True),
                             [QKTd], [Spd])
                        P.op('act', lambda e, Sp=Sp, Pt=Pt: e.activation(out=Pt[:, 0:256], in_=Sp[:, 0:256], func=AF.Exp, scale=0.125), [Spd], [Ptd])
                        P.op('pe', lambda e, O=O, h=h, kt=kt, Pt=Pt, i=i: e.matmul(O[:, 0:256], lhsT=Vaug[:, kt, h, :], rhs=Pt[:, 0:256], start=(i == 0), stop=(i == 1)),
                             [Vaugd, Ptd], [Od])
                else:
                    for ri in range(8):
                        i = qb * 8 + ri
                        qt0 = 256 + i * 64
                        tiles = [(kt, None) for kt in (0, 1)] + plan[i]
                        (Sp, Spd) = sr.next()
                        (Pt, Ptd) = pr.next()
                        for j, (kt, v) in enumerate(tiles):
                            P.op('pe', lambda e, Sp=Sp, h=h, kt=kt, qt0=qt0, j=j, v=v: e.matmul(Sp[:, j * 64:(j + 1) * 64], lhsT=QKT[:, 4 + h, kt * 128:(kt + 1) * 128], rhs=QKT[:, h, qt0:qt0 + 64], start=True, stop=(v is None)),
                                 [QKTd], [Spd])
                            if v is not None:
                                P.op('pe', lambda e, Sp=Sp, h=h, j=j, v=v: e.matmul(Sp[:, j * 64:(j + 1) * 64], lhsT=ident[:], rhs=tb[:, h, v, :], start=False, stop=True),
                                     [identd, tbd], [Spd])
                        nk = len(tiles)
                        P.op('act', lambda e, Sp=Sp, Pt=Pt, nk=nk: e.activation(out=Pt[:, 0:nk * 64], in_=Sp[:, 0:nk * 64], func=AF.Exp, scale=0.125), [Spd], [Ptd])
                        for j, (kt, v) in enumerate(tiles):
                            P.op('pe', lambda e, O=O, h=h, kt=kt, Pt=Pt, j=j, ri=ri, nk=nk: e.matmul(O[:, ri * 64:(ri + 1) * 64], lhsT=Vaug[:, kt, h, :], rhs=Pt[:, j * 64:(j + 1) * 64], start=(j == 0), stop=(j == nk - 1)),
                                 [Vaugd, Ptd], [Od])
                attn_finalize((osbr, ptr, rvr), O, Od, h, ot, otd, nq, ident32, ident32d)
            nj = nq // 128
            P.dma('sp', lambda e, ot=ot, q0=q0, nj=nj: e.dma_start(out=S['ocat'][q0:q0 + nj * 128, 768:1024].rearrange("(j p) c -> p j c", p=128), in_=ot[:, 0:nj, :]),
                  [otd], [S['ocat_d'][t] for t in range(q0 // 128, q0 // 128 + nj)])
        P.barrier()


def o_tt(q, out, in0, in1, op, rd, wr):
    K.P.op(q, lambda e: e.tensor_tensor(out=out, in0=in0, in1=in1, op=op), rd, wr)


def o_stt(q, out, in0, scalar, in1, op0, op1, rd, wr):
    K.P.op(q, lambda e: e.scalar_tensor_tensor(out=out, in0=in0, scalar=scalar, in1=in1, op0=op0, op1=op1), rd, wr)


def o_ts(q, out, in0, s1, op0, rd, wr):
    K.P.op(q, lambda e: e.tensor_scalar(out=out, in0=in0, scalar1=s1, scalar2=None, op0=op0), rd, wr)


def o_act(out, in_, func, rd, wr, **kw):
    K.P.op('act', lambda e: e.activation(out=out, in_=in_, func=func, **kw), rd, wr)


def o_red(q, out, in_, op, rd, wr):
    K.P.op(q, lambda e: e.tensor_reduce(out=out, in_=in_, axis=AX.X, op=op), rd, wr)


def o_mm(out, lhsT, rhs, rd, wr, start=True, stop=True):
    K.P.op('pe', lambda e: e.matmul(out, lhsT=lhsT, rhs=rhs, start=start, stop=stop), rd, wr)


def o_tr(out, in_, ident, rd, wr):
    K.P.op('pe', lambda e: e.transpose(out=out, in_=in_, identity=ident), rd, wr)


def o_cp(q, out, in_, rd, wr):
    if q == 'act':
        K.P.op('act', lambda e: e.copy(out=out, in_=in_), rd, wr)
    else:
        K.P.op(q, lambda e: e.tensor_copy(out=out, in_=in_), rd, wr)


def o_rcp(out, in_, rd, wr):
    K.P.op('dve', lambda e: e.reciprocal(out=out, in_=in_), rd, wr)


def o_ms(q, out, val, wr):
    K.P.op(q, lambda e: e.memset(out, val), [], wr)


def o_dma(out, in_, rd, wr, q='sp', **kw):
    K.P.dma(q, lambda e: e.dma_start(out=out, in_=in_, **kw), rd, wr)


def bc_load(st, name, src_row, n):
    t, d = sb(st, name, [128, n], F32)
    o_dma(t[:], src_row.partition_broadcast(128), [], [d])
    return t, d


RCH = 8


def rev_tile(c):
    return 1 - c if c < 2 else 35 - c


def phase_rwkv(l, I, S, need_ctx, ident32, ident32d, J32, J32d):
    rwkv_prep(l, I, S, ident32, ident32d, J32, J32d)
    rwkv_scan(S)
    rwkv_readout(l, I, S, need_ctx, J32, J32d)


def rwkv_prep(l, I, S, ident32, ident32d, J32, J32d):
    P = K.P
    MUL, ADD, SUB = ALU.mult, ALU.add, ALU.subtract
    with ExitStack() as st:
        xr = Ring(K, st, 'rv_x', [128, 1024], F32, 2)
        xo = Ring(K, st, 'rv_o', [128, 1024], F32, 2)
        pr = Ring(K, st, 'rv_ps', [128, 512], F32, 2, psum=True)
        for c in range(NTILE):
            tt_ = rev_tile(c)
            (x, xd) = xr.next()
            (o, od) = xo.next()
            o_dma(x[:], S['p'][tt_ * 128:(tt_ + 1) * 128, 0:1024], [S['p_d'][tt_]], [xd])
            for hf in range(2):
                (ps_, psd) = pr.next()
                o_mm(ps_[:], J32[:], x[:, hf * 512:(hf + 1) * 512], [J32d, xd], [psd])
                o_cp('act' if hf else 'dve', o[:, hf * 512:(hf + 1) * 512], ps_[:], [psd], [od])
            o_dma(S['prw1'][c * 128:(c + 1) * 128, :], o[:], [od], [S['prw1_d'][c]])
        P.barrier()
    with ExitStack() as st:
        mub, mubd = bc_load(st, 'rp_mu', I['rwkv_mu'][l, :], 1024)
        kkb, kkbd = bc_load(st, 'rp_kk', I['rwkv_k_k'][l, :], 256)
        kab, kabd = bc_load(st, 'rp_ka', I['rwkv_k_a'][l, :], 256)
        rkb, rkbd = bc_load(st, 'rp_rk', I['rwkv_r_k'][l].rearrange("h d -> (h d)"), 256)
        omka, omkad = sb(st, 'rp_omka', [128, 256], F32)
        K.P.op('dve', lambda e: e.tensor_scalar(out=omka[:], in0=kab[:], scalar1=-1.0, scalar2=1.0, op0=MUL, op1=ADD), [kabd], [omkad])
        w0b, a0b, wup, aup = [], [], [], []
        for d in range(2):
            w0b.append(bc_load(st, 'rp_w0%d' % d, I['rwkv_w0'][l, d, :], 256))
            a0b.append(bc_load(st, 'rp_a0%d' % d, I['rwkv_a0'][l, d, :], 256))
            t, td = sb(st, 'rp_wup%d' % d, [64, 256], F32)
            o_dma(t[:], I['rwkv_w_up'][l, d], [], [td])
            wup.append((t, td))
            t, td = sb(st, 'rp_aup%d' % d, [64, 256], F32)
            o_dma(t[:], I['rwkv_a_up'][l, d], [], [td])
            aup.append((t, td))
        gup, gupd = sb(st, 'rp_gup', [128, 256], F32)
        o_dma(gup[:], I['rwkv_g_up'][l], [], [gupd])

        xr = Ring(K, st, 'rp_x', [128, 1024], F32, 2)
        pvr = Ring(K, st, 'rp_pv', [128, 1024], F32, 2)
        nxr = Ring(K, st, 'rp_nx', [128, 1024], F32, 2)
        xsr = Ring(K, st, 'rp_xs', [128, 1024], F32, 2)
        kkr = Ring(K, st, 'rp_kkn', [128, 256], F32, 2)
        sqr = Ring(K, st, 'rp_sq', [128, 256], F32, 2)
        ssr = Ring(K, st, 'rp_ss', [128, 4], F32, 4)
        smr = Ring(K, st, 'rp_sm', [128, 128], F32, 4)
        sTr = Ring(K, st, 'rp_sT', [128, 128], F32, 4)
        ur = Ring(K, st, 'rp_u', [128, 256], F32, 2)
        ar_ = Ring(K, st, 'rp_a', [128, 256], F32, 2)
        mr = Ring(K, st, 'rp_m', [128, 256], F32, 2)
        kdr = Ring(K, st, 'rp_kd', [128, 256], F32, 3)
        kkar = Ring(K, st, 'rp_kka', [128, 256], F32, 3)
        bor = Ring(K, st, 'rp_bo', [128, 256], F32, 3)
        gtr = Ring(K, st, 'rp_gt', [128, 256], F32, 2)
        nkkr = Ring(K, st, 'rp_nkk', [128, 4, 2, 64], F32, 2)
        rrr = Ring(K, st, 'rp_rr', [128, 4, 2, 64], F32, 2)
        decr = Ring(K, st, 'rp_dec', [128, 4, 2, 64], F32, 2)
        fakr = Ring(K, st, 'rp_fak', [128, 128, 4, 2], F32, 2)
        farr = Ring(K, st, 'rp_far', [128, 128, 4, 2], F32, 2)
        fwr = Ring(K, st, 'rp_fw', [128, 128, 4], F32, 2)
        for ring in (fakr, farr):
            for (b_, bd_) in ring.bufs:
                o_ms('pool', b_[:], 0.0, [bd_])
        pbig = Ring(K, st, 'rp_pb', [128, 256], F32, 3, psum=True)
        ptr_ = Ring(K, st, 'rp_pt', [128, 128], F32, 3, psum=True)

        def v3(ap):
            return ap.rearrange("p (h k) -> p h k", k=64)

        for c in range(NTILE):
            first = c in (0, 2)
            last = c in (1, NTILE - 1)
            r0 = c * 128
            (nkk, nkkd) = nkkr.next()
            (rr, rrd) = rrr.next()
            (dec, decd) = decr.next()
            for d in range(2):
                if d == 0:
                    src = lambda a, b: S['p'][a:b, 0:1024]
                    sdeps = S['p_d']
                else:
                    src = lambda a, b: S['prw1'][a:b, :]
                    sdeps = S['prw1_d']
                nb = [sdeps[c]] + ([sdeps[c - 1]] if c > 0 else []) + ([sdeps[c + 1]] if c < NTILE - 1 else [])
                (x, xd) = xr.next()
                (pv, pvd) = pvr.next()
                (nx, nxd) = nxr.next()
                (xs, xsd) = xsr.next()
                o_dma(x[:], src(r0, r0 + 128), nb, [xd])
                if first:
                    o_ms('pool', pv[:], 0.0, [pvd])
                    o_dma(pv[1:128, :], src(r0, r0 + 127), nb, [pvd])
                else:
                    o_dma(pv[:], src(r0 - 1, r0 + 127), nb, [pvd])
                if last:
                    o_ms('pool', nx[:], 0.0, [nxd])
                    o_dma(nx[0:127, :], src(r0 + 1, r0 + 128), nb, [nxd])
                else:
                    o_dma(nx[:], src(r0 + 1, r0 + 129), nb, [nxd])
                o_tt('pool', pv[:], pv[:], nx[:], ADD, [pvd, nxd], [pvd])
                o_stt('dve', pv[:], pv[:], 0.5, x[:], MUL, SUB, [pvd, xd], [pvd])
                o_tt('pool', pv[:], pv[:], mub[:], MUL, [pvd, mubd], [pvd])
                o_tt('dve', xs[:], x[:], pv[:], ADD, [xd, pvd], [xsd])
                r_ = xs[:, 0:256]
                k_ = xs[:, 256:512]
                v_ = xs[:, 512:768]
                (kkn, kknd) = kkr.next()
                (sq, sqd) = sqr.next()
                (ss, ssd) = ssr.next()
                o_tt('dve', kkn[:], k_, kkb[:], MUL, [xsd, kkbd], [kknd])
                o_tt('pool', sq[:], kkn[:], kkn[:], MUL, [kknd], [sqd])
                o_red('dve', ss[:], v3(sq[:]), ADD, [sqd], [ssd])
                o_act(ss[:], ss[:], AF.Sqrt, [ssd], [ssd], bias=1e-12, scale=1.0)
                o_rcp(ss[:], ss[:], [ssd], [ssd])
                o_tt('dve', v3(kkn[:]), v3(kkn[:]), ss[:].unsqueeze(2).to_broadcast([128, 4, 64]), MUL, [kknd, ssd], [kknd])
                o_ts('dve', nkk[:, :, d, :], v3(kkn[:]), -1.0, MUL, [kknd], [nkkd])
                o_cp('pool', rr[:, :, d, :], v3(r_), [xsd], [rrd])
                (tw, twd) = smr.next()
                (twT, twTd) = sTr.next()
                (pt, ptd) = ptr_.next()
                (pb, pbd) = pbig.next()
                (u, ud) = ur.next()
                o_act(tw[:, 0:64], xs[:, 768:832], AF.Tanh, [xsd], [twd])
                o_tr(pt[0:64, :], tw[:, 0:64], ident32[:], [twd, ident32d], [ptd])
                o_cp('act', twT[0:64, :], pt[0:64, :], [ptd], [twTd])
                o_mm(pb[:], twT[0:64, :], wup[d][0][:], [twTd, wup[d][1]], [pbd])
                o_tt('dve', u[:], pb[:], w0b[d][0][:], ADD, [pbd, w0b[d][1]], [ud])
                o_act(u[:], u[:], AF.Sigmoid, [ud], [ud])
                o_act(dec[:, :, d, :], v3(u[:]), AF.Exp, [ud], [decd], scale=-0.6065306597126334)
                (xa, xad) = smr.next()
                (xaT, xaTd) = sTr.next()
                (pt, ptd) = ptr_.next()
                (pb, pbd) = pbig.next()
                (a, ad) = ar_.next()
                o_cp('pool', xa[:, 0:64], xs[:, 832:896], [xsd], [xad])
                o_tr(pt[0:64, :], xa[:, 0:64], ident32[:], [xad, ident32d], [ptd])
                o_cp('act', xaT[0:64, :], pt[0:64, :], [ptd], [xaTd])
                o_mm(pb[:], xaT[0:64, :], aup[d][0][:], [xaTd, aup[d][1]], [pbd])
                o_tt('dve', a[:], pb[:], a0b[d][0][:], ADD, [pbd, a0b[d][1]], [ad])
                o_act(a[:], a[:], AF.Sigmoid, [ad], [ad])
                (m, md) = mr.next()
                (kd, kdd) = kdr.next()
                (kka, kkad) = kkar.next()
                (bo, bod) = bor.next()
                o_tt('pool', m[:], a[:], kab[:], MUL, [ad, kabd], [md])
                o_tt('pool', m[:], m[:], omka[:], ADD, [md, omkad], [md])
                o_tt('dve', kd[:], k_, m[:], MUL, [xsd, md], [kdd])
                o_tt('pool', kka[:], kkn[:], a[:], MUL, [kknd, ad], [kkad])
                (ss2, ss2d) = ssr.next()
                o_tt('dve', m[:], r_, kd[:], MUL, [xsd, kdd], [md])
                o_tt('pool', m[:], m[:], rkb[:], MUL, [md, rkbd], [md])
                o_red('dve', ss2[:], v3(m[:]), ADD, [md], [ss2d])
                o_tt('dve', v3(bo[:]), v3(v_), ss2[:].unsqueeze(2).to_broadcast([128, 4, 64]), MUL, [xsd, ss2d], [bod])
                o_dma(S['V2'][d, r0:r0 + 128, :], v_, [xsd], [S['V2_d'][c]])
                o_dma(S['KKA'][d, r0:r0 + 128, :], kka[:], [kkad], [S['KKA_d'][c]])
                o_dma(S['KD'][d, r0:r0 + 128, :], kd[:], [kdd], [S['KD_d'][c]])
                o_dma(S['BON'][d, r0:r0 + 128, :], bo[:], [bod], [S['BON_d'][c]])
                if d == 0:
                    (sg, sgd) = smr.next()
                    (sgT, sgTd) = sTr.next()
                    (pt, ptd) = ptr_.next()
                    (pb, pbd) = pbig.next()
                    (gt, gtd) = gtr.next()
                    o_act(sg[:], xs[:, 896:1024], AF.Sigmoid, [xsd], [sgd])
                    o_tr(pt[:], sg[:], ident32[:], [sgd, ident32d], [ptd])
                    o_cp('act', sgT[:], pt[:], [ptd], [sgTd])
                    o_mm(pb[:], sgT[:], gup[:], [sgTd, gupd], [pbd])
                    o_cp('dve', gt[:], pb[:], [pbd], [gtd])
                    o_dma(S['GATE'][r0:r0 + 128, :], gt[:], [gtd], [S['GATE_d'][c]])
            (fak, fakd) = fakr.next()
            (far, fard) = farr.next()
            (fw, fwd) = fwr.next()
            for h in range(4):
                for (srcT, srcd, dstF, dstd) in ((nkk, nkkd, fak, fakd), (rr, rrd, far, fard)):
                    (pt, ptd) = ptr_.next()
                    o_tr(pt[:], srcT[:, h, :, :].rearrange("p d k -> p (d k)"), ident32[:], [srcd, ident32d], [ptd])
                    eng_ = 'act' if h % 2 == 0 else 'dve'
                    o_cp(eng_, dstF[0:64, :, h, 0], pt[0:64, :], [ptd], [dstd])
                    o_cp(eng_, dstF[64:128, :, h, 1], pt[64:128, :], [ptd], [dstd])
                (pt, ptd) = ptr_.next()
                o_tr(pt[:], dec[:, h, :, :].rearrange("p d k -> p (d k)"), ident32[:], [decd, ident32d], [ptd])
                o_cp('act', fw[:, :, h], pt[:], [ptd], [fwd])
            o_dma(S['AKK'][:, r0:r0 + 128, :], fak[:].rearrange("p s h d -> p s (h d)"), [fakd], [S['AKK_d'][c]])
            o_dma(S['AR'][:, r0:r0 + 128, :], far[:].rearrange("p s h d -> p s (h d)"), [fard], [S['AR_d'][c]])
            o_dma(S['WD'][:, r0:r0 + 128, :], fw[:], [fwd], [S['WD_d'][c]])
        P.barrier()


def rwkv_scan(S, T=None):
    P = K.P
    T = NT if T is None else T
    CH = RCH
    nch = T // CH
    with ExitStack() as st:
        ST, _ = sb(st, 'sc_ST', [128, 256], F32)
        STd = [Dep('sc_ST%d' % h) for h in range(4)]
        for ch in range(4):
            o_ms('pool', ST[:, ch * 64:(ch + 1) * 64], 0.0, [STd[ch]])
        NB = 3
        akk = [sb(st, 'sc_akk%d' % i, [128, CH, 8], F32) for i in range(NB)]
        ar = [sb(st, 'sc_ar%d' % i, [128, CH, 8], F32) for i in range(NB)]
        am = [sb(st, 'sc_am%d' % i, [128, CH, 4, 4], F32) for i in range(NB)]
        wt = [sb(st, 'sc_wt%d' % i, [128, CH, 4], F32) for i in range(NB)]
        bt = [sb(st, 'sc_bt%d' % i, [6, CH, 4, 128], F32) for i in range(NB)]
        rt = [sb(st, 'sc_rt%d' % i, [6, CH, 256], F32) for i in range(NB)]
        rtv = [Dep('sc_rtv%d' % i) for i in range(NB)]
        rts = [[Dep('sc_rts%d_%d' % (i, ch)) for ch in range(4)] for i in range(NB)]
        amf, amfd = sb(st, 'sc_amf', [128, 4, 4], F32)
        yfin, yfind = sb(st, 'sc_yfin', [4, 256], F32)
        for (b_, bd_) in bt:
            o_ms('pool', b_[:], 0.0, [bd_])
        sar = [Ring(K, st, 'sc_sa%d' % ch, [128, 64], F32, 1, psum=True) for ch in range(4)]
        ur = [Ring(K, st, 'sc_u%d' % ch, [128, 64], F32, 1, psum=True) for ch in range(4)]

        def load_chunk(c):
            i = c % NB
            s0 = c * CH
            tl = [s0 // 128]
            o_dma(akk[i][0][:], S['AKK'][:, s0:s0 + CH, :], [S['AKK_d'][t] for t in tl], [akk[i][1]])
            o_dma(ar[i][0][:], S['AR'][:, s0:s0 + CH, :], [S['AR_d'][t] for t in tl], [ar[i][1]])
            o_dma(wt[i][0][:], S['WD'][:, s0:s0 + CH, :], [S['WD_d'][t] for t in tl], [wt[i][1]])
            for which, nm in ((0, 'KKA'), (1, 'KD')):
                for d in range(2):
                    row = d if which == 0 else 4 + d
                    o_dma(bt[i][0][row:row + 1, :, :, d * 64:(d + 1) * 64],
                          S[nm][d:d + 1, s0:s0 + CH, :].rearrange("o s (h k) -> o s h k", k=64),
                          [S[nm + '_d'][t] for t in tl], [bt[i][1]], q='pool')
            o_dma(rt[i][0][4:6, :, :], S['V2'][:, s0:s0 + CH, :], [S['V2_d'][t] for t in tl], [rtv[i]])
            a4 = akk[i][0][:].rearrange("p s (h d) -> p s h d", d=2)
            r4 = ar[i][0][:].rearrange("p s (h d) -> p s h d", d=2)
            o_cp('pool', am[i][0][:, :, :, 0:2], a4, [akk[i][1]], [am[i][1]])
            o_cp('pool', am[i][0][:, 1:CH, :, 2:4], r4[:, 0:CH - 1, :, :], [ar[i][1]], [am[i][1]])
            if c == 0:
                o_ms('pool', am[i][0][:, 0, :, 2:4], 0.0, [am[i][1]])
            else:
                ip = (c - 1) % NB
                rp = ar[ip][0][:].rearrange("p s (h d) -> p s h d", d=2)
                o_cp('pool', am[i][0][:, 0, :, 2:4], rp[:, CH - 1, :, :], [ar[ip][1]], [am[i][1]])

        load_chunk(0)
        if nch > 1:
            load_chunk(1)
        for s in range(T):
            c, j = divmod(s, CH)
            i = c % NB
            if j == 0 and c + 2 < nch:
                load_chunk(c + 2)
            SAs = [sar[ch].next() for ch in range(4)]
            Us = [ur[ch].next() for ch in range(4)]
            for h in range(4):
                (SA, SAd) = SAs[h]
                o_mm(SA[0:4, :], am[i][0][:, j, h, :], ST[:, h * 64:(h + 1) * 64], [am[i][1], STd[h]], [SAd])
            for h in range(4):
                (SA, SAd) = SAs[h]
                (U, Ud) = Us[h]
                o_cp('act', rt[i][0][0:4, j, h * 64:(h + 1) * 64], SA[0:4, :], [SAd], [rts[i][h]])
                o_mm(U[:], bt[i][0][0:6, j, h, :], rt[i][0][0:6, j, h * 64:(h + 1) * 64], [bt[i][1], rts[i][h], rtv[i]], [Ud])
            for h in range(4):
                (U, Ud) = Us[h]
                STh = ST[:, h * 64:(h + 1) * 64]
                o_stt('dve', STh, STh, wt[i][0][:, j, h:h + 1], U[:], ALU.mult, ALU.add, [STd[h], wt[i][1], Ud], [STd[h]])
            if j == CH - 1:
                s0 = c * CH
                if c == 0:
                    o_dma(S['Y2'][:, 0:CH - 1, :], rt[i][0][2:4, 1:CH, :], rts[i], [S['Y2_d'][0]])
                else:
                    o_dma(S['Y2'][:, s0 - 1:s0 + CH - 1, :], rt[i][0][2:4, :, :], rts[i], [S['Y2_d'][(s0 - 1) // 128]])
        il = (nch - 1) % NB
        rl = ar[il][0][:].rearrange("p s (h d) -> p s h d", d=2)
        o_ms('pool', amf[:], 0.0, [amfd])
        o_cp('pool', amf[:, :, 2:4], rl[:, CH - 1, :, :], [ar[il][1], amfd], [amfd])
        for h in range(4):
            (SA, SAd) = sar[h].next()
            o_mm(SA[0:4, :], amf[:, h, :], ST[:, h * 64:(h + 1) * 64], [amfd, STd[h]], [SAd])
            o_cp('act', yfin[0:4, h * 64:(h + 1) * 64], SA[0:4, :], [SAd], [yfind])
        o_dma(S['Y2'][:, T - 1, :], yfin[2:4, :], [yfind], [S['Y2_d'][(T - 1) // 128]])
        P.barrier()


def rwkv_readout(l, I, S, need_ctx, J32, J32d):
    P = K.P
    MUL, ADD = ALU.mult, ALU.add
    with ExitStack() as st:
        lng, lngd = bc_load(st, 'ro_lng', I['rwkv_ln_g'][l, :], 256)
        lnb, lnbd = bc_load(st, 'ro_lnb', I['rwkv_ln_b'][l, :], 256)
        yr_ = Ring(K, st, 'ro_y', [128, 256], F32, 3)
        br_ = Ring(K, st, 'ro_b', [128, 256], F32, 3)
        gr_ = Ring(K, st, 'ro_g', [128, 256], F32, 2)
        ycr = Ring(K, st, 'ro_yc', [128, 256], F32, 3)
        sqr = Ring(K, st, 'ro_sq', [128, 256], F32, 2)
        smr = Ring(K, st, 'ro_sm', [128, 4], F32, 6)
        otr = Ring(K, st, 'ro_o', [128, 256], F32, 2)
        psr = Ring(K, st, 'ro_ps', [128, 256], F32, 2, psum=True)

        def v3(ap):
            return ap.rearrange("p (h k) -> p h k", k=64)

        def bc4(ap):
            return ap.unsqueeze(2).to_broadcast([128, 4, 64])

        for t in range(0 if need_ctx else 2, NTILE):
            outs = []
            for d in range(2):
                c = t if d == 0 else rev_tile(t)
                r0 = c * 128
                (y, yd) = yr_.next()
                (b, bd) = br_.next()
                (yc, ycd) = ycr.next()
                (sq, sqd) = sqr.next()
                (sm, smd) = smr.next()
                (vr, vrd) = smr.next()
                o_dma(y[:], S['Y2'][d, r0:r0 + 128, :], [S['Y2_d'][c]], [yd])
                o_dma(b[:], S['BON'][d, r0:r0 + 128, :], [S['BON_d'][c]], [bd])
                o_red('dve', sm[:], v3(y[:]), ADD, [yd], [smd])
                o_ts('dve', sm[:], sm[:], -1.0 / 64, MUL, [smd], [smd])
                o_tt('dve', v3(yc[:]), v3(y[:]), bc4(sm[:]), ADD, [yd, smd], [ycd])
                o_tt('pool', sq[:], yc[:], yc[:], MUL, [ycd], [sqd])
                o_red('dve', vr[:], v3(sq[:]), ADD, [sqd], [vrd])
                o_act(vr[:], vr[:], AF.Sqrt, [vrd], [vrd], bias=64e-5, scale=1.0 / 64)
                o_rcp(vr[:], vr[:], [vrd], [vrd])
                o_tt('dve', v3(yc[:]), v3(yc[:]), bc4(vr[:]), MUL, [ycd, vrd], [ycd])
                o_tt('pool', yc[:], yc[:], lng[:], MUL, [ycd, lngd], [ycd])
                o_tt('pool', yc[:], yc[:], lnb[:], ADD, [ycd, lnbd], [ycd])
                o_tt('dve', yc[:], yc[:], b[:], ADD, [ycd, bd], [ycd])
                outs.append((yc, ycd))
            (ps_, psd) = psr.next()
            (g, gd) = gr_.next()
            (ot, otd) = otr.next()
            o_dma(g[:], S['GATE'][t * 128:(t + 1) * 128, :], [S['GATE_d'][t]], [gd])
            o_mm(ps_[:], J32[:], outs[1][0][:], [J32d, outs[1][1]], [psd])
            o_tt('dve', ot[:], outs[0][0][:], ps_[:], ADD, [outs[0][1], psd], [otd])
            o_tt('dve', ot[:], ot[:], g[:], MUL, [otd, gd], [otd])
            o_dma(S['ocat'][t * 128:(t + 1) * 128, 0:256], ot[:], [otd], [S['ocat_d'][t]])
        P.barrier()


def phase_out(l, I, S, need_ctx, modL, modLd, modC, modCd, ident, identd):
    P = K.P
    MUL, ADD = ALU.mult, ALU.add
    with ExitStack() as st:
        wbf, wbfd = sb(st, 'wo_bf', [128, 8, D], BF16)
        wst = Ring(K, st, 'wo_st', [128, D], F32, 2)
        ocr = Ring(K, st, 'wo_oc', [128, D], F32, 2)
        ocbr = Ring(K, st, 'wo_ocb', [128, D], BF16, 2)
        oTr = Ring(K, st, 'wo_oT', [128, 8, 128], BF16, 2)
        xr = Ring(K, st, 'wo_x', [128, D], F32, 2)
        yr = Ring(K, st, 'wo_y', [128, D], F32, 2)
        sq, sqd = sb(st, 'wo_sq', [128, D], F32)
        ssr = Ring(K, st, 'wo_ss', [128, 1], F32, 4)
        pTr = Ring(K, st, 'wo_pT', [128, 8, 128], BF16, 2, psum=True)
        pyr = Ring(K, st, 'wo_py', [128, 512], F32, 4, psum=True)
        wo = I['w_out'][l].rearrange("(k p) n -> p k n", p=128)
        for kc in range(8):
            (ws, wsd) = wst.next()
            o_dma(ws[:], wo[:, kc, :], [], [wsd])
            o_cp('pool', wbf[:, kc, :], ws[:], [wsd], [wbfd])
        for t in range(0 if need_ctx else 2, NTILE):
            mt, mtd = (modC, modCd) if t < 2 else (modL, modLd)
            (oc, ocd) = ocr.next()
            (ocb, ocbd) = ocbr.next()
            (oT, oTd) = oTr.next()
            (x, xd) = xr.next()
            (y, yd) = yr.next()
            (ss, ssd) = ssr.next()
            (pT, pTd) = pTr.next()
            o_dma(oc[:], S['ocat'][t * 128:(t + 1) * 128, :], [S['ocat_d'][t]], [ocd])
            o_dma(x[:], S['xs'][t * 128:(t + 1) * 128, :], [S['xs_d'][t]], [xd])
            o_cp('pool', ocb[:], oc[:], [ocd], [ocbd])
            for kc in range(8):
                o_tr(pT[:, kc, :], ocb[:, kc * 128:(kc + 1) * 128], ident[:], [ocbd, identd], [pTd])
            o_cp('act', oT[:], pT[:], [pTd], [oTd])
            for hf in range(2):
                (py, pyd) = pyr.next()
                for kc in range(8):
                    o_mm(py[:], oT[:, kc, :], wbf[:, kc, hf * 512:(hf + 1) * 512], [oTd, wbfd], [pyd], start=(kc == 0), stop=(kc == 7))
                o_cp('act' if hf else 'dve', y[:, hf * 512:(hf + 1) * 512], py[:], [pyd], [yd])
            rms_rstd(y[:], yd, D, sq, sqd, ss, ssd, 0)
            o_stt('dve', y[:], y[:], ss[:, 0:1], mt[:, 2, :], MUL, MUL, [yd, ssd, mtd], [yd])
            o_tt('pool', x[:], x[:], y[:], ADD, [xd, yd], [xd])
            o_dma(S['xs'][t * 128:(t + 1) * 128, :], x[:], [xd], [S['xs_d'][t]])
        P.barrier()


def ffn_convert(I, S, moe, li):
    P = K.P
    NEx = NE if moe else 1
    tag = 'm' if moe else 'f'
    with ExitStack() as st:
        ldr = Ring(K, st, 'cv_ld', [128, DFF], F32, 3)
        cvr = Ring(K, st, 'cv_bf', [128, DFF], BF16, 3)
        n = 0
        engs = ('dve', 'pool', 'act')
        for e in range(NEx):
            for (nm, dst) in (('gate', 'wgb' + tag), ('up', 'wub' + tag)):
                src = (I['moe_w_' + nm][li, e] if moe else I['ffn_w_' + nm][li]).rearrange("(k p) n -> p k n", p=128)
                for kc in range(8):
                    (ld, ldd) = ldr.next()
                    (cv, cvd) = cvr.next()
                    o_dma(ld[:], src[:, kc, :], [], [ldd])
                    o_cp(engs[n % 3], cv[:], ld[:], [ldd], [cvd])
                    n += 1
                    for q4 in range(2):
                        f0, f1 = q4 * 11, (q4 + 1) * 11
                        o_dma(S[dst][e, f0:f1, :, kc, :].rearrange("f p n -> p f n"),
                              cv[:, f0 * 128:f1 * 128].rearrange("p (f n) -> p f n", n=128), [cvd], [Dep()])
            srcd = (I['moe_w_down'][li, e] if moe else I['ffn_w_down'][li]).rearrange("(f p) n -> p f n", p=128)
            for f0 in range(0, NFF, 2):
                (ld, ldd) = ldr.next()
                (cv, cvd) = cvr.next()
                o_dma(ld[:, 0:2048].rearrange("p (f n) -> p f n", n=1024), srcd[:, f0:f0 + 2, :], [], [ldd])
                o_cp(engs[n % 3], cv[:, 0:2048], ld[:, 0:2048], [ldd], [cvd])
                n += 1
                o_dma(S['wdb' + tag][e, f0:f0 + 2, :, :].rearrange("f p n -> p f n"),
                      cv[:, 0:2048].rearrange("p (f n) -> p f n", n=1024), [cvd], [Dep()])
        P.barrier()


def phase_ffn(l, I, S, need_ctx, modL, modLd, modC, modCd, ident32, ident32d, out_ap, out_d):
    P = K.P
    MUL, ADD, SUB = ALU.mult, ALU.add, ALU.subtract
    moe = (l % 2 == 1)
    li = l // 2
    NEx = NE if moe else 1
    tag = 'm' if moe else 'f'
    last_layer = not need_ctx
    ffn_convert(I, S, moe, li)
    tiles = list(range(0 if need_ctx else 2, NTILE))
    split = last_layer
    if split:
        tiles = tiles[0:16]
    with ExitStack() as st:
        xrr = Ring(K, st, 'ff_x', [128, 2, D], F32, 2)
        if split:
            sel, seld = sb(st, 'ff_sel', [128, 2], F32)
            o_dma(sel[:], I['halfsel'][:, :], [], [seld])
            xbr = Ring(K, st, 'ff_xb', [128, D], F32, 2)
            xar = Ring(K, st, 'ff_xa', [128, D], F32, 2)
        hfr = Ring(K, st, 'ff_hf', [128, D], F32, 2)
        hTr = Ring(K, st, 'ff_hT', [128, 8, 256], BF16, 2)
        h32r = Ring(K, st, 'ff_h32', [128, 8, 128], F32, 2)
        sq, sqd = sb(st, 'ff_sq', [128, D], F32)
        ssr = Ring(K, st, 'ff_ss', [128, 1], F32, 4)
        accr = Ring(K, st, 'ff_acc', [128, 2, D], F32, 2)
        gtr = Ring(K, st, 'ff_gate', [128, 2, 8], F32, 2)
        smr = Ring(K, st, 'ff_sm', [128, 8], F32, 8)
        s1r = Ring(K, st, 'ff_s1', [128, 1], F32, 12)
        wgr = Ring(K, st, 'ff_wg', [128, 8, 128], BF16, 3)
        wur = Ring(K, st, 'ff_wu', [128, 8, 128], BF16, 3)
        wdr = Ring(K, st, 'ff_wd', [128, D], BF16, 3)
        sgr = Ring(K, st, 'ff_sg', [128, 256], F32, 2)
        GTr = Ring(K, st, 'ff_GT', [128, 256], BF16, 3)
        pTr = Ring(K, st, 'ff_pT', [128, 4, 128], F32, 1, psum=True)
        gur = Ring(K, st, 'ff_gu', [128, 2, 256], F32, 2, psum=True)
        ops_ = [[ps(st, 'ff_o%d%d' % (a, b), [128, 512], F32) for b in range(2)] for a in range(2)]
        plr = Ring(K, st, 'ff_pl', [128, 8], F32, 1, psum=True)
        if moe:
            rt_, rtd = sb(st, 'ff_router', [128, 8, 8], F32)
            o_dma(rt_[:], I['moe_router'][li].rearrange("(k p) e -> p k e", p=128), [], [rtd])
        for blk in range(len(tiles) // 2):
            tl = tiles[2 * blk:2 * blk + 2]
            (xr, xrd) = xrr.next()
            (hT, hTd) = hTr.next()
            (acc, accd) = accr.next()
            (gate, gated) = gtr.next()
            for jj, t in enumerate(tl):
                mt, mtd = (modC, modCd) if t < 2 else (modL, modLd)
                (hf, hfd) = hfr.next()
                (h32, h32d) = h32r.next()
                (ss, ssd) = ssr.next()
                if split:
                    (xa, xad) = xar.next()
                    (xb, xbd) = xbr.next()
                    t2 = t + 16
                    o_dma(xa[:], S['xs'][t * 128:(t + 1) * 128, :], [S['xs_d'][t]], [xad])
                    o_dma(xb[:], S['xs'][t2 * 128:(t2 + 1) * 128, :], [S['xs_d'][t2]], [xbd], q='pool')
                    o_ts('dve', xa[:], xa[:], sel[:, 0:1], MUL, [xad, seld], [xad])
                    o_stt('dve', xr[:, jj, :], xb[:], sel[:, 1:2], xa[:], MUL, ADD, [xbd, seld, xad], [xrd])
                else:
                    o_dma(xr[:, jj, :], S['xs'][t * 128:(t + 1) * 128, :], [S['xs_d'][t]], [xrd])
                rms_rstd(xr[:, jj, :], xrd, D, sq, sqd, ss, ssd, 0)
                o_stt('dve', hf[:], xr[:, jj, :], ss[:, 0:1], mt[:, 4, :], MUL, MUL, [xrd, ssd, mtd], [hfd])
                o_tt('pool', hf[:], hf[:], mt[:, 3, :], ADD, [hfd, mtd], [hfd])
                for q4 in range(2):
                    (pT, pTd) = pTr.next()
                    for kk_ in range(4):
                        kc = q4 * 4 + kk_
                        o_tr(pT[:, kk_, :], hf[:, kc * 128:(kc + 1) * 128], ident32[:], [hfd, ident32d], [pTd])
                    o_cp('act', h32[:, q4 * 4:(q4 + 1) * 4, :], pT[:], [pTd], [h32d])
                    o_cp('pool', hT[:, q4 * 4:(q4 + 1) * 4, jj * 128:(jj + 1) * 128], h32[:, q4 * 4:(q4 + 1) * 4, :], [h32d], [hTd])
                if moe:
                    (pl, pld) = plr.next()
                    for kc in range(8):
                        o_mm(pl[:], h32[:, kc, :], rt_[:, kc, :], [h32d, rtd], [pld], start=(kc == 0), stop=(kc == 7))
                    (lg, lgd) = smr.next()
                    (lg2, lg2d) = smr.next()
                    (mk1, mk1d) = smr.next()
                    (mk2, mk2d) = smr.next()
                    (m1, m1d) = s1r.next()
                    (m2, m2d) = s1r.next()
                    (ex, exd) = s1r.next()
                    (w1, w1d) = s1r.next()
                    (w2, w2d) = s1r.next()
                    o_cp('dve', lg[:], pl[:], [pld], [lgd])
                    o_red('dve', m1[:], lg[:], ALU.max, [lgd], [m1d])
                    o_ts('dve', mk1[:], lg[:], m1[:, 0:1], ALU.is_equal, [lgd, m1d], [mk1d])
                    o_stt('dve', lg2[:], mk1[:], -1.0e30, lg[:], MUL, ADD, [mk1d, lgd], [lg2d])
                    o_red('dve', m2[:], lg2[:], ALU.max, [lg2d], [m2d])
                    o_ts('dve', mk2[:], lg2[:], m2[:, 0:1], ALU.is_equal, [lg2d, m2d], [mk2d])
                    o_tt('dve', ex[:], m2[:], m1[:], SUB, [m2d, m1d], [exd])
                    o_act(ex[:], ex[:], AF.Exp, [exd], [exd])
                    o_ts('dve', w1[:], ex[:], 1.0, ADD, [exd], [w1d])
                    o_rcp(w1[:], w1[:], [w1d], [w1d])
                    o_tt('dve', w2[:], ex[:], w1[:], MUL, [exd, w1d], [w2d])
                    o_ts('dve', gate[:, jj, :], mk1[:], w1[:, 0:1], MUL, [mk1d, w1d], [gated])
                    o_stt('dve', gate[:, jj, :], mk2[:], w2[:, 0:1], gate[:, jj, :], MUL, ADD, [mk2d, w2d, gated], [gated])
            for e in range(NEx):
                for f in range(NFF):
                    (wg, wgd) = wgr.next()
                    (wu, wud) = wur.next()
                    (wd, wdd) = wdr.next()
                    (gu, gud) = gur.next()
                    (sg, sgd) = sgr.next()
                    (GT, GTd) = GTr.next()
                    o_dma(wg[:], S['wgb' + tag][e, f], [], [wgd])
                    o_dma(wu[:], S['wub' + tag][e, f], [], [wud], q='pool')
                    o_dma(wd[:], S['wdb' + tag][e, f], [], [wdd])
                    for kc in range(8):
                        o_mm(gu[:, 0, :], wg[:, kc, :], hT[:, kc, :], [wgd, hTd], [gud], start=(kc == 0), stop=(kc == 7))
                    for kc in range(8):
                        o_mm(gu[:, 1, :], wu[:, kc, :], hT[:, kc, :], [wud, hTd], [gud], start=(kc == 0), stop=(kc == 7))
                    o_act(sg[:], gu[:, 0, :], AF.Silu, [gud], [sgd])
                    o_tt('dve', GT[:], sg[:], gu[:, 1, :], MUL, [sgd, gud], [GTd])
                    for jj in range(2):
                        for hf_ in range(2):
                            (op_, opd) = ops_[jj][hf_]
                            o_mm(op_[:], GT[:, jj * 128:(jj + 1) * 128], wd[:, hf_ * 512:(hf_ + 1) * 512], [GTd, wdd], [opd],
                                 start=(f == 0), stop=(f == NFF - 1))
                for jj in range(2):
                    for hf_ in range(2):
                        (op_, opd) = ops_[jj][hf_]
                        dst = acc[:, jj, hf_ * 512:(hf_ + 1) * 512]
                        if not moe:
                            o_cp('act', dst, op_[:], [opd], [accd])
                        elif e == 0:
                            o_ts('dve', dst, op_[:], gate[:, jj, e:e + 1], MUL, [opd, gated], [accd])
                        else:
                            o_stt('dve', dst, op_[:], gate[:, jj, e:e + 1], dst, MUL, ADD, [opd, gated, accd], [accd])
            for jj, t in enumerate(tl):
                mt, mtd = (modC, modCd) if t < 2 else (modL, modLd)
                (ss, ssd) = ssr.next()
                rms_rstd(acc[:, jj, :], accd, D, sq, sqd, ss, ssd, 0)
                o_stt('dve', acc[:, jj, :], acc[:, jj, :], ss[:, 0:1], mt[:, 5, :], MUL, MUL, [accd, ssd, mtd], [accd])
                o_tt('pool', acc[:, jj, :], acc[:, jj, :], xr[:, jj, :], ADD, [accd, xrd], [accd])
                if last_layer:
                    o_dma(out_ap[(t - 2) * 128:(t - 1) * 128, :], acc[:, jj, :], [accd], [out_d])
                else:
                    o_dma(S['xs'][t * 128:(t + 1) * 128, :], acc[:, jj, :], [accd], [S['xs_d'][t]])
        P.barrier()


IN_NAMES = ['x', 'c', 'ctx', 'c_ctx', 'mod_w', 'mod_b', 'norm_g', 'w_in', 'w_out', 'rwkv_mu', 'rwkv_w0', 'rwkv_w_up',
            'rwkv_a0', 'rwkv_a_up', 'rwkv_g_up', 'rwkv_k_k', 'rwkv_k_a', 'rwkv_r_k', 'rwkv_ln_g', 'rwkv_ln_b',
            'gqa_q_g', 'gqa_k_g', 'mla_q_norm_g', 'mla_w_uq', 'mla_kv_norm_g', 'mla_w_ukv', 'nat_bias',
            'ffn_w_gate', 'ffn_w_up', 'ffn_w_down', 'moe_router', 'moe_w_gate', 'moe_w_up', 'moe_w_down']


def build(shapes, stop_after=None, debug=(), only=None, layers=(0, 1), scan_T=None):
    nc = bass.Bass("TRN2", target_bir_lowering=False)
    K.nc = nc
    P = Prog(nc)
    K.P = P
    I = {}
    for name in IN_NAMES:
        I[name] = nc.dram_tensor(name, list(shapes[name]), F32, kind="ExternalInput").ap()
    out = nc.dram_tensor("out", [NL // 2, D], F32, kind="ExternalOutput").ap()
    I['halfsel'] = nc.dram_tensor('halfsel', [128, 2], F32, kind='ExternalInput').ap()
    out_d = Dep('out')
    S = {}

    def scratch(name, shape, dtype=F32, tiled=True):
        S[name] = dram(name, shape, dtype)
        S[name + '_d'] = [Dep('%s%d' % (name, t)) for t in range(NTILE)] if tiled else Dep(name)

    scratch('xs', [NT, D])
    scratch('p', [NT, INC])
    scratch('ocat', [NT, D])
    scratch('prw1', [NT, 1024])
    for nm in ('V2', 'KKA', 'KD', 'BON', 'Y2'):
        scratch(nm, [2, NT, 256])
    scratch('GATE', [NT, 256])
    scratch('AKK', [128, NT, 8])
    scratch('AR', [128, NT, 8])
    scratch('WD', [128, NT, 4])
    for tag, ne in (('f', 1), ('m', NE)):
        scratch('wgb' + tag, [ne, NFF, 128, 8, 128], BF16, tiled=False)
        scratch('wub' + tag, [ne, NFF, 128, 8, 128], BF16, tiled=False)
        scratch('wdb' + tag, [ne, NFF, 128, D], BF16, tiled=False)
    I['rope_g'] = nc.dram_tensor('rope_g', [NL, 2, 32], F32, kind='ExternalInput').ap()
    I['rope_m'] = nc.dram_tensor('rope_m', [NL, 2, 16], F32, kind='ExternalInput').ap()
    I['nat_tab'] = nc.dram_tensor('nat_tab', [2, 128, 4, len(nat_plan()[1]), 64], F32, kind='ExternalInput').ap()
    with ExitStack() as st:
        P.alloc_sems(st)
        ident, identd = sb(st, 'ident', [128, 128], BF16)
        modL, modLd = sb(st, 'modL', [128, 6, D], F32)
        modC, modCd = sb(st, 'modC', [128, 6, D], F32)
        ident32, ident32d = sb(st, 'ident32', [128, 128], F32)
        J32, J32d = sb(st, 'J32', [128, 128], F32)
        P.op('pool', lambda e: e.memset(ident[:], 1.0), [], [identd])
        P.op('pool', lambda e: e.memset(ident32[:], 1.0), [], [ident32d])
        P.op('pool', lambda e: e.memset(J32[:], 1.0), [], [J32d])
        P.op('pool', lambda e: e.affine_select(out=ident32[:], in_=ident32[:], pattern=[[-1, 128]], compare_op=ALU.is_equal, fill=0.0, base=0, channel_multiplier=1),
             [ident32d], [ident32d])
        P.op('pool', lambda e: e.affine_select(out=ident[:], in_=ident[:], pattern=[[-1, 128]], compare_op=ALU.is_equal, fill=0.0, base=0, channel_multiplier=1),
             [identd], [identd])
        P.op('pool', lambda e: e.affine_select(out=J32[:], in_=J32[:], pattern=[[1, 128]], compare_op=ALU.is_equal, fill=0.0, base=-127, channel_multiplier=1),
             [J32d], [J32d])
        P.dma('sp', lambda e: e.dma_start(out=S['xs'][0:NC_, :], in_=I['ctx'][:, :]), [], S['xs_d'][0:2])
        for j in range(4):
            P.dma('sp', lambda e, j=j: e.dma_start(out=S['xs'][NC_ + j * 1024:NC_ + (j + 1) * 1024, :], in_=I['x'][j * 1024:(j + 1) * 1024, :]),
                  [], S['xs_d'][2 + 8 * j:2 + 8 * (j + 1)])

        def want(name):
            return only is None or name in only

        for l in layers:
            need_ctx = (l == 0)
            if want('mod'):
                phase_mod(l, I, modL, modLd, modC, modCd)
            if want('in'):
                phase_in(l, I, S, modL, modLd, modC, modCd, ident, identd)
            if want('rwkv'):
                if scan_T is None:
                    phase_rwkv(l, I, S, need_ctx, ident32, ident32d, J32, J32d)
                else:
                    rwkv_prep(l, I, S, ident32, ident32d, J32, J32d)
                    rwkv_scan(S, scan_T)
            if want('gqa'):
                phase_gqa(l, I, S, need_ctx, ident, identd, ident32, ident32d)
            if want('mla'):
                phase_mla(l, I, S, need_ctx, ident, identd, ident32, ident32d)
            if want('nat'):
                phase_nat(l, I, S, need_ctx, ident, identd, ident32, ident32d)
            if stop_after == ('attn', l):
                break
            if want('out'):
                phase_out(l, I, S, need_ctx, modL, modLd, modC, modCd, ident, identd)
            if stop_after == ('out', l):
                break
            if want('ffn'):
                phase_ffn(l, I, S, need_ctx, modL, modLd, modC, modCd, ident32, ident32d, out, out_d)
        for name in debug:
            src = S[name]
            d = nc.dram_tensor('dbg_' + name, list(src.shape), src.dtype, kind="ExternalOutput").ap()
            dd = Dep('dbg_' + name)
            deps = S[name + '_d'] if isinstance(S[name + '_d'], list) else [S[name + '_d']]
            P.dma('sp', lambda e, d=d, src=src: e.dma_start(out=d, in_=src), deps, [dd])
        P.barrier()
        P.emit()
    return nc


def core_shapes(inputs):
    sh = {k: tuple(np.asarray(v).shape) for k, v in inputs.items()}
    sh['x'] = (NL, D)
    sh['c'] = (D,)
    sh['ctx'] = (NC_, D)
    return sh


def rope_tables(rot_dim):
    t = np.arange(NL)
    row = (t // 64).astype(np.float32)
    col = (t % 64).astype(np.float32)
    quarter = rot_dim // 4
    inv_freq = (np.float32(10000.0) ** (-np.arange(quarter, dtype=np.float32) / np.float32(quarter))).astype(np.float32)
    ang = np.concatenate([row[:, None] * inv_freq, col[:, None] * inv_freq], axis=-1).astype(np.float32)
    return np.ascontiguousarray(np.stack([np.cos(ang), np.sin(ang)], axis=1).astype(np.float32))


def core_inputs(inputs, b, shared=None):
    if shared is None:
        shared = {k: np.ascontiguousarray(np.asarray(v, dtype=np.float32)) for k, v in inputs.items() if k not in ('x', 'c', 'ctx')}
        shared['rope_g'] = rope_tables(64)
        shared['rope_m'] = rope_tables(32)
        nb = np.asarray(inputs['nat_bias'], np.float32)
        shared['nat_tab'] = np.ascontiguousarray(np.stack([nat_bias_table(nb[0]), nat_bias_table(nb[1])], 0))
    m = dict(shared)
    m['x'] = np.ascontiguousarray(np.asarray(inputs['x'][b], dtype=np.float32))
    m['c'] = np.ascontiguousarray(np.asarray(inputs['c'][b], dtype=np.float32))
    m['ctx'] = np.ascontiguousarray(np.asarray(inputs['ctx'][b], dtype=np.float32))
    return m


def kernel(**inputs):
    nb = 4
    shapes = core_shapes(inputs)
    nc = build(shapes)
    first = core_inputs(inputs, 0)
    shared = {k: v for k, v in first.items() if k not in ('x', 'c', 'ctx')}
    per_b = [first] + [core_inputs(inputs, b, shared) for b in range(1, nb)]
    maps = []
    for b in range(nb):
        for m in range(2):
            mm_ = dict(per_b[b])
            hs = np.zeros((128, 2), np.float32)
            hs[:, m] = 1.0
            mm_['halfsel'] = hs
            maps.append(mm_)
    res = run_bass_kernel_spmd(nc, maps, core_ids=list(range(2 * nb)))
    out = np.empty((nb, NL, D), np.float32)
    for b in range(nb):
        for m in range(2):
            out[b, m * (NL // 2):(m + 1) * (NL // 2)] = np.asarray(res.results[2 * b + m]['out'], dtype=np.float32)
    return out
```

```python
import numpy as np
from contextlib import ExitStack
import concourse.bass as bass
import concourse.mybir as mybir
from concourse.bass_utils import run_bass_kernel_spmd

F32 = mybir.dt.float32
BF16 = mybir.dt.bfloat16
AF = mybir.ActivationFunctionType
ALU = mybir.AluOpType
AX = mybir.AxisListType

COMPUTE = ('pe', 'act', 'dve', 'pool')
QUEUES = ('pe', 'act', 'dve', 'pool', 'sp')
NRING = 8

D = 1024
NL = 4096
NC_ = 256
NT = NL + NC_
NTILE = NT // 128
INC = 2720
RW0, GQ0, ML0, NA0 = 0, 1024, 1536, 1952
DFF = 2816
NFF = DFF // 128
NE = 8
RMS_EPS = 1e-6


class Dep:
    __slots__ = ('w', 'r', 'name')

    def __init__(self, name=''):
        self.w = None
        self.r = {}
        self.name = name


class Prog:
    def __init__(self, nc):
        self.nc = nc
        self.q = {e: [] for e in QUEUES}
        self.cnt = {e: 0 for e in COMPUTE}
        self.seen = {e: {} for e in QUEUES}
        self.sems = {}
        self.dma_cnt = {}
        self.dma_next = {e: 0 for e in QUEUES}
        self.ninstr = 0

    def alloc_sems(self, stack):
        for e in COMPUTE:
            self.sems[e] = stack.enter_context(self.nc.semaphore('s_' + e))
        for qn in ('sp', 'pool', 'act'):
            for j in range(NRING):
                key = ('dma', qn, j)
                self.sems[key] = stack.enter_context(self.nc.semaphore('d_%s_%d' % (qn, j)))
                self.dma_cnt[key] = 0

    def _need(self, queue, key, count, waits):
        if self.seen[queue].get(key, 0) >= count:
            return
        if waits.get(key, 0) < count:
            waits[key] = count

    def _emit_waits(self, queue, waits):
        for key, count in waits.items():
            sem = self.sems[key]
            val = count * (16 if isinstance(key, tuple) else 1)
            self.q[queue].append(lambda e, sem=sem, val=val: e.wait_ge(sem, val))
            self.seen[queue][key] = count
            self.ninstr += 1

    def _collect(self, queue, reads, writes):
        waits = {}
        for t in reads:
            if t.w is not None:
                self._need(queue, t.w[0], t.w[1], waits)
        for t in writes:
            if t.w is not None:
                if not (queue == 'pe' and t.w[0] == 'pe'):
                    self._need(queue, t.w[0], t.w[1], waits)
            for k, c in t.r.items():
                if k == queue:
                    continue
                self._need(queue, k, c, waits)
        return waits

    def op(self, queue, fn, reads=(), writes=()):
        waits = self._collect(queue, reads, writes)
        self._emit_waits(queue, waits)
        self.cnt[queue] += 1
        c = self.cnt[queue]
        sem = self.sems[queue]
        self.q[queue].append(lambda e, fn=fn, sem=sem: fn(e).then_inc(sem, 1))
        self.ninstr += 1
        for t in reads:
            if t.r.get(queue, 0) < c:
                t.r[queue] = c
        for t in writes:
            t.w = (queue, c)
            t.r = {}

    def dma(self, queue, fn, reads=(), writes=()):
        j = self.dma_next[queue]
        self.dma_next[queue] = (j + 1) % NRING
        key = ('dma', queue, j)
        waits = self._collect(queue, reads, writes)
        n = self.dma_cnt[key]
        if n > 0:
            self._need(queue, key, n, waits)
        self._emit_waits(queue, waits)
        self.dma_cnt[key] = n + 1
        sem = self.sems[key]
        self.q[queue].append(lambda e, fn=fn, sem=sem: fn(e).then_inc(sem, 16))
        self.ninstr += 1
        for t in reads:
            t.r[key] = n + 1
        for t in writes:
            t.w = (key, n + 1)
            t.r = {}

    def barrier(self, queues=QUEUES):
        for qn in queues:
            waits = {}
            for e in COMPUTE:
                if self.cnt[e] > 0 and e != qn:
                    self._need(qn, e, self.cnt[e], waits)
            for key, n in self.dma_cnt.items():
                if n > 0:
                    self._need(qn, key, n, waits)
            self._emit_waits(qn, waits)

    def emit(self):
        nc = self.nc
        with nc.Block() as block:
            @block.tensor
            def _(e):
                for f in self.q['pe']:
                    f(e)

            @block.scalar
            def _(e):
                for f in self.q['act']:
                    f(e)

            @block.vector
            def _(e):
                for f in self.q['dve']:
                    f(e)

            @block.gpsimd
            def _(e):
                for f in self.q['pool']:
                    f(e)

            @block.sync
            def _(e):
                for f in self.q['sp']:
                    f(e)


class Ring:
    def __init__(self, K, st, name, shape, dtype, n, psum=False):
        self.bufs = []
        for i in range(n):
            if psum:
                full = [128, 512] if dtype == F32 else [128, 1024]
                n = 1
                for d_ in shape[1:]:
                    n *= d_
                assert n <= full[1]
                t = st.enter_context(K.nc.psum_tensor(uname('%s%d' % (name, i)), full, dtype))
                t = t[0:shape[0], 0:n]
                if len(shape) == 3:
                    t = t.rearrange("p (a b) -> p a b", b=shape[2])
            else:
                t = st.enter_context(K.nc.sbuf_tensor(uname('%s%d' % (name, i)), shape, dtype))
            self.bufs.append((t, Dep('%s%d' % (name, i))))
        self.i = 0

    def next(self):
        b = self.bufs[self.i]
        self.i = (self.i + 1) % len(self.bufs)
        return b


class K:
    LVL = 99
    UID = 0
    CONVERTED = {}
    PENDING = []


def uname(name):
    K.UID += 1
    return '%s_u%d' % (name, K.UID)


def sb(st, name, shape, dtype):
    return st.enter_context(K.nc.sbuf_tensor(uname(name), shape, dtype)), Dep(name)


def ps(st, name, shape, dtype):
    return st.enter_context(K.nc.psum_tensor(uname(name), shape, dtype)), Dep(name)


def dram(name, shape, dtype):
    return K.nc.dram_tensor(name, shape, dtype, kind="Internal").ap()


def rms_rstd(x_ap, xdep, n, sq, sqd, ss, ssd, col):
    P = K.P
    P.op('dve', lambda e: e.tensor_tensor(out=sq[:, 0:n], in0=x_ap, in1=x_ap, op=ALU.mult), [xdep], [sqd])
    P.op('dve', lambda e: e.tensor_reduce(out=ss[:, col:col + 1], in_=sq[:, 0:n], axis=AX.X, op=ALU.add), [sqd], [ssd])
    P.op('act', lambda e: e.activation(out=ss[:, col:col + 1], in_=ss[:, col:col + 1], func=AF.Sqrt, bias=RMS_EPS, scale=1.0 / n), [ssd], [ssd])
    P.op('dve', lambda e: e.reciprocal(out=ss[:, col:col + 1], in_=ss[:, col:col + 1]), [ssd], [ssd])


def phase_mod(l, I, modL, modLd, modC, modCd):
    nc, P = K.nc, K.P
    with ExitStack() as st:
        cv, cvd = sb(st, 'cv', [128, 2, 8], F32)
        cs, csd = sb(st, 'cs', [128, 2, 8], F32)
        crep, crepd = sb(st, 'crep', [128, 2, 8, 128], BF16)
        gb, gbd = sb(st, 'gb', [128, 4, D], F32)
        wst = Ring(K, st, 'mw_st', [128, 8, 512], F32, 2)
        wbf = Ring(K, st, 'mw_bf', [128, 8, 512], BF16, 2)
        bb = Ring(K, st, 'mbb', [128, 512], F32, 2)
        pm = Ring(K, st, 'pmod', [128, 512], F32, 4, psum=True)
        P.dma('sp', lambda e: e.dma_start(out=cv[:, 0, :], in_=I['c'].rearrange("(k p) -> p k", p=128), allow_slow_non_contiguous=True), [], [cvd])
        P.dma('sp', lambda e: e.dma_start(out=cv[:, 1, :], in_=I['c_ctx'].rearrange("(k p) -> p k", p=128), allow_slow_non_contiguous=True), [], [cvd])
        P.dma('sp', lambda e: e.dma_start(out=gb[:], in_=I['norm_g'][l].partition_broadcast(128)), [], [gbd])
        P.op('act', lambda e: e.activation(out=cs[:], in_=cv[:], func=AF.Silu), [cvd], [csd])
        P.op('dve', lambda e: e.tensor_copy(out=crep[:], in_=cs[:].unsqueeze(3).to_broadcast([128, 2, 8, 128])), [csd], [crepd])
        mw = I['mod_w'][l].rearrange("(k p) n -> p k n", p=128)
        for nb in range(12):
            (ws, wsd) = wst.next()
            (wb, wbd) = wbf.next()
            (bt, btd) = bb.next()
            P.dma('sp', lambda e, ws=ws, nb=nb: e.dma_start(out=ws[:], in_=mw[:, :, nb * 512:(nb + 1) * 512]), [], [wsd])
            P.dma('sp', lambda e, bt=bt, nb=nb: e.dma_start(out=bt[:], in_=I['mod_b'][l, nb * 512:(nb + 1) * 512].partition_broadcast(128)), [], [btd])
            P.op('pool', lambda e, ws=ws, wb=wb: e.tensor_copy(out=wb[:], in_=ws[:]), [wsd], [wbd])
            j, off = nb // 2, (nb % 2) * 512
            for s, (mt, mtd) in enumerate(((modL, modLd), (modC, modCd))):
                (pt, ptd) = pm.next()
                for kc in range(8):
                    P.op('pe', lambda e, pt=pt, wb=wb, kc=kc, s=s: e.matmul(pt[:], lhsT=crep[:, s, kc, :], rhs=wb[:, kc, :], start=(kc == 0), stop=(kc == 7)),
                         [crepd, wbd], [ptd])
                P.op('dve', lambda e, pt=pt, bt=bt, mt=mt, j=j, off=off: e.tensor_tensor(out=mt[:, j, off:off + 512], in0=pt[:], in1=bt[:], op=ALU.add),
                     [ptd, btd], [mtd])
        for (mt, mtd) in ((modL, modLd), (modC, modCd)):
            for j, gi, plus1 in ((1, 0, True), (2, 1, False), (4, 2, True), (5, 3, False)):
                if plus1:
                    P.op('dve', lambda e, mt=mt, j=j, gi=gi: e.scalar_tensor_tensor(out=mt[:, j, :], in0=mt[:, j, :], scalar=1.0, in1=gb[:, gi, :], op0=ALU.add, op1=ALU.mult),
                         [mtd, gbd], [mtd])
                else:
                    P.op('dve', lambda e, mt=mt, j=j, gi=gi: e.tensor_tensor(out=mt[:, j, :], in0=mt[:, j, :], in1=gb[:, gi, :], op=ALU.mult),
                         [mtd, gbd], [mtd])
        P.barrier()


def phase_in(l, I, S, modL, modLd, modC, modCd, ident, identd):
    nc, P = K.nc, K.P
    with ExitStack() as st:
        wbf, wbfd = sb(st, 'win_bf', [128, 8, INC], BF16)
        wst = Ring(K, st, 'win_st', [128, INC], F32, 2)
        xr = Ring(K, st, 'in_x', [128, D], F32, 3)
        hr = Ring(K, st, 'in_h', [128, D], F32, 2)
        hbr = Ring(K, st, 'in_hb', [128, D], BF16, 2)
        hTr = Ring(K, st, 'in_hT', [128, 8, 128], BF16, 2)
        sq, sqd = sb(st, 'in_sq', [128, D], F32)
        ssr = Ring(K, st, 'in_ss', [128, 1], F32, 4)
        pr = Ring(K, st, 'in_p', [128, INC], F32, 2)
        ptr = Ring(K, st, 'in_pT', [128, 8, 128], BF16, 2, psum=True)
        pmr = Ring(K, st, 'in_pm', [128, 512], F32, 4, psum=True)
        win = I['w_in'][l].rearrange("(k p) n -> p k n", p=128)
        for kc in range(8):
            (ws, wsd) = wst.next()
            P.dma('sp', lambda e, ws=ws, kc=kc: e.dma_start(out=ws[:], in_=win[:, kc, :]), [], [wsd])
            P.op('pool', lambda e, ws=ws, kc=kc: e.tensor_copy(out=wbf[:, kc, :], in_=ws[:]), [wsd], [wbfd])
        for t in range(NTILE):
            mt, mtd = (modC, modCd) if t < 2 else (modL, modLd)
            (x, xd) = xr.next()
            (h, hd) = hr.next()
            (hb, hbd) = hbr.next()
            (hT, hTd) = hTr.next()
            (ss, ssd) = ssr.next()
            (pt, ptd) = pr.next()
            (pT, pTd) = ptr.next()
            P.dma('sp', lambda e, x=x, t=t: e.dma_start(out=x[:], in_=S['xs'][t * 128:(t + 1) * 128, :]), [S['xs_d'][t]], [xd])
            rms_rstd(x[:], xd, D, sq, sqd, ss, ssd, 0)
            P.op('dve', lambda e, h=h, x=x, ss=ss, mt=mt: e.scalar_tensor_tensor(out=h[:], in0=x[:], scalar=ss[:, 0:1], in1=mt[:, 1, :], op0=ALU.mult, op1=ALU.mult),
                 [xd, ssd, mtd], [hd])
            P.op('pool', lambda e, h=h, hb=hb, mt=mt: e.tensor_tensor(out=hb[:], in0=h[:], in1=mt[:, 0, :], op=ALU.add), [hd, mtd], [hbd])
            for kc in range(8):
                P.op('pe', lambda e, pT=pT, hb=hb, kc=kc: e.transpose(out=pT[:, kc, :], in_=hb[:, kc * 128:(kc + 1) * 128], identity=ident[:]),
                     [hbd, identd], [pTd])
            P.op('act', lambda e, hT=hT, pT=pT: e.copy(out=hT[:], in_=pT[:]), [pTd], [hTd])
            for nb in range(6):
                c0 = nb * 512
                cw = min(512, INC - c0)
                (pm, pmd) = pmr.next()
                for kc in range(8):
                    P.op('pe', lambda e, pm=pm, hT=hT, kc=kc, c0=c0, cw=cw: e.matmul(pm[:, 0:cw], lhsT=hT[:, kc, :], rhs=wbf[:, kc, c0:c0 + cw], start=(kc == 0), stop=(kc == 7)),
                         [hTd, wbfd], [pmd])
                if nb % 2 == 0:
                    P.op('act', lambda e, pm=pm, pt=pt, c0=c0, cw=cw: e.copy(out=pt[:, c0:c0 + cw], in_=pm[:, 0:cw]), [pmd], [ptd])
                else:
                    P.op('dve', lambda e, pm=pm, pt=pt, c0=c0, cw=cw: e.tensor_copy(out=pt[:, c0:c0 + cw], in_=pm[:, 0:cw]), [pmd], [ptd])
            P.dma('sp', lambda e, pt=pt, t=t: e.dma_start(out=S['p'][t * 128:(t + 1) * 128, :], in_=pt[:]), [ptd], [S['p_d'][t]])
        P.barrier()


def attn_finalize(st_rings, O, Od, h, ot, otd, nq, ident32, ident32d):
    P = K.P
    osbr, ptr, rvr = st_rings
    (osb, osbd) = osbr.next()
    P.op('dve', lambda e: e.tensor_copy(out=osb[:, 0:nq], in_=O[:, 0:nq]), [Od], [osbd])
    for j in range(nq // 128):
        (pt, ptd) = ptr.next()
        (rv, rvd) = rvr.next()
        P.op('pe', lambda e, pt=pt, osb=osb, j=j: e.transpose(out=pt[:], in_=osb[:, j * 128:(j + 1) * 128], identity=ident32[0:65, 0:65]),
             [osbd, ident32d], [ptd])
        P.op('dve', lambda e, pt=pt, rv=rv: e.reciprocal(out=rv[:], in_=pt[:, 64:65]), [ptd], [rvd])
        P.op('dve', lambda e, pt=pt, rv=rv, j=j: e.tensor_scalar(out=ot[:, j, h * 64:(h + 1) * 64], in0=pt[:, 0:64], scalar1=rv[:, 0:1], scalar2=None, op0=ALU.mult),
             [ptd, rvd], [otd])


def attn_core(st, S, QT, QTd, KT, KTd, kvmap, Vaug, Vaugd, scale, col0, need_ctx, ident32, ident32d, tag):
    P = K.P
    sr = Ring(K, st, tag + '_S', [128, 512], F32, 3, psum=True)
    orr = Ring(K, st, tag + '_O', [65, 512], F32, 2, psum=True)
    ptr = Ring(K, st, tag + '_fT', [128, 65], F32, 2, psum=True)
    pr = Ring(K, st, tag + '_P', [128, 512], BF16, 3)
    osbr = Ring(K, st, tag + '_osb', [65, 512], F32, 2)
    rvr = Ring(K, st, tag + '_rv', [128, 1], F32, 4)
    otr = Ring(K, st, tag + '_ot', [128, 4, 256], F32, 2)
    blocks = []
    if need_ctx:
        blocks.append((0, 256, [0, 1]))
    for qb in range(8):
        blocks.append((256 + qb * 512, 512, list(range(NTILE))))
    for (q0, nq, kts) in blocks:
        (ot, otd) = otr.next()
        for h in range(4):
            g = kvmap[h]
            (O, Od) = orr.next()
            pend = None
            for i, kt in enumerate(kts):
                (Sp, Spd) = sr.next()
                (Pt, Ptd) = pr.next()
                P.op('pe', lambda e, Sp=Sp, g=g, h=h, kt=kt, q0=q0, nq=nq: e.matmul(Sp[:, 0:nq], lhsT=KT(g)[:, kt * 128:(kt + 1) * 128], rhs=QT(h)[:, q0:q0 + nq], start=True, stop=True),
                     [QTd, KTd], [Spd])
                P.op('act', lambda e, Sp=Sp, Pt=Pt, nq=nq: e.activation(out=Pt[:, 0:nq], in_=Sp[:, 0:nq], func=AF.Exp, scale=scale), [Spd], [Ptd])
                if pend is not None:
                    pend()

                def pend(O=O, Od=Od, g=g, kt=kt, Pt=Pt, Ptd=Ptd, nq=nq, i=i, n=len(kts)):
                    P.op('pe', lambda e: e.matmul(O[:, 0:nq], lhsT=Vaug[:, kt, g, :], rhs=Pt[:, 0:nq], start=(i == 0), stop=(i == n - 1)),
                         [Vaugd, Ptd], [Od])
            pend()
            attn_finalize((osbr, ptr, rvr), O, Od, h, ot, otd, nq, ident32, ident32d)
        nj = nq // 128
        P.dma('sp', lambda e, ot=ot, q0=q0, nj=nj: e.dma_start(out=S['ocat'][q0:q0 + nj * 128, col0:col0 + 256].rearrange("(j p) c -> p j c", p=128), in_=ot[:, 0:nj, :]),
              [otd], [S['ocat_d'][t] for t in range(q0 // 128, q0 // 128 + nj)])


def phase_gqa(l, I, S, need_ctx, ident, identd, ident32, ident32d):
    nc, P = K.nc, K.P
    with ExitStack() as st:
        QKT, QKTd = sb(st, 'gq_QKT', [64, 6, NT], BF16)
        Vaug, Vaugd = sb(st, 'gq_V', [128, NTILE, 2, 65], BF16)
        with ExitStack() as st2:
            gain, gaind = sb(st2, 'gq_gain', [128, 6, 64], F32)
            xr = Ring(K, st2, 'gq_x', [128, 512], F32, 3)
            sq, sqd = sb(st2, 'gq_sq', [128, 384], F32)
            ssr = Ring(K, st2, 'gq_ss', [128, 6], F32, 3)
            qnr = Ring(K, st2, 'gq_qn', [128, 6, 64], F32, 2)
            qbr = Ring(K, st2, 'gq_qb', [128, 6, 64], BF16, 2)
            csr = Ring(K, st2, 'gq_cs', [128, 2, 32], F32, 3)
            t1r = Ring(K, st2, 'gq_t1', [128, 6, 32], F32, 2)
            t2r = Ring(K, st2, 'gq_t2', [128, 6, 32], F32, 2)
            pTr = Ring(K, st2, 'gq_pT', [64, 6, 128], BF16, 2, psum=True)
            P.dma('sp', lambda e: e.dma_start(out=gain[:, 0:4, :], in_=I['gqa_q_g'][l:l + 1, :].partition_broadcast(128).to_broadcast([128, 4, 64])), [], [gaind])
            P.dma('sp', lambda e: e.dma_start(out=gain[:, 4:6, :], in_=I['gqa_k_g'][l:l + 1, :].partition_broadcast(128).to_broadcast([128, 2, 64])), [], [gaind])
            P.op('pool', lambda e: e.memset(Vaug[:, :, :, 64:65], 1.0), [], [Vaugd])
            for t in range(NTILE):
                (x, xd) = xr.next()
                (ss, ssd) = ssr.next()
                (qn, qnd) = qnr.next()
                (qb, qbd) = qbr.next()
                (pT, pTd) = pTr.next()
                P.dma('sp', lambda e, x=x, t=t: e.dma_start(out=x[:], in_=S['p'][t * 128:(t + 1) * 128, GQ0:GQ0 + 512]), [S['p_d'][t]], [xd])
                P.op('dve', lambda e, x=x: e.tensor_tensor(out=sq[:], in0=x[:, 0:384], in1=x[:, 0:384], op=ALU.mult), [xd], [sqd])
                P.op('dve', lambda e, ss=ss: e.tensor_reduce(out=ss[:], in_=sq[:].rearrange("p (g d) -> p g d", d=64), axis=AX.X, op=ALU.add), [sqd], [ssd])
                P.op('act', lambda e, ss=ss: e.activation(out=ss[:], in_=ss[:], func=AF.Sqrt, bias=RMS_EPS, scale=1.0 / 64), [ssd], [ssd])
                P.op('dve', lambda e, ss=ss: e.reciprocal(out=ss[:], in_=ss[:]), [ssd], [ssd])
                P.op('dve', lambda e, x=x, qn=qn, ss=ss: e.tensor_tensor(out=qn[:], in0=x[:, 0:384].rearrange("p (g d) -> p g d", d=64), in1=ss[:].unsqueeze(2).to_broadcast([128, 6, 64]), op=ALU.mult),
                     [xd, ssd], [qnd])
                P.op('pool', lambda e, x=x, t=t: e.tensor_copy(out=Vaug[:, t, :, 0:64], in_=x[:, 384:512].rearrange("p (g d) -> p g d", d=64)), [xd], [Vaugd])
                if t < 2:
                    P.op('dve', lambda e, qn=qn, qb=qb: e.tensor_tensor(out=qb[:], in0=qn[:], in1=gain[:], op=ALU.mult), [qnd, gaind], [qbd])
                else:
                    (cs, csd) = csr.next()
                    (t1, t1d) = t1r.next()
                    (t2, t2d) = t2r.next()
                    r0 = (t - 2) * 128
                    P.dma('sp', lambda e, cs=cs, r0=r0: e.dma_start(out=cs[:], in_=I['rope_g'][r0:r0 + 128, :, :]), [], [csd])
                    P.op('dve', lambda e, qn=qn: e.tensor_tensor(out=qn[:], in0=qn[:], in1=gain[:], op=ALU.mult), [qnd, gaind], [qnd])
                    cosb = lambda cs=cs: cs[:, 0, :].unsqueeze(1).to_broadcast([128, 6, 32])
                    sinb = lambda cs=cs: cs[:, 1, :].unsqueeze(1).to_broadcast([128, 6, 32])
                    P.op('dve', lambda e, t1=t1, qn=qn, cosb=cosb: e.tensor_tensor(out=t1[:], in0=qn[:, :, 0:32], in1=cosb(), op=ALU.mult), [qnd, csd], [t1d])
                    P.op('pool', lambda e, t2=t2, qn=qn, sinb=sinb: e.tensor_tensor(out=t2[:], in0=qn[:, :, 32:64], in1=sinb(), op=ALU.mult), [qnd, csd], [t2d])
                    P.op('dve', lambda e, t1=t1, t2=t2, qb=qb: e.tensor_tensor(out=qb[:, :, 0:32], in0=t1[:], in1=t2[:], op=ALU.subtract), [t1d, t2d], [qbd])
                    P.op('dve', lambda e, t1=t1, qn=qn, sinb=sinb: e.tensor_tensor(out=t1[:], in0=qn[:, :, 0:32], in1=sinb(), op=ALU.mult), [qnd, csd], [t1d])
                    P.op('pool', lambda e, t2=t2, qn=qn, cosb=cosb: e.tensor_tensor(out=t2[:], in0=qn[:, :, 32:64], in1=cosb(), op=ALU.mult), [qnd, csd], [t2d])
                    P.op('dve', lambda e, t1=t1, t2=t2, qb=qb: e.tensor_tensor(out=qb[:, :, 32:64], in0=t1[:], in1=t2[:], op=ALU.add), [t1d, t2d], [qbd])
                for g in range(6):
                    P.op('pe', lambda e, pT=pT, qb=qb, g=g: e.transpose(out=pT[:, g, :], in_=qb[:, g, :], identity=ident[:]), [qbd, identd], [pTd])
                P.op('act', lambda e, pT=pT, t=t: e.copy(out=QKT[:, :, t * 128:(t + 1) * 128], in_=pT[:]), [pTd], [QKTd])
            P.barrier()
        attn_core(st, S, lambda h: QKT[:, h, :], QKTd, lambda g: QKT[:, 4 + g, :], QKTd, [0, 0, 1, 1], Vaug, Vaugd, 0.125, 256,
                  need_ctx, ident32, ident32d, 'gq')
        P.barrier()


def phase_mla(l, I, S, need_ctx, ident, identd, ident32, ident32d):
    nc, P = K.nc, K.P
    with ExitStack() as st:
        QKT, QKTd = sb(st, 'ml_QKT', [128, 8, NT], BF16)
        Vaug, Vaugd = sb(st, 'ml_V', [128, NTILE, 4, 65], BF16)
        with ExitStack() as st2:
            gain, gaind = sb(st2, 'ml_gain', [128, 384], F32)
            wst, wstd = sb(st2, 'ml_wst', [128, 2, 512], F32)
            wuq, wuqd = sb(st2, 'ml_wuq', [128, 2, 384], BF16)
            wukv, wukvd = sb(st2, 'ml_wukv', [128, 512], BF16)
            xr = Ring(K, st2, 'ml_x', [128, 416], F32, 3)
            sq, sqd = sb(st2, 'ml_sq', [128, 384], F32)
            ssr = Ring(K, st2, 'ml_ss', [128, 2], F32, 3)
            cnr = Ring(K, st2, 'ml_cn', [128, 384], F32, 2)
            cbr = Ring(K, st2, 'ml_cb', [128, 384], BF16, 2)
            cTr = Ring(K, st2, 'ml_cT', [128, 3, 128], BF16, 2)
            qsr = Ring(K, st2, 'ml_qs', [128, 4, 96], F32, 2)
            csr = Ring(K, st2, 'ml_cs', [128, 2, 16], F32, 3)
            t1r = Ring(K, st2, 'ml_t1', [128, 5, 16], F32, 2)
            t2r = Ring(K, st2, 'ml_t2', [128, 5, 16], F32, 2)
            rr = Ring(K, st2, 'ml_r', [128, 5, 32], F32, 2)
            qkr = Ring(K, st2, 'ml_qk', [128, 8, 128], BF16, 2)
            for (qk_, qkd_) in qkr.bufs:
                P.op('pool', lambda e, qk_=qk_: e.memset(qk_[:], 0.0), [], [qkd_])
            pcT = Ring(K, st2, 'ml_pcT', [128, 3, 128], BF16, 2, psum=True)
            pq = Ring(K, st2, 'ml_pq', [128, 384], F32, 1, psum=True)
            pkv = Ring(K, st2, 'ml_pkv', [128, 512], F32, 2, psum=True)
            pT2 = Ring(K, st2, 'ml_pT2', [128, 8, 128], BF16, 2, psum=True)
            P.dma('sp', lambda e: e.dma_start(out=gain[:, 0:256], in_=I['mla_q_norm_g'][l:l + 1, :].partition_broadcast(128)), [], [gaind])
            P.dma('sp', lambda e: e.dma_start(out=gain[:, 256:384], in_=I['mla_kv_norm_g'][l:l + 1, :].partition_broadcast(128)), [], [gaind])
            P.dma('sp', lambda e: e.dma_start(out=wst[:, :, 0:384], in_=I['mla_w_uq'][l].rearrange("(k p) n -> p k n", p=128)), [], [wstd])
            P.op('pool', lambda e: e.tensor_copy(out=wuq[:], in_=wst[:, :, 0:384]), [wstd], [wuqd])
            P.dma('sp', lambda e: e.dma_start(out=wst[:, 0, :], in_=I['mla_w_ukv'][l]), [wuqd], [wstd])
            P.op('pool', lambda e: e.tensor_copy(out=wukv[:], in_=wst[:, 0, :]), [wstd], [wukvd])
            P.op('pool', lambda e: e.memset(Vaug[:, :, :, 64:65], 1.0), [], [Vaugd])
            for t in range(NTILE):
                (x, xd) = xr.next()
                (ss, ssd) = ssr.next()
                (cn, cnd) = cnr.next()
                (cb, cbd) = cbr.next()
                (cT, cTd) = cTr.next()
                (qs, qsd) = qsr.next()
                (qk, qkd) = qkr.next()
                (r, rd) = rr.next()
                (pc, pcd) = pcT.next()
                (pqt, pqd) = pq.next()
                (pk, pkd) = pkv.next()
                (pT, pTd) = pT2.next()
                P.dma('sp', lambda e, x=x, t=t: e.dma_start(out=x[:], in_=S['p'][t * 128:(t + 1) * 128, ML0:ML0 + 416]), [S['p_d'][t]], [xd])
                if K.LVL < 2:
                    continue
                P.op('dve', lambda e, x=x: e.tensor_tensor(out=sq[:], in0=x[:, 0:384], in1=x[:, 0:384], op=ALU.mult), [xd], [sqd])
                P.op('dve', lambda e, ss=ss: e.tensor_reduce(out=ss[:, 0:1], in_=sq[:, 0:256], axis=AX.X, op=ALU.add), [sqd], [ssd])
                P.op('dve', lambda e, ss=ss: e.tensor_reduce(out=ss[:, 1:2], in_=sq[:, 256:384], axis=AX.X, op=ALU.add), [sqd], [ssd])
                P.op('act', lambda e, ss=ss: e.activation(out=ss[:, 0:1], in_=ss[:, 0:1], func=AF.Sqrt, bias=RMS_EPS, scale=1.0 / 256), [ssd], [ssd])
                P.op('act', lambda e, ss=ss: e.activation(out=ss[:, 1:2], in_=ss[:, 1:2], func=AF.Sqrt, bias=RMS_EPS, scale=1.0 / 128), [ssd], [ssd])
                P.op('dve', lambda e, ss=ss: e.reciprocal(out=ss[:], in_=ss[:]), [ssd], [ssd])
                P.op('dve', lambda e, x=x, cn=cn, ss=ss: e.scalar_tensor_tensor(out=cn[:, 0:256], in0=x[:, 0:256], scalar=ss[:, 0:1], in1=gain[:, 0:256], op0=ALU.mult, op1=ALU.mult),
                     [xd, ssd, gaind], [cnd])
                P.op('dve', lambda e, x=x, cn=cn, ss=ss: e.scalar_tensor_tensor(out=cn[:, 256:384], in0=x[:, 256:384], scalar=ss[:, 1:2], in1=gain[:, 256:384], op0=ALU.mult, op1=ALU.mult),
                     [xd, ssd, gaind], [cnd])
                P.op('pool', lambda e, cn=cn, cb=cb: e.tensor_copy(out=cb[:], in_=cn[:]), [cnd], [cbd])
                if K.LVL < 3:
                    continue
                for j in range(3):
                    P.op('pe', lambda e, pc=pc, cb=cb, j=j: e.transpose(out=pc[:, j, :], in_=cb[:, j * 128:(j + 1) * 128], identity=ident[:]), [cbd, identd], [pcd])
                if K.LVL < 2.3:
                    continue
                P.op('act', lambda e, cT=cT, pc=pc: e.copy(out=cT[:], in_=pc[:]), [pcd], [cTd])
                if K.LVL < 2.6:
                    continue
                for j in range(2):
                    P.op('pe', lambda e, pqt=pqt, cT=cT, j=j: e.matmul(pqt[:], lhsT=cT[:, j, :], rhs=wuq[:, j, :], start=(j == 0), stop=(j == 1)), [cTd, wuqd], [pqd])
                if K.LVL < 2.8:
                    continue
                for hf in range(2):
                    P.op('pe', lambda e, pk=pk, cT=cT, hf=hf: e.matmul(pk[:, hf * 256:(hf + 1) * 256], lhsT=cT[:, 2, :], rhs=wukv[:, hf * 256:(hf + 1) * 256], start=True, stop=True), [cTd, wukvd], [pkd])
                if K.LVL < 4:
                    continue
                P.op('act', lambda e, qs=qs, pqt=pqt: e.copy(out=qs[:], in_=pqt[:].rearrange("p (h d) -> p h d", d=96)), [pqd], [qsd])
                P.op('dve', lambda e, pk=pk, t=t: e.tensor_copy(out=Vaug[:, t, :, 0:64], in_=pk[:].rearrange("p (h d) -> p h d", d=128)[:, :, 64:128]), [pkd], [Vaugd])
                P.op('dve', lambda e, pk=pk, qk=qk: e.tensor_copy(out=qk[:, 4:8, 0:64], in_=pk[:].rearrange("p (h d) -> p h d", d=128)[:, :, 0:64]), [pkd], [qkd])
                P.op('pool', lambda e, qs=qs, qk=qk: e.tensor_copy(out=qk[:, 0:4, 0:64], in_=qs[:, :, 0:64]), [qsd], [qkd])
                P.op('pool', lambda e, r=r, qs=qs: e.tensor_copy(out=r[:, 0:4, :], in_=qs[:, :, 64:96]), [qsd], [rd])
                P.op('pool', lambda e, r=r, x=x: e.tensor_copy(out=r[:, 4, :], in_=x[:, 384:416]), [xd], [rd])
                if K.LVL < 5:
                    continue
                if t < 2:
                    P.op('dve', lambda e, r=r, qk=qk: e.tensor_copy(out=qk[:, 0:4, 64:96], in_=r[:, 0:4, :]), [rd], [qkd])
                    P.op('dve', lambda e, r=r, qk=qk: e.tensor_copy(out=qk[:, 4:8, 64:96], in_=r[:, 4, :].unsqueeze(1).to_broadcast([128, 4, 32])), [rd], [qkd])
                else:
                    (cs, csd) = csr.next()
                    (t1, t1d) = t1r.next()
                    (t2, t2d) = t2r.next()
                    r0 = (t - 2) * 128
                    P.dma('sp', lambda e, cs=cs, r0=r0: e.dma_start(out=cs[:], in_=I['rope_m'][r0:r0 + 128, :, :]), [], [csd])
                    cosb = lambda cs=cs: cs[:, 0, :].unsqueeze(1).to_broadcast([128, 5, 16])
                    sinb = lambda cs=cs: cs[:, 1, :].unsqueeze(1).to_broadcast([128, 5, 16])
                    P.op('dve', lambda e, t1=t1, r=r, cosb=cosb: e.tensor_tensor(out=t1[:], in0=r[:, :, 0:16], in1=cosb(), op=ALU.mult), [rd, csd], [t1d])
                    P.op('pool', lambda e, t2=t2, r=r, sinb=sinb: e.tensor_tensor(out=t2[:], in0=r[:, :, 16:32], in1=sinb(), op=ALU.mult), [rd, csd], [t2d])
                    P.op('dve', lambda e, t1=t1, t2=t2: e.tensor_tensor(out=t1[:], in0=t1[:], in1=t2[:], op=ALU.subtract), [t1d, t2d], [t1d])
                    P.op('dve', lambda e, t1=t1, qk=qk: e.tensor_copy(out=qk[:, 0:4, 64:80], in_=t1[:, 0:4, :]), [t1d], [qkd])
                    P.op('dve', lambda e, t1=t1, qk=qk: e.tensor_copy(out=qk[:, 4:8, 64:80], in_=t1[:, 4, :].unsqueeze(1).to_broadcast([128, 4, 16])), [t1d], [qkd])
                    (t1, t1d) = t1r.next()
                    (t2, t2d) = t2r.next()
                    P.op('dve', lambda e, t1=t1, r=r, sinb=sinb: e.tensor_tensor(out=t1[:], in0=r[:, :, 0:16], in1=sinb(), op=ALU.mult), [rd, csd], [t1d])
                    P.op('pool', lambda e, t2=t2, r=r, cosb=cosb: e.tensor_tensor(out=t2[:], in0=r[:, :, 16:32], in1=cosb(), op=ALU.mult), [rd, csd], [t2d])
                    P.op('dve', lambda e, t1=t1, t2=t2: e.tensor_tensor(out=t1[:], in0=t1[:], in1=t2[:], op=ALU.add), [t1d, t2d], [t1d])
                    P.op('dve', lambda e, t1=t1, qk=qk: e.tensor_copy(out=qk[:, 0:4, 80:96], in_=t1[:, 0:4, :]), [t1d], [qkd])
                    P.op('dve', lambda e, t1=t1, qk=qk: e.tensor_copy(out=qk[:, 4:8, 80:96], in_=t1[:, 4, :].unsqueeze(1).to_broadcast([128, 4, 16])), [t1d], [qkd])
                if K.LVL < 6:
                    continue
                for g in range(8):
                    P.op('pe', lambda e, pT=pT, qk=qk, g=g: e.transpose(out=pT[:, g, :], in_=qk[:, g, :], identity=ident[:]), [qkd, identd], [pTd])
                P.op('act', lambda e, pT=pT, t=t: e.copy(out=QKT[:, :, t * 128:(t + 1) * 128], in_=pT[:]), [pTd], [QKTd])
            P.barrier()
        if K.LVL < 7:
            return
        attn_core(st, S, lambda h: QKT[:, h, :], QKTd, lambda g: QKT[:, 4 + g, :], QKTd, [0, 1, 2, 3], Vaug, Vaugd, 96.0 ** -0.5, 512,
                  need_ctx, ident32, ident32d, 'ml')
        P.barrier()


BIG = 30000.0


def nat_plan():
    variants = {}
    plan = []
    for i in range(64):
        rs = min(max(i - 4, 0), 56)
        tiles = []
        for m in range(rs // 2, (rs + 7) // 2 + 1):
            dd = []
            for r in (2 * m, 2 * m + 1):
                dd.append(r - i + 7 if rs <= r < rs + 8 else -1)
            key = tuple(dd)
            if key not in variants:
                variants[key] = len(variants)
            tiles.append((2 + m, variants[key]))
        plan.append(tiles)
    vlist = [None] * len(variants)
    for k, v in variants.items():
        vlist[v] = k
    return plan, vlist


def nat_bias_table(nat_bias_l):
    plan, vlist = nat_plan()
    c = np.arange(64)
    cs = np.clip(c - 8, 0, 48)
    cp = np.arange(64)
    inwin = (cp[:, None] >= cs[None, :]) & (cp[:, None] < cs[None, :] + 16)
    off = np.clip(cp[:, None] - c[None, :] + 15, 0, 30)
    tab = np.full((128, 4, len(vlist), 64), -BIG, np.float32)
    for v, (d0, d1) in enumerate(vlist):
        for half, d in enumerate((d0, d1)):
            if d < 0:
                continue
            for h in range(4):
                vals = nat_bias_l[h, d][off]
                tab[half * 64:(half + 1) * 64, h, v, :] = np.where(inwin, vals, np.float32(-BIG))
    return tab


def phase_nat(l, I, S, need_ctx, ident, identd, ident32, ident32d):
    nc, P = K.nc, K.P
    plan, vlist = nat_plan()
    NV = len(vlist)
    with ExitStack() as st:
        QKT, QKTd = sb(st, 'na_QKT', [64, 8, NT], BF16)
        Vaug, Vaugd = sb(st, 'na_V', [128, NTILE, 4, 65], BF16)
        tb, tbd = sb(st, 'na_tb', [128, 4, NV, 64], BF16)
        with ExitStack() as st2:
            tbs, tbsd = sb(st2, 'na_tbs', [128, 4, NV, 64], F32)
            xr = Ring(K, st2, 'na_x', [128, 768], F32, 3)
            xbr = Ring(K, st2, 'na_xb', [128, 512], BF16, 2)
            pTr = Ring(K, st2, 'na_pT', [64, 8, 128], BF16, 2, psum=True)
            P.dma('sp', lambda e: e.dma_start(out=tbs[:], in_=I['nat_tab'][l]), [], [tbsd])
            P.op('dve', lambda e: e.tensor_scalar(out=tb[:], in0=tbs[:], scalar1=8.0, scalar2=None, op0=ALU.mult), [tbsd], [tbd])
            P.op('pool', lambda e: e.memset(Vaug[:, :, :, 64:65], 1.0), [], [Vaugd])
            for t in range(NTILE):
                (x, xd) = xr.next()
                (xb, xbd) = xbr.next()
                (pT, pTd) = pTr.next()
                P.dma('sp', lambda e, x=x, t=t: e.dma_start(out=x[:], in_=S['p'][t * 128:(t + 1) * 128, NA0:NA0 + 768]), [S['p_d'][t]], [xd])
                P.op('dve', lambda e, x=x, xb=xb: e.tensor_copy(out=xb[:], in_=x[:, 0:512]), [xd], [xbd])
                P.op('pool', lambda e, x=x, t=t: e.tensor_copy(out=Vaug[:, t, :, 0:64], in_=x[:, 512:768].rearrange("p (h d) -> p h d", d=64)), [xd], [Vaugd])
                for g in range(8):
                    P.op('pe', lambda e, pT=pT, xb=xb, g=g: e.transpose(out=pT[:, g, :], in_=xb[:, g * 64:(g + 1) * 64], identity=ident[:]), [xbd, identd], [pTd])
                P.op('act', lambda e, pT=pT, t=t: e.copy(out=QKT[:, :, t * 128:(t + 1) * 128], in_=pT[:]), [pTd], [QKTd])
            P.barrier()
        sr = Ring(K, st, 'na_S', [128, 512], F32, 3, psum=True)
        orr = Ring(K, st, 'na_O', [65, 512], F32, 2, psum=True)
        ptr = Ring(K, st, 'na_fT', [128, 65], F32, 2, psum=True)
        pr = Ring(K, st, 'na_P', [128, 512], BF16, 3)
        osbr = Ring(K, st, 'na_osb', [65, 512], F32, 2)
        rvr = Ring(K, st, 'na_rv', [128, 1], F32, 4)
        otr = Ring(K, st, 'na_ot', [128, 4, 256], F32, 2)
        blocks = []
        if need_ctx:
            blocks.append(None)
        for qb in range(8):
            blocks.append(qb)
        for qb in blocks:
            (ot, otd) = otr.next()
            if qb is None:
                q0, nq = 0, 256
            else:
                q0, nq = 256 + qb * 512, 512
            for h in range(4):
                (O, Od) = orr.next()
                if qb is None:
                    for i, kt in enumerate((0, 1)):
                        (Sp, Spd) = sr.next()
                        (Pt, Ptd) = pr.next()
                        P.op('pe', lambda e, Sp=Sp, h=h, kt=kt: e.matmul(Sp[:, 0:256], lhsT=QKT[:, 4 + h, kt * 128:(kt + 1) * 128], rhs=QKT[:, h, 0:256], start=True, stop=True),
                             [QKTd], [Spd])
                        P.op('act', lambda e, Sp=Sp, Pt=Pt: e.activation(out=Pt[:, 0:256], in_=Sp[:, 0:256], func=AF.Exp, scale=0.125), [Spd], [Ptd])
                        P.op('pe', lambda e, O=O, h=h, kt=kt, Pt=Pt, i=i: e.matmul(O[:, 0:256], lhsT=Vaug[:, kt, h, :], rhs=Pt[:, 0:256], start=(i == 0), stop=(i == 1)),
                             [Vaugd, Ptd], [Od])
                else:
                    for ri in range(8):
                        i = qb * 8 + ri
                        qt0 = 256 + i * 64
                        tiles = [(kt, None) for kt in (0, 1)] + plan[i]
                        (Sp, Spd) = sr.next()
                        (Pt, Ptd) = pr.next()
                        for j, (kt, v) in enumerate(tiles):
                            P.op('pe', lambda e, Sp=Sp, h=h, kt=kt, qt0=qt0, j=j, v=v: e.matmul(Sp[:, j * 64:(j + 1) * 64], lhsT=QKT[:, 4 + h, kt * 128:(kt + 1) * 128], rhs=QKT[:, h, qt0:qt0 + 64], start=True, stop=(v is None)),
                                 [QKTd], [Spd])
                            if v is not None:
                                P.op('pe', lambda e, Sp=Sp, h=h, j=j, v=v: e.matmul(Sp[:, j * 64:(j + 1) * 64], lhsT=ident[:], rhs=tb[:, h, v, :], start=False, stop=True),
                                     [identd, tbd], [Spd])
                        nk = len(tiles)
                        P.op('act', lambda e, Sp=Sp, Pt=Pt, nk=nk: e.activation(out=Pt[:, 0:nk * 64], in_=Sp[:, 0:nk * 64], func=AF.Exp, scale=0.125), [Spd], [Ptd])
                        for j, (kt, v) in enumerate(tiles):
                            P.op('pe', lambda e, O=O, h=h, kt=kt, Pt=Pt, j=j, ri=ri, nk=nk: e.matmul(O[:, ri * 64:(ri + 1) * 64], lhsT=Vaug[:, kt, h, :], rhs=Pt[:, j * 64:(j + 1) * 64], start=(j == 0), stop=(j == nk - 1)),
                                 [Vaugd, Ptd], [Od])
                attn_finalize((osbr, ptr, rvr), O, Od, h, ot, otd, nq, ident32, ident32d)
            nj = nq // 128
            P.dma('sp', lambda e, ot=ot, q0=q0, nj=nj: e.dma_start(out=S['ocat'][q0:q0 + nj * 128, 768:1024].rearrange("(j p) c -> p j c", p=128), in_=ot[:, 0:nj, :]),
                  [otd], [S['ocat_d'][t] for t in range(q0 // 128, q0 // 128 + nj)])
        P.barrier()


def o_tt(q, out, in0, in1, op, rd, wr):
    K.P.op(q, lambda e: e.tensor_tensor(out=out, in0=in0, in1=in1, op=op), rd, wr)


def o_stt(q, out, in0, scalar, in1, op0, op1, rd, wr):
    K.P.op(q, lambda e: e.scalar_tensor_tensor(out=out, in0=in0, scalar=scalar, in1=in1, op0=op0, op1=op1), rd, wr)


def o_ts(q, out, in0, s1, op0, rd, wr):
    K.P.op(q, lambda e: e.tensor_scalar(out=out, in0=in0, scalar1=s1, scalar2=None, op0=op0), rd, wr)


def o_act(out, in_, func, rd, wr, **kw):
    K.P.op('act', lambda e: e.activation(out=out, in_=in_, func=func, **kw), rd, wr)


def o_red(q, out, in_, op, rd, wr):
    K.P.op(q, lambda e: e.tensor_reduce(out=out, in_=in_, axis=AX.X, op=op), rd, wr)


def o_mm(out, lhsT, rhs, rd, wr, start=True, stop=True):
    K.P.op('pe', lambda e: e.matmul(out, lhsT=lhsT, rhs=rhs, start=start, stop=stop), rd, wr)


def o_tr(out, in_, ident, rd, wr):
    K.P.op('pe', lambda e: e.transpose(out=out, in_=in_, identity=ident), rd, wr)


def o_cp(q, out, in_, rd, wr):
    if q == 'act':
        K.P.op('act', lambda e: e.copy(out=out, in_=in_), rd, wr)
    else:
        K.P.op(q, lambda e: e.tensor_copy(out=out, in_=in_), rd, wr)


def o_rcp(out, in_, rd, wr):
    K.P.op('dve', lambda e: e.reciprocal(out=out, in_=in_), rd, wr)


def o_ms(q, out, val, wr):
    K.P.op(q, lambda e: e.memset(out, val), [], wr)


def o_dma(out, in_, rd, wr, q='sp', **kw):
    K.P.dma(q, lambda e: e.dma_start(out=out, in_=in_, **kw), rd, wr)


def bc_load(st, name, src_row, n):
    t, d = sb(st, name, [128, n], F32)
    o_dma(t[:], src_row.partition_broadcast(128), [], [d])
    return t, d


RCH = 8


def rev_tile(c):
    return 1 - c if c < 2 else 35 - c


def phase_rwkv(l, I, S, need_ctx, ident32, ident32d, J32, J32d):
    rwkv_prep(l, I, S, ident32, ident32d, J32, J32d)
    rwkv_scan(S)
    rwkv_readout(l, I, S, need_ctx, J32, J32d)


def rwkv_prep(l, I, S, ident32, ident32d, J32, J32d):
    P = K.P
    MUL, ADD, SUB = ALU.mult, ALU.add, ALU.subtract
    with ExitStack() as st:
        xr = Ring(K, st, 'rv_x', [128, 1024], F32, 2)
        xo = Ring(K, st, 'rv_o', [128, 1024], F32, 2)
        pr = Ring(K, st, 'rv_ps', [128, 512], F32, 2, psum=True)
        for c in range(NTILE):
            tt_ = rev_tile(c)
            (x, xd) = xr.next()
            (o, od) = xo.next()
            o_dma(x[:], S['p'][tt_ * 128:(tt_ + 1) * 128, 0:1024], [S['p_d'][tt_]], [xd])
            for hf in range(2):
                (ps_, psd) = pr.next()
                o_mm(ps_[:], J32[:], x[:, hf * 512:(hf + 1) * 512], [J32d, xd], [psd])
                o_cp('act' if hf else 'dve', o[:, hf * 512:(hf + 1) * 512], ps_[:], [psd], [od])
            o_dma(S['prw1'][c * 128:(c + 1) * 128, :], o[:], [od], [S['prw1_d'][c]])
        P.barrier()
    with ExitStack() as st:
        mub, mubd = bc_load(st, 'rp_mu', I['rwkv_mu'][l, :], 1024)
        kkb, kkbd = bc_load(st, 'rp_kk', I['rwkv_k_k'][l, :], 256)
        kab, kabd = bc_load(st, 'rp_ka', I['rwkv_k_a'][l, :], 256)
        rkb, rkbd = bc_load(st, 'rp_rk', I['rwkv_r_k'][l].rearrange("h d -> (h d)"), 256)
        omka, omkad = sb(st, 'rp_omka', [128, 256], F32)
        K.P.op('dve', lambda e: e.tensor_scalar(out=omka[:], in0=kab[:], scalar1=-1.0, scalar2=1.0, op0=MUL, op1=ADD), [kabd], [omkad])
        w0b, a0b, wup, aup = [], [], [], []
        for d in range(2):
            w0b.append(bc_load(st, 'rp_w0%d' % d, I['rwkv_w0'][l, d, :], 256))
            a0b.append(bc_load(st, 'rp_a0%d' % d, I['rwkv_a0'][l, d, :], 256))
            t, td = sb(st, 'rp_wup%d' % d, [64, 256], F32)
            o_dma(t[:], I['rwkv_w_up'][l, d], [], [td])
            wup.append((t, td))
            t, td = sb(st, 'rp_aup%d' % d, [64, 256], F32)
            o_dma(t[:], I['rwkv_a_up'][l, d], [], [td])
            aup.append((t, td))
        gup, gupd = sb(st, 'rp_gup', [128, 256], F32)
        o_dma(gup[:], I['rwkv_g_up'][l], [], [gupd])

        xr = Ring(K, st, 'rp_x', [128, 1024], F32, 2)
        pvr = Ring(K, st, 'rp_pv', [128, 1024], F32, 2)
        nxr = Ring(K, st, 'rp_nx', [128, 1024], F32, 2)
        xsr = Ring(K, st, 'rp_xs', [128, 1024], F32, 2)
        kkr = Ring(K, st, 'rp_kkn', [128, 256], F32, 2)
        sqr = Ring(K, st, 'rp_sq', [128, 256], F32, 2)
        ssr = Ring(K, st, 'rp_ss', [128, 4], F32, 4)
        smr = Ring(K, st, 'rp_sm', [128, 128], F32, 4)
        sTr = Ring(K, st, 'rp_sT', [128, 128], F32, 4)
        ur = Ring(K, st, 'rp_u', [128, 256], F32, 2)
        ar_ = Ring(K, st, 'rp_a', [128, 256], F32, 2)
        mr = Ring(K, st, 'rp_m', [128, 256], F32, 2)
        kdr = Ring(K, st, 'rp_kd', [128, 256], F32, 3)
        kkar = Ring(K, st, 'rp_kka', [128, 256], F32, 3)
        bor = Ring(K, st, 'rp_bo', [128, 256], F32, 3)
        gtr = Ring(K, st, 'rp_gt', [128, 256], F32, 2)
        nkkr = Ring(K, st, 'rp_nkk', [128, 4, 2, 64], F32, 2)
        rrr = Ring(K, st, 'rp_rr', [128, 4, 2, 64], F32, 2)
        decr = Ring(K, st, 'rp_dec', [128, 4, 2, 64], F32, 2)
        fakr = Ring(K, st, 'rp_fak', [128, 128, 4, 2], F32, 2)
        farr = Ring(K, st, 'rp_far', [128, 128, 4, 2], F32, 2)
        fwr = Ring(K, st, 'rp_fw', [128, 128, 4], F32, 2)
        for ring in (fakr, farr):
            for (b_, bd_) in ring.bufs:
                o_ms('pool', b_[:], 0.0, [bd_])
        pbig = Ring(K, st, 'rp_pb', [128, 256], F32, 3, psum=True)
        ptr_ = Ring(K, st, 'rp_pt', [128, 128], F32, 3, psum=True)

        def v3(ap):
            return ap.rearrange("p (h k) -> p h k", k=64)

        for c in range(NTILE):
            first = c in (0, 2)
            last = c in (1, NTILE - 1)
            r0 = c * 128
            (nkk, nkkd) = nkkr.next()
            (rr, rrd) = rrr.next()
            (dec, decd) = decr.next()
            for d in range(2):
                if d == 0:
                    src = lambda a, b: S['p'][a:b, 0:1024]
                    sdeps = S['p_d']
                else:
                    src = lambda a, b: S['prw1'][a:b, :]
                    sdeps = S['prw1_d']
                nb = [sdeps[c]] + ([sdeps[c - 1]] if c > 0 else []) + ([sdeps[c + 1]] if c < NTILE - 1 else [])
                (x, xd) = xr.next()
                (pv, pvd) = pvr.next()
                (nx, nxd) = nxr.next()
                (xs, xsd) = xsr.next()
                o_dma(x[:], src(r0, r0 + 128), nb, [xd])
                if first:
                    o_ms('pool', pv[:], 0.0, [pvd])
                    o_dma(pv[1:128, :], src(r0, r0 + 127), nb, [pvd])
                else:
                    o_dma(pv[:], src(r0 - 1, r0 + 127), nb, [pvd])
                if last:
                    o_ms('pool', nx[:], 0.0, [nxd])
                    o_dma(nx[0:127, :], src(r0 + 1, r0 + 128), nb, [nxd])
                else:
                    o_dma(nx[:], src(r0 + 1, r0 + 129), nb, [nxd])
                o_tt('pool', pv[:], pv[:], nx[:], ADD, [pvd, nxd], [pvd])
                o_stt('dve', pv[:], pv[:], 0.5, x[:], MUL, SUB, [pvd, xd], [pvd])
                o_tt('pool', pv[:], pv[:], mub[:], MUL, [pvd, mubd], [pvd])
                o_tt('dve', xs[:], x[:], pv[:], ADD, [xd, pvd], [xsd])
                r_ = xs[:, 0:256]
                k_ = xs[:, 256:512]
                v_ = xs[:, 512:768]
                (kkn, kknd) = kkr.next()
                (sq, sqd) = sqr.next()
                (ss, ssd) = ssr.next()
                o_tt('dve', kkn[:], k_, kkb[:], MUL, [xsd, kkbd], [kknd])
                o_tt('pool', sq[:], kkn[:], kkn[:], MUL, [kknd], [sqd])
                o_red('dve', ss[:], v3(sq[:]), ADD, [sqd], [ssd])
                o_act(ss[:], ss[:], AF.Sqrt, [ssd], [ssd], bias=1e-12, scale=1.0)
                o_rcp(ss[:], ss[:], [ssd], [ssd])
                o_tt('dve', v3(kkn[:]), v3(kkn[:]), ss[:].unsqueeze(2).to_broadcast([128, 4, 64]), MUL, [kknd, ssd], [kknd])
                o_ts('dve', nkk[:, :, d, :], v3(kkn[:]), -1.0, MUL, [kknd], [nkkd])
                o_cp('pool', rr[:, :, d, :], v3(r_), [xsd], [rrd])
                (tw, twd) = smr.next()
                (twT, twTd) = sTr.next()
                (pt, ptd) = ptr_.next()
                (pb, pbd) = pbig.next()
                (u, ud) = ur.next()
                o_act(tw[:, 0:64], xs[:, 768:832], AF.Tanh, [xsd], [twd])
                o_tr(pt[0:64, :], tw[:, 0:64], ident32[:], [twd, ident32d], [ptd])
                o_cp('act', twT[0:64, :], pt[0:64, :], [ptd], [twTd])
                o_mm(pb[:], twT[0:64, :], wup[d][0][:], [twTd, wup[d][1]], [pbd])
                o_tt('dve', u[:], pb[:], w0b[d][0][:], ADD, [pbd, w0b[d][1]], [ud])
                o_act(u[:], u[:], AF.Sigmoid, [ud], [ud])
                o_act(dec[:, :, d, :], v3(u[:]), AF.Exp, [ud], [decd], scale=-0.6065306597126334)
                (xa, xad) = smr.next()
                (xaT, xaTd) = sTr.next()
                (pt, ptd) = ptr_.next()
                (pb, pbd) = pbig.next()
                (a, ad) = ar_.next()
                o_cp('pool', xa[:, 0:64], xs[:, 832:896], [xsd], [xad])
                o_tr(pt[0:64, :], xa[:, 0:64], ident32[:], [xad, ident32d], [ptd])
                o_cp('act', xaT[0:64, :], pt[0:64, :], [ptd], [xaTd])
                o_mm(pb[:], xaT[0:64, :], aup[d][0][:], [xaTd, aup[d][1]], [pbd])
                o_tt('dve', a[:], pb[:], a0b[d][0][:], ADD, [pbd, a0b[d][1]], [ad])
                o_act(a[:], a[:], AF.Sigmoid, [ad], [ad])
                (m, md) = mr.next()
                (kd, kdd) = kdr.next()
                (kka, kkad) = kkar.next()
                (bo, bod) = bor.next()
                o_tt('pool', m[:], a[:], kab[:], MUL, [ad, kabd], [md])
                o_tt('pool', m[:], m[:], omka[:], ADD, [md, omkad], [md])
                o_tt('dve', kd[:], k_, m[:], MUL, [xsd, md], [kdd])
                o_tt('pool', kka[:], kkn[:], a[:], MUL, [kknd, ad], [kkad])
                (ss2, ss2d) = ssr.next()
                o_tt('dve', m[:], r_, kd[:], MUL, [xsd, kdd], [md])
                o_tt('pool', m[:], m[:], rkb[:], MUL, [md, rkbd], [md])
                o_red('dve', ss2[:], v3(m[:]), ADD, [md], [ss2d])
                o_tt('dve', v3(bo[:]), v3(v_), ss2[:].unsqueeze(2).to_broadcast([128, 4, 64]), MUL, [xsd, ss2d], [bod])
                o_dma(S['V2'][d, r0:r0 + 128, :], v_, [xsd], [S['V2_d'][c]])
                o_dma(S['KKA'][d, r0:r0 + 128, :], kka[:], [kkad], [S['KKA_d'][c]])
                o_dma(S['KD'][d, r0:r0 + 128, :], kd[:], [kdd], [S['KD_d'][c]])
                o_dma(S['BON'][d, r0:r0 + 128, :], bo[:], [bod], [S['BON_d'][c]])
                if d == 0:
                    (sg, sgd) = smr.next()
                    (sgT, sgTd) = sTr.next()
                    (pt, ptd) = ptr_.next()
                    (pb, pbd) = pbig.next()
                    (gt, gtd) = gtr.next()
                    o_act(sg[:], xs[:, 896:1024], AF.Sigmoid, [xsd], [sgd])
                    o_tr(pt[:], sg[:], ident32[:], [sgd, ident32d], [ptd])
                    o_cp('act', sgT[:], pt[:], [ptd], [sgTd])
                    o_mm(pb[:], sgT[:], gup[:], [sgTd, gupd], [pbd])
                    o_cp('dve', gt[:], pb[:], [pbd], [gtd])
                    o_dma(S['GATE'][r0:r0 + 128, :], gt[:], [gtd], [S['GATE_d'][c]])
            (fak, fakd) = fakr.next()
            (far, fard) = farr.next()
            (fw, fwd) = fwr.next()
            for h in range(4):
                for (srcT, srcd, dstF, dstd) in ((nkk, nkkd, fak, fakd), (rr, rrd, far, fard)):
                    (pt, ptd) = ptr_.next()
                    o_tr(pt[:], srcT[:, h, :, :].rearrange("p d k -> p (d k)"), ident32[:], [srcd, ident32d], [ptd])
                    eng_ = 'act' if h % 2 == 0 else 'dve'
                    o_cp(eng_, dstF[0:64, :, h, 0], pt[0:64, :], [ptd], [dstd])
                    o_cp(eng_, dstF[64:128, :, h, 1], pt[64:128, :], [ptd], [dstd])
                (pt, ptd) = ptr_.next()
                o_tr(pt[:], dec[:, h, :, :].rearrange("p d k -> p (d k)"), ident32[:], [decd, ident32d], [ptd])
                o_cp('act', fw[:, :, h], pt[:], [ptd], [fwd])
            o_dma(S['AKK'][:, r0:r0 + 128, :], fak[:].rearrange("p s h d -> p s (h d)"), [fakd], [S['AKK_d'][c]])
            o_dma(S['AR'][:, r0:r0 + 128, :], far[:].rearrange("p s h d -> p s (h d)"), [fard], [S['AR_d'][c]])
            o_dma(S['WD'][:, r0:r0 + 128, :], fw[:], [fwd], [S['WD_d'][c]])
        P.barrier()


def rwkv_scan(S, T=None):
    P = K.P
    T = NT if T is None else T
    CH = RCH
    nch = T // CH
    with ExitStack() as st:
        ST, _ = sb(st, 'sc_ST', [128, 256], F32)
        STd = [Dep('sc_ST%d' % h) for h in range(4)]
        for ch in range(4):
            o_ms('pool', ST[:, ch * 64:(ch + 1) * 64], 0.0, [STd[ch]])
        NB = 3
        akk = [sb(st, 'sc_akk%d' % i, [128, CH, 8], F32) for i in range(NB)]
        ar = [sb(st, 'sc_ar%d' % i, [128, CH, 8], F32) for i in range(NB)]
        am = [sb(st, 'sc_am%d' % i, [128, CH, 4, 4], F32) for i in range(NB)]
        wt = [sb(st, 'sc_wt%d' % i, [128, CH, 4], F32) for i in range(NB)]
        bt = [sb(st, 'sc_bt%d' % i, [6, CH, 4, 128], F32) for i in range(NB)]
        rt = [sb(st, 'sc_rt%d' % i, [6, CH, 256], F32) for i in range(NB)]
        rtv = [Dep('sc_rtv%d' % i) for i in range(NB)]
        rts = [[Dep('sc_rts%d_%d' % (i, ch)) for ch in range(4)] for i in range(NB)]
        amf, amfd = sb(st, 'sc_amf', [128, 4, 4], F32)
        yfin, yfind = sb(st, 'sc_yfin', [4, 256], F32)
        for (b_, bd_) in bt:
            o_ms('pool', b_[:], 0.0, [bd_])
        sar = [Ring(K, st, 'sc_sa%d' % ch, [128, 64], F32, 1, psum=True) for ch in range(4)]
        ur = [Ring(K, st, 'sc_u%d' % ch, [128, 64], F32, 1, psum=True) for ch in range(4)]

        def load_chunk(c):
            i = c % NB
            s0 = c * CH
            tl = [s0 // 128]
            o_dma(akk[i][0][:], S['AKK'][:, s0:s0 + CH, :], [S['AKK_d'][t] for t in tl], [akk[i][1]])
            o_dma(ar[i][0][:], S['AR'][:, s0:s0 + CH, :], [S['AR_d'][t] for t in tl], [ar[i][1]])
            o_dma(wt[i][0][:], S['WD'][:, s0:s0 + CH, :], [S['WD_d'][t] for t in tl], [wt[i][1]])
            for which, nm in ((0, 'KKA'), (1, 'KD')):
                for d in range(2):
                    row = d if which == 0 else 4 + d
                    o_dma(bt[i][0][row:row + 1, :, :, d * 64:(d + 1) * 64],
                          S[nm][d:d + 1, s0:s0 + CH, :].rearrange("o s (h k) -> o s h k", k=64),
                          [S[nm + '_d'][t] for t in tl], [bt[i][1]])
            o_dma(rt[i][0][4:6, :, :], S['V2'][:, s0:s0 + CH, :], [S['V2_d'][t] for t in tl], [rtv[i]])
            a4 = akk[i][0][:].rearrange("p s (h d) -> p s h d", d=2)
            r4 = ar[i][0][:].rearrange("p s (h d) -> p s h d", d=2)
            o_cp('pool', am[i][0][:, :, :, 0:2], a4, [akk[i][1]], [am[i][1]])
            o_cp('pool', am[i][0][:, 1:CH, :, 2:4], r4[:, 0:CH - 1, :, :], [ar[i][1]], [am[i][1]])
            if c == 0:
                o_ms('pool', am[i][0][:, 0, :, 2:4], 0.0, [am[i][1]])
            else:
                ip = (c - 1) % NB
                rp = ar[ip][0][:].rearrange("p s (h d) -> p s h d", d=2)
                o_cp('pool', am[i][0][:, 0, :, 2:4], rp[:, CH - 1, :, :], [ar[ip][1]], [am[i][1]])

        cv_ld = Ring(K, st, 'sc_cvld', [128, DFF], F32, 2)
        cv_bf = Ring(K, st, 'sc_cvbf', [128, DFF], BF16, 2)
        pend_items = K.PENDING
        K.PENDING = []
        every = max(1, T // max(1, len(pend_items)) - 1) if pend_items else 0
        load_chunk(0)
        if nch > 1:
            load_chunk(1)
        for s in range(T):
            c, j = divmod(s, CH)
            i = c % NB
            if pend_items and s % every == every - 1:
                pend_items.pop(0)(cv_ld, cv_bf, 'pool', 'pool')
            if j == 0 and c + 2 < nch:
                load_chunk(c + 2)
            SAs = [sar[ch].next() for ch in range(4)]
            Us = [ur[ch].next() for ch in range(4)]
            for h in range(4):
                (SA, SAd) = SAs[h]
                o_mm(SA[0:4, :], am[i][0][:, j, h, :], ST[:, h * 64:(h + 1) * 64], [am[i][1], STd[h]], [SAd])
            for h in range(4):
                (SA, SAd) = SAs[h]
                (U, Ud) = Us[h]
                o_cp('act', rt[i][0][0:4, j, h * 64:(h + 1) * 64], SA[0:4, :], [SAd], [rts[i][h]])
                o_mm(U[:], bt[i][0][0:6, j, h, :], rt[i][0][0:6, j, h * 64:(h + 1) * 64], [bt[i][1], rts[i][h], rtv[i]], [Ud])
            for h in range(4):
                (U, Ud) = Us[h]
                STh = ST[:, h * 64:(h + 1) * 64]
                o_stt('dve', STh, STh, wt[i][0][:, j, h:h + 1], U[:], ALU.mult, ALU.add, [STd[h], wt[i][1], Ud], [STd[h]])
            if j == CH - 1:
                s0 = c * CH
                if c == 0:
                    o_dma(S['Y2'][:, 0:CH - 1, :], rt[i][0][2:4, 1:CH, :], rts[i], [S['Y2_d'][0]])
                else:
                    o_dma(S['Y2'][:, s0 - 1:s0 + CH - 1, :], rt[i][0][2:4, :, :], rts[i], [S['Y2_d'][(s0 - 1) // 128]])
        while pend_items:
            pend_items.pop(0)(cv_ld, cv_bf, 'pool', 'pool')
        il = (nch - 1) % NB
        rl = ar[il][0][:].rearrange("p s (h d) -> p s h d", d=2)
        o_ms('pool', amf[:], 0.0, [amfd])
        o_cp('pool', amf[:, :, 2:4], rl[:, CH - 1, :, :], [ar[il][1], amfd], [amfd])
        for h in range(4):
            (SA, SAd) = sar[h].next()
            o_mm(SA[0:4, :], amf[:, h, :], ST[:, h * 64:(h + 1) * 64], [amfd, STd[h]], [SAd])
            o_cp('act', yfin[0:4, h * 64:(h + 1) * 64], SA[0:4, :], [SAd], [yfind])
        o_dma(S['Y2'][:, T - 1, :], yfin[2:4, :], [yfind], [S['Y2_d'][(T - 1) // 128]])
        P.barrier()


def rwkv_readout(l, I, S, need_ctx, J32, J32d):
    P = K.P
    MUL, ADD = ALU.mult, ALU.add
    with ExitStack() as st:
        lng, lngd = bc_load(st, 'ro_lng', I['rwkv_ln_g'][l, :], 256)
        lnb, lnbd = bc_load(st, 'ro_lnb', I['rwkv_ln_b'][l, :], 256)
        yr_ = Ring(K, st, 'ro_y', [128, 256], F32, 3)
        br_ = Ring(K, st, 'ro_b', [128, 256], F32, 3)
        gr_ = Ring(K, st, 'ro_g', [128, 256], F32, 2)
        ycr = Ring(K, st, 'ro_yc', [128, 256], F32, 3)
        sqr = Ring(K, st, 'ro_sq', [128, 256], F32, 2)
        smr = Ring(K, st, 'ro_sm', [128, 4], F32, 6)
        otr = Ring(K, st, 'ro_o', [128, 256], F32, 2)
        psr = Ring(K, st, 'ro_ps', [128, 256], F32, 2, psum=True)

        def v3(ap):
            return ap.rearrange("p (h k) -> p h k", k=64)

        def bc4(ap):
            return ap.unsqueeze(2).to_broadcast([128, 4, 64])

        for t in range(0 if need_ctx else 2, NTILE):
            outs = []
            for d in range(2):
                c = t if d == 0 else rev_tile(t)
                r0 = c * 128
                (y, yd) = yr_.next()
                (b, bd) = br_.next()
                (yc, ycd) = ycr.next()
                (sq, sqd) = sqr.next()
                (sm, smd) = smr.next()
                (vr, vrd) = smr.next()
                o_dma(y[:], S['Y2'][d, r0:r0 + 128, :], [S['Y2_d'][c]], [yd])
                o_dma(b[:], S['BON'][d, r0:r0 + 128, :], [S['BON_d'][c]], [bd])
                o_red('dve', sm[:], v3(y[:]), ADD, [yd], [smd])
                o_ts('dve', sm[:], sm[:], -1.0 / 64, MUL, [smd], [smd])
                o_tt('dve', v3(yc[:]), v3(y[:]), bc4(sm[:]), ADD, [yd, smd], [ycd])
                o_tt('pool', sq[:], yc[:], yc[:], MUL, [ycd], [sqd])
                o_red('dve', vr[:], v3(sq[:]), ADD, [sqd], [vrd])
                o_act(vr[:], vr[:], AF.Sqrt, [vrd], [vrd], bias=64e-5, scale=1.0 / 64)
                o_rcp(vr[:], vr[:], [vrd], [vrd])
                o_tt('dve', v3(yc[:]), v3(yc[:]), bc4(vr[:]), MUL, [ycd, vrd], [ycd])
                o_tt('pool', yc[:], yc[:], lng[:], MUL, [ycd, lngd], [ycd])
                o_tt('pool', yc[:], yc[:], lnb[:], ADD, [ycd, lnbd], [ycd])
                o_tt('dve', yc[:], yc[:], b[:], ADD, [ycd, bd], [ycd])
                outs.append((yc, ycd))
            (ps_, psd) = psr.next()
            (g, gd) = gr_.next()
            (ot, otd) = otr.next()
            o_dma(g[:], S['GATE'][t * 128:(t + 1) * 128, :], [S['GATE_d'][t]], [gd])
            o_mm(ps_[:], J32[:], outs[1][0][:], [J32d, outs[1][1]], [psd])
            o_tt('dve', ot[:], outs[0][0][:], ps_[:], ADD, [outs[0][1], psd], [otd])
            o_tt('dve', ot[:], ot[:], g[:], MUL, [otd, gd], [otd])
            o_dma(S['ocat'][t * 128:(t + 1) * 128, 0:256], ot[:], [otd], [S['ocat_d'][t]])
        P.barrier()


def phase_out(l, I, S, need_ctx, modL, modLd, modC, modCd, ident, identd):
    P = K.P
    MUL, ADD = ALU.mult, ALU.add
    with ExitStack() as st:
        wbf, wbfd = sb(st, 'wo_bf', [128, 8, D], BF16)
        wst = Ring(K, st, 'wo_st', [128, D], F32, 2)
        ocr = Ring(K, st, 'wo_oc', [128, D], F32, 2)
        ocbr = Ring(K, st, 'wo_ocb', [128, D], BF16, 2)
        oTr = Ring(K, st, 'wo_oT', [128, 8, 128], BF16, 2)
        xr = Ring(K, st, 'wo_x', [128, D], F32, 2)
        yr = Ring(K, st, 'wo_y', [128, D], F32, 2)
        sq, sqd = sb(st, 'wo_sq', [128, D], F32)
        ssr = Ring(K, st, 'wo_ss', [128, 1], F32, 4)
        pTr = Ring(K, st, 'wo_pT', [128, 8, 128], BF16, 2, psum=True)
        pyr = Ring(K, st, 'wo_py', [128, 512], F32, 4, psum=True)
        wo = I['w_out'][l].rearrange("(k p) n -> p k n", p=128)
        for kc in range(8):
            (ws, wsd) = wst.next()
            o_dma(ws[:], wo[:, kc, :], [], [wsd])
            o_cp('pool', wbf[:, kc, :], ws[:], [wsd], [wbfd])
        for t in range(0 if need_ctx else 2, NTILE):
            mt, mtd = (modC, modCd) if t < 2 else (modL, modLd)
            (oc, ocd) = ocr.next()
            (ocb, ocbd) = ocbr.next()
            (oT, oTd) = oTr.next()
            (x, xd) = xr.next()
            (y, yd) = yr.next()
            (ss, ssd) = ssr.next()
            (pT, pTd) = pTr.next()
            o_dma(oc[:], S['ocat'][t * 128:(t + 1) * 128, :], [S['ocat_d'][t]], [ocd])
            o_dma(x[:], S['xs'][t * 128:(t + 1) * 128, :], [S['xs_d'][t]], [xd])
            o_cp('pool', ocb[:], oc[:], [ocd], [ocbd])
            for kc in range(8):
                o_tr(pT[:, kc, :], ocb[:, kc * 128:(kc + 1) * 128], ident[:], [ocbd, identd], [pTd])
            o_cp('act', oT[:], pT[:], [pTd], [oTd])
            for hf in range(2):
                (py, pyd) = pyr.next()
                for kc in range(8):
                    o_mm(py[:], oT[:, kc, :], wbf[:, kc, hf * 512:(hf + 1) * 512], [oTd, wbfd], [pyd], start=(kc == 0), stop=(kc == 7))
                o_cp('act' if hf else 'dve', y[:, hf * 512:(hf + 1) * 512], py[:], [pyd], [yd])
            rms_rstd(y[:], yd, D, sq, sqd, ss, ssd, 0)
            o_stt('dve', y[:], y[:], ss[:, 0:1], mt[:, 2, :], MUL, MUL, [yd, ssd, mtd], [yd])
            o_tt('pool', x[:], x[:], y[:], ADD, [xd, yd], [xd])
            o_dma(S['xs'][t * 128:(t + 1) * 128, :], x[:], [xd], [S['xs_d'][t]])
        P.barrier()


def conv_items(I, S, moe, li):
    NEx = NE if moe else 1
    tag = 'm' if moe else 'f'
    items = []
    for e in range(NEx):
        for (nm, dst) in (('gate', 'wgb' + tag), ('up', 'wub' + tag)):
            src = (I['moe_w_' + nm][li, e] if moe else I['ffn_w_' + nm][li]).rearrange("(k p) n -> p k n", p=128)
            for kc in range(8):
                def it(ldr, cvr, eng, q, src=src, kc=kc, dst=dst, e=e):
                    (ld, ldd) = ldr.next()
                    (cv, cvd) = cvr.next()
                    o_dma(ld[:], src[:, kc, :], [], [ldd], q=q)
                    o_cp(eng, cv[:], ld[:], [ldd], [cvd])
                    for q4 in range(2):
                        f0, f1 = q4 * 11, (q4 + 1) * 11
                        o_dma(S[dst][e, f0:f1, :, kc, :].rearrange("f p n -> p f n"),
                              cv[:, f0 * 128:f1 * 128].rearrange("p (f n) -> p f n", n=128), [cvd], [Dep()], q=q)
                items.append(it)
        srcd = (I['moe_w_down'][li, e] if moe else I['ffn_w_down'][li]).rearrange("(f p) n -> p f n", p=128)
        for f0 in range(0, NFF, 2):
            def it(ldr, cvr, eng, q, srcd=srcd, f0=f0, e=e, tag=tag):
                (ld, ldd) = ldr.next()
                (cv, cvd) = cvr.next()
                o_dma(ld[:, 0:2048].rearrange("p (f n) -> p f n", n=1024), srcd[:, f0:f0 + 2, :], [], [ldd], q=q)
                o_cp(eng, cv[:, 0:2048], ld[:, 0:2048], [ldd], [cvd])
                o_dma(S['wdb' + tag][e, f0:f0 + 2, :, :].rearrange("f p n -> p f n"),
                      cv[:, 0:2048].rearrange("p (f n) -> p f n", n=1024), [cvd], [Dep()], q=q)
            items.append(it)
    return items


def ffn_convert(I, S, moe, li):
    if K.CONVERTED.get((moe, li)):
        return
    with ExitStack() as st:
        ldr = Ring(K, st, 'cv_ld', [128, DFF], F32, 3)
        cvr = Ring(K, st, 'cv_bf', [128, DFF], BF16, 3)
        engs = ('dve', 'pool', 'act')
        for n, it in enumerate(conv_items(I, S, moe, li)):
            it(ldr, cvr, engs[n % 3], 'sp')
        K.P.barrier()


def phase_ffn(l, I, S, need_ctx, modL, modLd, modC, modCd, ident32, ident32d, out_ap, out_d):
    P = K.P
    MUL, ADD, SUB = ALU.mult, ALU.add, ALU.subtract
    moe = (l % 2 == 1)
    li = l // 2
    NEx = NE if moe else 1
    tag = 'm' if moe else 'f'
    last_layer = not need_ctx
    ffn_convert(I, S, moe, li)
    tiles = list(range(0 if need_ctx else 2, NTILE))
    split = last_layer
    if split:
        tiles = tiles[0:16]
    with ExitStack() as st:
        xrr = Ring(K, st, 'ff_x', [128, 2, D], F32, 2)
        if split:
            sel, seld = sb(st, 'ff_sel', [128, 2], F32)
            o_dma(sel[:], I['halfsel'][:, :], [], [seld])
            xbr = Ring(K, st, 'ff_xb', [128, D], F32, 2)
            xar = Ring(K, st, 'ff_xa', [128, D], F32, 2)
        hfr = Ring(K, st, 'ff_hf', [128, D], F32, 2)
        hTr = Ring(K, st, 'ff_hT', [128, 8, 256], BF16, 2)
        h32r = Ring(K, st, 'ff_h32', [128, 8, 128], F32, 2)
        sq, sqd = sb(st, 'ff_sq', [128, D], F32)
        ssr = Ring(K, st, 'ff_ss', [128, 1], F32, 4)
        accr = Ring(K, st, 'ff_acc', [128, 2, D], F32, 2)
        gtr = Ring(K, st, 'ff_gate', [128, 2, 8], F32, 2)
        smr = Ring(K, st, 'ff_sm', [128, 8], F32, 8)
        s1r = Ring(K, st, 'ff_s1', [128, 1], F32, 12)
        wgr = Ring(K, st, 'ff_wg', [128, 8, 128], BF16, 3)
        wur = Ring(K, st, 'ff_wu', [128, 8, 128], BF16, 3)
        wdr = Ring(K, st, 'ff_wd', [128, D], BF16, 3)
        sgr = Ring(K, st, 'ff_sg', [128, 256], F32, 2)
        GTr = Ring(K, st, 'ff_GT', [128, 256], BF16, 3)
        pTr = Ring(K, st, 'ff_pT', [128, 4, 128], F32, 1, psum=True)
        gur = Ring(K, st, 'ff_gu', [128, 2, 256], F32, 2, psum=True)
        ops_ = [[ps(st, 'ff_o%d%d' % (a, b), [128, 512], F32) for b in range(2)] for a in range(2)]
        plr = Ring(K, st, 'ff_pl', [128, 8], F32, 1, psum=True)
        if moe:
            rt_, rtd = sb(st, 'ff_router', [128, 8, 8], F32)
            o_dma(rt_[:], I['moe_router'][li].rearrange("(k p) e -> p k e", p=128), [], [rtd])
        for blk in range(len(tiles) // 2):
            tl = tiles[2 * blk:2 * blk + 2]
            (xr, xrd) = xrr.next()
            (hT, hTd) = hTr.next()
            (acc, accd) = accr.next()
            (gate, gated) = gtr.next()
            for jj, t in enumerate(tl):
                mt, mtd = (modC, modCd) if t < 2 else (modL, modLd)
                (hf, hfd) = hfr.next()
                (h32, h32d) = h32r.next()
                (ss, ssd) = ssr.next()
                if split:
                    (xa, xad) = xar.next()
                    (xb, xbd) = xbr.next()
                    t2 = t + 16
                    o_dma(xa[:], S['xs'][t * 128:(t + 1) * 128, :], [S['xs_d'][t]], [xad])
                    o_dma(xb[:], S['xs'][t2 * 128:(t2 + 1) * 128, :], [S['xs_d'][t2]], [xbd], q='pool')
                    o_ts('dve', xa[:], xa[:], sel[:, 0:1], MUL, [xad, seld], [xad])
                    o_stt('dve', xr[:, jj, :], xb[:], sel[:, 1:2], xa[:], MUL, ADD, [xbd, seld, xad], [xrd])
                else:
                    o_dma(xr[:, jj, :], S['xs'][t * 128:(t + 1) * 128, :], [S['xs_d'][t]], [xrd])
                rms_rstd(xr[:, jj, :], xrd, D, sq, sqd, ss, ssd, 0)
                o_stt('dve', hf[:], xr[:, jj, :], ss[:, 0:1], mt[:, 4, :], MUL, MUL, [xrd, ssd, mtd], [hfd])
                o_tt('pool', hf[:], hf[:], mt[:, 3, :], ADD, [hfd, mtd], [hfd])
                for q4 in range(2):
                    (pT, pTd) = pTr.next()
                    for kk_ in range(4):
                        kc = q4 * 4 + kk_
                        o_tr(pT[:, kk_, :], hf[:, kc * 128:(kc + 1) * 128], ident32[:], [hfd, ident32d], [pTd])
                    o_cp('act', h32[:, q4 * 4:(q4 + 1) * 4, :], pT[:], [pTd], [h32d])
                    o_cp('pool', hT[:, q4 * 4:(q4 + 1) * 4, jj * 128:(jj + 1) * 128], h32[:, q4 * 4:(q4 + 1) * 4, :], [h32d], [hTd])
                if moe:
                    (pl, pld) = plr.next()
                    for kc in range(8):
                        o_mm(pl[:], h32[:, kc, :], rt_[:, kc, :], [h32d, rtd], [pld], start=(kc == 0), stop=(kc == 7))
                    (lg, lgd) = smr.next()
                    (lg2, lg2d) = smr.next()
                    (mk1, mk1d) = smr.next()
                    (mk2, mk2d) = smr.next()
                    (m1, m1d) = s1r.next()
                    (m2, m2d) = s1r.next()
                    (ex, exd) = s1r.next()
                    (w1, w1d) = s1r.next()
                    (w2, w2d) = s1r.next()
                    o_cp('dve', lg[:], pl[:], [pld], [lgd])
                    o_red('dve', m1[:], lg[:], ALU.max, [lgd], [m1d])
                    o_ts('dve', mk1[:], lg[:], m1[:, 0:1], ALU.is_equal, [lgd, m1d], [mk1d])
                    o_stt('dve', lg2[:], mk1[:], -1.0e30, lg[:], MUL, ADD, [mk1d, lgd], [lg2d])
                    o_red('dve', m2[:], lg2[:], ALU.max, [lg2d], [m2d])
                    o_ts('dve', mk2[:], lg2[:], m2[:, 0:1], ALU.is_equal, [lg2d, m2d], [mk2d])
                    o_tt('dve', ex[:], m2[:], m1[:], SUB, [m2d, m1d], [exd])
                    o_act(ex[:], ex[:], AF.Exp, [exd], [exd])
                    o_ts('dve', w1[:], ex[:], 1.0, ADD, [exd], [w1d])
                    o_rcp(w1[:], w1[:], [w1d], [w1d])
                    o_tt('dve', w2[:], ex[:], w1[:], MUL, [exd, w1d], [w2d])
                    o_ts('dve', gate[:, jj, :], mk1[:], w1[:, 0:1], MUL, [mk1d, w1d], [gated])
                    o_stt('dve', gate[:, jj, :], mk2[:], w2[:, 0:1], gate[:, jj, :], MUL, ADD, [mk2d, w2d, gated], [gated])
            for e in range(NEx):
                for f in range(NFF):
                    (wg, wgd) = wgr.next()
                    (wu, wud) = wur.next()
                    (wd, wdd) = wdr.next()
                    (gu, gud) = gur.next()
                    (sg, sgd) = sgr.next()
                    (GT, GTd) = GTr.next()
                    o_dma(wg[:], S['wgb' + tag][e, f], [], [wgd])
                    o_dma(wu[:], S['wub' + tag][e, f], [], [wud], q='pool')
                    o_dma(wd[:], S['wdb' + tag][e, f], [], [wdd])
                    for kc in range(8):
                        o_mm(gu[:, 0, :], wg[:, kc, :], hT[:, kc, :], [wgd, hTd], [gud], start=(kc == 0), stop=(kc == 7))
                    for kc in range(8):
                        o_mm(gu[:, 1, :], wu[:, kc, :], hT[:, kc, :], [wud, hTd], [gud], start=(kc == 0), stop=(kc == 7))
                    o_act(sg[:], gu[:, 0, :], AF.Silu, [gud], [sgd])
                    o_tt('dve', GT[:], sg[:], gu[:, 1, :], MUL, [sgd, gud], [GTd])
                    for jj in range(2):
                        for hf_ in range(2):
                            (op_, opd) = ops_[jj][hf_]
                            o_mm(op_[:], GT[:, jj * 128:(jj + 1) * 128], wd[:, hf_ * 512:(hf_ + 1) * 512], [GTd, wdd], [opd],
                                 start=(f == 0), stop=(f == NFF - 1))
                for jj in range(2):
                    for hf_ in range(2):
                        (op_, opd) = ops_[jj][hf_]
                        dst = acc[:, jj, hf_ * 512:(hf_ + 1) * 512]
                        if not moe:
                            o_cp('act', dst, op_[:], [opd], [accd])
                        elif e == 0:
                            o_ts('dve', dst, op_[:], gate[:, jj, e:e + 1], MUL, [opd, gated], [accd])
                        else:
                            o_stt('dve', dst, op_[:], gate[:, jj, e:e + 1], dst, MUL, ADD, [opd, gated, accd], [accd])
            for jj, t in enumerate(tl):
                mt, mtd = (modC, modCd) if t < 2 else (modL, modLd)
                (ss, ssd) = ssr.next()
                rms_rstd(acc[:, jj, :], accd, D, sq, sqd, ss, ssd, 0)
                o_stt('dve', acc[:, jj, :], acc[:, jj, :], ss[:, 0:1], mt[:, 5, :], MUL, MUL, [accd, ssd, mtd], [accd])
                o_tt('pool', acc[:, jj, :], acc[:, jj, :], xr[:, jj, :], ADD, [accd, xrd], [accd])
                if last_layer:
                    o_dma(out_ap[(t - 2) * 128:(t - 1) * 128, :], acc[:, jj, :], [accd], [out_d])
                else:
                    o_dma(S['xs'][t * 128:(t + 1) * 128, :], acc[:, jj, :], [accd], [S['xs_d'][t]])
        P.barrier()


IN_NAMES = ['x', 'c', 'ctx', 'c_ctx', 'mod_w', 'mod_b', 'norm_g', 'w_in', 'w_out', 'rwkv_mu', 'rwkv_w0', 'rwkv_w_up',
            'rwkv_a0', 'rwkv_a_up', 'rwkv_g_up', 'rwkv_k_k', 'rwkv_k_a', 'rwkv_r_k', 'rwkv_ln_g', 'rwkv_ln_b',
            'gqa_q_g', 'gqa_k_g', 'mla_q_norm_g', 'mla_w_uq', 'mla_kv_norm_g', 'mla_w_ukv', 'nat_bias',
            'ffn_w_gate', 'ffn_w_up', 'ffn_w_down', 'moe_router', 'moe_w_gate', 'moe_w_up', 'moe_w_down']


def build(shapes, stop_after=None, debug=(), only=None, layers=(0, 1), scan_T=None):
    nc = bass.Bass("TRN2", target_bir_lowering=False)
    K.nc = nc
    P = Prog(nc)
    K.P = P
    I = {}
    for name in IN_NAMES:
        I[name] = nc.dram_tensor(name, list(shapes[name]), F32, kind="ExternalInput").ap()
    out = nc.dram_tensor("out", [NL // 2, D], F32, kind="ExternalOutput").ap()
    I['halfsel'] = nc.dram_tensor('halfsel', [128, 2], F32, kind='ExternalInput').ap()
    out_d = Dep('out')
    S = {}

    def scratch(name, shape, dtype=F32, tiled=True):
        S[name] = dram(name, shape, dtype)
        S[name + '_d'] = [Dep('%s%d' % (name, t)) for t in range(NTILE)] if tiled else Dep(name)

    scratch('xs', [NT, D])
    scratch('p', [NT, INC])
    scratch('ocat', [NT, D])
    scratch('prw1', [NT, 1024])
    for nm in ('V2', 'KKA', 'KD', 'BON', 'Y2'):
        scratch(nm, [2, NT, 256])
    scratch('GATE', [NT, 256])
    scratch('AKK', [128, NT, 8])
    scratch('AR', [128, NT, 8])
    scratch('WD', [128, NT, 4])
    for tag, ne in (('f', 1), ('m', NE)):
        scratch('wgb' + tag, [ne, NFF, 128, 8, 128], BF16, tiled=False)
        scratch('wub' + tag, [ne, NFF, 128, 8, 128], BF16, tiled=False)
        scratch('wdb' + tag, [ne, NFF, 128, D], BF16, tiled=False)
    I['rope_g'] = nc.dram_tensor('rope_g', [NL, 2, 32], F32, kind='ExternalInput').ap()
    I['rope_m'] = nc.dram_tensor('rope_m', [NL, 2, 16], F32, kind='ExternalInput').ap()
    I['nat_tab'] = nc.dram_tensor('nat_tab', [2, 128, 4, len(nat_plan()[1]), 64], F32, kind='ExternalInput').ap()
    with ExitStack() as st:
        P.alloc_sems(st)
        ident, identd = sb(st, 'ident', [128, 128], BF16)
        modL, modLd = sb(st, 'modL', [128, 6, D], F32)
        modC, modCd = sb(st, 'modC', [128, 6, D], F32)
        ident32, ident32d = sb(st, 'ident32', [128, 128], F32)
        J32, J32d = sb(st, 'J32', [128, 128], F32)
        P.op('pool', lambda e: e.memset(ident[:], 1.0), [], [identd])
        P.op('pool', lambda e: e.memset(ident32[:], 1.0), [], [ident32d])
        P.op('pool', lambda e: e.memset(J32[:], 1.0), [], [J32d])
        P.op('pool', lambda e: e.affine_select(out=ident32[:], in_=ident32[:], pattern=[[-1, 128]], compare_op=ALU.is_equal, fill=0.0, base=0, channel_multiplier=1),
             [ident32d], [ident32d])
        P.op('pool', lambda e: e.affine_select(out=ident[:], in_=ident[:], pattern=[[-1, 128]], compare_op=ALU.is_equal, fill=0.0, base=0, channel_multiplier=1),
             [identd], [identd])
        P.op('pool', lambda e: e.affine_select(out=J32[:], in_=J32[:], pattern=[[1, 128]], compare_op=ALU.is_equal, fill=0.0, base=-127, channel_multiplier=1),
             [J32d], [J32d])
        P.dma('sp', lambda e: e.dma_start(out=S['xs'][0:NC_, :], in_=I['ctx'][:, :]), [], S['xs_d'][0:2])
        for j in range(4):
            P.dma('sp', lambda e, j=j: e.dma_start(out=S['xs'][NC_ + j * 1024:NC_ + (j + 1) * 1024, :], in_=I['x'][j * 1024:(j + 1) * 1024, :]),
                  [], S['xs_d'][2 + 8 * j:2 + 8 * (j + 1)])

        def want(name):
            return only is None or name in only

        K.CONVERTED = {}
        K.PENDING = []
        if only is None and tuple(layers) == (0, 1):
            K.PENDING = conv_items(I, S, False, 0) + conv_items(I, S, True, 0)
            K.CONVERTED = {(False, 0): True, (True, 0): True}
        for l in layers:
            need_ctx = (l == 0)
            if want('mod'):
                phase_mod(l, I, modL, modLd, modC, modCd)
            if want('in'):
                phase_in(l, I, S, modL, modLd, modC, modCd, ident, identd)
            if want('rwkv'):
                if scan_T is None:
                    phase_rwkv(l, I, S, need_ctx, ident32, ident32d, J32, J32d)
                else:
                    rwkv_prep(l, I, S, ident32, ident32d, J32, J32d)
                    rwkv_scan(S, scan_T)
            if want('gqa'):
                phase_gqa(l, I, S, need_ctx, ident, identd, ident32, ident32d)
            if want('mla'):
                phase_mla(l, I, S, need_ctx, ident, identd, ident32, ident32d)
            if want('nat'):
                phase_nat(l, I, S, need_ctx, ident, identd, ident32, ident32d)
            if stop_after == ('attn', l):
                break
            if want('out'):
                phase_out(l, I, S, need_ctx, modL, modLd, modC, modCd, ident, identd)
            if stop_after == ('out', l):
                break
            if want('ffn'):
                phase_ffn(l, I, S, need_ctx, modL, modLd, modC, modCd, ident32, ident32d, out, out_d)
        for name in debug:
            src = S[name]
            d = nc.dram_tensor('dbg_' + name, list(src.shape), src.dtype, kind="ExternalOutput").ap()
            dd = Dep('dbg_' + name)
            deps = S[name + '_d'] if isinstance(S[name + '_d'], list) else [S[name + '_d']]
            P.dma('sp', lambda e, d=d, src=src: e.dma_start(out=d, in_=src), deps, [dd])
        P.barrier()
        P.emit()
    return nc


def core_shapes(inputs):
    sh = {k: tuple(np.asarray(v).shape) for k, v in inputs.items()}
    sh['x'] = (NL, D)
    sh['c'] = (D,)
    sh['ctx'] = (NC_, D)
    return sh


def rope_tables(rot_dim):
    t = np.arange(NL)
    row = (t // 64).astype(np.float32)
    col = (t % 64).astype(np.float32)
    quarter = rot_dim // 4
    inv_freq = (np.float32(10000.0) ** (-np.arange(quarter, dtype=np.float32) / np.float32(quarter))).astype(np.float32)
    ang = np.concatenate([row[:, None] * inv_freq, col[:, None] * inv_freq], axis=-1).astype(np.float32)
    return np.ascontiguousarray(np.stack([np.cos(ang), np.sin(ang)], axis=1).astype(np.float32))


def core_inputs(inputs, b, shared=None):
    if shared is None:
        shared = {k: np.ascontiguousarray(np.asarray(v, dtype=np.float32)) for k, v in inputs.items() if k not in ('x', 'c', 'ctx')}
        shared['rope_g'] = rope_tables(64)
        shared['rope_m'] = rope_tables(32)
        nb = np.asarray(inputs['nat_bias'], np.float32)
        shared['nat_tab'] = np.ascontiguousarray(np.stack([nat_bias_table(nb[0]), nat_bias_table(nb[1])], 0))
    m = dict(shared)
    m['x'] = np.ascontiguousarray(np.asarray(inputs['x'][b], dtype=np.float32))
    m['c'] = np.ascontiguousarray(np.asarray(inputs['c'][b], dtype=np.float32))
    m['ctx'] = np.ascontiguousarray(np.asarray(inputs['ctx'][b], dtype=np.float32))
    return m


def kernel(**inputs):
    nb = 4
    shapes = core_shapes(inputs)
    nc = build(shapes)
    first = core_inputs(inputs, 0)
    shared = {k: v for k, v in first.items() if k not in ('x', 'c', 'ctx')}
    per_b = [first] + [core_inputs(inputs, b, shared) for b in range(1, nb)]
    maps = []
    for b in range(nb):
        for m in range(2):
            mm_ = dict(per_b[b])
            hs = np.zeros((128, 2), np.float32)
            hs[:, m] = 1.0
            mm_['halfsel'] = hs
            maps.append(mm_)
    res = run_bass_kernel_spmd(nc, maps, core_ids=list(range(2 * nb)))
    out = np.empty((nb, NL, D), np.float32)
    for b in range(nb):
        for m in range(2):
            out[b, m * (NL // 2):(m + 1) * (NL // 2)] = np.asarray(res.results[2 * b + m]['out'], dtype=np.float32)
    return out
```

```python
import numpy as np
from contextlib import ExitStack
import concourse.bass as bass
import concourse.mybir as mybir
from concourse.bass_utils import run_bass_kernel_spmd

F32 = mybir.dt.float32
BF16 = mybir.dt.bfloat16
AF = mybir.ActivationFunctionType
ALU = mybir.AluOpType
AX = mybir.AxisListType

COMPUTE = ('pe', 'act', 'dve', 'pool')
QUEUES = ('pe', 'act', 'dve', 'pool', 'sp')
NRING = 8

D = 1024
NL = 4096
NC_ = 256
NT = NL + NC_
NTILE = NT // 128
INC = 2720
RW0, GQ0, ML0, NA0 = 0, 1024, 1536, 1952
DFF = 2816
NFF = DFF // 128
NE = 8
RMS_EPS = 1e-6


class Dep:
    __slots__ = ('w', 'r', 'name')

    def __init__(self, name=''):
        self.w = None
        self.r = {}
        self.name = name


class Prog:
    def __init__(self, nc):
        self.nc = nc
        self.q = {e: [] for e in QUEUES}
        self.cnt = {e: 0 for e in COMPUTE}
        self.seen = {e: {} for e in QUEUES}
        self.sems = {}
        self.dma_cnt = {}
        self.dma_next = {e: 0 for e in QUEUES}
        self.ninstr = 0

    def alloc_sems(self, stack):
        for e in COMPUTE:
            self.sems[e] = stack.enter_context(self.nc.semaphore('s_' + e))
        for qn in ('sp', 'pool', 'act'):
            for j in range(NRING):
                key = ('dma', qn, j)
                self.sems[key] = stack.enter_context(self.nc.semaphore('d_%s_%d' % (qn, j)))
                self.dma_cnt[key] = 0

    def _need(self, queue, key, count, waits):
        if self.seen[queue].get(key, 0) >= count:
            return
        if waits.get(key, 0) < count:
            waits[key] = count

    def _emit_waits(self, queue, waits):
        for key, count in waits.items():
            sem = self.sems[key]
            val = count * (16 if isinstance(key, tuple) else 1)
            self.q[queue].append(lambda e, sem=sem, val=val: e.wait_ge(sem, val))
            self.seen[queue][key] = count
            self.ninstr += 1

    def _collect(self, queue, reads, writes):
        waits = {}
        for t in reads:
            if t.w is not None:
                self._need(queue, t.w[0], t.w[1], waits)
        for t in writes:
            if t.w is not None:
                if not (queue == 'pe' and t.w[0] == 'pe'):
                    self._need(queue, t.w[0], t.w[1], waits)
            for k, c in t.r.items():
                if k == queue:
                    continue
                self._need(queue, k, c, waits)
        return waits

    def op(self, queue, fn, reads=(), writes=()):
        waits = self._collect(queue, reads, writes)
        self._emit_waits(queue, waits)
        self.cnt[queue] += 1
        c = self.cnt[queue]
        sem = self.sems[queue]
        self.q[queue].append(lambda e, fn=fn, sem=sem: fn(e).then_inc(sem, 1))
        self.ninstr += 1
        for t in reads:
            if t.r.get(queue, 0) < c:
                t.r[queue] = c
        for t in writes:
            t.w = (queue, c)
            t.r = {}

    def dma(self, queue, fn, reads=(), writes=()):
        j = self.dma_next[queue]
        self.dma_next[queue] = (j + 1) % NRING
        key = ('dma', queue, j)
        waits = self._collect(queue, reads, writes)
        n = self.dma_cnt[key]
        if n > 0:
            self._need(queue, key, n, waits)
        self._emit_waits(queue, waits)
        self.dma_cnt[key] = n + 1
        sem = self.sems[key]
        self.q[queue].append(lambda e, fn=fn, sem=sem: fn(e).then_inc(sem, 16))
        self.ninstr += 1
        for t in reads:
            t.r[key] = n + 1
        for t in writes:
            t.w = (key, n + 1)
            t.r = {}

    def barrier(self, queues=QUEUES):
        for qn in queues:
            waits = {}
            for e in COMPUTE:
                if self.cnt[e] > 0 and e != qn:
                    self._need(qn, e, self.cnt[e], waits)
            for key, n in self.dma_cnt.items():
                if n > 0:
                    self._need(qn, key, n, waits)
            self._emit_waits(qn, waits)

    def emit(self):
        nc = self.nc
        with nc.Block() as block:
            @block.tensor
            def _(e):
                for f in self.q['pe']:
                    f(e)

            @block.scalar
            def _(e):
                for f in self.q['act']:
                    f(e)

            @block.vector
            def _(e):
                for f in self.q['dve']:
                    f(e)

            @block.gpsimd
            def _(e):
                for f in self.q['pool']:
                    f(e)

            @block.sync
            def _(e):
                for f in self.q['sp']:
                    f(e)


class Ring:
    def __init__(self, K, st, name, shape, dtype, n, psum=False):
        self.bufs = []
        for i in range(n):
            if psum:
                full = [128, 512] if dtype == F32 else [128, 1024]
                n = 1
                for d_ in shape[1:]:
                    n *= d_
                assert n <= full[1]
                t = st.enter_context(K.nc.psum_tensor(uname('%s%d' % (name, i)), full, dtype))
                t = t[0:shape[0], 0:n]
                if len(shape) == 3:
                    t = t.rearrange("p (a b) -> p a b", b=shape[2])
            else:
                t = st.enter_context(K.nc.sbuf_tensor(uname('%s%d' % (name, i)), shape, dtype))
            self.bufs.append((t, Dep('%s%d' % (name, i))))
        self.i = 0

    def next(self):
        b = self.bufs[self.i]
        self.i = (self.i + 1) % len(self.bufs)
        return b


class K:
    LVL = 99
    UID = 0
    CONVERTED = {}
    PENDING = []


def uname(name):
    K.UID += 1
    return '%s_u%d' % (name, K.UID)


def sb(st, name, shape, dtype):
    return st.enter_context(K.nc.sbuf_tensor(uname(name), shape, dtype)), Dep(name)


def ps(st, name, shape, dtype):
    return st.enter_context(K.nc.psum_tensor(uname(name), shape, dtype)), Dep(name)


def dram(name, shape, dtype):
    return K.nc.dram_tensor(name, shape, dtype, kind="Internal").ap()


def rms_rstd(x_ap, xdep, n, sq, sqd, ss, ssd, col):
    P = K.P
    P.op('dve', lambda e: e.tensor_tensor(out=sq[:, 0:n], in0=x_ap, in1=x_ap, op=ALU.mult), [xdep], [sqd])
    P.op('dve', lambda e: e.tensor_reduce(out=ss[:, col:col + 1], in_=sq[:, 0:n], axis=AX.X, op=ALU.add), [sqd], [ssd])
    P.op('act', lambda e: e.activation(out=ss[:, col:col + 1], in_=ss[:, col:col + 1], func=AF.Sqrt, bias=RMS_EPS, scale=1.0 / n), [ssd], [ssd])
    P.op('dve', lambda e: e.reciprocal(out=ss[:, col:col + 1], in_=ss[:, col:col + 1]), [ssd], [ssd])


def phase_mod(l, I, modL, modLd, modC, modCd):
    nc, P = K.nc, K.P
    with ExitStack() as st:
        cv, cvd = sb(st, 'cv', [128, 2, 8], F32)
        cs, csd = sb(st, 'cs', [128, 2, 8], F32)
        crep, crepd = sb(st, 'crep', [128, 2, 8, 128], BF16)
        gb, gbd = sb(st, 'gb', [128, 4, D], F32)
        wst = Ring(K, st, 'mw_st', [128, 8, 512], F32, 2)
        wbf = Ring(K, st, 'mw_bf', [128, 8, 512], BF16, 2)
        bb = Ring(K, st, 'mbb', [128, 512], F32, 2)
        pm = Ring(K, st, 'pmod', [128, 512], F32, 4, psum=True)
        P.dma('sp', lambda e: e.dma_start(out=cv[:, 0, :], in_=I['c'].rearrange("(k p) -> p k", p=128), allow_slow_non_contiguous=True), [], [cvd])
        P.dma('sp', lambda e: e.dma_start(out=cv[:, 1, :], in_=I['c_ctx'].rearrange("(k p) -> p k", p=128), allow_slow_non_contiguous=True), [], [cvd])
        P.dma('sp', lambda e: e.dma_start(out=gb[:], in_=I['norm_g'][l].partition_broadcast(128)), [], [gbd])
        P.op('act', lambda e: e.activation(out=cs[:], in_=cv[:], func=AF.Silu), [cvd], [csd])
        P.op('dve', lambda e: e.tensor_copy(out=crep[:], in_=cs[:].unsqueeze(3).to_broadcast([128, 2, 8, 128])), [csd], [crepd])
        mw = I['mod_w'][l].rearrange("(k p) n -> p k n", p=128)
        for nb in range(12):
            (ws, wsd) = wst.next()
            (wb, wbd) = wbf.next()
            (bt, btd) = bb.next()
            P.dma('sp', lambda e, ws=ws, nb=nb: e.dma_start(out=ws[:], in_=mw[:, :, nb * 512:(nb + 1) * 512]), [], [wsd])
            P.dma('sp', lambda e, bt=bt, nb=nb: e.dma_start(out=bt[:], in_=I['mod_b'][l, nb * 512:(nb + 1) * 512].partition_broadcast(128)), [], [btd])
            P.op('pool', lambda e, ws=ws, wb=wb: e.tensor_copy(out=wb[:], in_=ws[:]), [wsd], [wbd])
            j, off = nb // 2, (nb % 2) * 512
            for s, (mt, mtd) in enumerate(((modL, modLd), (modC, modCd))):
                (pt, ptd) = pm.next()
                for kc in range(8):
                    P.op('pe', lambda e, pt=pt, wb=wb, kc=kc, s=s: e.matmul(pt[:], lhsT=crep[:, s, kc, :], rhs=wb[:, kc, :], start=(kc == 0), stop=(kc == 7)),
                         [crepd, wbd], [ptd])
                P.op('dve', lambda e, pt=pt, bt=bt, mt=mt, j=j, off=off: e.tensor_tensor(out=mt[:, j, off:off + 512], in0=pt[:], in1=bt[:], op=ALU.add),
                     [ptd, btd], [mtd])
        for (mt, mtd) in ((modL, modLd), (modC, modCd)):
            for j, gi, plus1 in ((1, 0, True), (2, 1, False), (4, 2, True), (5, 3, False)):
                if plus1:
                    P.op('dve', lambda e, mt=mt, j=j, gi=gi: e.scalar_tensor_tensor(out=mt[:, j, :], in0=mt[:, j, :], scalar=1.0, in1=gb[:, gi, :], op0=ALU.add, op1=ALU.mult),
                         [mtd, gbd], [mtd])
                else:
                    P.op('dve', lambda e, mt=mt, j=j, gi=gi: e.tensor_tensor(out=mt[:, j, :], in0=mt[:, j, :], in1=gb[:, gi, :], op=ALU.mult),
                         [mtd, gbd], [mtd])
        P.barrier()


def phase_in(l, I, S, modL, modLd, modC, modCd, ident, identd):
    nc, P = K.nc, K.P
    with ExitStack() as st:
        wbf, wbfd = sb(st, 'win_bf', [128, 8, INC], BF16)
        wst = Ring(K, st, 'win_st', [128, INC], F32, 2)
        xr = Ring(K, st, 'in_x', [128, D], F32, 3)
        hr = Ring(K, st, 'in_h', [128, D], F32, 2)
        hbr = Ring(K, st, 'in_hb', [128, D], BF16, 2)
        hTr = Ring(K, st, 'in_hT', [128, 8, 128], BF16, 2)
        sq, sqd = sb(st, 'in_sq', [128, D], F32)
        ssr = Ring(K, st, 'in_ss', [128, 1], F32, 4)
        pr = Ring(K, st, 'in_p', [128, INC], F32, 2)
        ptr = Ring(K, st, 'in_pT', [128, 8, 128], BF16, 2, psum=True)
        pmr = Ring(K, st, 'in_pm', [128, 512], F32, 4, psum=True)
        win = I['w_in'][l].rearrange("(k p) n -> p k n", p=128)
        for kc in range(8):
            (ws, wsd) = wst.next()
            P.dma('sp', lambda e, ws=ws, kc=kc: e.dma_start(out=ws[:], in_=win[:, kc, :]), [], [wsd])
            P.op('pool', lambda e, ws=ws, kc=kc: e.tensor_copy(out=wbf[:, kc, :], in_=ws[:]), [wsd], [wbfd])
        for t in range(NTILE):
            mt, mtd = (modC, modCd) if t < 2 else (modL, modLd)
            (x, xd) = xr.next()
            (h, hd) = hr.next()
            (hb, hbd) = hbr.next()
            (hT, hTd) = hTr.next()
            (ss, ssd) = ssr.next()
            (pt, ptd) = pr.next()
            (pT, pTd) = ptr.next()
            P.dma('sp', lambda e, x=x, t=t: e.dma_start(out=x[:], in_=S['xs'][t * 128:(t + 1) * 128, :]), [S['xs_d'][t]], [xd])
            rms_rstd(x[:], xd, D, sq, sqd, ss, ssd, 0)
            P.op('dve', lambda e, h=h, x=x, ss=ss, mt=mt: e.scalar_tensor_tensor(out=h[:], in0=x[:], scalar=ss[:, 0:1], in1=mt[:, 1, :], op0=ALU.mult, op1=ALU.mult),
                 [xd, ssd, mtd], [hd])
            P.op('pool', lambda e, h=h, hb=hb, mt=mt: e.tensor_tensor(out=hb[:], in0=h[:], in1=mt[:, 0, :], op=ALU.add), [hd, mtd], [hbd])
            for kc in range(8):
                P.op('pe', lambda e, pT=pT, hb=hb, kc=kc: e.transpose(out=pT[:, kc, :], in_=hb[:, kc * 128:(kc + 1) * 128], identity=ident[:]),
                     [hbd, identd], [pTd])
            P.op('act', lambda e, hT=hT, pT=pT: e.copy(out=hT[:], in_=pT[:]), [pTd], [hTd])
            for nb in range(6):
                c0 = nb * 512
                cw = min(512, INC - c0)
                (pm, pmd) = pmr.next()
                for kc in range(8):
                    P.op('pe', lambda e, pm=pm, hT=hT, kc=kc, c0=c0, cw=cw: e.matmul(pm[:, 0:cw], lhsT=hT[:, kc, :], rhs=wbf[:, kc, c0:c0 + cw], start=(kc == 0), stop=(kc == 7)),
                         [hTd, wbfd], [pmd])
                if nb % 2 == 0:
                    P.op('act', lambda e, pm=pm, pt=pt, c0=c0, cw=cw: e.copy(out=pt[:, c0:c0 + cw], in_=pm[:, 0:cw]), [pmd], [ptd])
                else:
                    P.op('dve', lambda e, pm=pm, pt=pt, c0=c0, cw=cw: e.tensor_copy(out=pt[:, c0:c0 + cw], in_=pm[:, 0:cw]), [pmd], [ptd])
            P.dma('sp', lambda e, pt=pt, t=t: e.dma_start(out=S['p'][t * 128:(t + 1) * 128, :], in_=pt[:]), [ptd], [S['p_d'][t]])
        P.barrier()


def attn_finalize(st_rings, O, Od, h, ot, otd, nq, ident32, ident32d):
    P = K.P
    osbr, ptr, rvr = st_rings
    (osb, osbd) = osbr.next()
    P.op('dve', lambda e: e.tensor_copy(out=osb[:, 0:nq], in_=O[:, 0:nq]), [Od], [osbd])
    for j in range(nq // 128):
        (pt, ptd) = ptr.next()
        (rv, rvd) = rvr.next()
        P.op('pe', lambda e, pt=pt, osb=osb, j=j: e.transpose(out=pt[:], in_=osb[:, j * 128:(j + 1) * 128], identity=ident32[0:65, 0:65]),
             [osbd, ident32d], [ptd])
        P.op('dve', lambda e, pt=pt, rv=rv: e.reciprocal(out=rv[:], in_=pt[:, 64:65]), [ptd], [rvd])
        P.op('dve', lambda e, pt=pt, rv=rv, j=j: e.tensor_scalar(out=ot[:, j, h * 64:(h + 1) * 64], in0=pt[:, 0:64], scalar1=rv[:, 0:1], scalar2=None, op0=ALU.mult),
             [ptd, rvd], [otd])


def attn_core(st, S, QT, QTd, KT, KTd, kvmap, Vaug, Vaugd, scale, col0, need_ctx, ident32, ident32d, tag):
    P = K.P
    sr = Ring(K, st, tag + '_S', [128, 512], F32, 3, psum=True)
    orr = Ring(K, st, tag + '_O', [65, 512], F32, 2, psum=True)
    ptr = Ring(K, st, tag + '_fT', [128, 65], F32, 2, psum=True)
    pr = Ring(K, st, tag + '_P', [128, 512], BF16, 3)
    osbr = Ring(K, st, tag + '_osb', [65, 512], F32, 2)
    rvr = Ring(K, st, tag + '_rv', [128, 1], F32, 4)
    otr = Ring(K, st, tag + '_ot', [128, 4, 256], F32, 2)
    blocks = []
    if need_ctx:
        blocks.append((0, 256, [0, 1]))
    for qb in range(8):
        blocks.append((256 + qb * 512, 512, list(range(NTILE))))
    for (q0, nq, kts) in blocks:
        (ot, otd) = otr.next()
        for h in range(4):
            g = kvmap[h]
            (O, Od) = orr.next()
            pend = None
            for i, kt in enumerate(kts):
                (Sp, Spd) = sr.next()
                (Pt, Ptd) = pr.next()
                P.op('pe', lambda e, Sp=Sp, g=g, h=h, kt=kt, q0=q0, nq=nq: e.matmul(Sp[:, 0:nq], lhsT=KT(g)[:, kt * 128:(kt + 1) * 128], rhs=QT(h)[:, q0:q0 + nq], start=True, stop=True),
                     [QTd, KTd], [Spd])
                P.op('act', lambda e, Sp=Sp, Pt=Pt, nq=nq: e.activation(out=Pt[:, 0:nq], in_=Sp[:, 0:nq], func=AF.Exp, scale=scale), [Spd], [Ptd])
                if pend is not None:
                    pend()

                def pend(O=O, Od=Od, g=g, kt=kt, Pt=Pt, Ptd=Ptd, nq=nq, i=i, n=len(kts)):
                    P.op('pe', lambda e: e.matmul(O[:, 0:nq], lhsT=Vaug[:, kt, g, :], rhs=Pt[:, 0:nq], start=(i == 0), stop=(i == n - 1)),
                         [Vaugd, Ptd], [Od])
            pend()
            attn_finalize((osbr, ptr, rvr), O, Od, h, ot, otd, nq, ident32, ident32d)
        nj = nq // 128
        P.dma('sp', lambda e, ot=ot, q0=q0, nj=nj: e.dma_start(out=S['ocat'][q0:q0 + nj * 128, col0:col0 + 256].rearrange("(j p) c -> p j c", p=128), in_=ot[:, 0:nj, :]),
              [otd], [S['ocat_d'][t] for t in range(q0 // 128, q0 // 128 + nj)])


def phase_gqa(l, I, S, need_ctx, ident, identd, ident32, ident32d):
    nc, P = K.nc, K.P
    with ExitStack() as st:
        QKT, QKTd = sb(st, 'gq_QKT', [64, 6, NT], BF16)
        Vaug, Vaugd = sb(st, 'gq_V', [128, NTILE, 2, 65], BF16)
        with ExitStack() as st2:
            gain, gaind = sb(st2, 'gq_gain', [128, 6, 64], F32)
            xr = Ring(K, st2, 'gq_x', [128, 512], F32, 3)
            sq, sqd = sb(st2, 'gq_sq', [128, 384], F32)
            ssr = Ring(K, st2, 'gq_ss', [128, 6], F32, 3)
            qnr = Ring(K, st2, 'gq_qn', [128, 6, 64], F32, 2)
            qbr = Ring(K, st2, 'gq_qb', [128, 6, 64], BF16, 2)
            csr = Ring(K, st2, 'gq_cs', [128, 2, 32], F32, 3)
            t1r = Ring(K, st2, 'gq_t1', [128, 6, 32], F32, 2)
            t2r = Ring(K, st2, 'gq_t2', [128, 6, 32], F32, 2)
            pTr = Ring(K, st2, 'gq_pT', [64, 6, 128], BF16, 2, psum=True)
            P.dma('sp', lambda e: e.dma_start(out=gain[:, 0:4, :], in_=I['gqa_q_g'][l:l + 1, :].partition_broadcast(128).to_broadcast([128, 4, 64])), [], [gaind])
            P.dma('sp', lambda e: e.dma_start(out=gain[:, 4:6, :], in_=I['gqa_k_g'][l:l + 1, :].partition_broadcast(128).to_broadcast([128, 2, 64])), [], [gaind])
            P.op('pool', lambda e: e.memset(Vaug[:, :, :, 64:65], 1.0), [], [Vaugd])
            for t in range(NTILE):
                (x, xd) = xr.next()
                (ss, ssd) = ssr.next()
                (qn, qnd) = qnr.next()
                (qb, qbd) = qbr.next()
                (pT, pTd) = pTr.next()
                P.dma('sp', lambda e, x=x, t=t: e.dma_start(out=x[:], in_=S['p'][t * 128:(t + 1) * 128, GQ0:GQ0 + 512]), [S['p_d'][t]], [xd])
                P.op('dve', lambda e, x=x: e.tensor_tensor(out=sq[:], in0=x[:, 0:384], in1=x[:, 0:384], op=ALU.mult), [xd], [sqd])
                P.op('dve', lambda e, ss=ss: e.tensor_reduce(out=ss[:], in_=sq[:].rearrange("p (g d) -> p g d", d=64), axis=AX.X, op=ALU.add), [sqd], [ssd])
                P.op('act', lambda e, ss=ss: e.activation(out=ss[:], in_=ss[:], func=AF.Sqrt, bias=RMS_EPS, scale=1.0 / 64), [ssd], [ssd])
                P.op('dve', lambda e, ss=ss: e.reciprocal(out=ss[:], in_=ss[:]), [ssd], [ssd])
                P.op('dve', lambda e, x=x, qn=qn, ss=ss: e.tensor_tensor(out=qn[:], in0=x[:, 0:384].rearrange("p (g d) -> p g d", d=64), in1=ss[:].unsqueeze(2).to_broadcast([128, 6, 64]), op=ALU.mult),
                     [xd, ssd], [qnd])
                P.op('pool', lambda e, x=x, t=t: e.tensor_copy(out=Vaug[:, t, :, 0:64], in_=x[:, 384:512].rearrange("p (g d) -> p g d", d=64)), [xd], [Vaugd])
                if t < 2:
                    P.op('dve', lambda e, qn=qn, qb=qb: e.tensor_tensor(out=qb[:], in0=qn[:], in1=gain[:], op=ALU.mult), [qnd, gaind], [qbd])
                else:
                    (cs, csd) = csr.next()
                    (t1, t1d) = t1r.next()
                    (t2, t2d) = t2r.next()
                    r0 = (t - 2) * 128
                    P.dma('sp', lambda e, cs=cs, r0=r0: e.dma_start(out=cs[:], in_=I['rope_g'][r0:r0 + 128, :, :]), [], [csd])
                    P.op('dve', lambda e, qn=qn: e.tensor_tensor(out=qn[:], in0=qn[:], in1=gain[:], op=ALU.mult), [qnd, gaind], [qnd])
                    cosb = lambda cs=cs: cs[:, 0, :].unsqueeze(1).to_broadcast([128, 6, 32])
                    sinb = lambda cs=cs: cs[:, 1, :].unsqueeze(1).to_broadcast([128, 6, 32])
                    P.op('dve', lambda e, t1=t1, qn=qn, cosb=cosb: e.tensor_tensor(out=t1[:], in0=qn[:, :, 0:32], in1=cosb(), op=ALU.mult), [qnd, csd], [t1d])
                    P.op('pool', lambda e, t2=t2, qn=qn, sinb=sinb: e.tensor_tensor(out=t2[:], in0=qn[:, :, 32:64], in1=sinb(), op=ALU.mult), [qnd, csd], [t2d])
                    P.op('dve', lambda e, t1=t1, t2=t2, qb=qb: e.tensor_tensor(out=qb[:, :, 0:32], in0=t1[:], in1=t2[:], op=ALU.subtract), [t1d, t2d], [qbd])
                    P.op('dve', lambda e, t1=t1, qn=qn, sinb=sinb: e.tensor_tensor(out=t1[:], in0=qn[:, :, 0:32], in1=sinb(), op=ALU.mult), [qnd, csd], [t1d])
                    P.op('pool', lambda e, t2=t2, qn=qn, cosb=cosb: e.tensor_tensor(out=t2[:], in0=qn[:, :, 32:64], in1=cosb(), op=ALU.mult), [qnd, csd], [t2d])
                    P.op('dve', lambda e, t1=t1, t2=t2, qb=qb: e.tensor_tensor(out=qb[:, :, 32:64], in0=t1[:], in1=t2[:], op=ALU.add), [t1d, t2d], [qbd])
                for g in range(6):
                    P.op('pe', lambda e, pT=pT, qb=qb, g=g: e.transpose(out=pT[:, g, :], in_=qb[:, g, :], identity=ident[:]), [qbd, identd], [pTd])
                P.op('act', lambda e, pT=pT, t=t: e.copy(out=QKT[:, :, t * 128:(t + 1) * 128], in_=pT[:]), [pTd], [QKTd])
            P.barrier()
        attn_core(st, S, lambda h: QKT[:, h, :], QKTd, lambda g: QKT[:, 4 + g, :], QKTd, [0, 0, 1, 1], Vaug, Vaugd, 0.125, 256,
                  need_ctx, ident32, ident32d, 'gq')
        P.barrier()


def phase_mla(l, I, S, need_ctx, ident, identd, ident32, ident32d):
    nc, P = K.nc, K.P
    with ExitStack() as st:
        QKT, QKTd = sb(st, 'ml_QKT', [128, 8, NT], BF16)
        Vaug, Vaugd = sb(st, 'ml_V', [128, NTILE, 4, 65], BF16)
        with ExitStack() as st2:
            gain, gaind = sb(st2, 'ml_gain', [128, 384], F32)
            wst, wstd = sb(st2, 'ml_wst', [128, 2, 512], F32)
            wuq, wuqd = sb(st2, 'ml_wuq', [128, 2, 384], BF16)
            wukv, wukvd = sb(st2, 'ml_wukv', [128, 512], BF16)
            xr = Ring(K, st2, 'ml_x', [128, 416], F32, 3)
            sq, sqd = sb(st2, 'ml_sq', [128, 384], F32)
            ssr = Ring(K, st2, 'ml_ss', [128, 2], F32, 3)
            cnr = Ring(K, st2, 'ml_cn', [128, 384], F32, 2)
            cbr = Ring(K, st2, 'ml_cb', [128, 384], BF16, 2)
            cTr = Ring(K, st2, 'ml_cT', [128, 3, 128], BF16, 2)
            qsr = Ring(K, st2, 'ml_qs', [128, 4, 96], F32, 2)
            csr = Ring(K, st2, 'ml_cs', [128, 2, 16], F32, 3)
            t1r = Ring(K, st2, 'ml_t1', [128, 5, 16], F32, 2)
            t2r = Ring(K, st2, 'ml_t2', [128, 5, 16], F32, 2)
            rr = Ring(K, st2, 'ml_r', [128, 5, 32], F32, 2)
            qkr = Ring(K, st2, 'ml_qk', [128, 8, 128], BF16, 2)
            for (qk_, qkd_) in qkr.bufs:
                P.op('pool', lambda e, qk_=qk_: e.memset(qk_[:], 0.0), [], [qkd_])
            pcT = Ring(K, st2, 'ml_pcT', [128, 3, 128], BF16, 2, psum=True)
            pq = Ring(K, st2, 'ml_pq', [128, 384], F32, 1, psum=True)
            pkv = Ring(K, st2, 'ml_pkv', [128, 512], F32, 2, psum=True)
            pT2 = Ring(K, st2, 'ml_pT2', [128, 8, 128], BF16, 2, psum=True)
            P.dma('sp', lambda e: e.dma_start(out=gain[:, 0:256], in_=I['mla_q_norm_g'][l:l + 1, :].partition_broadcast(128)), [], [gaind])
            P.dma('sp', lambda e: e.dma_start(out=gain[:, 256:384], in_=I['mla_kv_norm_g'][l:l + 1, :].partition_broadcast(128)), [], [gaind])
            P.dma('sp', lambda e: e.dma_start(out=wst[:, :, 0:384], in_=I['mla_w_uq'][l].rearrange("(k p) n -> p k n", p=128)), [], [wstd])
            P.op('pool', lambda e: e.tensor_copy(out=wuq[:], in_=wst[:, :, 0:384]), [wstd], [wuqd])
            P.dma('sp', lambda e: e.dma_start(out=wst[:, 0, :], in_=I['mla_w_ukv'][l]), [wuqd], [wstd])
            P.op('pool', lambda e: e.tensor_copy(out=wukv[:], in_=wst[:, 0, :]), [wstd], [wukvd])
            P.op('pool', lambda e: e.memset(Vaug[:, :, :, 64:65], 1.0), [], [Vaugd])
            for t in range(NTILE):
                (x, xd) = xr.next()
                (ss, ssd) = ssr.next()
                (cn, cnd) = cnr.next()
                (cb, cbd) = cbr.next()
                (cT, cTd) = cTr.next()
                (qs, qsd) = qsr.next()
                (qk, qkd) = qkr.next()
                (r, rd) = rr.next()
                (pc, pcd) = pcT.next()
                (pqt, pqd) = pq.next()
                (pk, pkd) = pkv.next()
                (pT, pTd) = pT2.next()
                P.dma('sp', lambda e, x=x, t=t: e.dma_start(out=x[:], in_=S['p'][t * 128:(t + 1) * 128, ML0:ML0 + 416]), [S['p_d'][t]], [xd])
                if K.LVL < 2:
                    continue
                P.op('dve', lambda e, x=x: e.tensor_tensor(out=sq[:], in0=x[:, 0:384], in1=x[:, 0:384], op=ALU.mult), [xd], [sqd])
                P.op('dve', lambda e, ss=ss: e.tensor_reduce(out=ss[:, 0:1], in_=sq[:, 0:256], axis=AX.X, op=ALU.add), [sqd], [ssd])
                P.op('dve', lambda e, ss=ss: e.tensor_reduce(out=ss[:, 1:2], in_=sq[:, 256:384], axis=AX.X, op=ALU.add), [sqd], [ssd])
                P.op('act', lambda e, ss=ss: e.activation(out=ss[:, 0:1], in_=ss[:, 0:1], func=AF.Sqrt, bias=RMS_EPS, scale=1.0 / 256), [ssd], [ssd])
                P.op('act', lambda e, ss=ss: e.activation(out=ss[:, 1:2], in_=ss[:, 1:2], func=AF.Sqrt, bias=RMS_EPS, scale=1.0 / 128), [ssd], [ssd])
                P.op('dve', lambda e, ss=ss: e.reciprocal(out=ss[:], in_=ss[:]), [ssd], [ssd])
                P.op('dve', lambda e, x=x, cn=cn, ss=ss: e.scalar_tensor_tensor(out=cn[:, 0:256], in0=x[:, 0:256], scalar=ss[:, 0:1], in1=gain[:, 0:256], op0=ALU.mult, op1=ALU.mult),
                     [xd, ssd, gaind], [cnd])
                P.op('dve', lambda e, x=x, cn=cn, ss=ss: e.scalar_tensor_tensor(out=cn[:, 256:384], in0=x[:, 256:384], scalar=ss[:, 1:2], in1=gain[:, 256:384], op0=ALU.mult, op1=ALU.mult),
                     [xd, ssd, gaind], [cnd])
                P.op('pool', lambda e, cn=cn, cb=cb: e.tensor_copy(out=cb[:], in_=cn[:]), [cnd], [cbd])
                if K.LVL < 3:
                    continue
                for j in range(3):
                    P.op('pe', lambda e, pc=pc, cb=cb, j=j: e.transpose(out=pc[:, j, :], in_=cb[:, j * 128:(j + 1) * 128], identity=ident[:]), [cbd, identd], [pcd])
                if K.LVL < 2.3:
                    continue
                P.op('act', lambda e, cT=cT, pc=pc: e.copy(out=cT[:], in_=pc[:]), [pcd], [cTd])
                if K.LVL < 2.6:
                    continue
                for j in range(2):
                    P.op('pe', lambda e, pqt=pqt, cT=cT, j=j: e.matmul(pqt[:], lhsT=cT[:, j, :], rhs=wuq[:, j, :], start=(j == 0), stop=(j == 1)), [cTd, wuqd], [pqd])
                if K.LVL < 2.8:
                    continue
                for hf in range(2):
                    P.op('pe', lambda e, pk=pk, cT=cT, hf=hf: e.matmul(pk[:, hf * 256:(hf + 1) * 256], lhsT=cT[:, 2, :], rhs=wukv[:, hf * 256:(hf + 1) * 256], start=True, stop=True), [cTd, wukvd], [pkd])
                if K.LVL < 4:
                    continue
                P.op('act', lambda e, qs=qs, pqt=pqt: e.copy(out=qs[:], in_=pqt[:].rearrange("p (h d) -> p h d", d=96)), [pqd], [qsd])
                P.op('dve', lambda e, pk=pk, t=t: e.tensor_copy(out=Vaug[:, t, :, 0:64], in_=pk[:].rearrange("p (h d) -> p h d", d=128)[:, :, 64:128]), [pkd], [Vaugd])
                P.op('dve', lambda e, pk=pk, qk=qk: e.tensor_copy(out=qk[:, 4:8, 0:64], in_=pk[:].rearrange("p (h d) -> p h d", d=128)[:, :, 0:64]), [pkd], [qkd])
                P.op('pool', lambda e, qs=qs, qk=qk: e.tensor_copy(out=qk[:, 0:4, 0:64], in_=qs[:, :, 0:64]), [qsd], [qkd])
                P.op('pool', lambda e, r=r, qs=qs: e.tensor_copy(out=r[:, 0:4, :], in_=qs[:, :, 64:96]), [qsd], [rd])
                P.op('pool', lambda e, r=r, x=x: e.tensor_copy(out=r[:, 4, :], in_=x[:, 384:416]), [xd], [rd])
                if K.LVL < 5:
                    continue
                if t < 2:
                    P.op('dve', lambda e, r=r, qk=qk: e.tensor_copy(out=qk[:, 0:4, 64:96], in_=r[:, 0:4, :]), [rd], [qkd])
                    P.op('dve', lambda e, r=r, qk=qk: e.tensor_copy(out=qk[:, 4:8, 64:96], in_=r[:, 4, :].unsqueeze(1).to_broadcast([128, 4, 32])), [rd], [qkd])
                else:
                    (cs, csd) = csr.next()
                    (t1, t1d) = t1r.next()
                    (t2, t2d) = t2r.next()
                    r0 = (t - 2) * 128
                    P.dma('sp', lambda e, cs=cs, r0=r0: e.dma_start(out=cs[:], in_=I['rope_m'][r0:r0 + 128, :, :]), [], [csd])
                    cosb = lambda cs=cs: cs[:, 0, :].unsqueeze(1).to_broadcast([128, 5, 16])
                    sinb = lambda cs=cs: cs[:, 1, :].unsqueeze(1).to_broadcast([128, 5, 16])
                    P.op('dve', lambda e, t1=t1, r=r, cosb=cosb: e.tensor_tensor(out=t1[:], in0=r[:, :, 0:16], in1=cosb(), op=ALU.mult), [rd, csd], [t1d])
                    P.op('pool', lambda e, t2=t2, r=r, sinb=sinb: e.tensor_tensor(out=t2[:], in0=r[:, :, 16:32], in1=sinb(), op=ALU.mult), [rd, csd], [t2d])
                    P.op('dve', lambda e, t1=t1, t2=t2: e.tensor_tensor(out=t1[:], in0=t1[:], in1=t2[:], op=ALU.subtract), [t1d, t2d], [t1d])
                    P.op('dve', lambda e, t1=t1, qk=qk: e.tensor_copy(out=qk[:, 0:4, 64:80], in_=t1[:, 0:4, :]), [t1d], [qkd])
                    P.op('dve', lambda e, t1=t1, qk=qk: e.tensor_copy(out=qk[:, 4:8, 64:80], in_=t1[:, 4, :].unsqueeze(1).to_broadcast([128, 4, 16])), [t1d], [qkd])
                    (t1, t1d) = t1r.next()
                    (t2, t2d) = t2r.next()
                    P.op('dve', lambda e, t1=t1, r=r, sinb=sinb: e.tensor_tensor(out=t1[:], in0=r[:, :, 0:16], in1=sinb(), op=ALU.mult), [rd, csd], [t1d])
                    P.op('pool', lambda e, t2=t2, r=r, cosb=cosb: e.tensor_tensor(out=t2[:], in0=r[:, :, 16:32], in1=cosb(), op=ALU.mult), [rd, csd], [t2d])
                    P.op('dve', lambda e, t1=t1, t2=t2: e.tensor_tensor(out=t1[:], in0=t1[:], in1=t2[:], op=ALU.add), [t1d, t2d], [t1d])
                    P.op('dve', lambda e, t1=t1, qk=qk: e.tensor_copy(out=qk[:, 0:4, 80:96], in_=t1[:, 0:4, :]), [t1d], [qkd])
                    P.op('dve', lambda e, t1=t1, qk=qk: e.tensor_copy(out=qk[:, 4:8, 80:96], in_=t1[:, 4, :].unsqueeze(1).to_broadcast([128, 4, 16])), [t1d], [qkd])
                if K.LVL < 6:
                    continue
                for g in range(8):
                    P.op('pe', lambda e, pT=pT, qk=qk, g=g: e.transpose(out=pT[:, g, :], in_=qk[:, g, :], identity=ident[:]), [qkd, identd], [pTd])
                P.op('act', lambda e, pT=pT, t=t: e.copy(out=QKT[:, :, t * 128:(t + 1) * 128], in_=pT[:]), [pTd], [QKTd])
            P.barrier()
        if K.LVL < 7:
            return
        attn_core(st, S, lambda h: QKT[:, h, :], QKTd, lambda g: QKT[:, 4 + g, :], QKTd, [0, 1, 2, 3], Vaug, Vaugd, 96.0 ** -0.5, 512,
                  need_ctx, ident32, ident32d, 'ml')
        P.barrier()


BIG = 30000.0


def nat_plan():
    variants = {}
    plan = []
    for i in range(64):
        rs = min(max(i - 4, 0), 56)
        tiles = []
        for m in range(rs // 2, (rs + 7) // 2 + 1):
            dd = []
            for r in (2 * m, 2 * m + 1):
                dd.append(r - i + 7 if rs <= r < rs + 8 else -1)
            key = tuple(dd)
            if key not in variants:
                variants[key] = len(variants)
            tiles.append((2 + m, variants[key]))
        plan.append(tiles)
    vlist = [None] * len(variants)
    for k, v in variants.items():
        vlist[v] = k
    return plan, vlist


def nat_bias_table(nat_bias_l):
    plan, vlist = nat_plan()
    c = np.arange(64)
    cs = np.clip(c - 8, 0, 48)
    cp = np.arange(64)
    inwin = (cp[:, None] >= cs[None, :]) & (cp[:, None] < cs[None, :] + 16)
    off = np.clip(cp[:, None] - c[None, :] + 15, 0, 30)
    tab = np.full((128, 4, len(vlist), 64), -BIG, np.float32)
    for v, (d0, d1) in enumerate(vlist):
        for half, d in enumerate((d0, d1)):
            if d < 0:
                continue
            for h in range(4):
                vals = nat_bias_l[h, d][off]
                tab[half * 64:(half + 1) * 64, h, v, :] = np.where(inwin, vals, np.float32(-BIG))
    return tab


def phase_nat(l, I, S, need_ctx, ident, identd, ident32, ident32d):
    nc, P = K.nc, K.P
    plan, vlist = nat_plan()
    NV = len(vlist)
    with ExitStack() as st:
        QKT, QKTd = sb(st, 'na_QKT', [64, 8, NT], BF16)
        Vaug, Vaugd = sb(st, 'na_V', [128, NTILE, 4, 65], BF16)
        tb, tbd = sb(st, 'na_tb', [128, 4, NV, 64], BF16)
        with ExitStack() as st2:
            tbs, tbsd = sb(st2, 'na_tbs', [128, 4, NV, 64], F32)
            xr = Ring(K, st2, 'na_x', [128, 768], F32, 3)
            xbr = Ring(K, st2, 'na_xb', [128, 512], BF16, 2)
            pTr = Ring(K, st2, 'na_pT', [64, 8, 128], BF16, 2, psum=True)
            P.dma('sp', lambda e: e.dma_start(out=tbs[:], in_=I['nat_tab'][l]), [], [tbsd])
            P.op('dve', lambda e: e.tensor_scalar(out=tb[:], in0=tbs[:], scalar1=8.0, scalar2=None, op0=ALU.mult), [tbsd], [tbd])
            P.op('pool', lambda e: e.memset(Vaug[:, :, :, 64:65], 1.0), [], [Vaugd])
            for t in range(NTILE):
                (x, xd) = xr.next()
                (xb, xbd) = xbr.next()
                (pT, pTd) = pTr.next()
                P.dma('sp', lambda e, x=x, t=t: e.dma_start(out=x[:], in_=S['p'][t * 128:(t + 1) * 128, NA0:NA0 + 768]), [S['p_d'][t]], [xd])
                P.op('dve', lambda e, x=x, xb=xb: e.tensor_copy(out=xb[:], in_=x[:, 0:512]), [xd], [xbd])
                P.op('pool', lambda e, x=x, t=t: e.tensor_copy(out=Vaug[:, t, :, 0:64], in_=x[:, 512:768].rearrange("p (h d) -> p h d", d=64)), [xd], [Vaugd])
                for g in range(8):
                    P.op('pe', lambda e, pT=pT, xb=xb, g=g: e.transpose(out=pT[:, g, :], in_=xb[:, g * 64:(g + 1) * 64], identity=ident[:]), [xbd, identd], [pTd])
                P.op('act', lambda e, pT=pT, t=t: e.copy(out=QKT[:, :, t * 128:(t + 1) * 128], in_=pT[:]), [pTd], [QKTd])
            P.barrier()
        sr = Ring(K, st, 'na_S', [128, 512], F32, 3, psum=True)
        orr = Ring(K, st, 'na_O', [65, 512], F32, 2, psum=True)
        ptr = Ring(K, st, 'na_fT', [128, 65], F32, 2, psum=True)
        pr = Ring(K, st, 'na_P', [128, 512], BF16, 3)
        osbr = Ring(K, st, 'na_osb', [65, 512], F32, 2)
        rvr = Ring(K, st, 'na_rv', [128, 1], F32, 4)
        otr = Ring(K, st, 'na_ot', [128, 4, 256], F32, 2)
        blocks = []
        if need_ctx:
            blocks.append(None)
        for qb in range(8):
            blocks.append(qb)
        for qb in blocks:
            (ot, otd) = otr.next()
            if qb is None:
                q0, nq = 0, 256
            else:
                q0, nq = 256 + qb * 512, 512
            for h in range(4):
                (O, Od) = orr.next()
                if qb is None:
                    for i, kt in enumerate((0, 1)):
                        (Sp, Spd) = sr.next()
                        (Pt, Ptd) = pr.next()
                        P.op('pe', lambda e, Sp=Sp, h=h, kt=kt: e.matmul(Sp[:, 0:256], lhsT=QKT[:, 4 + h, kt * 128:(kt + 1) * 128], rhs=QKT[:, h, 0:256], start=True, stop=True),
                             [QKTd], [Spd])
                        P.op('act', lambda e, Sp=Sp, Pt=Pt: e.activation(out=Pt[:, 0:256], in_=Sp[:, 0:256], func=AF.Exp, scale=0.125), [Spd], [Ptd])
                        P.op('pe', lambda e, O=O, h=h, kt=kt, Pt=Pt, i=i: e.matmul(O[:, 0:256], lhsT=Vaug[:, kt, h, :], rhs=Pt[:, 0:256], start=(i == 0), stop=(i == 1)),
                             [Vaugd, Ptd], [Od])
                else:
                    npend = [None]
                    for ri in range(8):
                        i = qb * 8 + ri
                        qt0 = 256 + i * 64
                        tiles = [(kt, None) for kt in (0, 1)] + plan[i]
                        (Sp, Spd) = sr.next()
                        (Pt, Ptd) = pr.next()
                        for j, (kt, v) in enumerate(tiles):
                            P.op('pe', lambda e, Sp=Sp, h=h, kt=kt, qt0=qt0, j=j, v=v: e.matmul(Sp[:, j * 64:(j + 1) * 64], lhsT=QKT[:, 4 + h, kt * 128:(kt + 1) * 128], rhs=QKT[:, h, qt0:qt0 + 64], start=True, stop=(v is None)),
                                 [QKTd], [Spd])
                            if v is not None:
                                P.op('pe', lambda e, Sp=Sp, h=h, j=j, v=v: e.matmul(Sp[:, j * 64:(j + 1) * 64], lhsT=ident[:], rhs=tb[:, h, v, :], start=False, stop=True),
                                     [identd, tbd], [Spd])
                        nk = len(tiles)
                        P.op('act', lambda e, Sp=Sp, Pt=Pt, nk=nk: e.activation(out=Pt[:, 0:nk * 64], in_=Sp[:, 0:nk * 64], func=AF.Exp, scale=0.125), [Spd], [Ptd])
                        if npend[0] is not None:
                            npend[0]()

                        def _pend(O=O, Od=Od, h=h, Pt=Pt, Ptd=Ptd, ri=ri, nk=nk, tiles=tiles):
                            for j, (kt, v) in enumerate(tiles):
                                P.op('pe', lambda e, kt=kt, j=j: e.matmul(O[:, ri * 64:(ri + 1) * 64], lhsT=Vaug[:, kt, h, :], rhs=Pt[:, j * 64:(j + 1) * 64], start=(j == 0), stop=(j == nk - 1)),
                                     [Vaugd, Ptd], [Od])
                        npend[0] = _pend
                    npend[0]()
                    npend[0] = None
                attn_finalize((osbr, ptr, rvr), O, Od, h, ot, otd, nq, ident32, ident32d)
            nj = nq // 128
            P.dma('sp', lambda e, ot=ot, q0=q0, nj=nj: e.dma_start(out=S['ocat'][q0:q0 + nj * 128, 768:1024].rearrange("(j p) c -> p j c", p=128), in_=ot[:, 0:nj, :]),
                  [otd], [S['ocat_d'][t] for t in range(q0 // 128, q0 // 128 + nj)])
        P.barrier()


def o_tt(q, out, in0, in1, op, rd, wr):
    K.P.op(q, lambda e: e.tensor_tensor(out=out, in0=in0, in1=in1, op=op), rd, wr)


def o_stt(q, out, in0, scalar, in1, op0, op1, rd, wr):
    K.P.op(q, lambda e: e.scalar_tensor_tensor(out=out, in0=in0, scalar=scalar, in1=in1, op0=op0, op1=op1), rd, wr)


def o_ts(q, out, in0, s1, op0, rd, wr):
    K.P.op(q, lambda e: e.tensor_scalar(out=out, in0=in0, scalar1=s1, scalar2=None, op0=op0), rd, wr)


def o_act(out, in_, func, rd, wr, **kw):
    K.P.op('act', lambda e: e.activation(out=out, in_=in_, func=func, **kw), rd, wr)


def o_red(q, out, in_, op, rd, wr):
    K.P.op(q, lambda e: e.tensor_reduce(out=out, in_=in_, axis=AX.X, op=op), rd, wr)


def o_mm(out, lhsT, rhs, rd, wr, start=True, stop=True):
    K.P.op('pe', lambda e: e.matmul(out, lhsT=lhsT, rhs=rhs, start=start, stop=stop), rd, wr)


def o_tr(out, in_, ident, rd, wr):
    K.P.op('pe', lambda e: e.transpose(out=out, in_=in_, identity=ident), rd, wr)


def o_cp(q, out, in_, rd, wr):
    if q == 'act':
        K.P.op('act', lambda e: e.copy(out=out, in_=in_), rd, wr)
    else:
        K.P.op(q, lambda e: e.tensor_copy(out=out, in_=in_), rd, wr)


def o_rcp(out, in_, rd, wr):
    K.P.op('dve', lambda e: e.reciprocal(out=out, in_=in_), rd, wr)


def o_ms(q, out, val, wr):
    K.P.op(q, lambda e: e.memset(out, val), [], wr)


def o_dma(out, in_, rd, wr, q='sp', **kw):
    K.P.dma(q, lambda e: e.dma_start(out=out, in_=in_, **kw), rd, wr)


def bc_load(st, name, src_row, n):
    t, d = sb(st, name, [128, n], F32)
    o_dma(t[:], src_row.partition_broadcast(128), [], [d])
    return t, d


RCH = 8


def rev_tile(c):
    return 1 - c if c < 2 else 35 - c


def phase_rwkv(l, I, S, need_ctx, ident32, ident32d, J32, J32d):
    rwkv_prep(l, I, S, ident32, ident32d, J32, J32d)
    rwkv_scan(S)
    rwkv_readout(l, I, S, need_ctx, J32, J32d)


def rwkv_prep(l, I, S, ident32, ident32d, J32, J32d):
    P = K.P
    MUL, ADD, SUB = ALU.mult, ALU.add, ALU.subtract
    with ExitStack() as st:
        xr = Ring(K, st, 'rv_x', [128, 1024], F32, 2)
        xo = Ring(K, st, 'rv_o', [128, 1024], F32, 2)
        pr = Ring(K, st, 'rv_ps', [128, 512], F32, 2, psum=True)
        for c in range(NTILE):
            tt_ = rev_tile(c)
            (x, xd) = xr.next()
            (o, od) = xo.next()
            o_dma(x[:], S['p'][tt_ * 128:(tt_ + 1) * 128, 0:1024], [S['p_d'][tt_]], [xd])
            for hf in range(2):
                (ps_, psd) = pr.next()
                o_mm(ps_[:], J32[:], x[:, hf * 512:(hf + 1) * 512], [J32d, xd], [psd])
                o_cp('act' if hf else 'dve', o[:, hf * 512:(hf + 1) * 512], ps_[:], [psd], [od])
            o_dma(S['prw1'][c * 128:(c + 1) * 128, :], o[:], [od], [S['prw1_d'][c]])
        P.barrier()
    with ExitStack() as st:
        mub, mubd = bc_load(st, 'rp_mu', I['rwkv_mu'][l, :], 1024)
        kkb, kkbd = bc_load(st, 'rp_kk', I['rwkv_k_k'][l, :], 256)
        kab, kabd = bc_load(st, 'rp_ka', I['rwkv_k_a'][l, :], 256)
        rkb, rkbd = bc_load(st, 'rp_rk', I['rwkv_r_k'][l].rearrange("h d -> (h d)"), 256)
        omka, omkad = sb(st, 'rp_omka', [128, 256], F32)
        K.P.op('dve', lambda e: e.tensor_scalar(out=omka[:], in0=kab[:], scalar1=-1.0, scalar2=1.0, op0=MUL, op1=ADD), [kabd], [omkad])
        w0b, a0b, wup, aup = [], [], [], []
        for d in range(2):
            w0b.append(bc_load(st, 'rp_w0%d' % d, I['rwkv_w0'][l, d, :], 256))
            a0b.append(bc_load(st, 'rp_a0%d' % d, I['rwkv_a0'][l, d, :], 256))
            t, td = sb(st, 'rp_wup%d' % d, [64, 256], F32)
            o_dma(t[:], I['rwkv_w_up'][l, d], [], [td])
            wup.append((t, td))
            t, td = sb(st, 'rp_aup%d' % d, [64, 256], F32)
            o_dma(t[:], I['rwkv_a_up'][l, d], [], [td])
            aup.append((t, td))
        gup, gupd = sb(st, 'rp_gup', [128, 256], F32)
        o_dma(gup[:], I['rwkv_g_up'][l], [], [gupd])

        xr = Ring(K, st, 'rp_x', [128, 1024], F32, 2)
        pvr = Ring(K, st, 'rp_pv', [128, 1024], F32, 2)
        nxr = Ring(K, st, 'rp_nx', [128, 1024], F32, 2)
        xsr = Ring(K, st, 'rp_xs', [128, 1024], F32, 2)
        kkr = Ring(K, st, 'rp_kkn', [128, 256], F32, 2)
        sqr = Ring(K, st, 'rp_sq', [128, 256], F32, 2)
        ssr = Ring(K, st, 'rp_ss', [128, 4], F32, 4)
        smr = Ring(K, st, 'rp_sm', [128, 128], F32, 4)
        sTr = Ring(K, st, 'rp_sT', [128, 128], F32, 4)
        ur = Ring(K, st, 'rp_u', [128, 256], F32, 2)
        ar_ = Ring(K, st, 'rp_a', [128, 256], F32, 2)
        mr = Ring(K, st, 'rp_m', [128, 256], F32, 2)
        kdr = Ring(K, st, 'rp_kd', [128, 256], F32, 3)
        kkar = Ring(K, st, 'rp_kka', [128, 256], F32, 3)
        bor = Ring(K, st, 'rp_bo', [128, 256], F32, 3)
        gtr = Ring(K, st, 'rp_gt', [128, 256], F32, 2)
        nkkr = Ring(K, st, 'rp_nkk', [128, 4, 2, 64], F32, 2)
        rrr = Ring(K, st, 'rp_rr', [128, 4, 2, 64], F32, 2)
        decr = Ring(K, st, 'rp_dec', [128, 4, 2, 64], F32, 2)
        fakr = Ring(K, st, 'rp_fak', [128, 128, 4, 2], F32, 2)
        farr = Ring(K, st, 'rp_far', [128, 128, 4, 2], F32, 2)
        fwr = Ring(K, st, 'rp_fw', [128, 128, 4], F32, 2)
        for ring in (fakr, farr):
            for (b_, bd_) in ring.bufs:
                o_ms('pool', b_[:], 0.0, [bd_])
        pbig = Ring(K, st, 'rp_pb', [128, 256], F32, 3, psum=True)
        ptr_ = Ring(K, st, 'rp_pt', [128, 128], F32, 3, psum=True)

        def v3(ap):
            return ap.rearrange("p (h k) -> p h k", k=64)

        for c in range(NTILE):
            first = c in (0, 2)
            last = c in (1, NTILE - 1)
            r0 = c * 128
            (nkk, nkkd) = nkkr.next()
            (rr, rrd) = rrr.next()
            (dec, decd) = decr.next()
            for d in range(2):
                if d == 0:
                    src = lambda a, b: S['p'][a:b, 0:1024]
                    sdeps = S['p_d']
                else:
                    src = lambda a, b: S['prw1'][a:b, :]
                    sdeps = S['prw1_d']
                nb = [sdeps[c]] + ([sdeps[c - 1]] if c > 0 else []) + ([sdeps[c + 1]] if c < NTILE - 1 else [])
                (x, xd) = xr.next()
                (pv, pvd) = pvr.next()
                (nx, nxd) = nxr.next()
                (xs, xsd) = xsr.next()
                o_dma(x[:], src(r0, r0 + 128), nb, [xd])
                if first:
                    o_ms('pool', pv[:], 0.0, [pvd])
                    o_dma(pv[1:128, :], src(r0, r0 + 127), nb, [pvd])
                else:
                    o_dma(pv[:], src(r0 - 1, r0 + 127), nb, [pvd])
                if last:
                    o_ms('pool', nx[:], 0.0, [nxd])
                    o_dma(nx[0:127, :], src(r0 + 1, r0 + 128), nb, [nxd])
                else:
                    o_dma(nx[:], src(r0 + 1, r0 + 129), nb, [nxd])
                o_tt('pool', pv[:], pv[:], nx[:], ADD, [pvd, nxd], [pvd])
                o_stt('dve', pv[:], pv[:], 0.5, x[:], MUL, SUB, [pvd, xd], [pvd])
                o_tt('pool', pv[:], pv[:], mub[:], MUL, [pvd, mubd], [pvd])
                o_tt('dve', xs[:], x[:], pv[:], ADD, [xd, pvd], [xsd])
                r_ = xs[:, 0:256]
                k_ = xs[:, 256:512]
                v_ = xs[:, 512:768]
                (kkn, kknd) = kkr.next()
                (sq, sqd) = sqr.next()
                (ss, ssd) = ssr.next()
                o_tt('dve', kkn[:], k_, kkb[:], MUL, [xsd, kkbd], [kknd])
                o_tt('pool', sq[:], kkn[:], kkn[:], MUL, [kknd], [sqd])
                o_red('dve', ss[:], v3(sq[:]), ADD, [sqd], [ssd])
                o_act(ss[:], ss[:], AF.Sqrt, [ssd], [ssd], bias=1e-12, scale=1.0)
                o_rcp(ss[:], ss[:], [ssd], [ssd])
                o_tt('dve', v3(kkn[:]), v3(kkn[:]), ss[:].unsqueeze(2).to_broadcast([128, 4, 64]), MUL, [kknd, ssd], [kknd])
                o_ts('dve', nkk[:, :, d, :], v3(kkn[:]), -1.0, MUL, [kknd], [nkkd])
                o_cp('pool', rr[:, :, d, :], v3(r_), [xsd], [rrd])
                (tw, twd) = smr.next()
                (twT, twTd) = sTr.next()
                (pt, ptd) = ptr_.next()
                (pb, pbd) = pbig.next()
                (u, ud) = ur.next()
                o_act(tw[:, 0:64], xs[:, 768:832], AF.Tanh, [xsd], [twd])
                o_tr(pt[0:64, :], tw[:, 0:64], ident32[:], [twd, ident32d], [ptd])
                o_cp('act', twT[0:64, :], pt[0:64, :], [ptd], [twTd])
                o_mm(pb[:], twT[0:64, :], wup[d][0][:], [twTd, wup[d][1]], [pbd])
                o_tt('dve', u[:], pb[:], w0b[d][0][:], ADD, [pbd, w0b[d][1]], [ud])
                o_act(u[:], u[:], AF.Sigmoid, [ud], [ud])
                o_act(dec[:, :, d, :], v3(u[:]), AF.Exp, [ud], [decd], scale=-0.6065306597126334)
                (xa, xad) = smr.next()
                (xaT, xaTd) = sTr.next()
                (pt, ptd) = ptr_.next()
                (pb, pbd) = pbig.next()
                (a, ad) = ar_.next()
                o_cp('pool', xa[:, 0:64], xs[:, 832:896], [xsd], [xad])
                o_tr(pt[0:64, :], xa[:, 0:64], ident32[:], [xad, ident32d], [ptd])
                o_cp('act', xaT[0:64, :], pt[0:64, :], [ptd], [xaTd])
                o_mm(pb[:], xaT[0:64, :], aup[d][0][:], [xaTd, aup[d][1]], [pbd])
                o_tt('dve', a[:], pb[:], a0b[d][0][:], ADD, [pbd, a0b[d][1]], [ad])
                o_act(a[:], a[:], AF.Sigmoid, [ad], [ad])
                (m, md) = mr.next()
                (kd, kdd) = kdr.next()
                (kka, kkad) = kkar.next()
                (bo, bod) = bor.next()
                o_tt('pool', m[:], a[:], kab[:], MUL, [ad, kabd], [md])
                o_tt('pool', m[:], m[:], omka[:], ADD, [md, omkad], [md])
                o_tt('dve', kd[:], k_, m[:], MUL, [xsd, md], [kdd])
                o_tt('pool', kka[:], kkn[:], a[:], MUL, [kknd, ad], [kkad])
                (ss2, ss2d) = ssr.next()
                o_tt('dve', m[:], r_, kd[:], MUL, [xsd, kdd], [md])
                o_tt('pool', m[:], m[:], rkb[:], MUL, [md, rkbd], [md])
                o_red('dve', ss2[:], v3(m[:]), ADD, [md], [ss2d])
                o_tt('dve', v3(bo[:]), v3(v_), ss2[:].unsqueeze(2).to_broadcast([128, 4, 64]), MUL, [xsd, ss2d], [bod])
                o_dma(S['V2'][d, r0:r0 + 128, :], v_, [xsd], [S['V2_d'][c]])
                o_dma(S['KKA'][d, r0:r0 + 128, :], kka[:], [kkad], [S['KKA_d'][c]])
                o_dma(S['KD'][d, r0:r0 + 128, :], kd[:], [kdd], [S['KD_d'][c]])
                o_dma(S['BON'][d, r0:r0 + 128, :], bo[:], [bod], [S['BON_d'][c]])
                if d == 0:
                    (sg, sgd) = smr.next()
                    (sgT, sgTd) = sTr.next()
                    (pt, ptd) = ptr_.next()
                    (pb, pbd) = pbig.next()
                    (gt, gtd) = gtr.next()
                    o_act(sg[:], xs[:, 896:1024], AF.Sigmoid, [xsd], [sgd])
                    o_tr(pt[:], sg[:], ident32[:], [sgd, ident32d], [ptd])
                    o_cp('act', sgT[:], pt[:], [ptd], [sgTd])
                    o_mm(pb[:], sgT[:], gup[:], [sgTd, gupd], [pbd])
                    o_cp('dve', gt[:], pb[:], [pbd], [gtd])
                    o_dma(S['GATE'][r0:r0 + 128, :], gt[:], [gtd], [S['GATE_d'][c]])
            (fak, fakd) = fakr.next()
            (far, fard) = farr.next()
            (fw, fwd) = fwr.next()
            for h in range(4):
                for (srcT, srcd, dstF, dstd) in ((nkk, nkkd, fak, fakd), (rr, rrd, far, fard)):
                    (pt, ptd) = ptr_.next()
                    o_tr(pt[:], srcT[:, h, :, :].rearrange("p d k -> p (d k)"), ident32[:], [srcd, ident32d], [ptd])
                    eng_ = 'act' if h % 2 == 0 else 'dve'
                    o_cp(eng_, dstF[0:64, :, h, 0], pt[0:64, :], [ptd], [dstd])
                    o_cp(eng_, dstF[64:128, :, h, 1], pt[64:128, :], [ptd], [dstd])
                (pt, ptd) = ptr_.next()
                o_tr(pt[:], dec[:, h, :, :].rearrange("p d k -> p (d k)"), ident32[:], [decd, ident32d], [ptd])
                o_cp('act', fw[:, :, h], pt[:], [ptd], [fwd])
            o_dma(S['AKK'][:, r0:r0 + 128, :], fak[:].rearrange("p s h d -> p s (h d)"), [fakd], [S['AKK_d'][c]])
            o_dma(S['AR'][:, r0:r0 + 128, :], far[:].rearrange("p s h d -> p s (h d)"), [fard], [S['AR_d'][c]])
            o_dma(S['WD'][:, r0:r0 + 128, :], fw[:], [fwd], [S['WD_d'][c]])
        P.barrier()


def rwkv_scan(S, T=None):
    P = K.P
    T = NT if T is None else T
    CH = RCH
    nch = T // CH
    with ExitStack() as st:
        ST, _ = sb(st, 'sc_ST', [128, 256], F32)
        STd = [Dep('sc_ST%d' % h) for h in range(4)]
        for ch in range(4):
            o_ms('pool', ST[:, ch * 64:(ch + 1) * 64], 0.0, [STd[ch]])
        NB = 3
        akk = [sb(st, 'sc_akk%d' % i, [128, CH, 8], F32) for i in range(NB)]
        ar = [sb(st, 'sc_ar%d' % i, [128, CH, 8], F32) for i in range(NB)]
        am = [sb(st, 'sc_am%d' % i, [128, CH, 4, 4], F32) for i in range(NB)]
        wt = [sb(st, 'sc_wt%d' % i, [128, CH, 4], F32) for i in range(NB)]
        bt = [sb(st, 'sc_bt%d' % i, [6, CH, 4, 128], F32) for i in range(NB)]
        rt = [sb(st, 'sc_rt%d' % i, [6, CH, 256], F32) for i in range(NB)]
        rtv = [Dep('sc_rtv%d' % i) for i in range(NB)]
        rts = [[Dep('sc_rts%d_%d' % (i, ch)) for ch in range(4)] for i in range(NB)]
        amf, amfd = sb(st, 'sc_amf', [128, 4, 4], F32)
        yfin, yfind = sb(st, 'sc_yfin', [4, 256], F32)
        for (b_, bd_) in bt:
            o_ms('pool', b_[:], 0.0, [bd_])
        sar = [Ring(K, st, 'sc_sa%d' % ch, [128, 64], F32, 1, psum=True) for ch in range(4)]
        ur = [Ring(K, st, 'sc_u%d' % ch, [128, 64], F32, 1, psum=True) for ch in range(4)]

        def load_chunk(c):
            i = c % NB
            s0 = c * CH
            tl = [s0 // 128]
            o_dma(akk[i][0][:], S['AKK'][:, s0:s0 + CH, :], [S['AKK_d'][t] for t in tl], [akk[i][1]])
            o_dma(ar[i][0][:], S['AR'][:, s0:s0 + CH, :], [S['AR_d'][t] for t in tl], [ar[i][1]])
            o_dma(wt[i][0][:], S['WD'][:, s0:s0 + CH, :], [S['WD_d'][t] for t in tl], [wt[i][1]])
            for which, nm in ((0, 'KKA'), (1, 'KD')):
                for d in range(2):
                    row = d if which == 0 else 4 + d
                    o_dma(bt[i][0][row:row + 1, :, :, d * 64:(d + 1) * 64],
                          S[nm][d:d + 1, s0:s0 + CH, :].rearrange("o s (h k) -> o s h k", k=64),
                          [S[nm + '_d'][t] for t in tl], [bt[i][1]])
            o_dma(rt[i][0][4:6, :, :], S['V2'][:, s0:s0 + CH, :], [S['V2_d'][t] for t in tl], [rtv[i]])
            a4 = akk[i][0][:].rearrange("p s (h d) -> p s h d", d=2)
            r4 = ar[i][0][:].rearrange("p s (h d) -> p s h d", d=2)
            o_cp('pool', am[i][0][:, :, :, 0:2], a4, [akk[i][1]], [am[i][1]])
            o_cp('pool', am[i][0][:, 1:CH, :, 2:4], r4[:, 0:CH - 1, :, :], [ar[i][1]], [am[i][1]])
            if c == 0:
                o_ms('pool', am[i][0][:, 0, :, 2:4], 0.0, [am[i][1]])
            else:
                ip = (c - 1) % NB
                rp = ar[ip][0][:].rearrange("p s (h d) -> p s h d", d=2)
                o_cp('pool', am[i][0][:, 0, :, 2:4], rp[:, CH - 1, :, :], [ar[ip][1]], [am[i][1]])

        cv_ld = Ring(K, st, 'sc_cvld', [128, DFF], F32, 2)
        cv_bf = Ring(K, st, 'sc_cvbf', [128, DFF], BF16, 2)
        pend_items = K.PENDING
        K.PENDING = []
        every = max(1, T // max(1, len(pend_items)) - 1) if pend_items else 0
        load_chunk(0)
        if nch > 1:
            load_chunk(1)
        for s in range(T):
            c, j = divmod(s, CH)
            i = c % NB
            if pend_items and s % every == every - 1:
                pend_items.pop(0)(cv_ld, cv_bf, 'pool', 'pool')
            if j == 0 and c + 2 < nch:
                load_chunk(c + 2)
            SAs = [sar[ch].next() for ch in range(4)]
            Us = [ur[ch].next() for ch in range(4)]
            for h in range(4):
                (SA, SAd) = SAs[h]
                o_mm(SA[0:4, :], am[i][0][:, j, h, :], ST[:, h * 64:(h + 1) * 64], [am[i][1], STd[h]], [SAd])
            for h in range(4):
                (SA, SAd) = SAs[h]
                (U, Ud) = Us[h]
                o_cp('act', rt[i][0][0:4, j, h * 64:(h + 1) * 64], SA[0:4, :], [SAd], [rts[i][h]])
                o_mm(U[:], bt[i][0][0:6, j, h, :], rt[i][0][0:6, j, h * 64:(h + 1) * 64], [bt[i][1], rts[i][h], rtv[i]], [Ud])
            for h in range(4):
                (U, Ud) = Us[h]
                STh = ST[:, h * 64:(h + 1) * 64]
                o_stt('dve', STh, STh, wt[i][0][:, j, h:h + 1], U[:], ALU.mult, ALU.add, [STd[h], wt[i][1], Ud], [STd[h]])
            if j == CH - 1:
                s0 = c * CH
                if c == 0:
                    o_dma(S['Y2'][:, 0:CH - 1, :], rt[i][0][2:4, 1:CH, :], rts[i], [S['Y2_d'][0]])
                else:
                    o_dma(S['Y2'][:, s0 - 1:s0 + CH - 1, :], rt[i][0][2:4, :, :], rts[i], [S['Y2_d'][(s0 - 1) // 128]])
        while pend_items:
            pend_items.pop(0)(cv_ld, cv_bf, 'pool', 'pool')
        il = (nch - 1) % NB
        rl = ar[il][0][:].rearrange("p s (h d) -> p s h d", d=2)
        o_ms('pool', amf[:], 0.0, [amfd])
        o_cp('pool', amf[:, :, 2:4], rl[:, CH - 1, :, :], [ar[il][1], amfd], [amfd])
        for h in range(4):
            (SA, SAd) = sar[h].next()
            o_mm(SA[0:4, :], amf[:, h, :], ST[:, h * 64:(h + 1) * 64], [amfd, STd[h]], [SAd])
            o_cp('act', yfin[0:4, h * 64:(h + 1) * 64], SA[0:4, :], [SAd], [yfind])
        o_dma(S['Y2'][:, T - 1, :], yfin[2:4, :], [yfind], [S['Y2_d'][(T - 1) // 128]])
        P.barrier()


def rwkv_readout(l, I, S, need_ctx, J32, J32d):
    P = K.P
    MUL, ADD = ALU.mult, ALU.add
    with ExitStack() as st:
        lng, lngd = bc_load(st, 'ro_lng', I['rwkv_ln_g'][l, :], 256)
        lnb, lnbd = bc_load(st, 'ro_lnb', I['rwkv_ln_b'][l, :], 256)
        yr_ = Ring(K, st, 'ro_y', [128, 256], F32, 3)
        br_ = Ring(K, st, 'ro_b', [128, 256], F32, 3)
        gr_ = Ring(K, st, 'ro_g', [128, 256], F32, 2)
        ycr = Ring(K, st, 'ro_yc', [128, 256], F32, 3)
        sqr = Ring(K, st, 'ro_sq', [128, 256], F32, 2)
        smr = Ring(K, st, 'ro_sm', [128, 4], F32, 6)
        otr = Ring(K, st, 'ro_o', [128, 256], F32, 2)
        psr = Ring(K, st, 'ro_ps', [128, 256], F32, 2, psum=True)

        def v3(ap):
            return ap.rearrange("p (h k) -> p h k", k=64)

        def bc4(ap):
            return ap.unsqueeze(2).to_broadcast([128, 4, 64])

        for t in range(0 if need_ctx else 2, NTILE):
            outs = []
            for d in range(2):
                c = t if d == 0 else rev_tile(t)
                r0 = c * 128
                (y, yd) = yr_.next()
                (b, bd) = br_.next()
                (yc, ycd) = ycr.next()
                (sq, sqd) = sqr.next()
                (sm, smd) = smr.next()
                (vr, vrd) = smr.next()
                o_dma(y[:], S['Y2'][d, r0:r0 + 128, :], [S['Y2_d'][c]], [yd])
                o_dma(b[:], S['BON'][d, r0:r0 + 128, :], [S['BON_d'][c]], [bd])
                o_red('dve', sm[:], v3(y[:]), ADD, [yd], [smd])
                o_ts('dve', sm[:], sm[:], -1.0 / 64, MUL, [smd], [smd])
                o_tt('dve', v3(yc[:]), v3(y[:]), bc4(sm[:]), ADD, [yd, smd], [ycd])
                o_tt('pool', sq[:], yc[:], yc[:], MUL, [ycd], [sqd])
                o_red('dve', vr[:], v3(sq[:]), ADD, [sqd], [vrd])
                o_act(vr[:], vr[:], AF.Sqrt, [vrd], [vrd], bias=64e-5, scale=1.0 / 64)
                o_rcp(vr[:], vr[:], [vrd], [vrd])
                o_tt('dve', v3(yc[:]), v3(yc[:]), bc4(vr[:]), MUL, [ycd, vrd], [ycd])
                o_tt('pool', yc[:], yc[:], lng[:], MUL, [ycd, lngd], [ycd])
                o_tt('pool', yc[:], yc[:], lnb[:], ADD, [ycd, lnbd], [ycd])
                o_tt('dve', yc[:], yc[:], b[:], ADD, [ycd, bd], [ycd])
                outs.append((yc, ycd))
            (ps_, psd) = psr.next()
            (g, gd) = gr_.next()
            (ot, otd) = otr.next()
            o_dma(g[:], S['GATE'][t * 128:(t + 1) * 128, :], [S['GATE_d'][t]], [gd])
            o_mm(ps_[:], J32[:], outs[1][0][:], [J32d, outs[1][1]], [psd])
            o_tt('dve', ot[:], outs[0][0][:], ps_[:], ADD, [outs[0][1], psd], [otd])
            o_tt('dve', ot[:], ot[:], g[:], MUL, [otd, gd], [otd])
            o_dma(S['ocat'][t * 128:(t + 1) * 128, 0:256], ot[:], [otd], [S['ocat_d'][t]])
        P.barrier()


def phase_out(l, I, S, need_ctx, modL, modLd, modC, modCd, ident, identd):
    P = K.P
    MUL, ADD = ALU.mult, ALU.add
    with ExitStack() as st:
        wbf, wbfd = sb(st, 'wo_bf', [128, 8, D], BF16)
        wst = Ring(K, st, 'wo_st', [128, D], F32, 2)
        ocr = Ring(K, st, 'wo_oc', [128, D], F32, 2)
        ocbr = Ring(K, st, 'wo_ocb', [128, D], BF16, 2)
        oTr = Ring(K, st, 'wo_oT', [128, 8, 128], BF16, 2)
        xr = Ring(K, st, 'wo_x', [128, D], F32, 2)
        yr = Ring(K, st, 'wo_y', [128, D], F32, 2)
        sq, sqd = sb(st, 'wo_sq', [128, D], F32)
        ssr = Ring(K, st, 'wo_ss', [128, 1], F32, 4)
        pTr = Ring(K, st, 'wo_pT', [128, 8, 128], BF16, 2, psum=True)
        pyr = Ring(K, st, 'wo_py', [128, 512], F32, 4, psum=True)
        wo = I['w_out'][l].rearrange("(k p) n -> p k n", p=128)
        for kc in range(8):
            (ws, wsd) = wst.next()
            o_dma(ws[:], wo[:, kc, :], [], [wsd])
            o_cp('pool', wbf[:, kc, :], ws[:], [wsd], [wbfd])
        for t in range(0 if need_ctx else 2, NTILE):
            mt, mtd = (modC, modCd) if t < 2 else (modL, modLd)
            (oc, ocd) = ocr.next()
            (ocb, ocbd) = ocbr.next()
            (oT, oTd) = oTr.next()
            (x, xd) = xr.next()
            (y, yd) = yr.next()
            (ss, ssd) = ssr.next()
            (pT, pTd) = pTr.next()
            o_dma(oc[:], S['ocat'][t * 128:(t + 1) * 128, :], [S['ocat_d'][t]], [ocd])
            o_dma(x[:], S['xs'][t * 128:(t + 1) * 128, :], [S['xs_d'][t]], [xd])
            o_cp('pool', ocb[:], oc[:], [ocd], [ocbd])
            for kc in range(8):
                o_tr(pT[:, kc, :], ocb[:, kc * 128:(kc + 1) * 128], ident[:], [ocbd, identd], [pTd])
            o_cp('act', oT[:], pT[:], [pTd], [oTd])
            for hf in range(2):
                (py, pyd) = pyr.next()
                for kc in range(8):
                    o_mm(py[:], oT[:, kc, :], wbf[:, kc, hf * 512:(hf + 1) * 512], [oTd, wbfd], [pyd], start=(kc == 0), stop=(kc == 7))
                o_cp('act' if hf else 'dve', y[:, hf * 512:(hf + 1) * 512], py[:], [pyd], [yd])
            rms_rstd(y[:], yd, D, sq, sqd, ss, ssd, 0)
            o_stt('dve', y[:], y[:], ss[:, 0:1], mt[:, 2, :], MUL, MUL, [yd, ssd, mtd], [yd])
            o_tt('pool', x[:], x[:], y[:], ADD, [xd, yd], [xd])
            o_dma(S['xs'][t * 128:(t + 1) * 128, :], x[:], [xd], [S['xs_d'][t]])
        P.barrier()


def conv_items(I, S, moe, li):
    NEx = NE if moe else 1
    tag = 'm' if moe else 'f'
    items = []
    for e in range(NEx):
        for (nm, dst) in (('gate', 'wgb' + tag), ('up', 'wub' + tag)):
            src = (I['moe_w_' + nm][li, e] if moe else I['ffn_w_' + nm][li]).rearrange("(k p) n -> p k n", p=128)
            for kc in range(8):
                def it(ldr, cvr, eng, q, src=src, kc=kc, dst=dst, e=e):
                    (ld, ldd) = ldr.next()
                    (cv, cvd) = cvr.next()
                    o_dma(ld[:], src[:, kc, :], [], [ldd], q=q)
                    o_cp(eng, cv[:], ld[:], [ldd], [cvd])
                    for q4 in range(2):
                        f0, f1 = q4 * 11, (q4 + 1) * 11
                        o_dma(S[dst][e, f0:f1, :, kc, :].rearrange("f p n -> p f n"),
                              cv[:, f0 * 128:f1 * 128].rearrange("p (f n) -> p f n", n=128), [cvd], [Dep()], q=q)
                items.append(it)
        srcd = (I['moe_w_down'][li, e] if moe else I['ffn_w_down'][li]).rearrange("(f p) n -> p f n", p=128)
        for f0 in range(0, NFF, 2):
            def it(ldr, cvr, eng, q, srcd=srcd, f0=f0, e=e, tag=tag):
                (ld, ldd) = ldr.next()
                (cv, cvd) = cvr.next()
                o_dma(ld[:, 0:2048].rearrange("p (f n) -> p f n", n=1024), srcd[:, f0:f0 + 2, :], [], [ldd], q=q)
                o_cp(eng, cv[:, 0:2048], ld[:, 0:2048], [ldd], [cvd])
                o_dma(S['wdb' + tag][e, f0:f0 + 2, :, :].rearrange("f p n -> p f n"),
                      cv[:, 0:2048].rearrange("p (f n) -> p f n", n=1024), [cvd], [Dep()], q=q)
            items.append(it)
    return items


def ffn_convert(I, S, moe, li):
    if K.CONVERTED.get((moe, li)):
        return
    with ExitStack() as st:
        ldr = Ring(K, st, 'cv_ld', [128, DFF], F32, 3)
        cvr = Ring(K, st, 'cv_bf', [128, DFF], BF16, 3)
        engs = ('dve', 'pool', 'act')
        for n, it in enumerate(conv_items(I, S, moe, li)):
            it(ldr, cvr, engs[n % 3], 'sp')
        K.P.barrier()


def phase_ffn(l, I, S, need_ctx, modL, modLd, modC, modCd, ident32, ident32d, out_ap, out_d):
    P = K.P
    MUL, ADD, SUB = ALU.mult, ALU.add, ALU.subtract
    moe = (l % 2 == 1)
    li = l // 2
    NEx = NE if moe else 1
    tag = 'm' if moe else 'f'
    last_layer = not need_ctx
    ffn_convert(I, S, moe, li)
    tiles = list(range(0 if need_ctx else 2, NTILE))
    split = last_layer
    if split:
        tiles = tiles[0:16]
    with ExitStack() as st:
        xrr = Ring(K, st, 'ff_x', [128, 2, D], F32, 2)
        if split:
            sel, seld = sb(st, 'ff_sel', [128, 2], F32)
            o_dma(sel[:], I['halfsel'][:, :], [], [seld])
            xbr = Ring(K, st, 'ff_xb', [128, D], F32, 2)
            xar = Ring(K, st, 'ff_xa', [128, D], F32, 2)
        hfr = Ring(K, st, 'ff_hf', [128, D], F32, 2)
        hTr = Ring(K, st, 'ff_hT', [128, 8, 256], BF16, 2)
        h32r = Ring(K, st, 'ff_h32', [128, 8, 128], F32, 2)
        sq, sqd = sb(st, 'ff_sq', [128, D], F32)
        ssr = Ring(K, st, 'ff_ss', [128, 1], F32, 4)
        accr = Ring(K, st, 'ff_acc', [128, 2, D], F32, 2)
        gtr = Ring(K, st, 'ff_gate', [128, 2, 8], F32, 2)
        smr = Ring(K, st, 'ff_sm', [128, 8], F32, 8)
        s1r = Ring(K, st, 'ff_s1', [128, 1], F32, 12)
        wgr = Ring(K, st, 'ff_wg', [128, 8, 128], BF16, 3)
        wur = Ring(K, st, 'ff_wu', [128, 8, 128], BF16, 3)
        wdr = Ring(K, st, 'ff_wd', [128, D], BF16, 3)
        sgr = Ring(K, st, 'ff_sg', [128, 256], F32, 2)
        GTr = Ring(K, st, 'ff_GT', [128, 256], BF16, 3)
        pTr = Ring(K, st, 'ff_pT', [128, 4, 128], F32, 1, psum=True)
        gur = Ring(K, st, 'ff_gu', [128, 2, 256], F32, 2, psum=True)
        ops_ = [[ps(st, 'ff_o%d%d' % (a, b), [128, 512], F32) for b in range(2)] for a in range(2)]
        plr = Ring(K, st, 'ff_pl', [128, 8], F32, 1, psum=True)
        if moe:
            rt_, rtd = sb(st, 'ff_router', [128, 8, 8], F32)
            o_dma(rt_[:], I['moe_router'][li].rearrange("(k p) e -> p k e", p=128), [], [rtd])
        for blk in range(len(tiles) // 2):
            tl = tiles[2 * blk:2 * blk + 2]
            (xr, xrd) = xrr.next()
            (hT, hTd) = hTr.next()
            (acc, accd) = accr.next()
            (gate, gated) = gtr.next()
            for jj, t in enumerate(tl):
                mt, mtd = (modC, modCd) if t < 2 else (modL, modLd)
                (hf, hfd) = hfr.next()
                (h32, h32d) = h32r.next()
                (ss, ssd) = ssr.next()
                if split:
                    (xa, xad) = xar.next()
                    (xb, xbd) = xbr.next()
                    t2 = t + 16
                    o_dma(xa[:], S['xs'][t * 128:(t + 1) * 128, :], [S['xs_d'][t]], [xad])
                    o_dma(xb[:], S['xs'][t2 * 128:(t2 + 1) * 128, :], [S['xs_d'][t2]], [xbd], q='pool')
                    o_ts('dve', xa[:], xa[:], sel[:, 0:1], MUL, [xad, seld], [xad])
                    o_stt('dve', xr[:, jj, :], xb[:], sel[:, 1:2], xa[:], MUL, ADD, [xbd, seld, xad], [xrd])
                else:
                    o_dma(xr[:, jj, :], S['xs'][t * 128:(t + 1) * 128, :], [S['xs_d'][t]], [xrd])
                rms_rstd(xr[:, jj, :], xrd, D, sq, sqd, ss, ssd, 0)
                o_stt('dve', hf[:], xr[:, jj, :], ss[:, 0:1], mt[:, 4, :], MUL, MUL, [xrd, ssd, mtd], [hfd])
                o_tt('pool', hf[:], hf[:], mt[:, 3, :], ADD, [hfd, mtd], [hfd])
                for q4 in range(2):
                    (pT, pTd) = pTr.next()
                    for kk_ in range(4):
                        kc = q4 * 4 + kk_
                        o_tr(pT[:, kk_, :], hf[:, kc * 128:(kc + 1) * 128], ident32[:], [hfd, ident32d], [pTd])
                    o_cp('act', h32[:, q4 * 4:(q4 + 1) * 4, :], pT[:], [pTd], [h32d])
                    o_cp('pool', hT[:, q4 * 4:(q4 + 1) * 4, jj * 128:(jj + 1) * 128], h32[:, q4 * 4:(q4 + 1) * 4, :], [h32d], [hTd])
                if moe:
                    (pl, pld) = plr.next()
                    for kc in range(8):
                        o_mm(pl[:], h32[:, kc, :], rt_[:, kc, :], [h32d, rtd], [pld], start=(kc == 0), stop=(kc == 7))
                    (lg, lgd) = smr.next()
                    (lg2, lg2d) = smr.next()
                    (mk1, mk1d) = smr.next()
                    (mk2, mk2d) = smr.next()
                    (m1, m1d) = s1r.next()
                    (m2, m2d) = s1r.next()
                    (ex, exd) = s1r.next()
                    (w1, w1d) = s1r.next()
                    (w2, w2d) = s1r.next()
                    o_cp('dve', lg[:], pl[:], [pld], [lgd])
                    o_red('dve', m1[:], lg[:], ALU.max, [lgd], [m1d])
                    o_ts('dve', mk1[:], lg[:], m1[:, 0:1], ALU.is_equal, [lgd, m1d], [mk1d])
                    o_stt('dve', lg2[:], mk1[:], -1.0e30, lg[:], MUL, ADD, [mk1d, lgd], [lg2d])
                    o_red('dve', m2[:], lg2[:], ALU.max, [lg2d], [m2d])
                    o_ts('dve', mk2[:], lg2[:], m2[:, 0:1], ALU.is_equal, [lg2d, m2d], [mk2d])
                    o_tt('dve', ex[:], m2[:], m1[:], SUB, [m2d, m1d], [exd])
                    o_act(ex[:], ex[:], AF.Exp, [exd], [exd])
                    o_ts('dve', w1[:], ex[:], 1.0, ADD, [exd], [w1d])
                    o_rcp(w1[:], w1[:], [w1d], [w1d])
                    o_tt('dve', w2[:], ex[:], w1[:], MUL, [exd, w1d], [w2d])
                    o_ts('dve', gate[:, jj, :], mk1[:], w1[:, 0:1], MUL, [mk1d, w1d], [gated])
                    o_stt('dve', gate[:, jj, :], mk2[:], w2[:, 0:1], gate[:, jj, :], MUL, ADD, [mk2d, w2d, gated], [gated])
            for e in range(NEx):
                for f in range(NFF):
                    (wg, wgd) = wgr.next()
                    (wu, wud) = wur.next()
                    (wd, wdd) = wdr.next()
                    (gu, gud) = gur.next()
                    (sg, sgd) = sgr.next()
                    (GT, GTd) = GTr.next()
                    o_dma(wg[:], S['wgb' + tag][e, f], [], [wgd])
                    o_dma(wu[:], S['wub' + tag][e, f], [], [wud], q='pool')
                    o_dma(wd[:], S['wdb' + tag][e, f], [], [wdd])
                    for kc in range(8):
                        o_mm(gu[:, 0, :], wg[:, kc, :], hT[:, kc, :], [wgd, hTd], [gud], start=(kc == 0), stop=(kc == 7))
                    for kc in range(8):
                        o_mm(gu[:, 1, :], wu[:, kc, :], hT[:, kc, :], [wud, hTd], [gud], start=(kc == 0), stop=(kc == 7))
                    o_act(sg[:], gu[:, 0, :], AF.Silu, [gud], [sgd])
                    o_tt('dve', GT[:], sg[:], gu[:, 1, :], MUL, [sgd, gud], [GTd])
                    for jj in range(2):
                        for hf_ in range(2):
                            (op_, opd) = ops_[jj][hf_]
                            o_mm(op_[:], GT[:, jj * 128:(jj + 1) * 128], wd[:, hf_ * 512:(hf_ + 1) * 512], [GTd, wdd], [opd],
                                 start=(f == 0), stop=(f == NFF - 1))
                for jj in range(2):
                    for hf_ in range(2):
                        (op_, opd) = ops_[jj][hf_]
                        dst = acc[:, jj, hf_ * 512:(hf_ + 1) * 512]
                        if not moe:
                            o_cp('act', dst, op_[:], [opd], [accd])
                        elif e == 0:
                            o_ts('dve', dst, op_[:], gate[:, jj, e:e + 1], MUL, [opd, gated], [accd])
                        else:
                            o_stt('dve', dst, op_[:], gate[:, jj, e:e + 1], dst, MUL, ADD, [opd, gated, accd], [accd])
            for jj, t in enumerate(tl):
                mt, mtd = (modC, modCd) if t < 2 else (modL, modLd)
                (ss, ssd) = ssr.next()
                rms_rstd(acc[:, jj, :], accd, D, sq, sqd, ss, ssd, 0)
                o_stt('dve', acc[:, jj, :], acc[:, jj, :], ss[:, 0:1], mt[:, 5, :], MUL, MUL, [accd, ssd, mtd], [accd])
                o_tt('pool', acc[:, jj, :], acc[:, jj, :], xr[:, jj, :], ADD, [accd, xrd], [accd])
                if last_layer:
                    o_dma(out_ap[(t - 2) * 128:(t - 1) * 128, :], acc[:, jj, :], [accd], [out_d])
                else:
                    o_dma(S['xs'][t * 128:(t + 1) * 128, :], acc[:, jj, :], [accd], [S['xs_d'][t]])
        P.barrier()


IN_NAMES = ['x', 'c', 'ctx', 'c_ctx', 'mod_w', 'mod_b', 'norm_g', 'w_in', 'w_out', 'rwkv_mu', 'rwkv_w0', 'rwkv_w_up',
            'rwkv_a0', 'rwkv_a_up', 'rwkv_g_up', 'rwkv_k_k', 'rwkv_k_a', 'rwkv_r_k', 'rwkv_ln_g', 'rwkv_ln_b',
            'gqa_q_g', 'gqa_k_g', 'mla_q_norm_g', 'mla_w_uq', 'mla_kv_norm_g', 'mla_w_ukv', 'nat_bias',
            'ffn_w_gate', 'ffn_w_up', 'ffn_w_down', 'moe_router', 'moe_w_gate', 'moe_w_up', 'moe_w_down']


def build(shapes, stop_after=None, debug=(), only=None, layers=(0, 1), scan_T=None):
    nc = bass.Bass("TRN2", target_bir_lowering=False)
    K.nc = nc
    P = Prog(nc)
    K.P = P
    I = {}
    for name in IN_NAMES:
        I[name] = nc.dram_tensor(name, list(shapes[name]), F32, kind="ExternalInput").ap()
    out = nc.dram_tensor("out", [NL // 2, D], F32, kind="ExternalOutput").ap()
    I['halfsel'] = nc.dram_tensor('halfsel', [128, 2], F32, kind='ExternalInput').ap()
    out_d = Dep('out')
    S = {}

    def scratch(name, shape, dtype=F32, tiled=True):
        S[name] = dram(name, shape, dtype)
        S[name + '_d'] = [Dep('%s%d' % (name, t)) for t in range(NTILE)] if tiled else Dep(name)

    scratch('xs', [NT, D])
    scratch('p', [NT, INC])
    scratch('ocat', [NT, D])
    scratch('prw1', [NT, 1024])
    for nm in ('V2', 'KKA', 'KD', 'BON', 'Y2'):
        scratch(nm, [2, NT, 256])
    scratch('GATE', [NT, 256])
    scratch('AKK', [128, NT, 8])
    scratch('AR', [128, NT, 8])
    scratch('WD', [128, NT, 4])
    for tag, ne in (('f', 1), ('m', NE)):
        scratch('wgb' + tag, [ne, NFF, 128, 8, 128], BF16, tiled=False)
        scratch('wub' + tag, [ne, NFF, 128, 8, 128], BF16, tiled=False)
        scratch('wdb' + tag, [ne, NFF, 128, D], BF16, tiled=False)
    I['rope_g'] = nc.dram_tensor('rope_g', [NL, 2, 32], F32, kind='ExternalInput').ap()
    I['rope_m'] = nc.dram_tensor('rope_m', [NL, 2, 16], F32, kind='ExternalInput').ap()
    I['nat_tab'] = nc.dram_tensor('nat_tab', [2, 128, 4, len(nat_plan()[1]), 64], F32, kind='ExternalInput').ap()
    with ExitStack() as st:
        P.alloc_sems(st)
        ident, identd = sb(st, 'ident', [128, 128], BF16)
        modL, modLd = sb(st, 'modL', [128, 6, D], F32)
        modC, modCd = sb(st, 'modC', [128, 6, D], F32)
        ident32, ident32d = sb(st, 'ident32', [128, 128], F32)
        J32, J32d = sb(st, 'J32', [128, 128], F32)
        P.op('pool', lambda e: e.memset(ident[:], 1.0), [], [identd])
        P.op('pool', lambda e: e.memset(ident32[:], 1.0), [], [ident32d])
        P.op('pool', lambda e: e.memset(J32[:], 1.0), [], [J32d])
        P.op('pool', lambda e: e.affine_select(out=ident32[:], in_=ident32[:], pattern=[[-1, 128]], compare_op=ALU.is_equal, fill=0.0, base=0, channel_multiplier=1),
             [ident32d], [ident32d])
        P.op('pool', lambda e: e.affine_select(out=ident[:], in_=ident[:], pattern=[[-1, 128]], compare_op=ALU.is_equal, fill=0.0, base=0, channel_multiplier=1),
             [identd], [identd])
        P.op('pool', lambda e: e.affine_select(out=J32[:], in_=J32[:], pattern=[[1, 128]], compare_op=ALU.is_equal, fill=0.0, base=-127, channel_multiplier=1),
             [J32d], [J32d])
        P.dma('sp', lambda e: e.dma_start(out=S['xs'][0:NC_, :], in_=I['ctx'][:, :]), [], S['xs_d'][0:2])
        for j in range(4):
            P.dma('sp', lambda e, j=j: e.dma_start(out=S['xs'][NC_ + j * 1024:NC_ + (j + 1) * 1024, :], in_=I['x'][j * 1024:(j + 1) * 1024, :]),
                  [], S['xs_d'][2 + 8 * j:2 + 8 * (j + 1)])

        def want(name):
            return only is None or name in only

        K.CONVERTED = {}
        K.PENDING = []
        if only is None and tuple(layers) == (0, 1):
            K.PENDING = conv_items(I, S, False, 0) + conv_items(I, S, True, 0)
            K.CONVERTED = {(False, 0): True, (True, 0): True}
        for l in layers:
            need_ctx = (l == 0)
            if want('mod'):
                phase_mod(l, I, modL, modLd, modC, modCd)
            if want('in'):
                phase_in(l, I, S, modL, modLd, modC, modCd, ident, identd)
            if want('rwkv'):
                if scan_T is None:
                    phase_rwkv(l, I, S, need_ctx, ident32, ident32d, J32, J32d)
                else:
                    rwkv_prep(l, I, S, ident32, ident32d, J32, J32d)
                    rwkv_scan(S, scan_T)
            if want('gqa'):
                phase_gqa(l, I, S, need_ctx, ident, identd, ident32, ident32d)
            if want('mla'):
                phase_mla(l, I, S, need_ctx, ident, identd, ident32, ident32d)
            if want('nat'):
                phase_nat(l, I, S, need_ctx, ident, identd, ident32, ident32d)
            if stop_after == ('attn', l):
                break
            if want('out'):
                phase_out(l, I, S, need_ctx, modL, modLd, modC, modCd, ident, identd)
            if stop_after == ('out', l):
                break
            if want('ffn'):
                phase_ffn(l, I, S, need_ctx, modL, modLd, modC, modCd, ident32, ident32d, out, out_d)
        for name in debug:
            src = S[name]
            d = nc.dram_tensor('dbg_' + name, list(src.shape), src.dtype, kind="ExternalOutput").ap()
            dd = Dep('dbg_' + name)
            deps = S[name + '_d'] if isinstance(S[name + '_d'], list) else [S[name + '_d']]
            P.dma('sp', lambda e, d=d, src=src: e.dma_start(out=d, in_=src), deps, [dd])
        P.barrier()
        P.emit()
    return nc


def core_shapes(inputs):
    sh = {k: tuple(np.asarray(v).shape) for k, v in inputs.items()}
    sh['x'] = (NL, D)
    sh['c'] = (D,)
    sh['ctx'] = (NC_, D)
    return sh


def rope_tables(rot_dim):
    t = np.arange(NL)
    row = (t // 64).astype(np.float32)
    col = (t % 64).astype(np.float32)
    quarter = rot_dim // 4
    inv_freq = (np.float32(10000.0) ** (-np.arange(quarter, dtype=np.float32) / np.float32(quarter))).astype(np.float32)
    ang = np.concatenate([row[:, None] * inv_freq, col[:, None] * inv_freq], axis=-1).astype(np.float32)
    return np.ascontiguousarray(np.stack([np.cos(ang), np.sin(ang)], axis=1).astype(np.float32))


def core_inputs(inputs, b, shared=None):
    if shared is None:
        shared = {k: np.ascontiguousarray(np.asarray(v, dtype=np.float32)) for k, v in inputs.items() if k not in ('x', 'c', 'ctx')}
        shared['rope_g'] = rope_tables(64)
        shared['rope_m'] = rope_tables(32)
        nb = np.asarray(inputs['nat_bias'], np.float32)
        shared['nat_tab'] = np.ascontiguousarray(np.stack([nat_bias_table(nb[0]), nat_bias_table(nb[1])], 0))
    m = dict(shared)
    m['x'] = np.ascontiguousarray(np.asarray(inputs['x'][b], dtype=np.float32))
    m['c'] = np.ascontiguousarray(np.asarray(inputs['c'][b], dtype=np.float32))
    m['ctx'] = np.ascontiguousarray(np.asarray(inputs['ctx'][b], dtype=np.float32))
    return m


def kernel(**inputs):
    nb = 4
    shapes = core_shapes(inputs)
    nc = build(shapes)
    first = core_inputs(inputs, 0)
    shared = {k: v for k, v in first.items() if k not in ('x', 'c', 'ctx')}
    per_b = [first] + [core_inputs(inputs, b, shared) for b in range(1, nb)]
    maps = []
    for b in range(nb):
        for m in range(2):
            mm_ = dict(per_b[b])
            hs = np.zeros((128, 2), np.float32)
            hs[:, m] = 1.0
            mm_['halfsel'] = hs
            maps.append(mm_)
    res = run_bass_kernel_spmd(nc, maps, core_ids=list(range(2 * nb)))
    out = np.empty((nb, NL, D), np.float32)
    for b in range(nb):
        for m in range(2):
            out[b, m * (NL // 2):(m + 1) * (NL // 2)] = np.asarray(res.results[2 * b + m]['out'], dtype=np.float32)
    return out
```

```python
import numpy as np
from contextlib import ExitStack
import concourse.bass as bass
import concourse.mybir as mybir
from concourse.bass_utils import run_bass_kernel_spmd

F32 = mybir.dt.float32
BF16 = mybir.dt.bfloat16
AF = mybir.ActivationFunctionType
ALU = mybir.AluOpType
AX = mybir.AxisListType

COMPUTE = ('pe', 'act', 'dve', 'pool')
QUEUES = ('pe', 'act', 'dve', 'pool', 'sp')
NRING = 8

D = 1024
NL = 4096
NC_ = 256
NT = NL + NC_
NTILE = NT // 128
INC = 2720
RW0, GQ0, ML0, NA0 = 0, 1024, 1536, 1952
DFF = 2816
NFF = DFF // 128
NE = 8
RMS_EPS = 1e-6


class Dep:
    __slots__ = ('w', 'r', 'name')

    def __init__(self, name=''):
        self.w = None
        self.r = {}
        self.name = name


class Prog:
    def __init__(self, nc):
        self.nc = nc
        self.q = {e: [] for e in QUEUES}
        self.cnt = {e: 0 for e in COMPUTE}
        self.seen = {e: {} for e in QUEUES}
        self.sems = {}
        self.dma_cnt = {}
        self.dma_next = {e: 0 for e in QUEUES}
        self.ninstr = 0

    def alloc_sems(self, stack):
        for e in COMPUTE:
            self.sems[e] = stack.enter_context(self.nc.semaphore('s_' + e))
        for qn in ('sp', 'pool', 'act'):
            for j in range(NRING):
                key = ('dma', qn, j)
                self.sems[key] = stack.enter_context(self.nc.semaphore('d_%s_%d' % (qn, j)))
                self.dma_cnt[key] = 0

    def _need(self, queue, key, count, waits):
        if self.seen[queue].get(key, 0) >= count:
            return
        if waits.get(key, 0) < count:
            waits[key] = count

    def _emit_waits(self, queue, waits):
        for key, count in waits.items():
            sem = self.sems[key]
            val = count * (16 if isinstance(key, tuple) else 1)
            self.q[queue].append(lambda e, sem=sem, val=val: e.wait_ge(sem, val))
            self.seen[queue][key] = count
            self.ninstr += 1

    def _collect(self, queue, reads, writes):
        waits = {}
        for t in reads:
            if t.w is not None:
                self._need(queue, t.w[0], t.w[1], waits)
        for t in writes:
            if t.w is not None:
                if not (queue == 'pe' and t.w[0] == 'pe'):
                    self._need(queue, t.w[0], t.w[1], waits)
            for k, c in t.r.items():
                if k == queue:
                    continue
                self._need(queue, k, c, waits)
        return waits

    def op(self, queue, fn, reads=(), writes=()):
        waits = self._collect(queue, reads, writes)
        self._emit_waits(queue, waits)
        self.cnt[queue] += 1
        c = self.cnt[queue]
        sem = self.sems[queue]
        self.q[queue].append(lambda e, fn=fn, sem=sem: fn(e).then_inc(sem, 1))
        self.ninstr += 1
        for t in reads:
            if t.r.get(queue, 0) < c:
                t.r[queue] = c
        for t in writes:
            t.w = (queue, c)
            t.r = {}

    def dma(self, queue, fn, reads=(), writes=()):
        j = self.dma_next[queue]
        self.dma_next[queue] = (j + 1) % NRING
        key = ('dma', queue, j)
        waits = self._collect(queue, reads, writes)
        n = self.dma_cnt[key]
        if n > 0:
            self._need(queue, key, n, waits)
        self._emit_waits(queue, waits)
        self.dma_cnt[key] = n + 1
        sem = self.sems[key]
        self.q[queue].append(lambda e, fn=fn, sem=sem: fn(e).then_inc(sem, 16))
        self.ninstr += 1
        for t in reads:
            t.r[key] = n + 1
        for t in writes:
            t.w = (key, n + 1)
            t.r = {}

    def barrier(self, queues=QUEUES):
        for qn in queues:
            waits = {}
            for e in COMPUTE:
                if self.cnt[e] > 0 and e != qn:
                    self._need(qn, e, self.cnt[e], waits)
            for key, n in self.dma_cnt.items():
                if n > 0:
                    self._need(qn, key, n, waits)
            self._emit_waits(qn, waits)

    def emit(self):
        nc = self.nc
        with nc.Block() as block:
            @block.tensor
            def _(e):
                for f in self.q['pe']:
                    f(e)

            @block.scalar
            def _(e):
                for f in self.q['act']:
                    f(e)

            @block.vector
            def _(e):
                for f in self.q['dve']:
                    f(e)

            @block.gpsimd
            def _(e):
                for f in self.q['pool']:
                    f(e)

            @block.sync
            def _(e):
                for f in self.q['sp']:
                    f(e)


class Ring:
    def __init__(self, K, st, name, shape, dtype, n, psum=False):
        self.bufs = []
        for i in range(n):
            if psum:
                full = [128, 512] if dtype == F32 else [128, 1024]
                n = 1
                for d_ in shape[1:]:
                    n *= d_
                assert n <= full[1]
                t = st.enter_context(K.nc.psum_tensor(uname('%s%d' % (name, i)), full, dtype))
                t = t[0:shape[0], 0:n]
                if len(shape) == 3:
                    t = t.rearrange("p (a b) -> p a b", b=shape[2])
            else:
                t = st.enter_context(K.nc.sbuf_tensor(uname('%s%d' % (name, i)), shape, dtype))
            self.bufs.append((t, Dep('%s%d' % (name, i))))
        self.i = 0

    def next(self):
        b = self.bufs[self.i]
        self.i = (self.i + 1) % len(self.bufs)
        return b


class K:
    LVL = 99
    UID = 0
    CONVERTED = {}
    PENDING = []


def uname(name):
    K.UID += 1
    return '%s_u%d' % (name, K.UID)


def sb(st, name, shape, dtype):
    return st.enter_context(K.nc.sbuf_tensor(uname(name), shape, dtype)), Dep(name)


def ps(st, name, shape, dtype):
    return st.enter_context(K.nc.psum_tensor(uname(name), shape, dtype)), Dep(name)


def dram(name, shape, dtype):
    return K.nc.dram_tensor(name, shape, dtype, kind="Internal").ap()


def rms_rstd(x_ap, xdep, n, sq, sqd, ss, ssd, col):
    P = K.P
    P.op('dve', lambda e: e.tensor_tensor(out=sq[:, 0:n], in0=x_ap, in1=x_ap, op=ALU.mult), [xdep], [sqd])
    P.op('dve', lambda e: e.tensor_reduce(out=ss[:, col:col + 1], in_=sq[:, 0:n], axis=AX.X, op=ALU.add), [sqd], [ssd])
    P.op('act', lambda e: e.activation(out=ss[:, col:col + 1], in_=ss[:, col:col + 1], func=AF.Sqrt, bias=RMS_EPS, scale=1.0 / n), [ssd], [ssd])
    P.op('dve', lambda e: e.reciprocal(out=ss[:, col:col + 1], in_=ss[:, col:col + 1]), [ssd], [ssd])


def phase_mod(l, I, modL, modLd, modC, modCd):
    nc, P = K.nc, K.P
    with ExitStack() as st:
        cv, cvd = sb(st, 'cv', [128, 2, 8], F32)
        cs, csd = sb(st, 'cs', [128, 2, 8], F32)
        crep, crepd = sb(st, 'crep', [128, 2, 8, 128], BF16)
        gb, gbd = sb(st, 'gb', [128, 4, D], F32)
        wst = Ring(K, st, 'mw_st', [128, 8, 512], F32, 2)
        wbf = Ring(K, st, 'mw_bf', [128, 8, 512], BF16, 2)
        bb = Ring(K, st, 'mbb', [128, 512], F32, 2)
        pm = Ring(K, st, 'pmod', [128, 512], F32, 4, psum=True)
        P.dma('sp', lambda e: e.dma_start(out=cv[:, 0, :], in_=I['c'].rearrange("(k p) -> p k", p=128), allow_slow_non_contiguous=True), [], [cvd])
        P.dma('sp', lambda e: e.dma_start(out=cv[:, 1, :], in_=I['c_ctx'].rearrange("(k p) -> p k", p=128), allow_slow_non_contiguous=True), [], [cvd])
        P.dma('sp', lambda e: e.dma_start(out=gb[:], in_=I['norm_g'][l].partition_broadcast(128)), [], [gbd])
        P.op('act', lambda e: e.activation(out=cs[:], in_=cv[:], func=AF.Silu), [cvd], [csd])
        P.op('dve', lambda e: e.tensor_copy(out=crep[:], in_=cs[:].unsqueeze(3).to_broadcast([128, 2, 8, 128])), [csd], [crepd])
        mw = I['mod_w'][l].rearrange("(k p) n -> p k n", p=128)
        for nb in range(12):
            (ws, wsd) = wst.next()
            (wb, wbd) = wbf.next()
            (bt, btd) = bb.next()
            P.dma('sp', lambda e, ws=ws, nb=nb: e.dma_start(out=ws[:], in_=mw[:, :, nb * 512:(nb + 1) * 512]), [], [wsd])
            P.dma('sp', lambda e, bt=bt, nb=nb: e.dma_start(out=bt[:], in_=I['mod_b'][l, nb * 512:(nb + 1) * 512].partition_broadcast(128)), [], [btd])
            P.op('pool', lambda e, ws=ws, wb=wb: e.tensor_copy(out=wb[:], in_=ws[:]), [wsd], [wbd])
            j, off = nb // 2, (nb % 2) * 512
            for s, (mt, mtd) in enumerate(((modL, modLd), (modC, modCd))):
                (pt, ptd) = pm.next()
                for kc in range(8):
                    P.op('pe', lambda e, pt=pt, wb=wb, kc=kc, s=s: e.matmul(pt[:], lhsT=crep[:, s, kc, :], rhs=wb[:, kc, :], start=(kc == 0), stop=(kc == 7)),
                         [crepd, wbd], [ptd])
                P.op('dve', lambda e, pt=pt, bt=bt, mt=mt, j=j, off=off: e.tensor_tensor(out=mt[:, j, off:off + 512], in0=pt[:], in1=bt[:], op=ALU.add),
                     [ptd, btd], [mtd])
        for (mt, mtd) in ((modL, modLd), (modC, modCd)):
            for j, gi, plus1 in ((1, 0, True), (2, 1, False), (4, 2, True), (5, 3, False)):
                if plus1:
                    P.op('dve', lambda e, mt=mt, j=j, gi=gi: e.scalar_tensor_tensor(out=mt[:, j, :], in0=mt[:, j, :], scalar=1.0, in1=gb[:, gi, :], op0=ALU.add, op1=ALU.mult),
                         [mtd, gbd], [mtd])
                else:
                    P.op('dve', lambda e, mt=mt, j=j, gi=gi: e.tensor_tensor(out=mt[:, j, :], in0=mt[:, j, :], in1=gb[:, gi, :], op=ALU.mult),
                         [mtd, gbd], [mtd])
        P.barrier()


def phase_in(l, I, S, modL, modLd, modC, modCd, ident, identd):
    nc, P = K.nc, K.P
    with ExitStack() as st:
        wbf, wbfd = sb(st, 'win_bf', [128, 8, INC], BF16)
        wst = Ring(K, st, 'win_st', [128, INC], F32, 2)
        xr = Ring(K, st, 'in_x', [128, D], F32, 3)
        hr = Ring(K, st, 'in_h', [128, D], F32, 2)
        hbr = Ring(K, st, 'in_hb', [128, D], BF16, 2)
        hTr = Ring(K, st, 'in_hT', [128, 8, 128], BF16, 2)
        sq, sqd = sb(st, 'in_sq', [128, D], F32)
        ssr = Ring(K, st, 'in_ss', [128, 1], F32, 4)
        pr = Ring(K, st, 'in_p', [128, INC], F32, 2)
        ptr = Ring(K, st, 'in_pT', [128, 8, 128], BF16, 2, psum=True)
        pmr = Ring(K, st, 'in_pm', [128, 512], F32, 4, psum=True)
        win = I['w_in'][l].rearrange("(k p) n -> p k n", p=128)
        for kc in range(8):
            (ws, wsd) = wst.next()
            P.dma('sp', lambda e, ws=ws, kc=kc: e.dma_start(out=ws[:], in_=win[:, kc, :]), [], [wsd])
            P.op('pool', lambda e, ws=ws, kc=kc: e.tensor_copy(out=wbf[:, kc, :], in_=ws[:]), [wsd], [wbfd])
        for t in range(NTILE):
            mt, mtd = (modC, modCd) if t < 2 else (modL, modLd)
            (x, xd) = xr.next()
            (h, hd) = hr.next()
            (hb, hbd) = hbr.next()
            (hT, hTd) = hTr.next()
            (ss, ssd) = ssr.next()
            (pt, ptd) = pr.next()
            (pT, pTd) = ptr.next()
            P.dma('sp', lambda e, x=x, t=t: e.dma_start(out=x[:], in_=S['xs'][t * 128:(t + 1) * 128, :]), [S['xs_d'][t]], [xd])
            rms_rstd(x[:], xd, D, sq, sqd, ss, ssd, 0)
            P.op('dve', lambda e, h=h, x=x, ss=ss, mt=mt: e.scalar_tensor_tensor(out=h[:], in0=x[:], scalar=ss[:, 0:1], in1=mt[:, 1, :], op0=ALU.mult, op1=ALU.mult),
                 [xd, ssd, mtd], [hd])
            P.op('pool', lambda e, h=h, hb=hb, mt=mt: e.tensor_tensor(out=hb[:], in0=h[:], in1=mt[:, 0, :], op=ALU.add), [hd, mtd], [hbd])
            for kc in range(8):
                P.op('pe', lambda e, pT=pT, hb=hb, kc=kc: e.transpose(out=pT[:, kc, :], in_=hb[:, kc * 128:(kc + 1) * 128], identity=ident[:]),
                     [hbd, identd], [pTd])
            P.op('act', lambda e, hT=hT, pT=pT: e.copy(out=hT[:], in_=pT[:]), [pTd], [hTd])
            for nb in range(6):
                c0 = nb * 512
                cw = min(512, INC - c0)
                (pm, pmd) = pmr.next()
                for kc in range(8):
                    P.op('pe', lambda e, pm=pm, hT=hT, kc=kc, c0=c0, cw=cw: e.matmul(pm[:, 0:cw], lhsT=hT[:, kc, :], rhs=wbf[:, kc, c0:c0 + cw], start=(kc == 0), stop=(kc == 7)),
                         [hTd, wbfd], [pmd])
                if nb % 2 == 0:
                    P.op('act', lambda e, pm=pm, pt=pt, c0=c0, cw=cw: e.copy(out=pt[:, c0:c0 + cw], in_=pm[:, 0:cw]), [pmd], [ptd])
                else:
                    P.op('dve', lambda e, pm=pm, pt=pt, c0=c0, cw=cw: e.tensor_copy(out=pt[:, c0:c0 + cw], in_=pm[:, 0:cw]), [pmd], [ptd])
            P.dma('sp', lambda e, pt=pt, t=t: e.dma_start(out=S['p'][t * 128:(t + 1) * 128, :], in_=pt[:]), [ptd], [S['p_d'][t]])
        P.barrier()


def attn_finalize(st_rings, O, Od, h, ot, otd, nq, ident32, ident32d):
    P = K.P
    osbr, ptr, rvr = st_rings
    (osb, osbd) = osbr.next()
    P.op('dve', lambda e: e.tensor_copy(out=osb[:, 0:nq], in_=O[:, 0:nq]), [Od], [osbd])
    for j in range(nq // 128):
        (pt, ptd) = ptr.next()
        (rv, rvd) = rvr.next()
        P.op('pe', lambda e, pt=pt, osb=osb, j=j: e.transpose(out=pt[:], in_=osb[:, j * 128:(j + 1) * 128], identity=ident32[0:65, 0:65]),
             [osbd, ident32d], [ptd])
        P.op('dve', lambda e, pt=pt, rv=rv: e.reciprocal(out=rv[:], in_=pt[:, 64:65]), [ptd], [rvd])
        P.op('dve', lambda e, pt=pt, rv=rv, j=j: e.tensor_scalar(out=ot[:, j, h * 64:(h + 1) * 64], in0=pt[:, 0:64], scalar1=rv[:, 0:1], scalar2=None, op0=ALU.mult),
             [ptd, rvd], [otd])


def attn_core(st, S, QT, QTd, KT, KTd, kvmap, Vaug, Vaugd, scale, col0, need_ctx, ident32, ident32d, tag):
    P = K.P
    sr = Ring(K, st, tag + '_S', [128, 512], F32, 3, psum=True)
    orr = Ring(K, st, tag + '_O', [65, 512], F32, 2, psum=True)
    ptr = Ring(K, st, tag + '_fT', [128, 65], F32, 2, psum=True)
    pr = Ring(K, st, tag + '_P', [128, 512], BF16, 3)
    osbr = Ring(K, st, tag + '_osb', [65, 512], F32, 2)
    rvr = Ring(K, st, tag + '_rv', [128, 1], F32, 4)
    otr = Ring(K, st, tag + '_ot', [128, 4, 256], F32, 2)
    blocks = []
    if need_ctx:
        blocks.append((0, 256, [0, 1]))
    for qb in range(8):
        blocks.append((256 + qb * 512, 512, list(range(NTILE))))
    for (q0, nq, kts) in blocks:
        (ot, otd) = otr.next()
        for h in range(4):
            g = kvmap[h]
            (O, Od) = orr.next()
            pend = None
            for i, kt in enumerate(kts):
                (Sp, Spd) = sr.next()
                (Pt, Ptd) = pr.next()
                P.op('pe', lambda e, Sp=Sp, g=g, h=h, kt=kt, q0=q0, nq=nq: e.matmul(Sp[:, 0:nq], lhsT=KT(g)[:, kt * 128:(kt + 1) * 128], rhs=QT(h)[:, q0:q0 + nq], start=True, stop=True),
                     [QTd, KTd], [Spd])
                P.op('act', lambda e, Sp=Sp, Pt=Pt, nq=nq: e.activation(out=Pt[:, 0:nq], in_=Sp[:, 0:nq], func=AF.Exp, scale=scale), [Spd], [Ptd])
                if pend is not None:
                    pend()

                def pend(O=O, Od=Od, g=g, kt=kt, Pt=Pt, Ptd=Ptd, nq=nq, i=i, n=len(kts)):
                    P.op('pe', lambda e: e.matmul(O[:, 0:nq], lhsT=Vaug[:, kt, g, :], rhs=Pt[:, 0:nq], start=(i == 0), stop=(i == n - 1)),
                         [Vaugd, Ptd], [Od])
            pend()
            attn_finalize((osbr, ptr, rvr), O, Od, h, ot, otd, nq, ident32, ident32d)
        nj = nq // 128
        P.dma('sp', lambda e, ot=ot, q0=q0, nj=nj: e.dma_start(out=S['ocat'][q0:q0 + nj * 128, col0:col0 + 256].rearrange("(j p) c -> p j c", p=128), in_=ot[:, 0:nj, :]),
              [otd], [S['ocat_d'][t] for t in range(q0 // 128, q0 // 128 + nj)])


def phase_gqa(l, I, S, need_ctx, ident, identd, ident32, ident32d):
    nc, P = K.nc, K.P
    with ExitStack() as st:
        QKT, QKTd = sb(st, 'gq_QKT', [64, 6, NT], BF16)
        Vaug, Vaugd = sb(st, 'gq_V', [128, NTILE, 2, 65], BF16)
        with ExitStack() as st2:
            gain, gaind = sb(st2, 'gq_gain', [128, 6, 64], F32)
            xr = Ring(K, st2, 'gq_x', [128, 512], F32, 3)
            sq, sqd = sb(st2, 'gq_sq', [128, 384], F32)
            ssr = Ring(K, st2, 'gq_ss', [128, 6], F32, 3)
            qnr = Ring(K, st2, 'gq_qn', [128, 6, 64], F32, 2)
            qbr = Ring(K, st2, 'gq_qb', [128, 6, 64], BF16, 2)
            csr = Ring(K, st2, 'gq_cs', [128, 2, 32], F32, 3)
            t1r = Ring(K, st2, 'gq_t1', [128, 6, 32], F32, 2)
            t2r = Ring(K, st2, 'gq_t2', [128, 6, 32], F32, 2)
            pTr = Ring(K, st2, 'gq_pT', [64, 6, 128], BF16, 2, psum=True)
            P.dma('sp', lambda e: e.dma_start(out=gain[:, 0:4, :], in_=I['gqa_q_g'][l:l + 1, :].partition_broadcast(128).to_broadcast([128, 4, 64])), [], [gaind])
            P.dma('sp', lambda e: e.dma_start(out=gain[:, 4:6, :], in_=I['gqa_k_g'][l:l + 1, :].partition_broadcast(128).to_broadcast([128, 2, 64])), [], [gaind])
            P.op('pool', lambda e: e.memset(Vaug[:, :, :, 64:65], 1.0), [], [Vaugd])
            for t in range(NTILE):
                (x, xd) = xr.next()
                (ss, ssd) = ssr.next()
                (qn, qnd) = qnr.next()
                (qb, qbd) = qbr.next()
                (pT, pTd) = pTr.next()
                P.dma('sp', lambda e, x=x, t=t: e.dma_start(out=x[:], in_=S['p'][t * 128:(t + 1) * 128, GQ0:GQ0 + 512]), [S['p_d'][t]], [xd])
                P.op('dve', lambda e, x=x: e.tensor_tensor(out=sq[:], in0=x[:, 0:384], in1=x[:, 0:384], op=ALU.mult), [xd], [sqd])
                P.op('dve', lambda e, ss=ss: e.tensor_reduce(out=ss[:], in_=sq[:].rearrange("p (g d) -> p g d", d=64), axis=AX.X, op=ALU.add), [sqd], [ssd])
                P.op('act', lambda e, ss=ss: e.activation(out=ss[:], in_=ss[:], func=AF.Sqrt, bias=RMS_EPS, scale=1.0 / 64), [ssd], [ssd])
                P.op('dve', lambda e, ss=ss: e.reciprocal(out=ss[:], in_=ss[:]), [ssd], [ssd])
                P.op('dve', lambda e, x=x, qn=qn, ss=ss: e.tensor_tensor(out=qn[:], in0=x[:, 0:384].rearrange("p (g d) -> p g d", d=64), in1=ss[:].unsqueeze(2).to_broadcast([128, 6, 64]), op=ALU.mult),
                     [xd, ssd], [qnd])
                P.op('pool', lambda e, x=x, t=t: e.tensor_copy(out=Vaug[:, t, :, 0:64], in_=x[:, 384:512].rearrange("p (g d) -> p g d", d=64)), [xd], [Vaugd])
                if t < 2:
                    P.op('dve', lambda e, qn=qn, qb=qb: e.tensor_tensor(out=qb[:], in0=qn[:], in1=gain[:], op=ALU.mult), [qnd, gaind], [qbd])
                else:
                    (cs, csd) = csr.next()
                    (t1, t1d) = t1r.next()
                    (t2, t2d) = t2r.next()
                    r0 = (t - 2) * 128
                    P.dma('sp', lambda e, cs=cs, r0=r0: e.dma_start(out=cs[:], in_=I['rope_g'][r0:r0 + 128, :, :]), [], [csd])
                    P.op('dve', lambda e, qn=qn: e.tensor_tensor(out=qn[:], in0=qn[:], in1=gain[:], op=ALU.mult), [qnd, gaind], [qnd])
                    cosb = lambda cs=cs: cs[:, 0, :].unsqueeze(1).to_broadcast([128, 6, 32])
                    sinb = lambda cs=cs: cs[:, 1, :].unsqueeze(1).to_broadcast([128, 6, 32])
                    P.op('dve', lambda e, t1=t1, qn=qn, cosb=cosb: e.tensor_tensor(out=t1[:], in0=qn[:, :, 0:32], in1=cosb(), op=ALU.mult), [qnd, csd], [t1d])
                    P.op('pool', lambda e, t2=t2, qn=qn, sinb=sinb: e.tensor_tensor(out=t2[:], in0=qn[:, :, 32:64], in1=sinb(), op=ALU.mult), [qnd, csd], [t2d])
                    P.op('dve', lambda e, t1=t1, t2=t2, qb=qb: e.tensor_tensor(out=qb[:, :, 0:32], in0=t1[:], in1=t2[:], op=ALU.subtract), [t1d, t2d], [qbd])
                    P.op('dve', lambda e, t1=t1, qn=qn, sinb=sinb: e.tensor_tensor(out=t1[:], in0=qn[:, :, 0:32], in1=sinb(), op=ALU.mult), [qnd, csd], [t1d])
                    P.op('pool', lambda e, t2=t2, qn=qn, cosb=cosb: e.tensor_tensor(out=t2[:], in0=qn[:, :, 32:64], in1=cosb(), op=ALU.mult), [qnd, csd], [t2d])
                    P.op('dve', lambda e, t1=t1, t2=t2, qb=qb: e.tensor_tensor(out=qb[:, :, 32:64], in0=t1[:], in1=t2[:], op=ALU.add), [t1d, t2d], [qbd])
                for g in range(6):
                    P.op('pe', lambda e, pT=pT, qb=qb, g=g: e.transpose(out=pT[:, g, :], in_=qb[:, g, :], identity=ident[:]), [qbd, identd], [pTd])
                P.op('act', lambda e, pT=pT, t=t: e.copy(out=QKT[:, :, t * 128:(t + 1) * 128], in_=pT[:]), [pTd], [QKTd])
            P.barrier()
        attn_core(st, S, lambda h: QKT[:, h, :], QKTd, lambda g: QKT[:, 4 + g, :], QKTd, [0, 0, 1, 1], Vaug, Vaugd, 0.125, 256,
                  need_ctx, ident32, ident32d, 'gq')
        P.barrier()


def phase_mla(l, I, S, need_ctx, ident, identd, ident32, ident32d):
    nc, P = K.nc, K.P
    with ExitStack() as st:
        QKT, QKTd = sb(st, 'ml_QKT', [128, 8, NT], BF16)
        Vaug, Vaugd = sb(st, 'ml_V', [128, NTILE, 4, 65], BF16)
        with ExitStack() as st2:
            gain, gaind = sb(st2, 'ml_gain', [128, 384], F32)
            wst, wstd = sb(st2, 'ml_wst', [128, 2, 512], F32)
            wuq, wuqd = sb(st2, 'ml_wuq', [128, 2, 384], BF16)
            wukv, wukvd = sb(st2, 'ml_wukv', [128, 512], BF16)
            xr = Ring(K, st2, 'ml_x', [128, 416], F32, 3)
            sq, sqd = sb(st2, 'ml_sq', [128, 384], F32)
            ssr = Ring(K, st2, 'ml_ss', [128, 2], F32, 3)
            cnr = Ring(K, st2, 'ml_cn', [128, 384], F32, 2)
            cbr = Ring(K, st2, 'ml_cb', [128, 384], BF16, 2)
            cTr = Ring(K, st2, 'ml_cT', [128, 3, 128], BF16, 2)
            qsr = Ring(K, st2, 'ml_qs', [128, 4, 96], F32, 2)
            csr = Ring(K, st2, 'ml_cs', [128, 2, 16], F32, 3)
            t1r = Ring(K, st2, 'ml_t1', [128, 5, 16], F32, 2)
            t2r = Ring(K, st2, 'ml_t2', [128, 5, 16], F32, 2)
            rr = Ring(K, st2, 'ml_r', [128, 5, 32], F32, 2)
            qkr = Ring(K, st2, 'ml_qk', [128, 8, 128], BF16, 2)
            for (qk_, qkd_) in qkr.bufs:
                P.op('pool', lambda e, qk_=qk_: e.memset(qk_[:], 0.0), [], [qkd_])
            pcT = Ring(K, st2, 'ml_pcT', [128, 3, 128], BF16, 2, psum=True)
            pq = Ring(K, st2, 'ml_pq', [128, 384], F32, 1, psum=True)
            pkv = Ring(K, st2, 'ml_pkv', [128, 512], F32, 2, psum=True)
            pT2 = Ring(K, st2, 'ml_pT2', [128, 8, 128], BF16, 2, psum=True)
            P.dma('sp', lambda e: e.dma_start(out=gain[:, 0:256], in_=I['mla_q_norm_g'][l:l + 1, :].partition_broadcast(128)), [], [gaind])
            P.dma('sp', lambda e: e.dma_start(out=gain[:, 256:384], in_=I['mla_kv_norm_g'][l:l + 1, :].partition_broadcast(128)), [], [gaind])
            P.dma('sp', lambda e: e.dma_start(out=wst[:, :, 0:384], in_=I['mla_w_uq'][l].rearrange("(k p) n -> p k n", p=128)), [], [wstd])
            P.op('pool', lambda e: e.tensor_copy(out=wuq[:], in_=wst[:, :, 0:384]), [wstd], [wuqd])
            P.dma('sp', lambda e: e.dma_start(out=wst[:, 0, :], in_=I['mla_w_ukv'][l]), [wuqd], [wstd])
            P.op('pool', lambda e: e.tensor_copy(out=wukv[:], in_=wst[:, 0, :]), [wstd], [wukvd])
            P.op('pool', lambda e: e.memset(Vaug[:, :, :, 64:65], 1.0), [], [Vaugd])
            for t in range(NTILE):
                (x, xd) = xr.next()
                (ss, ssd) = ssr.next()
                (cn, cnd) = cnr.next()
                (cb, cbd) = cbr.next()
                (cT, cTd) = cTr.next()
                (qs, qsd) = qsr.next()
                (qk, qkd) = qkr.next()
                (r, rd) = rr.next()
                (pc, pcd) = pcT.next()
                (pqt, pqd) = pq.next()
                (pk, pkd) = pkv.next()
                (pT, pTd) = pT2.next()
                P.dma('sp', lambda e, x=x, t=t: e.dma_start(out=x[:], in_=S['p'][t * 128:(t + 1) * 128, ML0:ML0 + 416]), [S['p_d'][t]], [xd])
                if K.LVL < 2:
                    continue
                P.op('dve', lambda e, x=x: e.tensor_tensor(out=sq[:], in0=x[:, 0:384], in1=x[:, 0:384], op=ALU.mult), [xd], [sqd])
                P.op('dve', lambda e, ss=ss: e.tensor_reduce(out=ss[:, 0:1], in_=sq[:, 0:256], axis=AX.X, op=ALU.add), [sqd], [ssd])
                P.op('dve', lambda e, ss=ss: e.tensor_reduce(out=ss[:, 1:2], in_=sq[:, 256:384], axis=AX.X, op=ALU.add), [sqd], [ssd])
                P.op('act', lambda e, ss=ss: e.activation(out=ss[:, 0:1], in_=ss[:, 0:1], func=AF.Sqrt, bias=RMS_EPS, scale=1.0 / 256), [ssd], [ssd])
                P.op('act', lambda e, ss=ss: e.activation(out=ss[:, 1:2], in_=ss[:, 1:2], func=AF.Sqrt, bias=RMS_EPS, scale=1.0 / 128), [ssd], [ssd])
                P.op('dve', lambda e, ss=ss: e.reciprocal(out=ss[:], in_=ss[:]), [ssd], [ssd])
                P.op('dve', lambda e, x=x, cn=cn, ss=ss: e.scalar_tensor_tensor(out=cn[:, 0:256], in0=x[:, 0:256], scalar=ss[:, 0:1], in1=gain[:, 0:256], op0=ALU.mult, op1=ALU.mult),
                     [xd, ssd, gaind], [cnd])
                P.op('dve', lambda e, x=x, cn=cn, ss=ss: e.scalar_tensor_tensor(out=cn[:, 256:384], in0=x[:, 256:384], scalar=ss[:, 1:2], in1=gain[:, 256:384], op0=ALU.mult, op1=ALU.mult),
                     [xd, ssd, gaind], [cnd])
                P.op('pool', lambda e, cn=cn, cb=cb: e.tensor_copy(out=cb[:], in_=cn[:]), [cnd], [cbd])
                if K.LVL < 3:
                    continue
                for j in range(3):
                    P.op('pe', lambda e, pc=pc, cb=cb, j=j: e.transpose(out=pc[:, j, :], in_=cb[:, j * 128:(j + 1) * 128], identity=ident[:]), [cbd, identd], [pcd])
                if K.LVL < 2.3:
                    continue
                P.op('act', lambda e, cT=cT, pc=pc: e.copy(out=cT[:], in_=pc[:]), [pcd], [cTd])
                if K.LVL < 2.6:
                    continue
                for j in range(2):
                    P.op('pe', lambda e, pqt=pqt, cT=cT, j=j: e.matmul(pqt[:], lhsT=cT[:, j, :], rhs=wuq[:, j, :], start=(j == 0), stop=(j == 1)), [cTd, wuqd], [pqd])
                if K.LVL < 2.8:
                    continue
                for hf in range(2):
                    P.op('pe', lambda e, pk=pk, cT=cT, hf=hf: e.matmul(pk[:, hf * 256:(hf + 1) * 256], lhsT=cT[:, 2, :], rhs=wukv[:, hf * 256:(hf + 1) * 256], start=True, stop=True), [cTd, wukvd], [pkd])
                if K.LVL < 4:
                    continue
                P.op('act', lambda e, qs=qs, pqt=pqt: e.copy(out=qs[:], in_=pqt[:].rearrange("p (h d) -> p h d", d=96)), [pqd], [qsd])
                P.op('dve', lambda e, pk=pk, t=t: e.tensor_copy(out=Vaug[:, t, :, 0:64], in_=pk[:].rearrange("p (h d) -> p h d", d=128)[:, :, 64:128]), [pkd], [Vaugd])
                P.op('dve', lambda e, pk=pk, qk=qk: e.tensor_copy(out=qk[:, 4:8, 0:64], in_=pk[:].rearrange("p (h d) -> p h d", d=128)[:, :, 0:64]), [pkd], [qkd])
                P.op('pool', lambda e, qs=qs, qk=qk: e.tensor_copy(out=qk[:, 0:4, 0:64], in_=qs[:, :, 0:64]), [qsd], [qkd])
                P.op('pool', lambda e, r=r, qs=qs: e.tensor_copy(out=r[:, 0:4, :], in_=qs[:, :, 64:96]), [qsd], [rd])
                P.op('pool', lambda e, r=r, x=x: e.tensor_copy(out=r[:, 4, :], in_=x[:, 384:416]), [xd], [rd])
                if K.LVL < 5:
                    continue
                if t < 2:
                    P.op('dve', lambda e, r=r, qk=qk: e.tensor_copy(out=qk[:, 0:4, 64:96], in_=r[:, 0:4, :]), [rd], [qkd])
                    P.op('dve', lambda e, r=r, qk=qk: e.tensor_copy(out=qk[:, 4:8, 64:96], in_=r[:, 4, :].unsqueeze(1).to_broadcast([128, 4, 32])), [rd], [qkd])
                else:
                    (cs, csd) = csr.next()
                    (t1, t1d) = t1r.next()
                    (t2, t2d) = t2r.next()
                    r0 = (t - 2) * 128
                    P.dma('sp', lambda e, cs=cs, r0=r0: e.dma_start(out=cs[:], in_=I['rope_m'][r0:r0 + 128, :, :]), [], [csd])
                    cosb = lambda cs=cs: cs[:, 0, :].unsqueeze(1).to_broadcast([128, 5, 16])
                    sinb = lambda cs=cs: cs[:, 1, :].unsqueeze(1).to_broadcast([128, 5, 16])
                    P.op('dve', lambda e, t1=t1, r=r, cosb=cosb: e.tensor_tensor(out=t1[:], in0=r[:, :, 0:16], in1=cosb(), op=ALU.mult), [rd, csd], [t1d])
                    P.op('pool', lambda e, t2=t2, r=r, sinb=sinb: e.tensor_tensor(out=t2[:], in0=r[:, :, 16:32], in1=sinb(), op=ALU.mult), [rd, csd], [t2d])
                    P.op('dve', lambda e, t1=t1, t2=t2: e.tensor_tensor(out=t1[:], in0=t1[:], in1=t2[:], op=ALU.subtract), [t1d, t2d], [t1d])
                    P.op('dve', lambda e, t1=t1, qk=qk: e.tensor_copy(out=qk[:, 0:4, 64:80], in_=t1[:, 0:4, :]), [t1d], [qkd])
                    P.op('dve', lambda e, t1=t1, qk=qk: e.tensor_copy(out=qk[:, 4:8, 64:80], in_=t1[:, 4, :].unsqueeze(1).to_broadcast([128, 4, 16])), [t1d], [qkd])
                    (t1, t1d) = t1r.next()
                    (t2, t2d) = t2r.next()
                    P.op('dve', lambda e, t1=t1, r=r, sinb=sinb: e.tensor_tensor(out=t1[:], in0=r[:, :, 0:16], in1=sinb(), op=ALU.mult), [rd, csd], [t1d])
                    P.op('pool', lambda e, t2=t2, r=r, cosb=cosb: e.tensor_tensor(out=t2[:], in0=r[:, :, 16:32], in1=cosb(), op=ALU.mult), [rd, csd], [t2d])
                    P.op('dve', lambda e, t1=t1, t2=t2: e.tensor_tensor(out=t1[:], in0=t1[:], in1=t2[:], op=ALU.add), [t1d, t2d], [t1d])
                    P.op('dve', lambda e, t1=t1, qk=qk: e.tensor_copy(out=qk[:, 0:4, 80:96], in_=t1[:, 0:4, :]), [t1d], [qkd])
                    P.op('dve', lambda e, t1=t1, qk=qk: e.tensor_copy(out=qk[:, 4:8, 80:96], in_=t1[:, 4, :].unsqueeze(1).to_broadcast([128, 4, 16])), [t1d], [qkd])
                if K.LVL < 6:
                    continue
                for g in range(8):
                    P.op('pe', lambda e, pT=pT, qk=qk, g=g: e.transpose(out=pT[:, g, :], in_=qk[:, g, :], identity=ident[:]), [qkd, identd], [pTd])
                P.op('act', lambda e, pT=pT, t=t: e.copy(out=QKT[:, :, t * 128:(t + 1) * 128], in_=pT[:]), [pTd], [QKTd])
            P.barrier()
        if K.LVL < 7:
            return
        attn_core(st, S, lambda h: QKT[:, h, :], QKTd, lambda g: QKT[:, 4 + g, :], QKTd, [0, 1, 2, 3], Vaug, Vaugd, 96.0 ** -0.5, 512,
                  need_ctx, ident32, ident32d, 'ml')
        P.barrier()


BIG = 30000.0


def nat_plan():
    variants = {}
    plan = []
    for i in range(64):
        rs = min(max(i - 4, 0), 56)
        tiles = []
        for m in range(rs // 2, (rs + 7) // 2 + 1):
            dd = []
            for r in (2 * m, 2 * m + 1):
                dd.append(r - i + 7 if rs <= r < rs + 8 else -1)
            key = tuple(dd)
            if key not in variants:
                variants[key] = len(variants)
            tiles.append((2 + m, variants[key]))
        plan.append(tiles)
    vlist = [None] * len(variants)
    for k, v in variants.items():
        vlist[v] = k
    return plan, vlist


def nat_bias_table(nat_bias_l):
    plan, vlist = nat_plan()
    c = np.arange(64)
    cs = np.clip(c - 8, 0, 48)
    cp = np.arange(64)
    inwin = (cp[:, None] >= cs[None, :]) & (cp[:, None] < cs[None, :] + 16)
    off = np.clip(cp[:, None] - c[None, :] + 15, 0, 30)
    tab = np.full((128, 4, len(vlist), 64), -BIG, np.float32)
    for v, (d0, d1) in enumerate(vlist):
        for half, d in enumerate((d0, d1)):
            if d < 0:
                continue
            for h in range(4):
                vals = nat_bias_l[h, d][off]
                tab[half * 64:(half + 1) * 64, h, v, :] = np.where(inwin, vals, np.float32(-BIG))
    return tab


def phase_nat(l, I, S, need_ctx, ident, identd, ident32, ident32d):
    nc, P = K.nc, K.P
    plan, vlist = nat_plan()
    NV = len(vlist)
    with ExitStack() as st:
        QKT, QKTd = sb(st, 'na_QKT', [64, 8, NT], BF16)
        Vaug, Vaugd = sb(st, 'na_V', [128, NTILE, 4, 65], BF16)
        tb, tbd = sb(st, 'na_tb', [128, 4, NV, 64], BF16)
        with ExitStack() as st2:
            tbs, tbsd = sb(st2, 'na_tbs', [128, 4, NV, 64], F32)
            xr = Ring(K, st2, 'na_x', [128, 768], F32, 3)
            xbr = Ring(K, st2, 'na_xb', [128, 512], BF16, 2)
            pTr = Ring(K, st2, 'na_pT', [64, 8, 128], BF16, 2, psum=True)
            P.dma('sp', lambda e: e.dma_start(out=tbs[:], in_=I['nat_tab'][l]), [], [tbsd])
            P.op('dve', lambda e: e.tensor_scalar(out=tb[:], in0=tbs[:], scalar1=8.0, scalar2=None, op0=ALU.mult), [tbsd], [tbd])
            P.op('pool', lambda e: e.memset(Vaug[:, :, :, 64:65], 1.0), [], [Vaugd])
            for t in range(NTILE):
                (x, xd) = xr.next()
                (xb, xbd) = xbr.next()
                (pT, pTd) = pTr.next()
                P.dma('sp', lambda e, x=x, t=t: e.dma_start(out=x[:], in_=S['p'][t * 128:(t + 1) * 128, NA0:NA0 + 768]), [S['p_d'][t]], [xd])
                P.op('dve', lambda e, x=x, xb=xb: e.tensor_copy(out=xb[:], in_=x[:, 0:512]), [xd], [xbd])
                P.op('pool', lambda e, x=x, t=t: e.tensor_copy(out=Vaug[:, t, :, 0:64], in_=x[:, 512:768].rearrange("p (h d) -> p h d", d=64)), [xd], [Vaugd])
                for g in range(8):
                    P.op('pe', lambda e, pT=pT, xb=xb, g=g: e.transpose(out=pT[:, g, :], in_=xb[:, g * 64:(g + 1) * 64], identity=ident[:]), [xbd, identd], [pTd])
                P.op('act', lambda e, pT=pT, t=t: e.copy(out=QKT[:, :, t * 128:(t + 1) * 128], in_=pT[:]), [pTd], [QKTd])
            P.barrier()
        sr = Ring(K, st, 'na_S', [128, 512], F32, 3, psum=True)
        orr = Ring(K, st, 'na_O', [65, 512], F32, 2, psum=True)
        ptr = Ring(K, st, 'na_fT', [128, 65], F32, 2, psum=True)
        pr = Ring(K, st, 'na_P', [128, 512], BF16, 3)
        osbr = Ring(K, st, 'na_osb', [65, 512], F32, 2)
        rvr = Ring(K, st, 'na_rv', [128, 1], F32, 4)
        otr = Ring(K, st, 'na_ot', [128, 4, 256], F32, 2)
        blocks = []
        if need_ctx:
            blocks.append(None)
        for qb in range(8):
            blocks.append(qb)
        for qb in blocks:
            (ot, otd) = otr.next()
            if qb is None:
                q0, nq = 0, 256
            else:
                q0, nq = 256 + qb * 512, 512
            for h in range(4):
                (O, Od) = orr.next()
                if qb is None:
                    for i, kt in enumerate((0, 1)):
                        (Sp, Spd) = sr.next()
                        (Pt, Ptd) = pr.next()
                        P.op('pe', lambda e, Sp=Sp, h=h, kt=kt: e.matmul(Sp[:, 0:256], lhsT=QKT[:, 4 + h, kt * 128:(kt + 1) * 128], rhs=QKT[:, h, 0:256], start=True, stop=True),
                             [QKTd], [Spd])
                        P.op('act', lambda e, Sp=Sp, Pt=Pt: e.activation(out=Pt[:, 0:256], in_=Sp[:, 0:256], func=AF.Exp, scale=0.125), [Spd], [Ptd])
                        P.op('pe', lambda e, O=O, h=h, kt=kt, Pt=Pt, i=i: e.matmul(O[:, 0:256], lhsT=Vaug[:, kt, h, :], rhs=Pt[:, 0:256], start=(i == 0), stop=(i == 1)),
                             [Vaugd, Ptd], [Od])
                else:
                    npend = [None]
                    for ri in range(8):
                        i = qb * 8 + ri
                        qt0 = 256 + i * 64
                        tiles = [(kt, None) for kt in (0, 1)] + plan[i]
                        (Sp, Spd) = sr.next()
                        (Pt, Ptd) = pr.next()
                        for j, (kt, v) in enumerate(tiles):
                            P.op('pe', lambda e, Sp=Sp, h=h, kt=kt, qt0=qt0, j=j, v=v: e.matmul(Sp[:, j * 64:(j + 1) * 64], lhsT=QKT[:, 4 + h, kt * 128:(kt + 1) * 128], rhs=QKT[:, h, qt0:qt0 + 64], start=True, stop=(v is None)),
                                 [QKTd], [Spd])
                            if v is not None:
                                P.op('pe', lambda e, Sp=Sp, h=h, j=j, v=v: e.matmul(Sp[:, j * 64:(j + 1) * 64], lhsT=ident[:], rhs=tb[:, h, v, :], start=False, stop=True),
                                     [identd, tbd], [Spd])
                        nk = len(tiles)
                        P.op('act', lambda e, Sp=Sp, Pt=Pt, nk=nk: e.activation(out=Pt[:, 0:nk * 64], in_=Sp[:, 0:nk * 64], func=AF.Exp, scale=0.125), [Spd], [Ptd])
                        if npend[0] is not None:
                            npend[0]()

                        def _pend(O=O, Od=Od, h=h, Pt=Pt, Ptd=Ptd, ri=ri, nk=nk, tiles=tiles):
                            for j, (kt, v) in enumerate(tiles):
                                P.op('pe', lambda e, kt=kt, j=j: e.matmul(O[:, ri * 64:(ri + 1) * 64], lhsT=Vaug[:, kt, h, :], rhs=Pt[:, j * 64:(j + 1) * 64], start=(j == 0), stop=(j == nk - 1)),
                                     [Vaugd, Ptd], [Od])
                        npend[0] = _pend
                    npend[0]()
                    npend[0] = None
                attn_finalize((osbr, ptr, rvr), O, Od, h, ot, otd, nq, ident32, ident32d)
            nj = nq // 128
            P.dma('sp', lambda e, ot=ot, q0=q0, nj=nj: e.dma_start(out=S['ocat'][q0:q0 + nj * 128, 768:1024].rearrange("(j p) c -> p j c", p=128), in_=ot[:, 0:nj, :]),
                  [otd], [S['ocat_d'][t] for t in range(q0 // 128, q0 // 128 + nj)])
        P.barrier()


def o_tt(q, out, in0, in1, op, rd, wr):
    K.P.op(q, lambda e: e.tensor_tensor(out=out, in0=in0, in1=in1, op=op), rd, wr)


def o_stt(q, out, in0, scalar, in1, op0, op1, rd, wr):
    K.P.op(q, lambda e: e.scalar_tensor_tensor(out=out, in0=in0, scalar=scalar, in1=in1, op0=op0, op1=op1), rd, wr)


def o_ts(q, out, in0, s1, op0, rd, wr):
    K.P.op(q, lambda e: e.tensor_scalar(out=out, in0=in0, scalar1=s1, scalar2=None, op0=op0), rd, wr)


def o_act(out, in_, func, rd, wr, **kw):
    K.P.op('act', lambda e: e.activation(out=out, in_=in_, func=func, **kw), rd, wr)


def o_red(q, out, in_, op, rd, wr):
    K.P.op(q, lambda e: e.tensor_reduce(out=out, in_=in_, axis=AX.X, op=op), rd, wr)


def o_mm(out, lhsT, rhs, rd, wr, start=True, stop=True):
    K.P.op('pe', lambda e: e.matmul(out, lhsT=lhsT, rhs=rhs, start=start, stop=stop), rd, wr)


def o_tr(out, in_, ident, rd, wr):
    K.P.op('pe', lambda e: e.transpose(out=out, in_=in_, identity=ident), rd, wr)


def o_cp(q, out, in_, rd, wr):
    if q == 'act':
        K.P.op('act', lambda e: e.copy(out=out, in_=in_), rd, wr)
    else:
        K.P.op(q, lambda e: e.tensor_copy(out=out, in_=in_), rd, wr)


def o_rcp(out, in_, rd, wr):
    K.P.op('dve', lambda e: e.reciprocal(out=out, in_=in_), rd, wr)


def o_ms(q, out, val, wr):
    K.P.op(q, lambda e: e.memset(out, val), [], wr)


def o_dma(out, in_, rd, wr, q='sp', **kw):
    K.P.dma(q, lambda e: e.dma_start(out=out, in_=in_, **kw), rd, wr)


def bc_load(st, name, src_row, n):
    t, d = sb(st, name, [128, n], F32)
    o_dma(t[:], src_row.partition_broadcast(128), [], [d])
    return t, d


RCH = 8


def rev_tile(c):
    return 1 - c if c < 2 else 35 - c


def phase_rwkv(l, I, S, need_ctx, ident32, ident32d, J32, J32d):
    rwkv_prep(l, I, S, ident32, ident32d, J32, J32d)
    rwkv_scan(S)
    rwkv_readout(l, I, S, need_ctx, J32, J32d)


def rwkv_prep(l, I, S, ident32, ident32d, J32, J32d):
    P = K.P
    MUL, ADD, SUB = ALU.mult, ALU.add, ALU.subtract
    with ExitStack() as st:
        xr = Ring(K, st, 'rv_x', [128, 1024], F32, 2)
        xo = Ring(K, st, 'rv_o', [128, 1024], F32, 2)
        pr = Ring(K, st, 'rv_ps', [128, 512], F32, 2, psum=True)
        for c in range(NTILE):
            tt_ = rev_tile(c)
            (x, xd) = xr.next()
            (o, od) = xo.next()
            o_dma(x[:], S['p'][tt_ * 128:(tt_ + 1) * 128, 0:1024], [S['p_d'][tt_]], [xd])
            for hf in range(2):
                (ps_, psd) = pr.next()
                o_mm(ps_[:], J32[:], x[:, hf * 512:(hf + 1) * 512], [J32d, xd], [psd])
                o_cp('act' if hf else 'dve', o[:, hf * 512:(hf + 1) * 512], ps_[:], [psd], [od])
            o_dma(S['prw1'][c * 128:(c + 1) * 128, :], o[:], [od], [S['prw1_d'][c]])
        P.barrier()
    with ExitStack() as st:
        mub, mubd = bc_load(st, 'rp_mu', I['rwkv_mu'][l, :], 1024)
        kkb, kkbd = bc_load(st, 'rp_kk', I['rwkv_k_k'][l, :], 256)
        kab, kabd = bc_load(st, 'rp_ka', I['rwkv_k_a'][l, :], 256)
        rkb, rkbd = bc_load(st, 'rp_rk', I['rwkv_r_k'][l].rearrange("h d -> (h d)"), 256)
        omka, omkad = sb(st, 'rp_omka', [128, 256], F32)
        K.P.op('dve', lambda e: e.tensor_scalar(out=omka[:], in0=kab[:], scalar1=-1.0, scalar2=1.0, op0=MUL, op1=ADD), [kabd], [omkad])
        w0b, a0b, wup, aup = [], [], [], []
        for d in range(2):
            w0b.append(bc_load(st, 'rp_w0%d' % d, I['rwkv_w0'][l, d, :], 256))
            a0b.append(bc_load(st, 'rp_a0%d' % d, I['rwkv_a0'][l, d, :], 256))
            t, td = sb(st, 'rp_wup%d' % d, [64, 256], F32)
            o_dma(t[:], I['rwkv_w_up'][l, d], [], [td])
            wup.append((t, td))
            t, td = sb(st, 'rp_aup%d' % d, [64, 256], F32)
            o_dma(t[:], I['rwkv_a_up'][l, d], [], [td])
            aup.append((t, td))
        gup, gupd = sb(st, 'rp_gup', [128, 256], F32)
        o_dma(gup[:], I['rwkv_g_up'][l], [], [gupd])

        xr = Ring(K, st, 'rp_x', [128, 1024], F32, 2)
        pvr = Ring(K, st, 'rp_pv', [128, 1024], F32, 2)
        nxr = Ring(K, st, 'rp_nx', [128, 1024], F32, 2)
        xsr = Ring(K, st, 'rp_xs', [128, 1024], F32, 2)
        kkr = Ring(K, st, 'rp_kkn', [128, 256], F32, 2)
        sqr = Ring(K, st, 'rp_sq', [128, 256], F32, 2)
        ssr = Ring(K, st, 'rp_ss', [128, 4], F32, 4)
        smr = Ring(K, st, 'rp_sm', [128, 128], F32, 4)
        sTr = Ring(K, st, 'rp_sT', [128, 128], F32, 4)
        ur = Ring(K, st, 'rp_u', [128, 256], F32, 2)
        ar_ = Ring(K, st, 'rp_a', [128, 256], F32, 2)
        mr = Ring(K, st, 'rp_m', [128, 256], F32, 2)
        kdr = Ring(K, st, 'rp_kd', [128, 256], F32, 3)
        kkar = Ring(K, st, 'rp_kka', [128, 256], F32, 3)
        bor = Ring(K, st, 'rp_bo', [128, 256], F32, 3)
        gtr = Ring(K, st, 'rp_gt', [128, 256], F32, 2)
        nkkr = Ring(K, st, 'rp_nkk', [128, 4, 2, 64], F32, 2)
        rrr = Ring(K, st, 'rp_rr', [128, 4, 2, 64], F32, 2)
        decr = Ring(K, st, 'rp_dec', [128, 4, 2, 64], F32, 2)
        fakr = Ring(K, st, 'rp_fak', [128, 128, 4, 2], F32, 2)
        farr = Ring(K, st, 'rp_far', [128, 128, 4, 2], F32, 2)
        fwr = Ring(K, st, 'rp_fw', [128, 128, 4], F32, 2)
        for ring in (fakr, farr):
            for (b_, bd_) in ring.bufs:
                o_ms('pool', b_[:], 0.0, [bd_])
        pbig = Ring(K, st, 'rp_pb', [128, 256], F32, 3, psum=True)
        ptr_ = Ring(K, st, 'rp_pt', [128, 128], F32, 3, psum=True)

        def v3(ap):
            return ap.rearrange("p (h k) -> p h k", k=64)

        for c in range(NTILE):
            first = c in (0, 2)
            last = c in (1, NTILE - 1)
            r0 = c * 128
            (nkk, nkkd) = nkkr.next()
            (rr, rrd) = rrr.next()
            (dec, decd) = decr.next()
            for d in range(2):
                if d == 0:
                    src = lambda a, b: S['p'][a:b, 0:1024]
                    sdeps = S['p_d']
                else:
                    src = lambda a, b: S['prw1'][a:b, :]
                    sdeps = S['prw1_d']
                nb = [sdeps[c]] + ([sdeps[c - 1]] if c > 0 else []) + ([sdeps[c + 1]] if c < NTILE - 1 else [])
                (x, xd) = xr.next()
                (pv, pvd) = pvr.next()
                (nx, nxd) = nxr.next()
                (xs, xsd) = xsr.next()
                o_dma(x[:], src(r0, r0 + 128), nb, [xd])
                if first:
                    o_ms('pool', pv[:], 0.0, [pvd])
                    o_dma(pv[1:128, :], src(r0, r0 + 127), nb, [pvd])
                else:
                    o_dma(pv[:], src(r0 - 1, r0 + 127), nb, [pvd])
                if last:
                    o_ms('pool', nx[:], 0.0, [nxd])
                    o_dma(nx[0:127, :], src(r0 + 1, r0 + 128), nb, [nxd])
                else:
                    o_dma(nx[:], src(r0 + 1, r0 + 129), nb, [nxd])
                o_tt('pool', pv[:], pv[:], nx[:], ADD, [pvd, nxd], [pvd])
                o_stt('dve', pv[:], pv[:], 0.5, x[:], MUL, SUB, [pvd, xd], [pvd])
                o_tt('pool', pv[:], pv[:], mub[:], MUL, [pvd, mubd], [pvd])
                o_tt('dve', xs[:], x[:], pv[:], ADD, [xd, pvd], [xsd])
                r_ = xs[:, 0:256]
                k_ = xs[:, 256:512]
                v_ = xs[:, 512:768]
                (kkn, kknd) = kkr.next()
                (sq, sqd) = sqr.next()
                (ss, ssd) = ssr.next()
                o_tt('dve', kkn[:], k_, kkb[:], MUL, [xsd, kkbd], [kknd])
                o_tt('pool', sq[:], kkn[:], kkn[:], MUL, [kknd], [sqd])
                o_red('dve', ss[:], v3(sq[:]), ADD, [sqd], [ssd])
                o_act(ss[:], ss[:], AF.Sqrt, [ssd], [ssd], bias=1e-12, scale=1.0)
                o_rcp(ss[:], ss[:], [ssd], [ssd])
                o_tt('dve', v3(kkn[:]), v3(kkn[:]), ss[:].unsqueeze(2).to_broadcast([128, 4, 64]), MUL, [kknd, ssd], [kknd])
                o_ts('dve', nkk[:, :, d, :], v3(kkn[:]), -1.0, MUL, [kknd], [nkkd])
                o_cp('pool', rr[:, :, d, :], v3(r_), [xsd], [rrd])
                (tw, twd) = smr.next()
                (twT, twTd) = sTr.next()
                (pt, ptd) = ptr_.next()
                (pb, pbd) = pbig.next()
                (u, ud) = ur.next()
                o_act(tw[:, 0:64], xs[:, 768:832], AF.Tanh, [xsd], [twd])
                o_tr(pt[0:64, :], tw[:, 0:64], ident32[:], [twd, ident32d], [ptd])
                o_cp('act', twT[0:64, :], pt[0:64, :], [ptd], [twTd])
                o_mm(pb[:], twT[0:64, :], wup[d][0][:], [twTd, wup[d][1]], [pbd])
                o_tt('dve', u[:], pb[:], w0b[d][0][:], ADD, [pbd, w0b[d][1]], [ud])
                o_act(u[:], u[:], AF.Sigmoid, [ud], [ud])
                o_act(dec[:, :, d, :], v3(u[:]), AF.Exp, [ud], [decd], scale=-0.6065306597126334)
                (xa, xad) = smr.next()
                (xaT, xaTd) = sTr.next()
                (pt, ptd) = ptr_.next()
                (pb, pbd) = pbig.next()
                (a, ad) = ar_.next()
                o_cp('pool', xa[:, 0:64], xs[:, 832:896], [xsd], [xad])
                o_tr(pt[0:64, :], xa[:, 0:64], ident32[:], [xad, ident32d], [ptd])
                o_cp('act', xaT[0:64, :], pt[0:64, :], [ptd], [xaTd])
                o_mm(pb[:], xaT[0:64, :], aup[d][0][:], [xaTd, aup[d][1]], [pbd])
                o_tt('dve', a[:], pb[:], a0b[d][0][:], ADD, [pbd, a0b[d][1]], [ad])
                o_act(a[:], a[:], AF.Sigmoid, [ad], [ad])
                (m, md) = mr.next()
                (kd, kdd) = kdr.next()
                (kka, kkad) = kkar.next()
                (bo, bod) = bor.next()
                o_tt('pool', m[:], a[:], kab[:], MUL, [ad, kabd], [md])
                o_tt('pool', m[:], m[:], omka[:], ADD, [md, omkad], [md])
                o_tt('dve', kd[:], k_, m[:], MUL, [xsd, md], [kdd])
                o_tt('pool', kka[:], kkn[:], a[:], MUL, [kknd, ad], [kkad])
                (ss2, ss2d) = ssr.next()
                o_tt('dve', m[:], r_, kd[:], MUL, [xsd, kdd], [md])
                o_tt('pool', m[:], m[:], rkb[:], MUL, [md, rkbd], [md])
                o_red('dve', ss2[:], v3(m[:]), ADD, [md], [ss2d])
                o_tt('dve', v3(bo[:]), v3(v_), ss2[:].unsqueeze(2).to_broadcast([128, 4, 64]), MUL, [xsd, ss2d], [bod])
                o_dma(S['V2'][d, r0:r0 + 128, :], v_, [xsd], [S['V2_d'][c]])
                o_dma(S['KKA'][d, r0:r0 + 128, :], kka[:], [kkad], [S['KKA_d'][c]])
                o_dma(S['KD'][d, r0:r0 + 128, :], kd[:], [kdd], [S['KD_d'][c]])
                o_dma(S['BON'][d, r0:r0 + 128, :], bo[:], [bod], [S['BON_d'][c]])
                if d == 0:
                    (sg, sgd) = smr.next()
                    (sgT, sgTd) = sTr.next()
                    (pt, ptd) = ptr_.next()
                    (pb, pbd) = pbig.next()
                    (gt, gtd) = gtr.next()
                    o_act(sg[:], xs[:, 896:1024], AF.Sigmoid, [xsd], [sgd])
                    o_tr(pt[:], sg[:], ident32[:], [sgd, ident32d], [ptd])
                    o_cp('act', sgT[:], pt[:], [ptd], [sgTd])
                    o_mm(pb[:], sgT[:], gup[:], [sgTd, gupd], [pbd])
                    o_cp('dve', gt[:], pb[:], [pbd], [gtd])
                    o_dma(S['GATE'][r0:r0 + 128, :], gt[:], [gtd], [S['GATE_d'][c]])
            (fak, fakd) = fakr.next()
            (far, fard) = farr.next()
            (fw, fwd) = fwr.next()
            for h in range(4):
                for (srcT, srcd, dstF, dstd) in ((nkk, nkkd, fak, fakd), (rr, rrd, far, fard)):
                    (pt, ptd) = ptr_.next()
                    o_tr(pt[:], srcT[:, h, :, :].rearrange("p d k -> p (d k)"), ident32[:], [srcd, ident32d], [ptd])
                    eng_ = 'act' if h % 2 == 0 else 'dve'
                    o_cp(eng_, dstF[0:64, :, h, 0], pt[0:64, :], [ptd], [dstd])
                    o_cp(eng_, dstF[64:128, :, h, 1], pt[64:128, :], [ptd], [dstd])
                (pt, ptd) = ptr_.next()
                o_tr(pt[:], dec[:, h, :, :].rearrange("p d k -> p (d k)"), ident32[:], [decd, ident32d], [ptd])
                o_cp('act', fw[:, :, h], pt[:], [ptd], [fwd])
            o_dma(S['AKK'][:, r0:r0 + 128, :], fak[:].rearrange("p s h d -> p s (h d)"), [fakd], [S['AKK_d'][c]])
            o_dma(S['AR'][:, r0:r0 + 128, :], far[:].rearrange("p s h d -> p s (h d)"), [fard], [S['AR_d'][c]])
            o_dma(S['WD'][:, r0:r0 + 128, :], fw[:], [fwd], [S['WD_d'][c]])
        P.barrier()


def rwkv_scan(S, T=None):
    P = K.P
    T = NT if T is None else T
    CH = RCH
    nch = T // CH
    with ExitStack() as st:
        ST, _ = sb(st, 'sc_ST', [128, 256], F32)
        STd = [Dep('sc_ST%d' % h) for h in range(4)]
        for ch in range(4):
            o_ms('pool', ST[:, ch * 64:(ch + 1) * 64], 0.0, [STd[ch]])
        NB = 3
        akk = [sb(st, 'sc_akk%d' % i, [128, CH, 8], F32) for i in range(NB)]
        ar = [sb(st, 'sc_ar%d' % i, [128, CH, 8], F32) for i in range(NB)]
        am = [sb(st, 'sc_am%d' % i, [128, CH, 4, 4], F32) for i in range(NB)]
        wt = [sb(st, 'sc_wt%d' % i, [128, CH, 4], F32) for i in range(NB)]
        bt = [sb(st, 'sc_bt%d' % i, [6, CH, 4, 128], F32) for i in range(NB)]
        rt = [sb(st, 'sc_rt%d' % i, [6, CH, 256], F32) for i in range(NB)]
        rtv = [Dep('sc_rtv%d' % i) for i in range(NB)]
        rts = [[Dep('sc_rts%d_%d' % (i, ch)) for ch in range(4)] for i in range(NB)]
        amf, amfd = sb(st, 'sc_amf', [128, 4, 4], F32)
        yfin, yfind = sb(st, 'sc_yfin', [4, 256], F32)
        for (b_, bd_) in bt:
            o_ms('pool', b_[:], 0.0, [bd_])
        sar = [Ring(K, st, 'sc_sa%d' % ch, [128, 64], F32, 1, psum=True) for ch in range(4)]
        ur = [Ring(K, st, 'sc_u%d' % ch, [128, 64], F32, 1, psum=True) for ch in range(4)]

        def load_chunk(c):
            i = c % NB
            s0 = c * CH
            tl = [s0 // 128]
            o_dma(akk[i][0][:], S['AKK'][:, s0:s0 + CH, :], [S['AKK_d'][t] for t in tl], [akk[i][1]])
            o_dma(ar[i][0][:], S['AR'][:, s0:s0 + CH, :], [S['AR_d'][t] for t in tl], [ar[i][1]])
            o_dma(wt[i][0][:], S['WD'][:, s0:s0 + CH, :], [S['WD_d'][t] for t in tl], [wt[i][1]])
            for which, nm in ((0, 'KKA'), (1, 'KD')):
                for d in range(2):
                    row = d if which == 0 else 4 + d
                    o_dma(bt[i][0][row:row + 1, :, :, d * 64:(d + 1) * 64],
                          S[nm][d:d + 1, s0:s0 + CH, :].rearrange("o s (h k) -> o s h k", k=64),
                          [S[nm + '_d'][t] for t in tl], [bt[i][1]])
            o_dma(rt[i][0][4:6, :, :], S['V2'][:, s0:s0 + CH, :], [S['V2_d'][t] for t in tl], [rtv[i]])
            a4 = akk[i][0][:].rearrange("p s (h d) -> p s h d", d=2)
            r4 = ar[i][0][:].rearrange("p s (h d) -> p s h d", d=2)
            o_cp('pool', am[i][0][:, :, :, 0:2], a4, [akk[i][1]], [am[i][1]])
            o_cp('pool', am[i][0][:, 1:CH, :, 2:4], r4[:, 0:CH - 1, :, :], [ar[i][1]], [am[i][1]])
            if c == 0:
                o_ms('pool', am[i][0][:, 0, :, 2:4], 0.0, [am[i][1]])
            else:
                ip = (c - 1) % NB
                rp = ar[ip][0][:].rearrange("p s (h d) -> p s h d", d=2)
                o_cp('pool', am[i][0][:, 0, :, 2:4], rp[:, CH - 1, :, :], [ar[ip][1]], [am[i][1]])

        cv_ld = Ring(K, st, 'sc_cvld', [128, DFF], F32, 2)
        cv_bf = Ring(K, st, 'sc_cvbf', [128, DFF], BF16, 2)
        pend_items = K.PENDING
        K.PENDING = []
        every = max(1, T // max(1, len(pend_items)) - 1) if pend_items else 0
        load_chunk(0)
        if nch > 1:
            load_chunk(1)
        for s in range(T):
            c, j = divmod(s, CH)
            i = c % NB
            if pend_items and s % every == every - 1:
                pend_items.pop(0)(cv_ld, cv_bf, 'pool', 'pool')
            if j == 0 and c + 2 < nch:
                load_chunk(c + 2)
            SAs = [sar[ch].next() for ch in range(4)]
            Us = [ur[ch].next() for ch in range(4)]
            for h in range(4):
                (SA, SAd) = SAs[h]
                o_mm(SA[0:4, :], am[i][0][:, j, h, :], ST[:, h * 64:(h + 1) * 64], [am[i][1], STd[h]], [SAd])
            for h in range(4):
                (SA, SAd) = SAs[h]
                (U, Ud) = Us[h]
                o_cp('act', rt[i][0][0:4, j, h * 64:(h + 1) * 64], SA[0:4, :], [SAd], [rts[i][h]])
                o_mm(U[:], bt[i][0][0:6, j, h, :], rt[i][0][0:6, j, h * 64:(h + 1) * 64], [bt[i][1], rts[i][h], rtv[i]], [Ud])
            for h in range(4):
                (U, Ud) = Us[h]
                STh = ST[:, h * 64:(h + 1) * 64]
                o_stt('dve', STh, STh, wt[i][0][:, j, h:h + 1], U[:], ALU.mult, ALU.add, [STd[h], wt[i][1], Ud], [STd[h]])
            if j == CH - 1:
                s0 = c * CH
                if c == 0:
                    o_dma(S['Y2'][:, 0:CH - 1, :], rt[i][0][2:4, 1:CH, :], rts[i], [S['Y2_d'][0]])
                else:
                    o_dma(S['Y2'][:, s0 - 1:s0 + CH - 1, :], rt[i][0][2:4, :, :], rts[i], [S['Y2_d'][(s0 - 1) // 128]])
        while pend_items:
            pend_items.pop(0)(cv_ld, cv_bf, 'pool', 'pool')
        il = (nch - 1) % NB
        rl = ar[il][0][:].rearrange("p s (h d) -> p s h d", d=2)
        o_ms('pool', amf[:], 0.0, [amfd])
        o_cp('pool', amf[:, :, 2:4], rl[:, CH - 1, :, :], [ar[il][1], amfd], [amfd])
        for h in range(4):
            (SA, SAd) = sar[h].next()
            o_mm(SA[0:4, :], amf[:, h, :], ST[:, h * 64:(h + 1) * 64], [amfd, STd[h]], [SAd])
            o_cp('act', yfin[0:4, h * 64:(h + 1) * 64], SA[0:4, :], [SAd], [yfind])
        o_dma(S['Y2'][:, T - 1, :], yfin[2:4, :], [yfind], [S['Y2_d'][(T - 1) // 128]])
        P.barrier()


def rwkv_readout(l, I, S, need_ctx, J32, J32d):
    P = K.P
    MUL, ADD = ALU.mult, ALU.add
    with ExitStack() as st:
        lng, lngd = bc_load(st, 'ro_lng', I['rwkv_ln_g'][l, :], 256)
        lnb, lnbd = bc_load(st, 'ro_lnb', I['rwkv_ln_b'][l, :], 256)
        yr_ = Ring(K, st, 'ro_y', [128, 256], F32, 3)
        br_ = Ring(K, st, 'ro_b', [128, 256], F32, 3)
        gr_ = Ring(K, st, 'ro_g', [128, 256], F32, 2)
        ycr = Ring(K, st, 'ro_yc', [128, 256], F32, 3)
        sqr = Ring(K, st, 'ro_sq', [128, 256], F32, 2)
        smr = Ring(K, st, 'ro_sm', [128, 4], F32, 6)
        otr = Ring(K, st, 'ro_o', [128, 256], F32, 2)
        psr = Ring(K, st, 'ro_ps', [128, 256], F32, 2, psum=True)

        def v3(ap):
            return ap.rearrange("p (h k) -> p h k", k=64)

        def bc4(ap):
            return ap.unsqueeze(2).to_broadcast([128, 4, 64])

        for t in range(0 if need_ctx else 2, NTILE):
            outs = []
            for d in range(2):
                c = t if d == 0 else rev_tile(t)
                r0 = c * 128
                (y, yd) = yr_.next()
                (b, bd) = br_.next()
                (yc, ycd) = ycr.next()
                (sq, sqd) = sqr.next()
                (sm, smd) = smr.next()
                (vr, vrd) = smr.next()
                o_dma(y[:], S['Y2'][d, r0:r0 + 128, :], [S['Y2_d'][c]], [yd])
                o_dma(b[:], S['BON'][d, r0:r0 + 128, :], [S['BON_d'][c]], [bd])
                o_red('dve', sm[:], v3(y[:]), ADD, [yd], [smd])
                o_ts('dve', sm[:], sm[:], -1.0 / 64, MUL, [smd], [smd])
                o_tt('dve', v3(yc[:]), v3(y[:]), bc4(sm[:]), ADD, [yd, smd], [ycd])
                o_tt('pool', sq[:], yc[:], yc[:], MUL, [ycd], [sqd])
                o_red('dve', vr[:], v3(sq[:]), ADD, [sqd], [vrd])
                o_act(vr[:], vr[:], AF.Sqrt, [vrd], [vrd], bias=64e-5, scale=1.0 / 64)
                o_rcp(vr[:], vr[:], [vrd], [vrd])
                o_tt('dve', v3(yc[:]), v3(yc[:]), bc4(vr[:]), MUL, [ycd, vrd], [ycd])
                o_tt('pool', yc[:], yc[:], lng[:], MUL, [ycd, lngd], [ycd])
                o_tt('pool', yc[:], yc[:], lnb[:], ADD, [ycd, lnbd], [ycd])
                o_tt('dve', yc[:], yc[:], b[:], ADD, [ycd, bd], [ycd])
                outs.append((yc, ycd))
            (ps_, psd) = psr.next()
            (g, gd) = gr_.next()
            (ot, otd) = otr.next()
            o_dma(g[:], S['GATE'][t * 128:(t + 1) * 128, :], [S['GATE_d'][t]], [gd])
            o_mm(ps_[:], J32[:], outs[1][0][:], [J32d, outs[1][1]], [psd])
            o_tt('dve', ot[:], outs[0][0][:], ps_[:], ADD, [outs[0][1], psd], [otd])
            o_tt('dve', ot[:], ot[:], g[:], MUL, [otd, gd], [otd])
            o_dma(S['ocat'][t * 128:(t + 1) * 128, 0:256], ot[:], [otd], [S['ocat_d'][t]])
        P.barrier()


def phase_out(l, I, S, need_ctx, modL, modLd, modC, modCd, ident, identd):
    P = K.P
    MUL, ADD = ALU.mult, ALU.add
    with ExitStack() as st:
        wbf, wbfd = sb(st, 'wo_bf', [128, 8, D], BF16)
        wst = Ring(K, st, 'wo_st', [128, D], F32, 2)
        ocr = Ring(K, st, 'wo_oc', [128, D], F32, 2)
        ocbr = Ring(K, st, 'wo_ocb', [128, D], BF16, 2)
        oTr = Ring(K, st, 'wo_oT', [128, 8, 128], BF16, 2)
        xr = Ring(K, st, 'wo_x', [128, D], F32, 2)
        yr = Ring(K, st, 'wo_y', [128, D], F32, 2)
        sq, sqd = sb(st, 'wo_sq', [128, D], F32)
        ssr = Ring(K, st, 'wo_ss', [128, 1], F32, 4)
        pTr = Ring(K, st, 'wo_pT', [128, 8, 128], BF16, 2, psum=True)
        pyr = Ring(K, st, 'wo_py', [128, 512], F32, 4, psum=True)
        wo = I['w_out'][l].rearrange("(k p) n -> p k n", p=128)
        for kc in range(8):
            (ws, wsd) = wst.next()
            o_dma(ws[:], wo[:, kc, :], [], [wsd])
            o_cp('pool', wbf[:, kc, :], ws[:], [wsd], [wbfd])
        for t in range(0 if need_ctx else 2, NTILE):
            mt, mtd = (modC, modCd) if t < 2 else (modL, modLd)
            (oc, ocd) = ocr.next()
            (ocb, ocbd) = ocbr.next()
            (oT, oTd) = oTr.next()
            (x, xd) = xr.next()
            (y, yd) = yr.next()
            (ss, ssd) = ssr.next()
            (pT, pTd) = pTr.next()
            o_dma(oc[:], S['ocat'][t * 128:(t + 1) * 128, :], [S['ocat_d'][t]], [ocd])
            o_dma(x[:], S['xs'][t * 128:(t + 1) * 128, :], [S['xs_d'][t]], [xd])
            o_cp('pool', ocb[:], oc[:], [ocd], [ocbd])
            for kc in range(8):
                o_tr(pT[:, kc, :], ocb[:, kc * 128:(kc + 1) * 128], ident[:], [ocbd, identd], [pTd])
            o_cp('act', oT[:], pT[:], [pTd], [oTd])
            for hf in range(2):
                (py, pyd) = pyr.next()
                for kc in range(8):
                    o_mm(py[:], oT[:, kc, :], wbf[:, kc, hf * 512:(hf + 1) * 512], [oTd, wbfd], [pyd], start=(kc == 0), stop=(kc == 7))
                o_cp('act' if hf else 'dve', y[:, hf * 512:(hf + 1) * 512], py[:], [pyd], [yd])
            rms_rstd(y[:], yd, D, sq, sqd, ss, ssd, 0)
            o_stt('dve', y[:], y[:], ss[:, 0:1], mt[:, 2, :], MUL, MUL, [yd, ssd, mtd], [yd])
            o_tt('pool', x[:], x[:], y[:], ADD, [xd, yd], [xd])
            o_dma(S['xs'][t * 128:(t + 1) * 128, :], x[:], [xd], [S['xs_d'][t]])
        P.barrier()


def conv_items(I, S, moe, li):
    NEx = NE if moe else 1
    tag = 'm' if moe else 'f'
    items = []
    for e in range(NEx):
        for (nm, dst) in (('gate', 'wgb' + tag), ('up', 'wub' + tag)):
            src = (I['moe_w_' + nm][li, e] if moe else I['ffn_w_' + nm][li]).rearrange("(k p) n -> p k n", p=128)
            for kc in range(8):
                def it(ldr, cvr, eng, q, src=src, kc=kc, dst=dst, e=e):
                    (ld, ldd) = ldr.next()
                    (cv, cvd) = cvr.next()
                    o_dma(ld[:], src[:, kc, :], [], [ldd], q=q)
                    o_cp(eng, cv[:], ld[:], [ldd], [cvd])
                    for q4 in range(2):
                        f0, f1 = q4 * 11, (q4 + 1) * 11
                        o_dma(S[dst][e, f0:f1, :, kc, :].rearrange("f p n -> p f n"),
                              cv[:, f0 * 128:f1 * 128].rearrange("p (f n) -> p f n", n=128), [cvd], [Dep()], q=q)
                items.append(it)
        srcd = (I['moe_w_down'][li, e] if moe else I['ffn_w_down'][li]).rearrange("(f p) n -> p f n", p=128)
        for f0 in range(0, NFF, 2):
            def it(ldr, cvr, eng, q, srcd=srcd, f0=f0, e=e, tag=tag):
                (ld, ldd) = ldr.next()
                (cv, cvd) = cvr.next()
                o_dma(ld[:, 0:2048].rearrange("p (f n) -> p f n", n=1024), srcd[:, f0:f0 + 2, :], [], [ldd], q=q)
                o_cp(eng, cv[:, 0:2048], ld[:, 0:2048], [ldd], [cvd])
                o_dma(S['wdb' + tag][e, f0:f0 + 2, :, :].rearrange("f p n -> p f n"),
                      cv[:, 0:2048].rearrange("p (f n) -> p f n", n=1024), [cvd], [Dep()], q=q)
            items.append(it)
    return items


def ffn_convert(I, S, moe, li):
    if K.CONVERTED.get((moe, li)):
        return
    with ExitStack() as st:
        ldr = Ring(K, st, 'cv_ld', [128, DFF], F32, 3)
        cvr = Ring(K, st, 'cv_bf', [128, DFF], BF16, 3)
        engs = ('dve', 'pool', 'act')
        for n, it in enumerate(conv_items(I, S, moe, li)):
            it(ldr, cvr, engs[n % 3], 'sp')
        K.P.barrier()


def phase_ffn(l, I, S, need_ctx, modL, modLd, modC, modCd, ident32, ident32d, out_ap, out_d):
    P = K.P
    MUL, ADD, SUB = ALU.mult, ALU.add, ALU.subtract
    moe = (l % 2 == 1)
    li = l // 2
    NEx = NE if moe else 1
    tag = 'm' if moe else 'f'
    last_layer = not need_ctx
    ffn_convert(I, S, moe, li)
    tiles = list(range(0 if need_ctx else 2, NTILE))
    split = last_layer
    if split:
        tiles = tiles[0:16]
    with ExitStack() as st:
        xrr = Ring(K, st, 'ff_x', [128, 2, D], F32, 2)
        if split:
            sel, seld = sb(st, 'ff_sel', [128, 2], F32)
            o_dma(sel[:], I['halfsel'][:, :], [], [seld])
            xbr = Ring(K, st, 'ff_xb', [128, D], F32, 2)
            xar = Ring(K, st, 'ff_xa', [128, D], F32, 2)
        hfr = Ring(K, st, 'ff_hf', [128, D], F32, 2)
        hTr = Ring(K, st, 'ff_hT', [128, 8, 256], BF16, 2)
        h32r = Ring(K, st, 'ff_h32', [128, 8, 128], F32, 2)
        sq, sqd = sb(st, 'ff_sq', [128, D], F32)
        ssr = Ring(K, st, 'ff_ss', [128, 1], F32, 4)
        accr = Ring(K, st, 'ff_acc', [128, 2, D], F32, 2)
        gtr = Ring(K, st, 'ff_gate', [128, 2, 8], F32, 2)
        smr = Ring(K, st, 'ff_sm', [128, 8], F32, 8)
        s1r = Ring(K, st, 'ff_s1', [128, 1], F32, 12)
        wgr = Ring(K, st, 'ff_wg', [128, 8, 128], BF16, 3)
        wur = Ring(K, st, 'ff_wu', [128, 8, 128], BF16, 3)
        wdr = Ring(K, st, 'ff_wd', [128, D], BF16, 3)
        sgr = Ring(K, st, 'ff_sg', [128, 256], F32, 2)
        GTr = Ring(K, st, 'ff_GT', [128, 256], BF16, 3)
        pTr = Ring(K, st, 'ff_pT', [128, 4, 128], F32, 1, psum=True)
        gur = Ring(K, st, 'ff_gu', [128, 2, 256], F32, 2, psum=True)
        ops_ = [[ps(st, 'ff_o%d%d' % (a, b), [128, 512], F32) for b in range(2)] for a in range(2)]
        plr = Ring(K, st, 'ff_pl', [128, 8], F32, 1, psum=True)
        if moe:
            rt_, rtd = sb(st, 'ff_router', [128, 8, 8], F32)
            o_dma(rt_[:], I['moe_router'][li].rearrange("(k p) e -> p k e", p=128), [], [rtd])
        for blk in range(len(tiles) // 2):
            tl = tiles[2 * blk:2 * blk + 2]
            (xr, xrd) = xrr.next()
            (hT, hTd) = hTr.next()
            (acc, accd) = accr.next()
            (gate, gated) = gtr.next()
            for jj, t in enumerate(tl):
                mt, mtd = (modC, modCd) if t < 2 else (modL, modLd)
                (hf, hfd) = hfr.next()
                (h32, h32d) = h32r.next()
                (ss, ssd) = ssr.next()
                if split:
                    (xa, xad) = xar.next()
                    (xb, xbd) = xbr.next()
                    t2 = t + 16
                    o_dma(xa[:], S['xs'][t * 128:(t + 1) * 128, :], [S['xs_d'][t]], [xad])
                    o_dma(xb[:], S['xs'][t2 * 128:(t2 + 1) * 128, :], [S['xs_d'][t2]], [xbd], q='pool')
                    o_ts('dve', xa[:], xa[:], sel[:, 0:1], MUL, [xad, seld], [xad])
                    o_stt('dve', xr[:, jj, :], xb[:], sel[:, 1:2], xa[:], MUL, ADD, [xbd, seld, xad], [xrd])
                else:
                    o_dma(xr[:, jj, :], S['xs'][t * 128:(t + 1) * 128, :], [S['xs_d'][t]], [xrd])
                rms_rstd(xr[:, jj, :], xrd, D, sq, sqd, ss, ssd, 0)
                o_stt('dve', hf[:], xr[:, jj, :], ss[:, 0:1], mt[:, 4, :], MUL, MUL, [xrd, ssd, mtd], [hfd])
                o_tt('pool', hf[:], hf[:], mt[:, 3, :], ADD, [hfd, mtd], [hfd])
                for q4 in range(2):
                    (pT, pTd) = pTr.next()
                    for kk_ in range(4):
                        kc = q4 * 4 + kk_
                        o_tr(pT[:, kk_, :], hf[:, kc * 128:(kc + 1) * 128], ident32[:], [hfd, ident32d], [pTd])
                    o_cp('act', h32[:, q4 * 4:(q4 + 1) * 4, :], pT[:], [pTd], [h32d])
                    o_cp('pool', hT[:, q4 * 4:(q4 + 1) * 4, jj * 128:(jj + 1) * 128], h32[:, q4 * 4:(q4 + 1) * 4, :], [h32d], [hTd])
                if moe:
                    (pl, pld) = plr.next()
                    for kc in range(8):
                        o_mm(pl[:], h32[:, kc, :], rt_[:, kc, :], [h32d, rtd], [pld], start=(kc == 0), stop=(kc == 7))
                    (lg, lgd) = smr.next()
                    (lg2, lg2d) = smr.next()
                    (mk1, mk1d) = smr.next()
                    (mk2, mk2d) = smr.next()
                    (m1, m1d) = s1r.next()
                    (m2, m2d) = s1r.next()
                    (ex, exd) = s1r.next()
                    (w1, w1d) = s1r.next()
                    (w2, w2d) = s1r.next()
                    o_cp('dve', lg[:], pl[:], [pld], [lgd])
                    o_red('dve', m1[:], lg[:], ALU.max, [lgd], [m1d])
                    o_ts('dve', mk1[:], lg[:], m1[:, 0:1], ALU.is_equal, [lgd, m1d], [mk1d])
                    o_stt('dve', lg2[:], mk1[:], -1.0e30, lg[:], MUL, ADD, [mk1d, lgd], [lg2d])
                    o_red('dve', m2[:], lg2[:], ALU.max, [lg2d], [m2d])
                    o_ts('dve', mk2[:], lg2[:], m2[:, 0:1], ALU.is_equal, [lg2d, m2d], [mk2d])
                    o_tt('dve', ex[:], m2[:], m1[:], SUB, [m2d, m1d], [exd])
                    o_act(ex[:], ex[:], AF.Exp, [exd], [exd])
                    o_ts('dve', w1[:], ex[:], 1.0, ADD, [exd], [w1d])
                    o_rcp(w1[:], w1[:], [w1d], [w1d])
                    o_tt('dve', w2[:], ex[:], w1[:], MUL, [exd, w1d], [w2d])
                    o_ts('dve', gate[:, jj, :], mk1[:], w1[:, 0:1], MUL, [mk1d, w1d], [gated])
                    o_stt('dve', gate[:, jj, :], mk2[:], w2[:, 0:1], gate[:, jj, :], MUL, ADD, [mk2d, w2d, gated], [gated])
            for e in range(NEx):
                fpend = [None]
                for f in range(NFF):
                    (wg, wgd) = wgr.next()
                    (wu, wud) = wur.next()
                    (wd, wdd) = wdr.next()
                    (gu, gud) = gur.next()
                    (sg, sgd) = sgr.next()
                    (GT, GTd) = GTr.next()
                    o_dma(wg[:], S['wgb' + tag][e, f], [], [wgd])
                    o_dma(wu[:], S['wub' + tag][e, f], [], [wud], q='pool')
                    o_dma(wd[:], S['wdb' + tag][e, f], [], [wdd])
                    for kc in range(8):
                        o_mm(gu[:, 0, :], wg[:, kc, :], hT[:, kc, :], [wgd, hTd], [gud], start=(kc == 0), stop=(kc == 7))
                    for kc in range(8):
                        o_mm(gu[:, 1, :], wu[:, kc, :], hT[:, kc, :], [wud, hTd], [gud], start=(kc == 0), stop=(kc == 7))
                    o_act(sg[:], gu[:, 0, :], AF.Silu, [gud], [sgd])
                    o_tt('dve', GT[:], sg[:], gu[:, 1, :], MUL, [sgd, gud], [GTd])
                    if fpend[0] is not None:
                        fpend[0]()

                    def _down(GT=GT, GTd=GTd, wd=wd, wdd=wdd, f=f):
                        for jj in range(2):
                            for hf_ in range(2):
                                (op_, opd) = ops_[jj][hf_]
                                o_mm(op_[:], GT[:, jj * 128:(jj + 1) * 128], wd[:, hf_ * 512:(hf_ + 1) * 512], [GTd, wdd], [opd],
                                     start=(f == 0), stop=(f == NFF - 1))
                    fpend[0] = _down
                fpend[0]()
                fpend[0] = None
                for jj in range(2):
                    for hf_ in range(2):
                        (op_, opd) = ops_[jj][hf_]
                        dst = acc[:, jj, hf_ * 512:(hf_ + 1) * 512]
                        if not moe:
                            o_cp('act', dst, op_[:], [opd], [accd])
                        elif e == 0:
                            o_ts('dve', dst, op_[:], gate[:, jj, e:e + 1], MUL, [opd, gated], [accd])
                        else:
                            o_stt('dve', dst, op_[:], gate[:, jj, e:e + 1], dst, MUL, ADD, [opd, gated, accd], [accd])
            for jj, t in enumerate(tl):
                mt, mtd = (modC, modCd) if t < 2 else (modL, modLd)
                (ss, ssd) = ssr.next()
                rms_rstd(acc[:, jj, :], accd, D, sq, sqd, ss, ssd, 0)
                o_stt('dve', acc[:, jj, :], acc[:, jj, :], ss[:, 0:1], mt[:, 5, :], MUL, MUL, [accd, ssd, mtd], [accd])
                o_tt('pool', acc[:, jj, :], acc[:, jj, :], xr[:, jj, :], ADD, [accd, xrd], [accd])
                if last_layer:
                    o_dma(out_ap[(t - 2) * 128:(t - 1) * 128, :], acc[:, jj, :], [accd], [out_d])
                else:
                    o_dma(S['xs'][t * 128:(t + 1) * 128, :], acc[:, jj, :], [accd], [S['xs_d'][t]])
        P.barrier()


IN_NAMES = ['x', 'c', 'ctx', 'c_ctx', 'mod_w', 'mod_b', 'norm_g', 'w_in', 'w_out', 'rwkv_mu', 'rwkv_w0', 'rwkv_w_up',
            'rwkv_a0', 'rwkv_a_up', 'rwkv_g_up', 'rwkv_k_k', 'rwkv_k_a', 'rwkv_r_k', 'rwkv_ln_g', 'rwkv_ln_b',
            'gqa_q_g', 'gqa_k_g', 'mla_q_norm_g', 'mla_w_uq', 'mla_kv_norm_g', 'mla_w_ukv', 'nat_bias',
            'ffn_w_gate', 'ffn_w_up', 'ffn_w_down', 'moe_router', 'moe_w_gate', 'moe_w_up', 'moe_w_down']


def build(shapes, stop_after=None, debug=(), only=None, layers=(0, 1), scan_T=None):
    nc = bass.Bass("TRN2", target_bir_lowering=False)
    K.nc = nc
    P = Prog(nc)
    K.P = P
    I = {}
    for name in IN_NAMES:
        I[name] = nc.dram_tensor(name, list(shapes[name]), F32, kind="ExternalInput").ap()
    out = nc.dram_tensor("out", [NL // 2, D], F32, kind="ExternalOutput").ap()
    I['halfsel'] = nc.dram_tensor('halfsel', [128, 2], F32, kind='ExternalInput').ap()
    out_d = Dep('out')
    S = {}

    def scratch(name, shape, dtype=F32, tiled=True):
        S[name] = dram(name, shape, dtype)
        S[name + '_d'] = [Dep('%s%d' % (name, t)) for t in range(NTILE)] if tiled else Dep(name)

    scratch('xs', [NT, D])
    scratch('p', [NT, INC])
    scratch('ocat', [NT, D])
    scratch('prw1', [NT, 1024])
    for nm in ('V2', 'KKA', 'KD', 'BON', 'Y2'):
        scratch(nm, [2, NT, 256])
    scratch('GATE', [NT, 256])
    scratch('AKK', [128, NT, 8])
    scratch('AR', [128, NT, 8])
    scratch('WD', [128, NT, 4])
    for tag, ne in (('f', 1), ('m', NE)):
        scratch('wgb' + tag, [ne, NFF, 128, 8, 128], BF16, tiled=False)
        scratch('wub' + tag, [ne, NFF, 128, 8, 128], BF16, tiled=False)
        scratch('wdb' + tag, [ne, NFF, 128, D], BF16, tiled=False)
    I['rope_g'] = nc.dram_tensor('rope_g', [NL, 2, 32], F32, kind='ExternalInput').ap()
    I['rope_m'] = nc.dram_tensor('rope_m', [NL, 2, 16], F32, kind='ExternalInput').ap()
    I['nat_tab'] = nc.dram_tensor('nat_tab', [2, 128, 4, len(nat_plan()[1]), 64], F32, kind='ExternalInput').ap()
    with ExitStack() as st:
        P.alloc_sems(st)
        ident, identd = sb(st, 'ident', [128, 128], BF16)
        modL, modLd = sb(st, 'modL', [128, 6, D], F32)
        modC, modCd = sb(st, 'modC', [128, 6, D], F32)
        ident32, ident32d = sb(st, 'ident32', [128, 128], F32)
        J32, J32d = sb(st, 'J32', [128, 128], F32)
        P.op('pool', lambda e: e.memset(ident[:], 1.0), [], [identd])
        P.op('pool', lambda e: e.memset(ident32[:], 1.0), [], [ident32d])
        P.op('pool', lambda e: e.memset(J32[:], 1.0), [], [J32d])
        P.op('pool', lambda e: e.affine_select(out=ident32[:], in_=ident32[:], pattern=[[-1, 128]], compare_op=ALU.is_equal, fill=0.0, base=0, channel_multiplier=1),
             [ident32d], [ident32d])
        P.op('pool', lambda e: e.affine_select(out=ident[:], in_=ident[:], pattern=[[-1, 128]], compare_op=ALU.is_equal, fill=0.0, base=0, channel_multiplier=1),
             [identd], [identd])
        P.op('pool', lambda e: e.affine_select(out=J32[:], in_=J32[:], pattern=[[1, 128]], compare_op=ALU.is_equal, fill=0.0, base=-127, channel_multiplier=1),
             [J32d], [J32d])
        P.dma('sp', lambda e: e.dma_start(out=S['xs'][0:NC_, :], in_=I['ctx'][:, :]), [], S['xs_d'][0:2])
        for j in range(4):
            P.dma('sp', lambda e, j=j: e.dma_start(out=S['xs'][NC_ + j * 1024:NC_ + (j + 1) * 1024, :], in_=I['x'][j * 1024:(j + 1) * 1024, :]),
                  [], S['xs_d'][2 + 8 * j:2 + 8 * (j + 1)])

        def want(name):
            return only is None or name in only

        K.CONVERTED = {}
        K.PENDING = []
        if only is None and tuple(layers) == (0, 1):
            K.PENDING = conv_items(I, S, False, 0) + conv_items(I, S, True, 0)
            K.CONVERTED = {(False, 0): True, (True, 0): True}
        for l in layers:
            need_ctx = (l == 0)
            if want('mod'):
                phase_mod(l, I, modL, modLd, modC, modCd)
            if want('in'):
                phase_in(l, I, S, modL, modLd, modC, modCd, ident, identd)
            if want('rwkv'):
                if scan_T is None:
                    phase_rwkv(l, I, S, need_ctx, ident32, ident32d, J32, J32d)
                else:
                    rwkv_prep(l, I, S, ident32, ident32d, J32, J32d)
                    rwkv_scan(S, scan_T)
            if want('gqa'):
                phase_gqa(l, I, S, need_ctx, ident, identd, ident32, ident32d)
            if want('mla'):
                phase_mla(l, I, S, need_ctx, ident, identd, ident32, ident32d)
            if want('nat'):
                phase_nat(l, I, S, need_ctx, ident, identd, ident32, ident32d)
            if stop_after == ('attn', l):
                break
            if want('out'):
                phase_out(l, I, S, need_ctx, modL, modLd, modC, modCd, ident, identd)
            if stop_after == ('out', l):
                break
            if want('ffn'):
                phase_ffn(l, I, S, need_ctx, modL, modLd, modC, modCd, ident32, ident32d, out, out_d)
        for name in debug:
            src = S[name]
            d = nc.dram_tensor('dbg_' + name, list(src.shape), src.dtype, kind="ExternalOutput").ap()
            dd = Dep('dbg_' + name)
            deps = S[name + '_d'] if isinstance(S[name + '_d'], list) else [S[name + '_d']]
            P.dma('sp', lambda e, d=d, src=src: e.dma_start(out=d, in_=src), deps, [dd])
        P.barrier()
        P.emit()
    return nc


def core_shapes(inputs):
    sh = {k: tuple(np.asarray(v).shape) for k, v in inputs.items()}
    sh['x'] = (NL, D)
    sh['c'] = (D,)
    sh['ctx'] = (NC_, D)
    return sh


def rope_tables(rot_dim):
    t = np.arange(NL)
    row = (t // 64).astype(np.float32)
    col = (t % 64).astype(np.float32)
    quarter = rot_dim // 4
    inv_freq = (np.float32(10000.0) ** (-np.arange(quarter, dtype=np.float32) / np.float32(quarter))).astype(np.float32)
    ang = np.concatenate([row[:, None] * inv_freq, col[:, None] * inv_freq], axis=-1).astype(np.float32)
    return np.ascontiguousarray(np.stack([np.cos(ang), np.sin(ang)], axis=1).astype(np.float32))


def core_inputs(inputs, b, shared=None):
    if shared is None:
        shared = {k: np.ascontiguousarray(np.asarray(v, dtype=np.float32)) for k, v in inputs.items() if k not in ('x', 'c', 'ctx')}
        shared['rope_g'] = rope_tables(64)
        shared['rope_m'] = rope_tables(32)
        nb = np.asarray(inputs['nat_bias'], np.float32)
        shared['nat_tab'] = np.ascontiguousarray(np.stack([nat_bias_table(nb[0]), nat_bias_table(nb[1])], 0))
    m = dict(shared)
    m['x'] = np.ascontiguousarray(np.asarray(inputs['x'][b], dtype=np.float32))
    m['c'] = np.ascontiguousarray(np.asarray(inputs['c'][b], dtype=np.float32))
    m['ctx'] = np.ascontiguousarray(np.asarray(inputs['ctx'][b], dtype=np.float32))
    return m


def kernel(**inputs):
    nb = 4
    shapes = core_shapes(inputs)
    nc = build(shapes)
    first = core_inputs(inputs, 0)
    shared = {k: v for k, v in first.items() if k not in ('x', 'c', 'ctx')}
    per_b = [first] + [core_inputs(inputs, b, shared) for b in range(1, nb)]
    maps = []
    for b in range(nb):
        for m in range(2):
            mm_ = dict(per_b[b])
            hs = np.zeros((128, 2), np.float32)
            hs[:, m] = 1.0
            mm_['halfsel'] = hs
            maps.append(mm_)
    res = run_bass_kernel_spmd(nc, maps, core_ids=list(range(2 * nb)))
    out = np.empty((nb, NL, D), np.float32)
    for b in range(nb):
        for m in range(2):
            out[b, m * (NL // 2):(m + 1) * (NL // 2)] = np.asarray(res.results[2 * b + m]['out'], dtype=np.float32)
    return out
```
